# Optimizing a Trainium2 kernel written in Bass

```python
import jax, jax.numpy as jnp
from jax import lax
import numpy as np

D_MODEL = 1024
BATCH = 16
SEQ = 2048
DEPTH = 4

GRID_W = 64
CTX_LEN = 256
N_MIXERS = 3
N_HEADS = 16
N_KV_HEADS = 4
HEAD_DIM = D_MODEL // N_HEADS
Q_PER_KV = N_HEADS // N_KV_HEADS
QKV_DIM = (N_HEADS + 2 * N_KV_HEADS) * HEAD_DIM
ATTN_SCALE = HEAD_DIM ** -0.5
ROPE_THETA = 10000.0
BLOCK_Q = 128
WINDOW = 128
NA_WIN_H = 8
NA_WIN_W = 16
N_GROUPS = 4
EXPERTS_PER_GROUP = 8
N_EXPERTS = N_GROUPS * EXPERTS_PER_GROUP
TOP_K_IN_GROUP = 2
EXPERT_HIDDEN = D_MODEL // 2
MOE_BLOCK = 128
NORM_EPS = 1e-6
NEG_INF = -1e30
N_A_LAYERS = (DEPTH + N_MIXERS - 1) // N_MIXERS
N_B_LAYERS = (DEPTH + N_MIXERS - 2) // N_MIXERS
N_C_LAYERS = DEPTH // N_MIXERS

kernel_name = 'hybrid_dit_window_dense_natten_hmoe'


def _rmsnorm(x, g):
    x32 = x.astype(jnp.float32)
    y = x32 * lax.rsqrt(jnp.mean(x32 * x32, axis=-1, keepdims=True) + NORM_EPS)
    return (y * g.astype(jnp.float32)).astype(x.dtype)


def _modulate(h, shift, scale):
    return h * (1 + scale) + shift


def _axial_rope_tables(n, dtype):
    t = jnp.arange(n)
    row = (t // GRID_W).astype(jnp.float32)
    col = (t % GRID_W).astype(jnp.float32)
    quarter = HEAD_DIM // 4
    inv = ROPE_THETA ** (-jnp.arange(quarter, dtype=jnp.float32) / quarter)
    ar = row[:, None] * inv
    ac = col[:, None] * inv
    ang = jnp.concatenate([ar, ar, ac, ac], axis=-1)
    return jnp.cos(ang).astype(dtype), jnp.sin(ang).astype(dtype)


def _rot_axial(x):
    r1, r2, c1, c2 = jnp.split(x, 4, axis=-1)
    return jnp.concatenate([-r2, r1, -c2, c1], axis=-1)


def _apply_rope(x, cos, sin):
    shp = (1, x.shape[1]) + (1,) * (x.ndim - 3) + (HEAD_DIM,)
    return x * cos.reshape(shp) + _rot_axial(x) * sin.reshape(shp)


def _split_qkv(h, w_qkv):
    b, n, _ = h.shape
    qkv = h @ w_qkv
    q, k, v = jnp.split(qkv, [N_HEADS * HEAD_DIM, (N_HEADS + N_KV_HEADS) * HEAD_DIM], axis=-1)
    return (q.reshape(b, n, N_KV_HEADS, Q_PER_KV, HEAD_DIM),
            k.reshape(b, n, N_KV_HEADS, HEAD_DIM),
            v.reshape(b, n, N_KV_HEADS, HEAD_DIM))


def _sink_column(sink, lead_shape):
    return jnp.broadcast_to(sink.astype(jnp.float32).reshape(1, N_KV_HEADS, Q_PER_KV, 1, 1), lead_shape + (1,))


def _joint_softmax(s_lat, s_ctx, sink):
    parts = [s_lat, s_ctx]
    if sink is not None:
        parts.append(_sink_column(sink, s_lat.shape[:-1]))
    p = jax.nn.softmax(jnp.concatenate(parts, axis=-1), axis=-1)
    k1 = s_lat.shape[-1]
    c = s_ctx.shape[-1]
    return p[..., :k1], p[..., k1:k1 + c]


def _ctx_self_attention(qc, kc, vc, sink):
    b, n = qc.shape[:2]
    s = jnp.einsum('bqhgd,bkhd->bhgqk', qc, kc).astype(jnp.float32)
    if sink is not None:
        s = jnp.concatenate([s, _sink_column(sink, s.shape[:-1])], axis=-1)
    p = jax.nn.softmax(s, axis=-1)[..., :kc.shape[1]]
    o = jnp.einsum('bhgqk,bkhd->bqhgd', p.astype(vc.dtype), vc)
    return o.reshape(b, n, N_HEADS * HEAD_DIM)


def _mixer_window(q, k, v, kc, vc, sink, cos, sin):
    b, n = q.shape[:2]
    nb = n // BLOCK_Q
    span = BLOCK_Q + 2 * WINDOW
    q_rot = _apply_rope(q, cos, sin)
    pad = ((0, 0), (WINDOW, WINDOW), (0, 0), (0, 0))
    k_pad = jnp.pad(_apply_rope(k, cos, sin), pad)
    v_pad = jnp.pad(v, pad)

    def block(i):
        start = i * BLOCK_Q
        qb = lax.dynamic_slice_in_dim(q_rot, start, BLOCK_Q, 1)
        qpb = lax.dynamic_slice_in_dim(q, start, BLOCK_Q, 1)
        kb = lax.dynamic_slice_in_dim(k_pad, start, span, 1)
        vb = lax.dynamic_slice_in_dim(v_pad, start, span, 1)
        qpos = start + jnp.arange(BLOCK_Q)
        kpos = start - WINDOW + jnp.arange(span)
        mask = ((jnp.abs(kpos[None, :] - qpos[:, None]) <= WINDOW)
                & (kpos >= 0)[None, :] & (kpos < n)[None, :])
        s_lat = jnp.einsum('bqhgd,bkhd->bhgqk', qb, kb).astype(jnp.float32)
        s_lat = jnp.where(mask, s_lat, NEG_INF)
        s_ctx = jnp.einsum('bqhgd,bkhd->bhgqk', qpb, kc).astype(jnp.float32)
        p_lat, p_ctx = _joint_softmax(s_lat, s_ctx, sink)
        return (jnp.einsum('bhgqk,bkhd->bqhgd', p_lat.astype(v.dtype), vb)
                + jnp.einsum('bhgqk,bkhd->bqhgd', p_ctx.astype(vc.dtype), vc))

    o = lax.map(block, jnp.arange(nb))
    return jnp.moveaxis(o, 0, 1).reshape(b, n, N_HEADS * HEAD_DIM)


def _mixer_global(q, k, v, kc, vc, cos, sin):
    b, n = q.shape[:2]
    nb = n // BLOCK_Q
    q_rot = _apply_rope(q, cos, sin)
    k_rot = _apply_rope(k, cos, sin)

    def block(i):
        start = i * BLOCK_Q
        qb = lax.dynamic_slice_in_dim(q_rot, start, BLOCK_Q, 1)
        qpb = lax.dynamic_slice_in_dim(q, start, BLOCK_Q, 1)
        s_lat = jnp.einsum('bqhgd,bkhd->bhgqk', qb, k_rot).astype(jnp.float32)
        s_ctx = jnp.einsum('bqhgd,bkhd->bhgqk', qpb, kc).astype(jnp.float32)
        p_lat, p_ctx = _joint_softmax(s_lat, s_ctx, None)
        return (jnp.einsum('bhgqk,bkhd->bqhgd', p_lat.astype(v.dtype), v)
                + jnp.einsum('bhgqk,bkhd->bqhgd', p_ctx.astype(vc.dtype), vc))

    o = lax.map(block, jnp.arange(nb))
    return jnp.moveaxis(o, 0, 1).reshape(b, n, N_HEADS * HEAD_DIM)


def _na_tables(n):
    rows = n // GRID_W
    wh = min(NA_WIN_H, rows)
    ww = min(NA_WIN_W, GRID_W)
    t = np.arange(n)
    r = t // GRID_W
    cl = t % GRID_W
    rs = np.clip(r - wh // 2, 0, rows - wh)
    cs = np.clip(cl - ww // 2, 0, GRID_W - ww)
    kr = rs[:, None, None] + np.arange(wh)[None, :, None]
    kcol = cs[:, None, None] + np.arange(ww)[None, None, :]
    idx = (kr * GRID_W + kcol).reshape(n, wh * ww)
    rel = ((kr - r[:, None, None] + NA_WIN_H - 1) * (2 * NA_WIN_W - 1)
           + (kcol - cl[:, None, None] + NA_WIN_W - 1)).reshape(n, wh * ww)
    return idx.astype(np.int32), rel.astype(np.int32)


def _mixer_neighbourhood(q, k, v, kc, vc, rpb):
    b, n = q.shape[:2]
    nb = n // BLOCK_Q
    idx, rel = _na_tables(n)
    n_keys = idx.shape[1]
    idx_blocks = jnp.asarray(idx).reshape(nb, BLOCK_Q, n_keys)
    rel_blocks = jnp.asarray(rel).reshape(nb, BLOCK_Q, n_keys)
    rpb_flat = rpb.reshape(N_HEADS, -1)

    def block(args):
        i, idx_b, rel_b = args
        qb = lax.dynamic_slice_in_dim(q, i * BLOCK_Q, BLOCK_Q, 1)
        kg = k[:, idx_b]
        vg = v[:, idx_b]
        bias = rpb_flat[:, rel_b].reshape(N_KV_HEADS, Q_PER_KV, BLOCK_Q, n_keys).astype(jnp.float32)
        s_lat = jnp.einsum('bqhgd,bqkhd->bhgqk', qb, kg).astype(jnp.float32) + bias
        s_ctx = jnp.einsum('bqhgd,bkhd->bhgqk', qb, kc).astype(jnp.float32)
        p_lat, p_ctx = _joint_softmax(s_lat, s_ctx, None)
        return (jnp.einsum('bhgqk,bqkhd->bqhgd', p_lat.astype(v.dtype), vg)
                + jnp.einsum('bhgqk,bkhd->bqhgd', p_ctx.astype(vc.dtype), vc))

    o = lax.map(block, (jnp.arange(nb), idx_blocks, rel_blocks))
    return jnp.moveaxis(o, 0, 1).reshape(b, n, N_HEADS * HEAD_DIM)


def _hier_moe(h, w_group, b_group, w_router, b_router, w_gate, w_up, w_down):
    t, d = h.shape
    p_grp = jax.nn.softmax((h @ w_group + b_group).astype(jnp.float32), axis=-1)
    grp = jnp.argmax(p_grp, axis=-1)
    p_top = jnp.take_along_axis(p_grp, grp[:, None], axis=-1)
    lg = (h @ w_router + b_router).astype(jnp.float32).reshape(t, N_GROUPS, EXPERTS_PER_GROUP)
    lg = jnp.take_along_axis(lg, grp[:, None, None], axis=1)[:, 0]
    p_in, e_in = lax.top_k(jax.nn.softmax(lg, axis=-1), TOP_K_IN_GROUP)
    wts = (p_top * p_in / jnp.sum(p_in, axis=-1, keepdims=True)).reshape(-1)
    eid = (grp[:, None] * EXPERTS_PER_GROUP + e_in).reshape(-1).astype(jnp.int32)
    tok = jnp.repeat(jnp.arange(t, dtype=jnp.int32), TOP_K_IN_GROUP)
    n_asg = t * TOP_K_IN_GROUP
    cap = -(-n_asg // MOE_BLOCK) * MOE_BLOCK + N_EXPERTS * MOE_BLOCK
    n_blocks = cap // MOE_BLOCK
    order = jnp.argsort(eid)
    eid_s = eid[order]
    tok_s = tok[order]
    wts_s = wts[order]
    counts = jnp.bincount(eid, length=N_EXPERTS).astype(jnp.int32)
    padded = (counts + MOE_BLOCK - 1) // MOE_BLOCK * MOE_BLOCK
    pad_end = jnp.cumsum(padded)
    pad_start = pad_end - padded
    start = jnp.cumsum(counts) - counts
    dest = pad_start[eid_s] + jnp.arange(n_asg, dtype=jnp.int32) - start[eid_s]
    tok_buf = jnp.full((cap,), t, jnp.int32).at[dest].set(tok_s)
    w_buf = jnp.zeros((cap,), jnp.float32).at[dest].set(wts_s)
    blk_e = jnp.minimum(jnp.searchsorted(pad_end, jnp.arange(n_blocks, dtype=jnp.int32) * MOE_BLOCK, side='right'),
                        N_EXPERTS - 1)
    h_pad = jnp.concatenate([h, jnp.zeros((1, d), h.dtype)], axis=0)
    xb = h_pad[tok_buf].reshape(n_blocks, MOE_BLOCK, d)

    def expert_block(args):
        xe, e = args
        return (jax.nn.silu(xe @ w_gate[e]) * (xe @ w_up[e])) @ w_down[e]

    y = lax.map(expert_block, (xb, blk_e)).reshape(cap, d)
    out = jax.ops.segment_sum(y.astype(jnp.float32) * w_buf[:, None], tok_buf, num_segments=t + 1)
    return out[:t]


def setup_inputs(seed: int = 0) -> dict:
    key = jax.random.key(seed)
    ks = jax.random.split(key, 24)

    def nrm(k, shape, s):
        return jax.random.normal(k, shape, jnp.float32) * s

    d, f, hd = D_MODEL, EXPERT_HIDDEN, N_HEADS * HEAD_DIM
    return {
        'x': nrm(ks[0], (BATCH, SEQ, d), 1.0),
        'c': nrm(ks[1], (BATCH, d), 1.0),
        'ctx': nrm(ks[2], (BATCH, CTX_LEN, d), 1.0),
        'c_ctx': nrm(ks[3], (d,), 1.0),
        'w_ada': nrm(ks[4], (DEPTH, d, 6 * d), 0.5 * d ** -0.5),
        'b_ada': nrm(ks[5], (DEPTH, 6 * d), 0.02),
        'g_attn': 1.0 + nrm(ks[6], (DEPTH, d), 0.05),
        'w_qkv': nrm(ks[7], (DEPTH, d, QKV_DIM), d ** -0.5),
        'w_o': nrm(ks[8], (DEPTH, hd, d), hd ** -0.5),
        'sink_a': nrm(ks[9], (N_A_LAYERS, N_HEADS), 1.0),
        'gq_b': 1.0 + nrm(ks[10], (N_B_LAYERS, HEAD_DIM), 0.05),
        'gk_b': 1.0 + nrm(ks[11], (N_B_LAYERS, HEAD_DIM), 0.05),
        'rpb_c': nrm(ks[12], (N_C_LAYERS, N_HEADS, 2 * NA_WIN_H - 1, 2 * NA_WIN_W - 1), 0.5),
        'g_ffn': 1.0 + nrm(ks[13], (DEPTH, d), 0.05),
        'w_group': nrm(ks[14], (DEPTH, d, N_GROUPS), d ** -0.5),
        'b_group': nrm(ks[15], (DEPTH, N_GROUPS), 0.01),
        'w_router': nrm(ks[16], (DEPTH, d, N_EXPERTS), d ** -0.5),
        'b_router': nrm(ks[17], (DEPTH, N_EXPERTS), 0.01),
        'w_gate': nrm(ks[18], (DEPTH, N_EXPERTS, d, f), d ** -0.5),
        'w_up': nrm(ks[19], (DEPTH, N_EXPERTS, d, f), d ** -0.5),
        'w_down': nrm(ks[20], (DEPTH, N_EXPERTS, f, d), f ** -0.5),
        'g_final': 1.0 + nrm(ks[21], (d,), 0.05),
    }


def reference(x, c, ctx, c_ctx, w_ada, b_ada, g_attn, w_qkv, w_o, sink_a, gq_b, gk_b, rpb_c, g_ffn,
              w_group, b_group, w_router, b_router, w_gate, w_up, w_down, g_final):
    b, n, d = x.shape
    cos, sin = _axial_rope_tables(n, x.dtype)
    silu_c = jax.nn.silu(c)
    silu_cc = jax.nn.silu(c_ctx)
    xc = ctx
    for i in range(DEPTH):
        m = i % N_MIXERS
        j = i // N_MIXERS
        last = i == DEPTH - 1
        mod = (silu_c @ w_ada[i] + b_ada[i])[:, None, :]
        modc = (silu_cc @ w_ada[i] + b_ada[i])[None, None, :]
        sh_a, sc_a, gt_a, sh_f, sc_f, gt_f = jnp.split(mod, 6, axis=-1)
        shc_a, scc_a, gtc_a, shc_f, scc_f, gtc_f = jnp.split(modc, 6, axis=-1)

        h = _modulate(_rmsnorm(x, g_attn[i]), sh_a, sc_a)
        hc = _modulate(_rmsnorm(xc, g_attn[i]), shc_a, scc_a)
        q, k, v = _split_qkv(h, w_qkv[i])
        qc, kc, vc = _split_qkv(hc, w_qkv[i])
        sink = None
        if m == 0:
            sink = sink_a[j]
        elif m == 1:
            q, k = _rmsnorm(q, gq_b[j]), _rmsnorm(k, gk_b[j])
            qc, kc = _rmsnorm(qc, gq_b[j]), _rmsnorm(kc, gk_b[j])
        q = q * ATTN_SCALE
        qc = qc * ATTN_SCALE
        if m == 0:
            o = _mixer_window(q, k, v, kc, vc, sink, cos, sin)
        elif m == 1:
            o = _mixer_global(q, k, v, kc, vc, cos, sin)
        else:
            o = _mixer_neighbourhood(q, k, v, kc, vc, rpb_c[j])
        x = x + gt_a * (o @ w_o[i])
        if not last:
            oc = _ctx_self_attention(qc, kc, vc, sink)
            xc = xc + gtc_a * (oc @ w_o[i])

        hf = _modulate(_rmsnorm(x, g_ffn[i]), sh_f, sc_f).reshape(b * n, d)
        if last:
            tokens = hf
        else:
            hfc = _modulate(_rmsnorm(xc, g_ffn[i]), shc_f, scc_f).reshape(-1, d)
            tokens = jnp.concatenate([hf, hfc], axis=0)
        y = _hier_moe(tokens, w_group[i], b_group[i], w_router[i], b_router[i],
                      w_gate[i], w_up[i], w_down[i]).astype(x.dtype)
        x = x + gt_f * y[:b * n].reshape(b, n, d)
        if not last:
            xc = xc + gtc_f * y[b * n:].reshape(xc.shape)
    return _rmsnorm(x, g_final)
```

```python
import numpy as np
import concourse.bass as bass
import concourse.mybir as mybir
from concourse.bass_utils import run_bass_kernel_spmd

F32 = mybir.dt.float32
BF16 = mybir.dt.bfloat16
I32 = mybir.dt.int32
ALU = mybir.AluOpType
AF = mybir.ActivationFunctionType
AX = mybir.AxisListType

D = 1024
SEQ = 2048
CTX = 256
DEPTH = 4
NH = 16
NKV = 4
HD = 64
NE = 32
FH = 512
GRID_W = 64
CAP = 512
NT_L = SEQ // 128
NT_C = CTX // 128
NT_B = NT_L + NT_C
EPS = 1e-6
NEG = -1e30


class _Op:
    __slots__ = ("idx", "eng", "fn", "deps", "dma", "sig", "waits", "signal", "ksnap")

    def __init__(self, idx, eng, fn, dma):
        self.idx = idx
        self.eng = eng
        self.fn = fn
        self.deps = set()
        self.dma = dma
        self.sig = None
        self.waits = []
        self.signal = False
        self.ksnap = None


class Prog:
    ENGS = ("pe", "act", "dve", "pool", "sp")
    EPOCH = 20000
    NDMA = {"sp": 24, "pool": 24, "act": 8}

    def __init__(self, nc):
        self.nc = nc
        self.ops = []
        self.last_w = {}
        self.readers = {}
        self.pending = {}
        self.last_eng = {}
        self.dma_since = []

    def add(self, eng, fn, r=(), w=(), dma=False):
        op = _Op(len(self.ops), eng, fn, dma)
        ops = self.ops

        def same_eng(j):
            o = ops[j]
            return (not dma) and (not o.dma) and o.eng == eng

        for k in r:
            lw = self.last_w.get(k)
            if lw is not None and not (eng == "pe" and same_eng(lw)):
                op.deps.add(lw)
        for k in w:
            lw = self.last_w.get(k)
            if lw is not None and not (eng == "pe" and same_eng(lw)):
                op.deps.add(lw)
            rs = self.readers.get(k)
            if rs:
                for rd in rs:
                    if not same_eng(rd):
                        op.deps.add(rd)
        for k in r:
            self.readers.setdefault(k, []).append(op.idx)
        for k in w:
            self.last_w[k] = op.idx
            self.readers[k] = []
        if eng in self.pending:
            op.deps.update(self.pending.pop(eng))
        op.deps.discard(op.idx)
        self.ops.append(op)
        if dma:
            self.dma_since.append(op.idx)
        else:
            self.last_eng[eng] = op.idx
        return op

    def barrier(self):
        deps = set(self.last_eng.values()) | set(self.dma_since)
        for e in self.ENGS:
            self.pending[e] = set(deps) | self.pending.get(e, set())
        self.dma_since = []

    def finalize(self, final_engine="sp"):
        ops = self.ops
        dma_rr = {q: 0 for q in self.NDMA}
        dma_last = {}
        dma_cnt = {}
        for op in ops:
            if op.dma:
                q = op.eng
                s = dma_rr[q] % self.NDMA[q]
                dma_rr[q] += 1
                key = ("dma", q, s)
                prev = dma_last.get(key)
                if prev is not None:
                    op.deps.add(prev)
                dma_last[key] = op.idx
                dma_cnt[key] = dma_cnt.get(key, 0) + 16
                op.sig = (key, dma_cnt[key])
        has_dep = [False] * len(ops)
        for op in ops:
            for d in op.deps:
                has_dep[d] = True
        cnt = {e: 0 for e in self.ENGS}
        know = {e: {} for e in self.ENGS}
        for op in ops:
            e = op.eng
            K = know[e]
            for d in sorted(op.deps):
                sk, sv = ops[d].sig
                if K.get(sk, 0) >= sv:
                    continue
                op.waits.append((sk, sv))
                K[sk] = sv
                for k2, v2 in ops[d].ksnap.items():
                    if K.get(k2, 0) < v2:
                        K[k2] = v2
            if op.dma:
                op.signal = True
            elif has_dep[op.idx]:
                cnt[e] += 1
                ep = cnt[e] // self.EPOCH
                op.sig = (("eng", e, ep), cnt[e] - ep * self.EPOCH + (1 if ep else 0))
                op.signal = True
            op.ksnap = dict(K)
        self.final_waits = []
        Kf = know[final_engine]
        for key, v in dma_cnt.items():
            if Kf.get(key, 0) < v:
                self.final_waits.append((key, v))
        self.semkeys = set(op.sig[0] for op in ops if op.signal)
        self.final_engine = final_engine

    def emit(self):
        nc = self.nc
        sems = {}
        for i, k in enumerate(sorted(self.semkeys, key=str)):
            sems[k] = nc.alloc_semaphore("s%d" % i)
        by_eng = {e: [] for e in self.ENGS}
        for op in self.ops:
            by_eng[op.eng].append(op)
        fin_eng = self.final_engine
        fin_waits = self.final_waits

        def run(eng_name, eng):
            for op in by_eng[eng_name]:
                for sk, sv in op.waits:
                    eng.wait_ge(sems[sk], sv)
                ins = op.fn(eng)
                if op.signal:
                    ins.then_inc(sems[op.sig[0]], 16 if op.dma else 1)
            if eng_name == fin_eng:
                for sk, sv in fin_waits:
                    eng.wait_ge(sems[sk], sv)

        with nc.Block() as block:
            @block.tensor
            def _(e):
                run("pe", e)

            @block.scalar
            def _(e):
                run("act", e)

            @block.vector
            def _(e):
                run("dve", e)

            @block.gpsimd
            def _(e):
                run("pool", e)

            @block.sync
            def _(e):
                run("sp", e)


class T:
    __slots__ = ("ap", "key")

    def __init__(self, ap, key):
        self.ap = ap
        self.key = key

    def __getitem__(self, idx):
        return T(self.ap[idx], self.key)

    def v(self, ap):
        return T(ap, self.key)


class Ring:
    def __init__(self, items):
        self.items = items
        self.i = 0

    def next(self):
        it = self.items[self.i % len(self.items)]
        self.i += 1
        return it


def _rope_tables():
    t = np.arange(SEQ)
    row = (t // GRID_W).astype(np.float32)
    col = (t % GRID_W).astype(np.float32)
    quarter = HD // 4
    inv = (np.float32(10000.0) ** (-np.arange(quarter, dtype=np.float32) / np.float32(quarter))).astype(np.float32)
    ar = row[:, None] * inv
    ac = col[:, None] * inv
    ang = np.concatenate([ar, ar, ac, ac], axis=-1).astype(np.float32)
    cos = np.cos(ang).astype(np.float32)
    sin = np.sin(ang).astype(np.float32)
    sgn = np.concatenate([-np.ones(16), np.ones(16), -np.ones(16), np.ones(16)]).astype(np.float32)
    return cos, (sin * sgn[None, :]).astype(np.float32)


NA_PAT_TILES = [0, 1, 2, 14, 15]


def _na_pattern(t):
    return {0: 0, 1: 1, 14: 3, 15: 4}.get(t, 2)


def _na_keytiles(t):
    if t <= 1:
        return [0, 1, 2, 3]
    if t >= 14:
        return [12, 13, 14, 15]
    return [t - 2, t - 1, t, t + 1, t + 2]


def _na_index_table():
    rows = SEQ // GRID_W
    wh, ww = 8, 16
    nrel = 15 * 31
    masked = NH * nrel
    tab = np.full((5, 5, 128, NH, 128), masked, dtype=np.int64)
    for pi, t in enumerate(NA_PAT_TILES):
        kts = _na_keytiles(t)
        q = t * 128 + np.arange(128)
        r = q // GRID_W
        c = q % GRID_W
        rs = np.clip(r - wh // 2, 0, rows - wh)
        cs = np.clip(c - ww // 2, 0, GRID_W - ww)
        for j, kt in enumerate(kts):
            k = kt * 128 + np.arange(128)
            kr = k // GRID_W
            kc = k % GRID_W
            inwin = ((kr[:, None] >= rs[None, :]) & (kr[:, None] < rs[None, :] + wh)
                     & (kc[:, None] >= cs[None, :]) & (kc[:, None] < cs[None, :] + ww))
            rel = (kr[:, None] - r[None, :] + 7) * 31 + (kc[:, None] - c[None, :] + 15)
            for h in range(NH):
                tab[pi, j, :, h, :] = np.where(inwin, h * nrel + rel, masked)
    return tab


def build(NB=2, n_layers=DEPTH):
    nc = bass.Bass("TRN2", target_bir_lowering=False)
    R = NB + 1
    NTOK = NB * NT_B * 128

    def din(name, shape, dt=F32):
        return nc.dram_tensor(name, list(shape), dt, kind="ExternalInput").ap()

    x_in = din("x", [NB * SEQ, D])
    ctx_in = din("ctx", [NB * CTX, D])
    cvec = din("cvec", [R, D])
    w_ada = din("w_ada", [DEPTH, D, 6 * D])
    b_ada = din("b_ada", [DEPTH, 6 * D])
    g_attn = din("g_attn", [DEPTH, D])
    w_qkv = din("w_qkv", [DEPTH, D, 1536])
    w_o = din("w_o", [DEPTH, D, D])
    sink_a = din("sink_a", [2, NH])
    gq_b = din("gq_b", [1, HD])
    gk_b = din("gk_b", [1, HD])
    g_ffn = din("g_ffn", [DEPTH, D])
    w_rg = din("w_rg", [DEPTH, D, 36])
    b_rg = din("b_rg", [DEPTH, 36])
    w_gate = din("w_gate", [DEPTH, NE, D, FH])
    w_up = din("w_up", [DEPTH, NE, D, FH])
    w_down = din("w_down", [DEPTH, NE, FH, D])
    g_final = din("g_final", [1, D])
    c_ident = din("c_ident", [128, 128])
    c_cos = din("c_cos", [SEQ, HD])
    c_sin = din("c_sin", [SEQ, HD])
    c_masks = din("c_masks", [2, 128, 512])
    c_tri = din("c_tri", [128, 128])
    c_ec = din("c_ec", [1, NE])
    c_natab = din("c_natab", [5, 5, 128, NH * 128])
    out = nc.dram_tensor("out", [NB * SEQ, D], F32, kind="ExternalOutput").ap()

    xs = nc.dram_tensor("xs", [NTOK, D], F32).ap()
    modd = nc.dram_tensor("modd", [DEPTH, R, 6 * D], F32).ap()
    XG = nc.dram_tensor("XG", [NE * CAP + 1, D], BF16).ap()
    YG = nc.dram_tensor("YG", [NE * CAP + 1, D], F32).ap()

    P = Prog(nc)

    POOL_KB = 188
    pool = nc.alloc_sbuf_tensor("pool", [128, POOL_KB * 256], F32)
    alloc_state = {"off": 0}

    def a32(name, n, parts=128):
        off = alloc_state["off"]
        alloc_state["off"] = off + n
        assert alloc_state["off"] <= POOL_KB * 256, (name, alloc_state["off"])
        return T(pool[0:parts, off:off + n], name)

    def a16(name, n, parts=128):
        n32 = (n + 1) // 2
        off = alloc_state["off"]
        alloc_state["off"] = off + n32
        assert alloc_state["off"] <= POOL_KB * 256, (name, alloc_state["off"])
        return T(pool[0:parts, off:off + n32].bitcast(BF16), name)

    ps = [T(nc.alloc_psum_tensor("ps%d" % i, [128, 512], F32)[:, :], "ps%d" % i) for i in range(8)]

    def psbf(i):
        return T(ps[i].ap.bitcast(BF16), ps[i].key)

    ident = a32("ident", 128)
    identb = a16("identb", 128)
    onesb = a16("onesb", 128)
    trib = a16("trib", 128)
    cos_t = a32("cos", NT_L * HD)
    sin_t = a32("sin", NT_L * HD)
    maskLo = a16("maskLo", 512)
    maskHi = a16("maskHi", 512)
    gq_bc = a32("gq_bc", HD)
    gk_bc = a32("gk_bc", HD)
    ec_bc = a32("ec_bc", NE)
    brg_bc = a32("brg_bc", 36)
    tot = a32("tot", NE)
    NTT = NB * NT_B
    rt_w1 = a32("rt_w1", NTT)
    rt_w2 = a32("rt_w2", NTT)
    rt_d1 = T(nc.alloc_sbuf_tensor("rt_d1", [128, NTT], I32)[:, :], "rt_d1")
    rt_d2 = T(nc.alloc_sbuf_tensor("rt_d2", [128, NTT], I32)[:, :], "rt_d2")
    small = a32("small", 64)
    PERSIST = alloc_state["off"]

    def dma(q, out_, in_, r=(), w=()):
        oa = out_.ap if isinstance(out_, T) else out_
        ia = in_.ap if isinstance(in_, T) else in_
        rr = list(r) + ([in_.key] if isinstance(in_, T) else [])
        ww = list(w) + ([out_.key] if isinstance(out_, T) else [])
        P.add(q, lambda e: e.dma_start(out=oa, in_=ia), rr, ww, dma=True)

    def act(out_, in_, func, scale=1.0, bias=0.0, accum=None, r=(), w=()):
        kw = {}
        rr = [in_.key] + list(r)
        ww = [out_.key] + list(w)
        if isinstance(bias, T):
            rr.append(bias.key)
            bias = bias.ap
        if isinstance(scale, T):
            rr.append(scale.key)
            scale = scale.ap
        if accum is not None:
            kw["accum_out"] = accum.ap
            ww.append(accum.key)
        oa, ia = out_.ap, in_.ap
        P.add("act", lambda e: e.activation(out=oa, in_=ia, func=func, bias=bias, scale=scale, **kw), rr, ww)

    def tt(eng, out_, a, b, op):
        oa, aa, ba = out_.ap, a.ap, b.ap
        P.add(eng, lambda e: e.tensor_tensor(out=oa, in0=aa, in1=ba, op=op), [a.key, b.key], [out_.key])

    def ts(eng, out_, a, s1, op0, s2=None, op1=None, accum=None):
        rr = [a.key]
        ww = [out_.key]
        if isinstance(s1, T):
            rr.append(s1.key)
            s1 = s1.ap
        if isinstance(s2, T):
            rr.append(s2.key)
            s2 = s2.ap
        kw = {}
        if op1 is not None:
            kw["op1"] = op1
        if accum is not None:
            kw["accum_out"] = accum.ap
            ww.append(accum.key)
        oa, aa = out_.ap, a.ap
        P.add(eng, lambda e: e.tensor_scalar(out=oa, in0=aa, scalar1=s1, scalar2=s2, op0=op0, **kw), rr, ww)

    def stt(eng, out_, a, s, b, op0, op1, accum=None):
        rr = [a.key, b.key]
        ww = [out_.key]
        if isinstance(s, T):
            rr.append(s.key)
            s = s.ap
        kw = {}
        if accum is not None:
            kw["accum_out"] = accum.ap
            ww.append(accum.key)
        oa, aa, ba = out_.ap, a.ap, b.ap
        P.add(eng, lambda e: e.scalar_tensor_tensor(out=oa, in0=aa, scalar=s, in1=ba, op0=op0, op1=op1, **kw), rr, ww)

    def cp(eng, out_, in_):
        oa, ia = out_.ap, in_.ap
        P.add(eng, lambda e: e.tensor_copy(out=oa, in_=ia), [in_.key], [out_.key])

    def recip(out_, in_):
        oa, ia = out_.ap, in_.ap
        P.add("dve", lambda e: e.reciprocal(out=oa, in_=ia), [in_.key], [out_.key])

    def red(out_, in_, op, axis=AX.X):
        oa, ia = out_.ap, in_.ap
        P.add("dve", lambda e: e.tensor_reduce(out=oa, in_=ia, axis=axis, op=op), [in_.key], [out_.key])

    def memset(eng, out_, val):
        oa = out_.ap
        P.add(eng, lambda e: e.memset(oa, val), [], [out_.key])

    def mm_group(out_, pairs, r=()):
        oa = out_.ap
        n = len(pairs)

        def fn(e):
            ins = None
            for i, (l, rh) in enumerate(pairs):
                ins = e.matmul(oa, lhsT=l, rhs=rh, start=(i == 0), stop=(i == n - 1))
            return ins
        P.add("pe", fn, list(r), [out_.key])

    def tr_group(items, r=(), w=()):
        def fn(e):
            ins = None
            for (o, i, idn) in items:
                ins = e.transpose(out=o, in_=i, identity=idn)
            return ins
        P.add("pe", fn, list(r), list(w))

    dma("sp", ident, c_ident)
    dma("pool", identb, c_ident)
    dma("pool", trib, c_tri)
    dma("sp", cos_t.v(cos_t.ap.rearrange("p (t d) -> p t d", d=HD)), c_cos.rearrange("(t p) d -> p t d", p=128))
    dma("sp", sin_t.v(sin_t.ap.rearrange("p (t d) -> p t d", d=HD)), c_sin.rearrange("(t p) d -> p t d", p=128))
    dma("pool", maskLo, c_masks[0])
    dma("pool", maskHi, c_masks[1])
    dma("sp", gq_bc, gq_b.partition_broadcast(128))
    dma("sp", gk_bc, gk_b.partition_broadcast(128))
    dma("sp", ec_bc, c_ec.partition_broadcast(128))
    memset("dve", onesb, 1.0)

    for b in range(NB):
        base = b * NT_B * 128
        dma("sp", xs[base:base + SEQ, :], x_in[b * SEQ:(b + 1) * SEQ, :], w=[("xs", b * NT_B + t) for t in range(NT_L)])
        dma("sp", xs[base + SEQ:base + SEQ + CTX, :], ctx_in[b * CTX:(b + 1) * CTX, :],
            w=[("xs", b * NT_B + NT_L + t) for t in range(NT_C)])

    alloc_state["off"] = PERSIST
    zrow = a32("zrow", D, parts=1)
    memset("dve", zrow, 0.0)
    dma("sp", YG[NE * CAP:NE * CAP + 1, :], zrow, w=["YG"])
    cv = a32("cv", D, parts=R)
    scT = a32("scT", 8 * R)
    wada = [a32("wada%d" % i, 8 * 512) for i in range(2)]
    modsb = a32("modsb", 6 * D, parts=R)
    bada = a32("bada", 6 * D, parts=R)
    gA = a32("gA", D, parts=R)
    gF = a32("gF", D, parts=R)
    dma("sp", cv, cvec)
    act(cv, cv, AF.Silu)
    tr_group([(ps[0].ap[:, kc * R:(kc + 1) * R], cv.ap[:, kc * 128:(kc + 1) * 128], ident.ap[0:R, 0:R]) for kc in range(8)],
             r=[cv.key, ident.key], w=[ps[0].key])
    cp("dve", scT, ps[0][:, 0:8 * R])
    for li in range(n_layers):
        dma("sp", bada, b_ada[li:li + 1, :].partition_broadcast(R))
        dma("sp", gA, g_attn[li:li + 1, :].partition_broadcast(R))
        dma("sp", gF, g_ffn[li:li + 1, :].partition_broadcast(R))
        for n in range(12):
            wt = wada[n % 2]
            dma("sp" if n % 2 == 0 else "act", wt.v(wt.ap.rearrange("p (k f) -> p k f", f=512)),
                w_ada[li, :, n * 512:(n + 1) * 512].rearrange("(k p) f -> p k f", p=128))
            pp = ps[1 + (n % 2)]
            mm_group(pp[0:R, :], [(scT.ap[:, kc * R:(kc + 1) * R], wt.ap[:, kc * 512:(kc + 1) * 512]) for kc in range(8)],
                     r=[scT.key, wt.key])
            tt("dve", modsb[:, n * 512:(n + 1) * 512], pp[0:R, :], bada[:, n * 512:(n + 1) * 512], ALU.add)
        stt("dve", modsb[:, D:2 * D], modsb[:, D:2 * D], 1.0, gA, ALU.add, ALU.mult)
        stt("dve", modsb[:, 4 * D:5 * D], modsb[:, 4 * D:5 * D], 1.0, gF, ALU.add, ALU.mult)
        dma("sp", modd[li], modsb, w=[("modd", li)])

    def norm_h(xt, h, gs_bc, sh_bc, st):
        stt("dve", h, xt, 1.0 / D, xt, ALU.mult, ALU.mult, accum=st[:, 0:1])
        act(st[:, 1:2], st[:, 0:1], AF.Ln, bias=EPS)
        act(st[:, 2:3], st[:, 1:2], AF.Exp, scale=-0.5)
        stt("dve", h, xt, st[:, 2:3], gs_bc, ALU.mult, ALU.mult)
        tt("pool", h, h, sh_bc, ALU.add)

    def transpose_h(h, dst, dst_is_bf16, pbanks):
        for half in range(2):
            pp = pbanks[half]
            tr_group([(pp.ap[:, j * 128:(j + 1) * 128], h.ap[:, (half * 4 + j) * 128:(half * 4 + j + 1) * 128], ident.ap)
                      for j in range(4)], r=[h.key, ident.key], w=[pp.key])
            d = dst[:, half * 512:(half + 1) * 512]
            if half == 0:
                act(d, pp, AF.Copy)
            else:
                cp("dve", d, pp)

    stat_ring = Ring([T(small.ap[:, i * 4:(i + 1) * 4], ("stat", i)) for i in range(4)])

    for li in range(n_layers):
        mtype = li % 3
        jidx = li // 3
        last = li == n_layers - 1
        rope = mtype in (0, 1)

        P.barrier()
        alloc_state["off"] = PERSIST
        wqkv = a16("wqkv", 8 * 1536)
        wo = a16("wo", NH * D, parts=64)
        kT = a16("kT", NKV * NT_B * 128, parts=64)
        Vs = a16("Vs", NT_B * 256)
        modA = [a32("modA%d" % i, D) for i in range(3)]
        xts = Ring([a32("xt%d" % i, D) for i in range(2)])
        h = a32("h", D)
        hTs = Ring([a16("hT%d" % i, 8 * 128) for i in range(2)])
        q32 = a32("q32", D)
        qro = a32("qro", D)
        qtmp = a32("qtmp", D)
        qbf = [a16("qbf%d" % i, D) for i in range(2)]
        qT = [a16("qT%d" % i, NH * 128, parts=64) for i in range(2)]
        kb = a16("kb", 256)
        pTs = Ring([a16("pT%d" % i, 512) for i in range(3)])
        sbS = Ring([a32("sbS%d" % i, 512) for i in range(2)])
        nat = Ring([a32("nat%d" % i, 512) for i in range(2)])
        den = a32("den", 512, parts=64)
        rden = a32("rden", 512, parts=64)
        oT = a16("oT", NH * 128, parts=64)
        ytmp = Ring([a32("ytmp%d" % i, 512) for i in range(2)])
        sinke = a32("sinke", NH * 128, parts=64)
        sink_s = a32("sink_s", NH, parts=64)
        hst = a32("hst", 64)

        dma("pool", wqkv.v(wqkv.ap.rearrange("p (k n) -> p k n", n=1536)),
            w_qkv[li].rearrange("(k p) n -> p k n", p=128))
        dma("pool", wo.v(wo.ap.rearrange("p (h n) -> p h n", n=D)), w_o[li].rearrange("(h p) n -> p h n", p=64))
        use_sink = mtype == 0
        if use_sink:
            dma("sp", sink_s, sink_a[jidx:jidx + 1, :].partition_broadcast(64))
            act(sink_s, sink_s, AF.Exp)
            cp("dve", sinke.v(sinke.ap.rearrange("p (h q) -> p h q", q=128)),
               sink_s.v(sink_s.ap.unsqueeze(2).to_broadcast([64, NH, 128])))

        def load_modA(row):
            for i, c in enumerate((1, 0, 2)):
                dma("sp", modA[i], modd[li, row:row + 1, c * D:(c + 1) * D].partition_broadcast(128), r=[("modd", li)])

        def head_norm(src, nheads, g_bc, dst):
            W = nheads * HD
            sq = qtmp[:, 0:W]
            act(sq, src, AF.Square)
            ssum = hst[:, 0:nheads]
            red(ssum, sq.v(sq.ap.rearrange("p (h d) -> p h d", d=HD)), ALU.add)
            act(hst[:, 16:16 + nheads], ssum, AF.Ln, scale=1.0 / HD, bias=EPS)
            act(hst[:, 32:32 + nheads], hst[:, 16:16 + nheads], AF.Exp, scale=-0.5)
            rs_b = hst.v(hst.ap[:, 32:32 + nheads].unsqueeze(2).to_broadcast([128, nheads, HD]))
            d3 = dst.v(dst.ap.rearrange("p (h d) -> p h d", d=HD))
            s3 = src.v(src.ap.rearrange("p (h d) -> p h d", d=HD))
            tt("dve", d3, s3, rs_b, ALU.mult)
            g_b = g_bc.v(g_bc.ap.unsqueeze(1).to_broadcast([128, nheads, HD]))
            tt("pool", d3, d3, g_b, ALU.mult)

        def apply_rope(src, nheads, t, dst):
            W = nheads * HD
            cs = cos_t.v(cos_t.ap[:, t * HD:(t + 1) * HD].unsqueeze(1).to_broadcast([128, nheads, HD]))
            s3 = src.v(src.ap.rearrange("p (h d) -> p h d", d=HD))
            d3 = dst.v(dst.ap.rearrange("p (h d) -> p h d", d=HD))
            t3 = qtmp.v(qtmp.ap[:, 0:W].rearrange("p (h d) -> p h d", d=HD))
            tt("dve", d3, s3, cs, ALU.mult)
            s5 = src.ap.rearrange("p (h a b d) -> p h a b d", a=2, b=2, d=16)
            t5 = qtmp.ap[:, 0:W].rearrange("p (h a b d) -> p h a b d", a=2, b=2, d=16)
            sn5 = sin_t.ap[:, t * HD:(t + 1) * HD].rearrange("p (a b d) -> p a b d", a=2, b=2, d=16)
            for bsel in range(2):
                o_ = T(t5[:, :, :, bsel, :], qtmp.key)
                i_ = T(s5[:, :, :, 1 - bsel, :], src.key)
                sn = T(sn5[:, :, bsel, :].unsqueeze(1).to_broadcast([128, nheads, 2, 16]), sin_t.key)
                tt("dve" if bsel == 0 else "pool", o_, i_, sn, ALU.mult)
            tt("pool", d3, d3, t3, ALU.add)

        for b in range(NB):
            tile0 = b * NT_B
            cur_row = None
            for t in range(NT_B):
                is_ctx = t >= NT_L
                row = NB if is_ctx else b
                if row != cur_row:
                    load_modA(row)
                    cur_row = row
                gt = tile0 + t
                xt = xts.next()
                dma("sp", xt, xs[gt * 128:(gt + 1) * 128, :], r=[("xs", gt)])
                st = stat_ring.next()
                norm_h(xt, h, modA[0], modA[1], st)
                hT = hTs.next()
                transpose_h(h, hT, True, (ps[0], ps[1]))
                mm_group(ps[2], [(hT.ap[:, kc * 128:(kc + 1) * 128], wqkv.ap[:, kc * 1536 + 1024:kc * 1536 + 1536])
                                 for kc in range(8)], r=[hT.key, wqkv.key])
                ksrc = ps[2][:, 0:256]
                if mtype == 1:
                    head_norm(ksrc, NKV, gk_bc, q32[:, 0:256])
                    ksrc = q32[:, 0:256]
                if rope and not is_ctx:
                    if mtype != 1:
                        cp("dve", q32[:, 0:256], ksrc)
                        ksrc = q32[:, 0:256]
                    apply_rope(ksrc, NKV, t, qro[:, 0:256])
                    ksrc = qro[:, 0:256]
                act(kb, ksrc, AF.Copy)
                act(Vs[:, t * 256:(t + 1) * 256], ps[2][:, 256:512], AF.Copy)
                pb = psbf(3)
                tr_group([(pb.ap[0:64, g * 128:(g + 1) * 128], kb.ap[:, g * HD:(g + 1) * HD], identb.ap) for g in range(NKV)],
                         r=[kb.key, identb.key], w=[pb.key])
                kTv = kT.v(kT.ap.rearrange("p (g n) -> p g n", g=NKV)[:, :, t * 128:(t + 1) * 128])
                cp("dve", kTv, pb.v(pb.ap[0:64, 0:512].rearrange("p (g n) -> p g n", g=NKV)))

            q_tiles = list(range(NT_L)) + ([] if last else list(range(NT_L, NT_B)))
            cur_row = None
            for t in q_tiles:
                is_ctx = t >= NT_L
                row = NB if is_ctx else b
                if row != cur_row:
                    load_modA(row)
                    cur_row = row
                gt = tile0 + t
                xt = xts.next()
                dma("sp", xt, xs[gt * 128:(gt + 1) * 128, :], r=[("xs", gt)])
                st = stat_ring.next()
                norm_h(xt, h, modA[0], modA[1], st)
                hT = hTs.next()
                transpose_h(h, hT, True, (ps[0], ps[1]))
                for half in range(2):
                    mm_group(ps[2 + half], [(hT.ap[:, kc * 128:(kc + 1) * 128],
                                             wqkv.ap[:, kc * 1536 + half * 512:kc * 1536 + (half + 1) * 512])
                                            for kc in range(8)], r=[hT.key, wqkv.key])
                for half in range(2):
                    if mtype == 1:
                        head_norm(ps[2 + half], 8, gq_bc, q32[:, half * 512:(half + 1) * 512])
                    else:
                        cp("dve", q32[:, half * 512:(half + 1) * 512], ps[2 + half])
                variants = [0]
                act(qbf[0], q32, AF.Copy, scale=0.125)
                if rope and not is_ctx:
                    for half in range(2):
                        apply_rope(q32[:, half * 512:(half + 1) * 512], 8, t, qro[:, half * 512:(half + 1) * 512])
                    act(qbf[1], qro, AF.Copy, scale=0.125)
                    variants = [0, 1]
                for vi in variants:
                    for hh in range(2):
                        pb = psbf(4 + hh)
                        tr_group([(pb.ap[0:64, j * 128:(j + 1) * 128], qbf[vi].ap[:, (hh * 8 + j) * HD:(hh * 8 + j + 1) * HD],
                                   identb.ap) for j in range(8)], r=[qbf[vi].key, identb.key], w=[pb.key])
                        if hh == 0:
                            cp("dve", qT[vi][:, hh * 1024:(hh + 1) * 1024], pb[0:64, :])
                        else:
                            act(qT[vi][:, hh * 1024:(hh + 1) * 1024], pb[0:64, :], AF.Copy)
                if is_ctx:
                    KL = [(NT_L + j, 0, None, None) for j in range(NT_C)]
                else:
                    KL = []
                    if mtype == 0:
                        for kt_ in (t - 1, t, t + 1):
                            if 0 <= kt_ < NT_L:
                                m = maskLo if kt_ == t - 1 else (maskHi if kt_ == t + 1 else None)
                                KL.append((kt_, 1, m, None))
                    elif mtype == 1:
                        KL = [(kt_, 1, None, None) for kt_ in range(NT_L)]
                    else:
                        for j, kt_ in enumerate(_na_keytiles(t)):
                            KL.append((kt_, 0, None, (_na_pattern(t), j)))
                    KL += [(NT_L + j, 0, None, None) for j in range(NT_C)]
                nk = len(KL)
                items = [(g, ki) + KL[ki] for g in range(NKV) for ki in range(nk)]

                def emit_S(i, items=None, qT=qT):
                    g, ki, kt_, vi, msk, natp = ITEMS[i]
                    pS = ps[4 + (i % 2)]
                    kslice = kT.ap[:, g * NT_B * 128 + kt_ * 128: g * NT_B * 128 + (kt_ + 1) * 128]
                    mm_group(pS, [(kslice, qT[vi].ap[:, g * 512:(g + 1) * 512])], r=[kT.key, qT[vi].key])

                ITEMS = items
                emit_S(0)
                for i, (g, ki, kt_, vi, msk, natp) in enumerate(items):
                    if i + 1 < len(items):
                        emit_S(i + 1)
                    pS = ps[4 + (i % 2)]
                    pT = pTs.next()
                    if natp is not None:
                        nb_ = nat.next()
                        dma("sp" if i % 2 == 0 else "act", nb_, c_natab[natp[0], natp[1], :, g * 512:(g + 1) * 512])
                        sS = sbS.next()
                        tt("dve", sS, pS, nb_, ALU.add)
                        act(pT, sS, AF.Exp)
                    else:
                        act(pT, pS, AF.Exp)
                    if msk is not None:
                        tt("pool", pT, pT, msk, ALU.mult)
                    oa6, oa7 = ps[6].ap[0:64, :], ps[7].ap[0:64, :]
                    vsl = Vs.ap[:, kt_ * 256 + g * HD: kt_ * 256 + (g + 1) * HD]
                    pTa = pT.ap
                    first, lastk = (ki == 0), (ki == nk - 1)

                    def pv(e, oa6=oa6, oa7=oa7, vsl=vsl, pTa=pTa, first=first, lastk=lastk):
                        e.matmul(oa6, lhsT=vsl, rhs=pTa, start=first, stop=lastk)
                        return e.matmul(oa7, lhsT=onesb.ap[:, 0:64], rhs=pTa, start=first, stop=lastk)
                    P.add("pe", pv, [Vs.key, pT.key, onesb.key], [ps[6].key, ps[7].key])
                    if lastk:
                        if use_sink:
                            tt("dve", den, ps[7][0:64, :], sinke[:, g * 512:(g + 1) * 512], ALU.add)
                            recip(rden, den)
                        else:
                            recip(rden, ps[7][0:64, :])
                        tt("dve", oT[:, g * 512:(g + 1) * 512], ps[6][0:64, :], rden, ALU.mult)
                for half in range(2):
                    mm_group(ps[2 + half], [(oT.ap[:, hh * 128:(hh + 1) * 128], wo.ap[:, hh * D + half * 512: hh * D + (half + 1) * 512])
                                            for hh in range(NH)], r=[oT.key, wo.key])
                    yt = ytmp.next()
                    tt("dve", yt, ps[2 + half], modA[2][:, half * 512:(half + 1) * 512], ALU.mult)
                    tt("pool", xt[:, half * 512:(half + 1) * 512], xt[:, half * 512:(half + 1) * 512], yt, ALU.add)
                dma("sp", xs[gt * 128:(gt + 1) * 128, :], xt, w=[("xs", gt)])

        P.barrier()
        alloc_state["off"] = PERSIST
        wgs = [a16("wg%d" % i, 8 * FH) for i in range(2)]
        wus = [a16("wu%d" % i, 8 * FH) for i in range(2)]
        wds = [a16("wd%d" % i, 4 * D) for i in range(2)]
        xgs = [a16("xg%d" % i, (CAP // 128) * D) for i in range(2)]
        xeT = a16("xeT", 8 * CAP)
        HT = a16("HT", 4 * CAP)
        sgs = Ring([a32("sg%d" % i, CAP) for i in range(2)])
        Ysb = Ring([a32("Ysb%d" % i, D) for i in range(2)])
        xts = Ring([a32("mxt%d" % i, D) for i in range(2)])
        h = a32("mh", D)
        hbfs = Ring([a16("hbf%d" % i, D) for i in range(2)])
        hT32 = a32("hT32", 8 * 128)
        modF = [[a32("modF%d_%d" % (s, i), D) for i in range(2)] for s in range(2)]
        gtF = [a32("gtF%d" % s, D) for s in range(2)]
        y1s = Ring([a32("y1_%d" % i, D) for i in range(2)])
        y2s = Ring([a32("y2_%d" % i, D) for i in range(2)])
        gfin = a32("gfin", D)
        wrg = a32("wrg", 8 * 36)
        rl = a32("rl", 512)

        def RL(name, off, n):
            return T(rl.ap[:, off:off + n], ("rl", name))

        L = RL("L", 0, 36)
        mg = RL("mg", 40, 1)
        nmg = RL("nmg", 41, 1)
        eg = RL("eg", 44, 4)
        sg_ = RL("sg", 48, 1)
        ptop = RL("ptop", 49, 1)
        ohg = RL("ohg", 52, 4)
        pen = RL("pen", 56, 4)
        Lm = RL("Lm", 64, 32)
        mx8 = RL("mx8", 96, 8)
        oh1 = RL("oh1", 104, 32)
        oh2 = RL("oh2", 136, 32)
        dd = RL("dd", 168, 1)
        ed = RL("ed", 169, 1)
        rd = RL("rd", 170, 1)
        Asum = RL("A", 172, 32)
        Abf = T(rl.ap[:, 204:220].bitcast(BF16), ("rl", "Abf"))
        slot = RL("slot", 224, 32)
        tmp32 = RL("tmp32", 256, 32)
        d1f = RL("d1f", 288, 1)
        d2f = RL("d2f", 289, 1)
        ov = RL("ov", 290, 1)
        slotp = RL("slotp", 296, 32)
        nov = RL("nov", 291, 1)

        m_tiles = []
        for b in range(NB):
            m_tiles += [(b * NT_B + t, b) for t in range(NT_L)]
        if not last:
            for b in range(NB):
                m_tiles += [(b * NT_B + NT_L + t, NB) for t in range(NT_C)]

        dma("sp", wrg.v(wrg.ap.rearrange("p (k n) -> p k n", n=36)), w_rg[li].rearrange("(k p) n -> p k n", p=128))
        dma("sp", brg_bc, b_rg[li:li + 1, :].partition_broadcast(128))
        memset("dve", tot, 0.0)
        if last:
            dma("sp", gfin, g_final.partition_broadcast(128))

        cur_row = None
        mset = -1
        for (gt, row) in m_tiles:
            if row != cur_row:
                mset += 1
                mf = modF[mset % 2]
                dma("sp", mf[0], modd[li, row:row + 1, 4 * D:5 * D].partition_broadcast(128), r=[("modd", li)])
                dma("sp", mf[1], modd[li, row:row + 1, 3 * D:4 * D].partition_broadcast(128), r=[("modd", li)])
                cur_row = row
            xt = xts.next()
            dma("sp", xt, xs[gt * 128:(gt + 1) * 128, :], r=[("xs", gt)])
            st = stat_ring.next()
            norm_h(xt, h, mf[0], mf[1], st)
            hbf = hbfs.next()
            act(hbf, h, AF.Copy)
            transpose_h(h, hT32, False, (ps[0], ps[1]))
            mm_group(ps[2][:, 0:36], [(hT32.ap[:, kc * 128:(kc + 1) * 128], wrg.ap[:, kc * 36:(kc + 1) * 36]) for kc in range(8)],
                     r=[hT32.key, wrg.key])
            tt("dve", L, ps[2][:, 0:36], brg_bc, ALU.add)
            red(mg, L[:, 0:4], ALU.max)
            ts("dve", nmg, mg, -1.0, ALU.mult)
            act(eg, L[:, 0:4], AF.Exp, bias=nmg, accum=sg_)
            recip(ptop, sg_)
            ts("dve", ohg, L[:, 0:4], mg, ALU.is_equal)
            ts("dve", pen, ohg, -1.0, ALU.add, 1e30, ALU.mult)
            tt("dve", Lm.v(Lm.ap.rearrange("p (g e) -> p g e", e=8)), L.v(L.ap[:, 4:36].rearrange("p (g e) -> p g e", e=8)),
               pen.v(pen.ap.unsqueeze(2).to_broadcast([128, 4, 8])), ALU.add)
            mxo, lmi = mx8.ap, Lm.ap
            P.add("dve", lambda e, mxo=mxo, lmi=lmi: e.max(out=mxo, in_=lmi), [Lm.key], [mx8.key])
            ts("dve", oh1, Lm, mx8[:, 0:1], ALU.is_equal)
            ts("dve", oh2, Lm, mx8[:, 1:2], ALU.is_equal)
            tt("dve", dd, mx8[:, 1:2], mx8[:, 0:1], ALU.subtract)
            act(ed, dd, AF.Exp)
            ts("dve", ed, ed, 1.0, ALU.add)
            recip(rd, ed)
            tt("dve", rt_w1[:, gt:gt + 1], ptop, rd, ALU.mult)
            tt("dve", rt_w2[:, gt:gt + 1], ptop, rt_w1[:, gt:gt + 1], ALU.subtract)
            tt("dve", Asum, oh1, oh2, ALU.add)
            cp("dve", Abf, Asum)
            mm_group(ps[3][:, 0:32], [(trib.ap, Abf.ap)], r=[trib.key, Abf.key])
            stt("dve", slot, ps[3][:, 0:32], 0.0, tot, ALU.add, ALU.add)
            mm_group(ps[3][:, 32:64], [(onesb.ap, Abf.ap)], r=[onesb.key, Abf.key])
            tt("dve", slotp, slot, ec_bc, ALU.add)
            for (oh, df, rtd) in ((oh1, d1f, rt_d1), (oh2, d2f, rt_d2)):
                tt("dve", tmp32, oh, slot, ALU.mult)
                red(ov, tmp32, ALU.add)
                ts("dve", ov, ov, float(CAP), ALU.is_ge)
                ts("dve", nov, ov, -1.0, ALU.mult, 1.0, ALU.add)
                tt("dve", tmp32, oh, slotp, ALU.mult)
                red(df, tmp32, ALU.add)
                tt("dve", df, df, nov, ALU.mult)
                stt("dve", df, ov, float(NE * CAP), df, ALU.mult, ALU.add)
                cp("dve", rtd[:, gt:gt + 1], df)
            tt("dve", tot, tot, ps[3][:, 32:64], ALU.add)
            for rtd in (rt_d1, rt_d2):
                ia = rtd.ap[:, gt:gt + 1]
                ha = hbf.ap
                P.add("pool", lambda e, ia=ia, ha=ha: e.indirect_dma_start(
                    out=XG, out_offset=bass.IndirectOffsetOnAxis(ap=ia, axis=0), in_=ha, in_offset=None),
                    [rtd.key, hbf.key], ["XG"], dma=True)

        for ex in range(NE):
            wg, wu, wd, xg = wgs[ex % 2], wus[ex % 2], wds[ex % 2], xgs[ex % 2]
            dma("pool", wg.v(wg.ap.rearrange("p (k f) -> p k f", f=FH)), w_gate[li, ex].rearrange("(k p) f -> p k f", p=128))
            dma("pool", wu.v(wu.ap.rearrange("p (k f) -> p k f", f=FH)), w_up[li, ex].rearrange("(k p) f -> p k f", p=128))
            dma("pool", wd.v(wd.ap.rearrange("p (k f) -> p k f", f=D)), w_down[li, ex].rearrange("(k p) f -> p k f", p=128))
            dma("sp", xg.v(xg.ap.rearrange("p (s d) -> p s d", d=D)),
                XG[ex * CAP:(ex + 1) * CAP, :].rearrange("(s p) d -> p s d", p=128), r=["XG"])
            NS = CAP // 128
            for s in range(NS):
                pb = psbf(s % 2)
                tr_group([(pb.ap[:, kc * 128:(kc + 1) * 128], xg.ap[:, s * D + kc * 128: s * D + (kc + 1) * 128], identb.ap)
                          for kc in range(8)], r=[xg.key, identb.key], w=[pb.key])
                dst = xeT.v(xeT.ap.rearrange("p (k c) -> p k c", c=CAP)[:, :, s * 128:(s + 1) * 128])
                src = pb.v(pb.ap.rearrange("p (k c) -> p k c", c=128))
                if s % 2 == 0:
                    cp("dve", dst, src)
                else:
                    act(dst, src, AF.Copy)
            for m in range(4):
                mm_group(ps[2 + (m % 2)], [(wg.ap[:, kc * FH + m * 128: kc * FH + (m + 1) * 128], xeT.ap[:, kc * CAP:(kc + 1) * CAP])
                                           for kc in range(8)], r=[wg.key, xeT.key])
                mm_group(ps[4 + (m % 2)], [(wu.ap[:, kc * FH + m * 128: kc * FH + (m + 1) * 128], xeT.ap[:, kc * CAP:(kc + 1) * CAP])
                                           for kc in range(8)], r=[wu.key, xeT.key])
                sgt = sgs.next()
                act(sgt, ps[2 + (m % 2)], AF.Silu)
                tt("dve", HT[:, m * CAP:(m + 1) * CAP], sgt, ps[4 + (m % 2)], ALU.mult)
            for s in range(NS):
                ysb = Ysb.next()
                for half in range(2):
                    pp = ps[6 + half]
                    mm_group(pp, [(HT.ap[:, m * CAP + s * 128: m * CAP + (s + 1) * 128], wd.ap[:, m * D + half * 512: m * D + (half + 1) * 512])
                                  for m in range(4)], r=[HT.key, wd.key])
                    if half == 0:
                        act(ysb[:, 0:512], pp, AF.Copy)
                    else:
                        cp("dve", ysb[:, 512:1024], pp)
                dma("sp", YG[ex * CAP + s * 128: ex * CAP + (s + 1) * 128, :], ysb, w=["YG"])

        cur_row = None
        mset = -1
        for (gt, row) in m_tiles:
            if row != cur_row:
                mset += 1
                gtf = gtF[mset % 2]
                dma("sp", gtf, modd[li, row:row + 1, 5 * D:6 * D].partition_broadcast(128), r=[("modd", li)])
                cur_row = row
            y1, y2 = y1s.next(), y2s.next()
            for (yy, rtd) in ((y1, rt_d1), (y2, rt_d2)):
                ia = rtd.ap[:, gt:gt + 1]
                ya = yy.ap
                P.add("pool", lambda e, ia=ia, ya=ya: e.indirect_dma_start(
                    out=ya, out_offset=None, in_=YG, in_offset=bass.IndirectOffsetOnAxis(ap=ia, axis=0)),
                    [rtd.key, "YG"], [yy.key], dma=True)
            xt = xts.next()
            dma("sp", xt, xs[gt * 128:(gt + 1) * 128, :], r=[("xs", gt)])
            ts("dve", y1, y1, rt_w1[:, gt:gt + 1], ALU.mult)
            stt("dve", y1, y2, rt_w2[:, gt:gt + 1], y1, ALU.mult, ALU.add)
            tt("pool", y1, y1, gtf, ALU.mult)
            tt("pool", xt, xt, y1, ALU.add)
            if not last:
                dma("sp", xs[gt * 128:(gt + 1) * 128, :], xt, w=[("xs", gt)])
            else:
                st = stat_ring.next()
                stt("dve", h, xt, 1.0 / D, xt, ALU.mult, ALU.mult, accum=st[:, 0:1])
                act(st[:, 1:2], st[:, 0:1], AF.Ln, bias=EPS)
                act(st[:, 2:3], st[:, 1:2], AF.Exp, scale=-0.5)
                stt("dve", h, xt, st[:, 2:3], gfin, ALU.mult, ALU.mult)
                b_ = gt // NT_B
                t_ = gt % NT_B
                r0 = b_ * SEQ + t_ * 128
                dma("sp", out[r0:r0 + 128, :], h)

    P.finalize()
    P.emit()
    return nc


_CACHE = {}


def _consts():
    if "c" not in _CACHE:
        cos, sin_s = _rope_tables()
        kk = np.arange(128)[:, None]
        qq = np.arange(128)[None, :]
        mlo = np.tile((kk >= qq).astype(np.float32), (1, 4))
        mhi = np.tile((kk <= qq).astype(np.float32), (1, 4))
        tri = (np.arange(128)[:, None] < np.arange(128)[None, :]).astype(np.float32)
        ec = (np.arange(NE, dtype=np.float32) * CAP).reshape(1, NE)
        _CACHE["c"] = dict(c_ident=np.eye(128, dtype=np.float32), c_cos=cos, c_sin=sin_s,
                           c_masks=np.stack([mlo, mhi]).astype(np.float32), c_tri=tri, c_ec=ec)
        _CACHE["naidx"] = _na_index_table()
    return _CACHE["c"], _CACHE["naidx"]


def make_in_maps(inputs, n_cores, NB):
    f = lambda a: np.ascontiguousarray(np.asarray(a, dtype=np.float32))
    consts, naidx = _consts()
    rpb = f(inputs["rpb_c"])[0].reshape(-1)
    rpb_ext = np.concatenate([rpb, np.array([NEG], dtype=np.float32)])
    natab = np.ascontiguousarray(rpb_ext[naidx].reshape(5, 5, 128, NH * 128))
    w_rg = np.ascontiguousarray(np.concatenate([f(inputs["w_group"]), f(inputs["w_router"])], axis=-1))
    b_rg = np.ascontiguousarray(np.concatenate([f(inputs["b_group"]), f(inputs["b_router"])], axis=-1))
    shared = dict(
        w_ada=f(inputs["w_ada"]), b_ada=f(inputs["b_ada"]), g_attn=f(inputs["g_attn"]), w_qkv=f(inputs["w_qkv"]),
        w_o=f(inputs["w_o"]), sink_a=f(inputs["sink_a"]), gq_b=f(inputs["gq_b"]), gk_b=f(inputs["gk_b"]),
        g_ffn=f(inputs["g_ffn"]), w_rg=w_rg, b_rg=b_rg, w_gate=f(inputs["w_gate"]), w_up=f(inputs["w_up"]),
        w_down=f(inputs["w_down"]), g_final=f(inputs["g_final"]).reshape(1, D), c_natab=natab, **consts)
    x = f(inputs["x"])
    ctx = f(inputs["ctx"])
    c = f(inputs["c"])
    c_ctx = f(inputs["c_ctx"]).reshape(1, D)
    maps = []
    for i in range(n_cores):
        sl = slice(i * NB, (i + 1) * NB)
        m = dict(shared)
        m["x"] = np.ascontiguousarray(x[sl].reshape(NB * SEQ, D))
        m["ctx"] = np.ascontiguousarray(ctx[sl].reshape(NB * CTX, D))
        m["cvec"] = np.ascontiguousarray(np.concatenate([c[sl], c_ctx], axis=0))
        maps.append(m)
    return maps


def kernel(**inputs):
    n_cores = 8
    NB = 2
    if "nc" not in _CACHE:
        _CACHE["nc"] = build(NB=NB)
    nc = _CACHE["nc"]
    maps = make_in_maps(inputs, n_cores, NB)
    res = run_bass_kernel_spmd(nc, maps, core_ids=list(range(n_cores)))
    outs = [r["out"].reshape(NB, SEQ, D) for r in res.results]
    return np.concatenate(outs, axis=0).astype(np.float32)
```

```python
import numpy as np
import concourse.bass as bass
import concourse.mybir as mybir
from concourse.bass_utils import run_bass_kernel_spmd

F32 = mybir.dt.float32
BF16 = mybir.dt.bfloat16
I32 = mybir.dt.int32
ALU = mybir.AluOpType
AF = mybir.ActivationFunctionType
AX = mybir.AxisListType

D = 1024
SEQ = 2048
CTX = 256
DEPTH = 4
NH = 16
NKV = 4
HD = 64
NE = 32
FH = 512
GRID_W = 64
import os as _os
OPT = dict(W_KV=int(_os.environ.get("W_KV", 2)), W_M1=int(_os.environ.get("W_M1", 2)), W_M3=int(_os.environ.get("W_M3", 2)),
           Q_OVERLAP=int(_os.environ.get("Q_OVERLAP", 1)), S_AHEAD=int(_os.environ.get("S_AHEAD", 1)),
           SINK_BC=int(_os.environ.get("SINK_BC", 1)), SKIP_ATT=int(_os.environ.get("SKIP_ATT", 0)),
           SKIP_M1=int(_os.environ.get("SKIP_M1", 0)), SKIP_M2=int(_os.environ.get("SKIP_M2", 0)), SKIP_M3=int(_os.environ.get("SKIP_M3", 0)))
CAP = 512
NT_L = SEQ // 128
NT_C = CTX // 128
NT_B = NT_L + NT_C
EPS = 1e-6
NEG = -1e30


class _Op:
    __slots__ = ("idx", "eng", "fn", "deps", "dma", "sig", "waits", "signal", "ksnap")

    def __init__(self, idx, eng, fn, dma):
        self.idx = idx
        self.eng = eng
        self.fn = fn
        self.deps = set()
        self.dma = dma
        self.sig = None
        self.waits = []
        self.signal = False
        self.ksnap = None


class Prog:
    ENGS = ("pe", "act", "dve", "pool", "sp")
    EPOCH = 20000
    NDMA = {"sp": 24, "pool": 24, "act": 8}

    def __init__(self, nc):
        self.nc = nc
        self.ops = []
        self.last_w = {}
        self.readers = {}
        self.pending = {}
        self.last_eng = {}
        self.dma_since = []

    def add(self, eng, fn, r=(), w=(), dma=False):
        op = _Op(len(self.ops), eng, fn, dma)
        ops = self.ops

        def same_eng(j):
            o = ops[j]
            return (not dma) and (not o.dma) and o.eng == eng

        for k in r:
            lw = self.last_w.get(k)
            if lw is not None and not (eng == "pe" and same_eng(lw)):
                op.deps.add(lw)
        for k in w:
            lw = self.last_w.get(k)
            if lw is not None and not (eng == "pe" and same_eng(lw)):
                op.deps.add(lw)
            rs = self.readers.get(k)
            if rs:
                for rd in rs:
                    if not same_eng(rd):
                        op.deps.add(rd)
        for k in r:
            self.readers.setdefault(k, []).append(op.idx)
        for k in w:
            self.last_w[k] = op.idx
            self.readers[k] = []
        if eng in self.pending:
            op.deps.update(self.pending.pop(eng))
        op.deps.discard(op.idx)
        self.ops.append(op)
        if dma:
            self.dma_since.append(op.idx)
        else:
            self.last_eng[eng] = op.idx
        return op

    def barrier(self):
        deps = set(self.last_eng.values()) | set(self.dma_since)
        for e in self.ENGS:
            self.pending[e] = set(deps) | self.pending.get(e, set())
        self.dma_since = []

    def finalize(self, final_engine="sp"):
        ops = self.ops
        dma_rr = {q: 0 for q in self.NDMA}
        dma_last = {}
        dma_cnt = {}
        for op in ops:
            if op.dma:
                q = op.eng
                s = dma_rr[q] % self.NDMA[q]
                dma_rr[q] += 1
                key = ("dma", q, s)
                prev = dma_last.get(key)
                if prev is not None:
                    op.deps.add(prev)
                dma_last[key] = op.idx
                dma_cnt[key] = dma_cnt.get(key, 0) + 16
                op.sig = (key, dma_cnt[key])
        has_dep = [False] * len(ops)
        for op in ops:
            for d in op.deps:
                has_dep[d] = True
        cnt = {e: 0 for e in self.ENGS}
        know = {e: {} for e in self.ENGS}
        for op in ops:
            e = op.eng
            K = know[e]
            for d in sorted(op.deps):
                sk, sv = ops[d].sig
                if K.get(sk, 0) >= sv:
                    continue
                op.waits.append((sk, sv))
                K[sk] = sv
                for k2, v2 in ops[d].ksnap.items():
                    if K.get(k2, 0) < v2:
                        K[k2] = v2
            if op.dma:
                op.signal = True
            elif has_dep[op.idx]:
                cnt[e] += 1
                ep = cnt[e] // self.EPOCH
                op.sig = (("eng", e, ep), cnt[e] - ep * self.EPOCH + (1 if ep else 0))
                op.signal = True
            op.ksnap = dict(K)
        self.final_waits = []
        Kf = know[final_engine]
        for key, v in dma_cnt.items():
            if Kf.get(key, 0) < v:
                self.final_waits.append((key, v))
        self.semkeys = set(op.sig[0] for op in ops if op.signal)
        self.final_engine = final_engine

    def emit(self):
        nc = self.nc
        sems = {}
        for i, k in enumerate(sorted(self.semkeys, key=str)):
            sems[k] = nc.alloc_semaphore("s%d" % i)
        by_eng = {e: [] for e in self.ENGS}
        for op in self.ops:
            by_eng[op.eng].append(op)
        fin_eng = self.final_engine
        fin_waits = self.final_waits

        def run(eng_name, eng):
            for op in by_eng[eng_name]:
                for sk, sv in op.waits:
                    eng.wait_ge(sems[sk], sv)
                ins = op.fn(eng)
                if op.signal:
                    ins.then_inc(sems[op.sig[0]], 16 if op.dma else 1)
            if eng_name == fin_eng:
                for sk, sv in fin_waits:
                    eng.wait_ge(sems[sk], sv)

        with nc.Block() as block:
            @block.tensor
            def _(e):
                run("pe", e)

            @block.scalar
            def _(e):
                run("act", e)

            @block.vector
            def _(e):
                run("dve", e)

            @block.gpsimd
            def _(e):
                run("pool", e)

            @block.sync
            def _(e):
                run("sp", e)


def interleave(gens, width, stagger):
    active = []
    it = iter(gens)
    pending_start = 0
    done = False
    while True:
        if not done and len(active) < width and pending_start <= 0:
            g_ = next(it, None)
            if g_ is None:
                done = True
            else:
                active.append(g_)
                pending_start = stagger
        if not active:
            if done:
                break
            pending_start = 0
            continue
        pending_start -= 1
        for g_ in list(active):
            try:
                next(g_)
            except StopIteration:
                active.remove(g_)


class T:
    __slots__ = ("ap", "key")

    def __init__(self, ap, key):
        self.ap = ap
        self.key = key

    def __getitem__(self, idx):
        return T(self.ap[idx], self.key)

    def v(self, ap):
        return T(ap, self.key)


class Ring:
    def __init__(self, items):
        self.items = items
        self.i = 0

    def next(self):
        it = self.items[self.i % len(self.items)]
        self.i += 1
        return it


def _rope_tables():
    t = np.arange(SEQ)
    row = (t // GRID_W).astype(np.float32)
    col = (t % GRID_W).astype(np.float32)
    quarter = HD // 4
    inv = (np.float32(10000.0) ** (-np.arange(quarter, dtype=np.float32) / np.float32(quarter))).astype(np.float32)
    ar = row[:, None] * inv
    ac = col[:, None] * inv
    ang = np.concatenate([ar, ar, ac, ac], axis=-1).astype(np.float32)
    cos = np.cos(ang).astype(np.float32)
    sin = np.sin(ang).astype(np.float32)
    sgn = np.concatenate([-np.ones(16), np.ones(16), -np.ones(16), np.ones(16)]).astype(np.float32)
    return cos, (sin * sgn[None, :]).astype(np.float32)


NA_PAT_TILES = [0, 1, 2, 14, 15]


def _na_pattern(t):
    return {0: 0, 1: 1, 14: 3, 15: 4}.get(t, 2)


def _na_keytiles(t):
    if t <= 1:
        return [0, 1, 2, 3]
    if t >= 14:
        return [12, 13, 14, 15]
    return [t - 2, t - 1, t, t + 1, t + 2]


def _na_index_table():
    rows = SEQ // GRID_W
    wh, ww = 8, 16
    nrel = 15 * 31
    masked = NH * nrel
    tab = np.full((5, 5, 128, NH, 128), masked, dtype=np.int64)
    for pi, t in enumerate(NA_PAT_TILES):
        kts = _na_keytiles(t)
        q = t * 128 + np.arange(128)
        r = q // GRID_W
        c = q % GRID_W
        rs = np.clip(r - wh // 2, 0, rows - wh)
        cs = np.clip(c - ww // 2, 0, GRID_W - ww)
        for j, kt in enumerate(kts):
            k = kt * 128 + np.arange(128)
            kr = k // GRID_W
            kc = k % GRID_W
            inwin = ((kr[:, None] >= rs[None, :]) & (kr[:, None] < rs[None, :] + wh)
                     & (kc[:, None] >= cs[None, :]) & (kc[:, None] < cs[None, :] + ww))
            rel = (kr[:, None] - r[None, :] + 7) * 31 + (kc[:, None] - c[None, :] + 15)
            for h in range(NH):
                tab[pi, j, :, h, :] = np.where(inwin, h * nrel + rel, masked)
    return tab


def build(NB=2, n_layers=DEPTH):
    nc = bass.Bass("TRN2", target_bir_lowering=False)
    R = NB + 1
    NTOK = NB * NT_B * 128

    def din(name, shape, dt=F32):
        return nc.dram_tensor(name, list(shape), dt, kind="ExternalInput").ap()

    x_in = din("x", [NB * SEQ, D])
    ctx_in = din("ctx", [NB * CTX, D])
    cvec = din("cvec", [R, D])
    w_ada = din("w_ada", [DEPTH, D, 6 * D])
    b_ada = din("b_ada", [DEPTH, 6 * D])
    g_attn = din("g_attn", [DEPTH, D])
    w_qkv = din("w_qkv", [DEPTH, D, 1536])
    w_o = din("w_o", [DEPTH, D, D])
    sink_a = din("sink_a", [2, NH])
    gq_b = din("gq_b", [1, HD])
    gk_b = din("gk_b", [1, HD])
    g_ffn = din("g_ffn", [DEPTH, D])
    w_rg = din("w_rg", [DEPTH, D, 36])
    b_rg = din("b_rg", [DEPTH, 36])
    w_gate = din("w_gate", [DEPTH, NE, D, FH])
    w_up = din("w_up", [DEPTH, NE, D, FH])
    w_down = din("w_down", [DEPTH, NE, FH, D])
    g_final = din("g_final", [1, D])
    c_ident = din("c_ident", [128, 128])
    c_cos = din("c_cos", [SEQ, HD])
    c_sin = din("c_sin", [SEQ, HD])
    c_masks = din("c_masks", [2, 128, 512])
    c_tri = din("c_tri", [128, 128])
    c_ec = din("c_ec", [1, NE])
    c_natab = din("c_natab", [5, 5, 128, NH * 128])
    out = nc.dram_tensor("out", [NB * SEQ, D], F32, kind="ExternalOutput").ap()

    xs = nc.dram_tensor("xs", [NTOK, D], F32).ap()
    modd = nc.dram_tensor("modd", [DEPTH, R, 6 * D], F32).ap()
    XG = nc.dram_tensor("XG", [NE * CAP + 1, D], BF16).ap()
    YG = nc.dram_tensor("YG", [NE * CAP + 1, D], F32).ap()

    P = Prog(nc)

    POOL_KB = 188
    pool = nc.alloc_sbuf_tensor("pool", [128, POOL_KB * 256], F32)
    alloc_state = {"off": 0}

    def a32(name, n, parts=128):
        off = alloc_state["off"]
        alloc_state["off"] = off + n
        assert alloc_state["off"] <= POOL_KB * 256, (name, alloc_state["off"])
        return T(pool[0:parts, off:off + n], name)

    def a16(name, n, parts=128):
        n32 = (n + 1) // 2
        off = alloc_state["off"]
        alloc_state["off"] = off + n32
        assert alloc_state["off"] <= POOL_KB * 256, (name, alloc_state["off"])
        return T(pool[0:parts, off:off + n32].bitcast(BF16), name)

    ps = [T(nc.alloc_psum_tensor("ps%d" % i, [128, 512], F32)[:, :], "ps%d" % i) for i in range(8)]

    def psbf(i):
        return T(ps[i].ap.bitcast(BF16), ps[i].key)

    ident = a32("ident", 128)
    identb = a16("identb", 128)
    onesb = a16("onesb", 128)
    trib = a16("trib", 128)
    cos_t = a32("cos", NT_L * HD)
    sin_t = a32("sin", NT_L * HD)
    maskLo = a16("maskLo", 512)
    maskHi = a16("maskHi", 512)
    gq_bc = a32("gq_bc", HD)
    gk_bc = a32("gk_bc", HD)
    ec_bc = a32("ec_bc", NE)
    brg_bc = a32("brg_bc", 36)
    tot = a32("tot", NE)
    NTT = NB * NT_B
    rt_w1 = a32("rt_w1", NTT)
    rt_w2 = a32("rt_w2", NTT)
    rt_d1 = T(nc.alloc_sbuf_tensor("rt_d1", [128, NTT], I32)[:, :], "rt_d1")
    rt_d2 = T(nc.alloc_sbuf_tensor("rt_d2", [128, NTT], I32)[:, :], "rt_d2")
    small = a32("small", 64)
    PERSIST = alloc_state["off"]

    def dma(q, out_, in_, r=(), w=()):
        oa = out_.ap if isinstance(out_, T) else out_
        ia = in_.ap if isinstance(in_, T) else in_
        rr = list(r) + ([in_.key] if isinstance(in_, T) else [])
        ww = list(w) + ([out_.key] if isinstance(out_, T) else [])
        P.add(q, lambda e: e.dma_start(out=oa, in_=ia), rr, ww, dma=True)

    def act(out_, in_, func, scale=1.0, bias=0.0, accum=None, r=(), w=()):
        kw = {}
        rr = [in_.key] + list(r)
        ww = [out_.key] + list(w)
        if isinstance(bias, T):
            rr.append(bias.key)
            bias = bias.ap
        if isinstance(scale, T):
            rr.append(scale.key)
            scale = scale.ap
        if accum is not None:
            kw["accum_out"] = accum.ap
            ww.append(accum.key)
        oa, ia = out_.ap, in_.ap
        P.add("act", lambda e: e.activation(out=oa, in_=ia, func=func, bias=bias, scale=scale, **kw), rr, ww)

    def tt(eng, out_, a, b, op):
        oa, aa, ba = out_.ap, a.ap, b.ap
        P.add(eng, lambda e: e.tensor_tensor(out=oa, in0=aa, in1=ba, op=op), [a.key, b.key], [out_.key])

    def ts(eng, out_, a, s1, op0, s2=None, op1=None, accum=None):
        rr = [a.key]
        ww = [out_.key]
        if isinstance(s1, T):
            rr.append(s1.key)
            s1 = s1.ap
        if isinstance(s2, T):
            rr.append(s2.key)
            s2 = s2.ap
        kw = {}
        if op1 is not None:
            kw["op1"] = op1
        if accum is not None:
            kw["accum_out"] = accum.ap
            ww.append(accum.key)
        oa, aa = out_.ap, a.ap
        P.add(eng, lambda e: e.tensor_scalar(out=oa, in0=aa, scalar1=s1, scalar2=s2, op0=op0, **kw), rr, ww)

    def stt(eng, out_, a, s, b, op0, op1, accum=None):
        rr = [a.key, b.key]
        ww = [out_.key]
        if isinstance(s, T):
            rr.append(s.key)
            s = s.ap
        kw = {}
        if accum is not None:
            kw["accum_out"] = accum.ap
            ww.append(accum.key)
        oa, aa, ba = out_.ap, a.ap, b.ap
        P.add(eng, lambda e: e.scalar_tensor_tensor(out=oa, in0=aa, scalar=s, in1=ba, op0=op0, op1=op1, **kw), rr, ww)

    def cp(eng, out_, in_):
        oa, ia = out_.ap, in_.ap
        P.add(eng, lambda e: e.tensor_copy(out=oa, in_=ia), [in_.key], [out_.key])

    def recip(out_, in_):
        oa, ia = out_.ap, in_.ap
        P.add("dve", lambda e: e.reciprocal(out=oa, in_=ia), [in_.key], [out_.key])

    def red(out_, in_, op, axis=AX.X):
        oa, ia = out_.ap, in_.ap
        P.add("dve", lambda e: e.tensor_reduce(out=oa, in_=ia, axis=axis, op=op), [in_.key], [out_.key])

    def memset(eng, out_, val):
        oa = out_.ap
        P.add(eng, lambda e: e.memset(oa, val), [], [out_.key])

    def mm_group(out_, pairs, r=()):
        oa = out_.ap
        n = len(pairs)

        def fn(e):
            ins = None
            for i, (l, rh) in enumerate(pairs):
                ins = e.matmul(oa, lhsT=l, rhs=rh, start=(i == 0), stop=(i == n - 1))
            return ins
        P.add("pe", fn, list(r), [out_.key])

    def tr_group(items, r=(), w=()):
        def fn(e):
            ins = None
            for (o, i, idn) in items:
                ins = e.transpose(out=o, in_=i, identity=idn)
            return ins
        P.add("pe", fn, list(r), list(w))

    dma("sp", ident, c_ident)
    dma("pool", identb, c_ident)
    dma("pool", trib, c_tri)
    dma("sp", cos_t.v(cos_t.ap.rearrange("p (t d) -> p t d", d=HD)), c_cos.rearrange("(t p) d -> p t d", p=128))
    dma("sp", sin_t.v(sin_t.ap.rearrange("p (t d) -> p t d", d=HD)), c_sin.rearrange("(t p) d -> p t d", p=128))
    dma("pool", maskLo, c_masks[0])
    dma("pool", maskHi, c_masks[1])
    dma("sp", gq_bc, gq_b.partition_broadcast(128))
    dma("sp", gk_bc, gk_b.partition_broadcast(128))
    dma("sp", ec_bc, c_ec.partition_broadcast(128))
    memset("dve", onesb, 1.0)

    for b in range(NB):
        base = b * NT_B * 128
        dma("sp", xs[base:base + SEQ, :], x_in[b * SEQ:(b + 1) * SEQ, :], w=[("xs", b * NT_B + t) for t in range(NT_L)])
        dma("sp", xs[base + SEQ:base + SEQ + CTX, :], ctx_in[b * CTX:(b + 1) * CTX, :],
            w=[("xs", b * NT_B + NT_L + t) for t in range(NT_C)])

    alloc_state["off"] = PERSIST
    zrow = a32("zrow", D, parts=1)
    memset("dve", zrow, 0.0)
    dma("sp", YG[NE * CAP:NE * CAP + 1, :], zrow, w=["YG"])
    cv = a32("cv", D, parts=R)
    scT = a32("scT", 8 * R)
    wada = [a32("wada%d" % i, 8 * 512) for i in range(2)]
    modsb = a32("modsb", 6 * D, parts=R)
    bada = a32("bada", 6 * D, parts=R)
    gA = a32("gA", D, parts=R)
    gF = a32("gF", D, parts=R)
    dma("sp", cv, cvec)
    act(cv, cv, AF.Silu)
    tr_group([(ps[0].ap[:, kc * R:(kc + 1) * R], cv.ap[:, kc * 128:(kc + 1) * 128], ident.ap[0:R, 0:R]) for kc in range(8)],
             r=[cv.key, ident.key], w=[ps[0].key])
    cp("dve", scT, ps[0][:, 0:8 * R])
    for li in range(n_layers):
        dma("sp", bada, b_ada[li:li + 1, :].partition_broadcast(R))
        dma("sp", gA, g_attn[li:li + 1, :].partition_broadcast(R))
        dma("sp", gF, g_ffn[li:li + 1, :].partition_broadcast(R))
        for n in range(12):
            wt = wada[n % 2]
            dma("sp" if n % 2 == 0 else "act", wt.v(wt.ap.rearrange("p (k f) -> p k f", f=512)),
                w_ada[li, :, n * 512:(n + 1) * 512].rearrange("(k p) f -> p k f", p=128))
            pp = ps[1 + (n % 2)]
            mm_group(pp[0:R, :], [(scT.ap[:, kc * R:(kc + 1) * R], wt.ap[:, kc * 512:(kc + 1) * 512]) for kc in range(8)],
                     r=[scT.key, wt.key])
            tt("dve", modsb[:, n * 512:(n + 1) * 512], pp[0:R, :], bada[:, n * 512:(n + 1) * 512], ALU.add)
        stt("dve", modsb[:, D:2 * D], modsb[:, D:2 * D], 1.0, gA, ALU.add, ALU.mult)
        stt("dve", modsb[:, 4 * D:5 * D], modsb[:, 4 * D:5 * D], 1.0, gF, ALU.add, ALU.mult)
        dma("sp", modd[li], modsb, w=[("modd", li)])

    def norm_h(xt, h, gs_bc, sh_bc, st):
        stt("dve", h, xt, 1.0 / D, xt, ALU.mult, ALU.mult, accum=st[:, 0:1])
        act(st[:, 1:2], st[:, 0:1], AF.Ln, bias=EPS)
        act(st[:, 2:3], st[:, 1:2], AF.Exp, scale=-0.5)
        stt("dve", h, xt, st[:, 2:3], gs_bc, ALU.mult, ALU.mult)
        tt("pool", h, h, sh_bc, ALU.add)

    def transpose_h(h, dst, dst_is_bf16, pbanks):
        for half in range(2):
            pp = pbanks[half]
            tr_group([(pp.ap[:, j * 128:(j + 1) * 128], h.ap[:, (half * 4 + j) * 128:(half * 4 + j + 1) * 128], ident.ap)
                      for j in range(4)], r=[h.key, ident.key], w=[pp.key])
            d = dst[:, half * 512:(half + 1) * 512]
            if half == 0:
                act(d, pp, AF.Copy)
            else:
                cp("dve", d, pp)

    stat_ring = Ring([T(small.ap[:, i * 4:(i + 1) * 4], ("stat", i)) for i in range(4)])

    for li in range(n_layers):
        mtype = li % 3
        jidx = li // 3
        last = li == n_layers - 1
        rope = mtype in (0, 1)

        P.barrier()
        alloc_state["off"] = PERSIST
        wqkv = a16("wqkv", 8 * 1536)
        wo = a16("wo", NH * D, parts=64)
        kT = a16("kT", NKV * NT_B * 128, parts=64)
        Vs = a16("Vs", NT_B * 256)
        modA = [a32("modA%d" % i, D) for i in range(3)]
        xts = Ring([a32("xt%d" % i, D) for i in range(2)])
        h = a32("h", D)
        hTs = Ring([a16("hT%d" % i, 8 * 128) for i in range(2)])
        q32 = a32("q32", D)
        qro = a32("qro", D)
        qtmp = a32("qtmp", D)
        qbf = [a16("qbf%d" % i, D) for i in range(2)]
        qT = [a16("qT%d" % i, NH * 128, parts=64) for i in range(2)]
        kb = a16("kb", 256)
        pTs = Ring([a16("pT%d" % i, 512) for i in range(3)])
        sbS = Ring([a32("sbS%d" % i, 512) for i in range(2)])
        nat = Ring([a32("nat%d" % i, 512) for i in range(2)])
        den = a32("den", 512, parts=64)
        rden = a32("rden", 512, parts=64)
        oT = a16("oT", NH * 128, parts=64)
        ytmp = Ring([a32("ytmp%d" % i, 512) for i in range(2)])
        sinke = a32("sinke", NH * 128, parts=64)
        sink_s = a32("sink_s", NH, parts=64)
        hst = a32("hst", 64)

        dma("pool", wqkv.v(wqkv.ap.rearrange("p (k n) -> p k n", n=1536)),
            w_qkv[li].rearrange("(k p) n -> p k n", p=128))
        dma("pool", wo.v(wo.ap.rearrange("p (h n) -> p h n", n=D)), w_o[li].rearrange("(h p) n -> p h n", p=64))
        use_sink = mtype == 0
        if use_sink:
            dma("sp", sink_s, sink_a[jidx:jidx + 1, :].partition_broadcast(64))
            act(sink_s, sink_s, AF.Exp)
            cp("dve", sinke.v(sinke.ap.rearrange("p (h q) -> p h q", q=128)),
               sink_s.v(sink_s.ap.unsqueeze(2).to_broadcast([64, NH, 128])))

        def load_modA(row):
            for i, c in enumerate((1, 0, 2)):
                dma("sp", modA[i], modd[li, row:row + 1, c * D:(c + 1) * D].partition_broadcast(128), r=[("modd", li)])

        def head_norm(src, nheads, g_bc, dst):
            W = nheads * HD
            sq = qtmp[:, 0:W]
            act(sq, src, AF.Square)
            ssum = hst[:, 0:nheads]
            red(ssum, sq.v(sq.ap.rearrange("p (h d) -> p h d", d=HD)), ALU.add)
            act(hst[:, 16:16 + nheads], ssum, AF.Ln, scale=1.0 / HD, bias=EPS)
            act(hst[:, 32:32 + nheads], hst[:, 16:16 + nheads], AF.Exp, scale=-0.5)
            rs_b = hst.v(hst.ap[:, 32:32 + nheads].unsqueeze(2).to_broadcast([128, nheads, HD]))
            d3 = dst.v(dst.ap.rearrange("p (h d) -> p h d", d=HD))
            s3 = src.v(src.ap.rearrange("p (h d) -> p h d", d=HD))
            tt("dve", d3, s3, rs_b, ALU.mult)
            g_b = g_bc.v(g_bc.ap.unsqueeze(1).to_broadcast([128, nheads, HD]))
            tt("pool", d3, d3, g_b, ALU.mult)

        def apply_rope(src, nheads, t, dst):
            W = nheads * HD
            cs = cos_t.v(cos_t.ap[:, t * HD:(t + 1) * HD].unsqueeze(1).to_broadcast([128, nheads, HD]))
            s3 = src.v(src.ap.rearrange("p (h d) -> p h d", d=HD))
            d3 = dst.v(dst.ap.rearrange("p (h d) -> p h d", d=HD))
            t3 = qtmp.v(qtmp.ap[:, 0:W].rearrange("p (h d) -> p h d", d=HD))
            tt("dve", d3, s3, cs, ALU.mult)
            s5 = src.ap.rearrange("p (h a b d) -> p h a b d", a=2, b=2, d=16)
            t5 = qtmp.ap[:, 0:W].rearrange("p (h a b d) -> p h a b d", a=2, b=2, d=16)
            sn5 = sin_t.ap[:, t * HD:(t + 1) * HD].rearrange("p (a b d) -> p a b d", a=2, b=2, d=16)
            for bsel in range(2):
                o_ = T(t5[:, :, :, bsel, :], qtmp.key)
                i_ = T(s5[:, :, :, 1 - bsel, :], src.key)
                sn = T(sn5[:, :, bsel, :].unsqueeze(1).to_broadcast([128, nheads, 2, 16]), sin_t.key)
                tt("dve" if bsel == 0 else "pool", o_, i_, sn, ALU.mult)
            tt("pool", d3, d3, t3, ALU.add)

        for b in range(NB):
            tile0 = b * NT_B
            cur_row = None
            for t in range(NT_B):
                is_ctx = t >= NT_L
                row = NB if is_ctx else b
                if row != cur_row:
                    load_modA(row)
                    cur_row = row
                gt = tile0 + t
                xt = xts.next()
                dma("sp", xt, xs[gt * 128:(gt + 1) * 128, :], r=[("xs", gt)])
                st = stat_ring.next()
                norm_h(xt, h, modA[0], modA[1], st)
                hT = hTs.next()
                transpose_h(h, hT, True, (ps[0], ps[1]))
                mm_group(ps[2], [(hT.ap[:, kc * 128:(kc + 1) * 128], wqkv.ap[:, kc * 1536 + 1024:kc * 1536 + 1536])
                                 for kc in range(8)], r=[hT.key, wqkv.key])
                ksrc = ps[2][:, 0:256]
                if mtype == 1:
                    head_norm(ksrc, NKV, gk_bc, q32[:, 0:256])
                    ksrc = q32[:, 0:256]
                if rope and not is_ctx:
                    if mtype != 1:
                        cp("dve", q32[:, 0:256], ksrc)
                        ksrc = q32[:, 0:256]
                    apply_rope(ksrc, NKV, t, qro[:, 0:256])
                    ksrc = qro[:, 0:256]
                act(kb, ksrc, AF.Copy)
                act(Vs[:, t * 256:(t + 1) * 256], ps[2][:, 256:512], AF.Copy)
                pb = psbf(3)
                tr_group([(pb.ap[0:64, g * 128:(g + 1) * 128], kb.ap[:, g * HD:(g + 1) * HD], identb.ap) for g in range(NKV)],
                         r=[kb.key, identb.key], w=[pb.key])
                kTv = kT.v(kT.ap.rearrange("p (g n) -> p g n", g=NKV)[:, :, t * 128:(t + 1) * 128])
                cp("dve", kTv, pb.v(pb.ap[0:64, 0:512].rearrange("p (g n) -> p g n", g=NKV)))

            q_tiles = list(range(NT_L)) + ([] if last else list(range(NT_L, NT_B)))
            cur_row = None
            for t in q_tiles:
                is_ctx = t >= NT_L
                row = NB if is_ctx else b
                if row != cur_row:
                    load_modA(row)
                    cur_row = row
                gt = tile0 + t
                xt = xts.next()
                dma("sp", xt, xs[gt * 128:(gt + 1) * 128, :], r=[("xs", gt)])
                st = stat_ring.next()
                norm_h(xt, h, modA[0], modA[1], st)
                hT = hTs.next()
                transpose_h(h, hT, True, (ps[0], ps[1]))
                for half in range(2):
                    mm_group(ps[2 + half], [(hT.ap[:, kc * 128:(kc + 1) * 128],
                                             wqkv.ap[:, kc * 1536 + half * 512:kc * 1536 + (half + 1) * 512])
                                            for kc in range(8)], r=[hT.key, wqkv.key])
                for half in range(2):
                    if mtype == 1:
                        head_norm(ps[2 + half], 8, gq_bc, q32[:, half * 512:(half + 1) * 512])
                    else:
                        cp("dve", q32[:, half * 512:(half + 1) * 512], ps[2 + half])
                variants = [0]
                act(qbf[0], q32, AF.Copy, scale=0.125)
                if rope and not is_ctx:
                    for half in range(2):
                        apply_rope(q32[:, half * 512:(half + 1) * 512], 8, t, qro[:, half * 512:(half + 1) * 512])
                    act(qbf[1], qro, AF.Copy, scale=0.125)
                    variants = [0, 1]
                for vi in variants:
                    for hh in range(2):
                        pb = psbf(4 + hh)
                        tr_group([(pb.ap[0:64, j * 128:(j + 1) * 128], qbf[vi].ap[:, (hh * 8 + j) * HD:(hh * 8 + j + 1) * HD],
                                   identb.ap) for j in range(8)], r=[qbf[vi].key, identb.key], w=[pb.key])
                        if hh == 0:
                            cp("dve", qT[vi][:, hh * 1024:(hh + 1) * 1024], pb[0:64, :])
                        else:
                            act(qT[vi][:, hh * 1024:(hh + 1) * 1024], pb[0:64, :], AF.Copy)
                if is_ctx:
                    KL = [(NT_L + j, 0, None, None) for j in range(NT_C)]
                else:
                    KL = []
                    if mtype == 0:
                        for kt_ in (t - 1, t, t + 1):
                            if 0 <= kt_ < NT_L:
                                m = maskLo if kt_ == t - 1 else (maskHi if kt_ == t + 1 else None)
                                KL.append((kt_, 1, m, None))
                    elif mtype == 1:
                        KL = [(kt_, 1, None, None) for kt_ in range(NT_L)]
                    else:
                        for j, kt_ in enumerate(_na_keytiles(t)):
                            KL.append((kt_, 0, None, (_na_pattern(t), j)))
                    KL += [(NT_L + j, 0, None, None) for j in range(NT_C)]
                nk = len(KL)
                items = [(g, ki) + KL[ki] for g in range(NKV) for ki in range(nk)]

                def emit_S(i, items=None, qT=qT):
                    g, ki, kt_, vi, msk, natp = ITEMS[i]
                    pS = ps[4 + (i % 2)]
                    kslice = kT.ap[:, g * NT_B * 128 + kt_ * 128: g * NT_B * 128 + (kt_ + 1) * 128]
                    mm_group(pS, [(kslice, qT[vi].ap[:, g * 512:(g + 1) * 512])], r=[kT.key, qT[vi].key])

                ITEMS = items
                emit_S(0)
                for i, (g, ki, kt_, vi, msk, natp) in enumerate(items):
                    if i + 1 < len(items):
                        emit_S(i + 1)
                    pS = ps[4 + (i % 2)]
                    pT = pTs.next()
                    if natp is not None:
                        nb_ = nat.next()
                        dma("sp" if i % 2 == 0 else "act", nb_, c_natab[natp[0], natp[1], :, g * 512:(g + 1) * 512])
                        sS = sbS.next()
                        tt("dve", sS, pS, nb_, ALU.add)
                        act(pT, sS, AF.Exp)
                    else:
                        act(pT, pS, AF.Exp)
                    if msk is not None:
                        tt("pool", pT, pT, msk, ALU.mult)
                    oa6, oa7 = ps[6].ap[0:64, :], ps[7].ap[0:64, :]
                    vsl = Vs.ap[:, kt_ * 256 + g * HD: kt_ * 256 + (g + 1) * HD]
                    pTa = pT.ap
                    first, lastk = (ki == 0), (ki == nk - 1)

                    def pv(e, oa6=oa6, oa7=oa7, vsl=vsl, pTa=pTa, first=first, lastk=lastk):
                        e.matmul(oa6, lhsT=vsl, rhs=pTa, start=first, stop=lastk)
                        return e.matmul(oa7, lhsT=onesb.ap[:, 0:64], rhs=pTa, start=first, stop=lastk)
                    P.add("pe", pv, [Vs.key, pT.key, onesb.key], [ps[6].key, ps[7].key])
                    if lastk:
                        if use_sink:
                            tt("dve", den, ps[7][0:64, :], sinke[:, g * 512:(g + 1) * 512], ALU.add)
                            recip(rden, den)
                        else:
                            recip(rden, ps[7][0:64, :])
                        tt("dve", oT[:, g * 512:(g + 1) * 512], ps[6][0:64, :], rden, ALU.mult)
                for half in range(2):
                    mm_group(ps[2 + half], [(oT.ap[:, hh * 128:(hh + 1) * 128], wo.ap[:, hh * D + half * 512: hh * D + (half + 1) * 512])
                                            for hh in range(NH)], r=[oT.key, wo.key])
                    yt = ytmp.next()
                    tt("dve", yt, ps[2 + half], modA[2][:, half * 512:(half + 1) * 512], ALU.mult)
                    tt("pool", xt[:, half * 512:(half + 1) * 512], xt[:, half * 512:(half + 1) * 512], yt, ALU.add)
                dma("sp", xs[gt * 128:(gt + 1) * 128, :], xt, w=[("xs", gt)])

        P.barrier()
        alloc_state["off"] = PERSIST
        wgs = [a16("wg%d" % i, 8 * FH) for i in range(2)]
        wus = [a16("wu%d" % i, 8 * FH) for i in range(2)]
        wds = [a16("wd%d" % i, 4 * D) for i in range(2)]
        xgs = [a16("xg%d" % i, (CAP // 128) * D) for i in range(2)]
        xeT = a16("xeT", 8 * CAP)
        HT = a16("HT", 4 * CAP)
        sgs = Ring([a32("sg%d" % i, CAP) for i in range(2)])
        Ysb = Ring([a32("Ysb%d" % i, D) for i in range(2)])
        xts = Ring([a32("mxt%d" % i, D) for i in range(2)])
        h = a32("mh", D)
        hbfs = Ring([a16("hbf%d" % i, D) for i in range(2)])
        hT32 = a32("hT32", 8 * 128)
        modF = [[a32("modF%d_%d" % (s, i), D) for i in range(2)] for s in range(2)]
        gtF = [a32("gtF%d" % s, D) for s in range(2)]
        y1s = Ring([a32("y1_%d" % i, D) for i in range(2)])
        y2s = Ring([a32("y2_%d" % i, D) for i in range(2)])
        gfin = a32("gfin", D) if last else None
        wrg = a32("wrg", 8 * 36)
        print("moe SBUF KB (before rl)", alloc_state["off"] / 256.0)
        rls = [a32("rl%d" % i, 512) for i in range(2)]
        hT32s = [hT32, a32("hT32b", 8 * 128)]
        mhs = [h, a32("mh2", D)]

        def mk_rl(si):
            rl = rls[si]

            def RL(name, off, n):
                return T(rl.ap[:, off:off + n], ("rl", si, name))
            d = dict(L=RL("L", 0, 36), mg=RL("mg", 40, 1), nmg=RL("nmg", 41, 1), eg=RL("eg", 44, 4), sg=RL("sg", 48, 1),
                     ptop=RL("ptop", 49, 1), ohg=RL("ohg", 52, 4), pen=RL("pen", 56, 4), Lm=RL("Lm", 64, 32),
                     mx8=RL("mx8", 96, 8), oh1=RL("oh1", 104, 32), oh2=RL("oh2", 136, 32), dd=RL("dd", 168, 1),
                     ed=RL("ed", 169, 1), rd=RL("rd", 170, 1), A=RL("A", 172, 32),
                     Abf=T(rl.ap[:, 204:220].bitcast(BF16), ("rl", si, "Abf")), slot=RL("slot", 224, 32),
                     tmpa=RL("tmpa", 256, 32), tmpb=RL("tmpb", 328, 32), d1f=RL("d1f", 288, 1), d2f=RL("d2f", 289, 1),
                     ov=RL("ov", 290, 1), nov=RL("nov", 291, 1), slotp=RL("slotp", 296, 32))
            return d
        RLS = [mk_rl(0), mk_rl(1)]

        m_tiles = []
        for b in range(NB):
            m_tiles += [(b * NT_B + t, b) for t in range(NT_L)]
        if not last:
            for b in range(NB):
                m_tiles += [(b * NT_B + NT_L + t, NB) for t in range(NT_C)]
        XGKEYS = [("XG", gt) for (gt, _) in m_tiles]

        dma("sp", wrg.v(wrg.ap.rearrange("p (k n) -> p k n", n=36)), w_rg[li].rearrange("(k p) n -> p k n", p=128))
        dma("sp", brg_bc, b_rg[li:li + 1, :].partition_broadcast(128))
        memset("dve", tot, 0.0)
        if last:
            dma("sp", gfin, g_final.partition_broadcast(128))

        m1_state = {"row": None, "mset": -1, "mf": None}

        def m1_gen(gt, row, si):
            R_ = RLS[si]
            pbase = 4 * si
            if row != m1_state["row"]:
                m1_state["mset"] += 1
                mf_ = modF[m1_state["mset"] % 2]
                dma("sp", mf_[0], modd[li, row:row + 1, 4 * D:5 * D].partition_broadcast(128), r=[("modd", li)])
                dma("sp", mf_[1], modd[li, row:row + 1, 3 * D:4 * D].partition_broadcast(128), r=[("modd", li)])
                m1_state["row"] = row
                m1_state["mf"] = mf_
            mf = m1_state["mf"]
            xt = xts.next()
            dma("sp", xt, xs[gt * 128:(gt + 1) * 128, :], r=[("xs", gt)])
            st = stat_ring.next()
            hh_ = mhs[si]
            norm_h(xt, hh_, mf[0], mf[1], st)
            yield
            hbf = hbfs.next()
            act(hbf, hh_, AF.Copy)
            hT32_ = hT32s[si]
            transpose_h(hh_, hT32_, False, (ps[pbase], ps[pbase + 1]))
            yield
            mm_group(ps[pbase + 2][:, 0:36], [(hT32_.ap[:, kc * 128:(kc + 1) * 128], wrg.ap[:, kc * 36:(kc + 1) * 36]) for kc in range(8)],
                     r=[hT32_.key, wrg.key])
            yield
            L, mg, nmg, eg, sg_, ptop, ohg, pen, Lm, mx8 = (R_[k] for k in ("L", "mg", "nmg", "eg", "sg", "ptop", "ohg", "pen", "Lm", "mx8"))
            oh1, oh2, dd, ed, rd, Asum, Abf, slot = (R_[k] for k in ("oh1", "oh2", "dd", "ed", "rd", "A", "Abf", "slot"))
            tt("dve", L, ps[pbase + 2][:, 0:36], brg_bc, ALU.add)
            red(mg, L[:, 0:4], ALU.max)
            yield
            ts("dve", nmg, mg, -1.0, ALU.mult)
            ts("dve", ohg, L[:, 0:4], mg, ALU.is_equal)
            yield
            act(eg, L[:, 0:4], AF.Exp, bias=nmg, accum=sg_)
            ts("dve", pen, ohg, -1.0, ALU.add, 1e30, ALU.mult)
            yield
            recip(ptop, sg_)
            tt("dve", Lm.v(Lm.ap.rearrange("p (g e) -> p g e", e=8)), L.v(L.ap[:, 4:36].rearrange("p (g e) -> p g e", e=8)),
               pen.v(pen.ap.unsqueeze(2).to_broadcast([128, 4, 8])), ALU.add)
            yield
            mxo, lmi = mx8.ap, Lm.ap
            P.add("dve", lambda e, mxo=mxo, lmi=lmi: e.max(out=mxo, in_=lmi), [Lm.key], [mx8.key])
            yield
            ts("dve", oh1, Lm, mx8[:, 0:1], ALU.is_equal)
            ts("dve", oh2, Lm, mx8[:, 1:2], ALU.is_equal)
            tt("dve", dd, mx8[:, 1:2], mx8[:, 0:1], ALU.subtract)
            yield
            act(ed, dd, AF.Exp)
            tt("dve", Asum, oh1, oh2, ALU.add)
            yield
            ts("dve", ed, ed, 1.0, ALU.add)
            cp("dve", Abf, Asum)
            yield
            recip(rd, ed)
            w1 = T(rt_w1.ap[:, gt:gt + 1], ("rt_w1", gt))
            w2 = T(rt_w2.ap[:, gt:gt + 1], ("rt_w2", gt))
            mm_group(ps[pbase + 3][:, 0:32], [(trib.ap, Abf.ap)], r=[trib.key, Abf.key])
            mm_group(ps[pbase + 3][:, 32:64], [(onesb.ap, Abf.ap)], r=[onesb.key, Abf.key])
            stt("dve", slot, ps[pbase + 3][:, 0:32], 0.0, tot, ALU.add, ALU.add)
            tt("dve", tot, tot, ps[pbase + 3][:, 32:64], ALU.add)
            yield
            tt("dve", w1, ptop, rd, ALU.mult)
            tt("dve", R_["slotp"], slot, ec_bc, ALU.add)
            yield
            tt("dve", w2, ptop, w1, ALU.subtract)
            tt("dve", R_["tmpa"], oh1, slot, ALU.mult)
            tt("dve", R_["tmpb"], oh2, slot, ALU.mult)
            yield
            ovs = []
            for ci, (oh, df, rtd, tmp) in enumerate(((oh1, R_["d1f"], rt_d1, R_["tmpa"]), (oh2, R_["d2f"], rt_d2, R_["tmpb"]))):
                ov = T(rls[si].ap[:, 400 + ci:401 + ci], ("rl", si, "ov%d" % ci))
                nov = T(rls[si].ap[:, 404 + ci:405 + ci], ("rl", si, "nov%d" % ci))
                red(ov, tmp, ALU.add)
                yield
                ts("dve", ov, ov, float(CAP), ALU.is_ge)
                tt("dve", tmp, oh, R_["slotp"], ALU.mult)
                yield
                ts("dve", nov, ov, -1.0, ALU.mult, 1.0, ALU.add)
                red(df, tmp, ALU.add)
                yield
                tt("dve", df, df, nov, ALU.mult)
                yield
                stt("dve", df, ov, float(NE * CAP), df, ALU.mult, ALU.add)
                yield
                rcol = T(rtd.ap[:, gt:gt + 1], (rtd.key, gt))
                cp("dve", rcol, df)
                yield
                ia = rcol.ap
                ha = hbf.ap
                P.add("pool", lambda e, ia=ia, ha=ha: e.indirect_dma_start(
                    out=XG, out_offset=bass.IndirectOffsetOnAxis(ap=ia, axis=0), in_=ha, in_offset=None),
                    [rcol.key, hbf.key], [("XG", gt)], dma=True)

        if not OPT["SKIP_M1"]:
            interleave((m1_gen(gt, row, i % 2) for i, (gt, row) in enumerate(m_tiles)), OPT["W_M1"], 9)

        for ex in (range(NE) if not OPT["SKIP_M2"] else []):
            wg, wu, wd, xg = wgs[ex % 2], wus[ex % 2], wds[ex % 2], xgs[ex % 2]
            dma("pool", wg.v(wg.ap.rearrange("p (k f) -> p k f", f=FH)), w_gate[li, ex].rearrange("(k p) f -> p k f", p=128))
            dma("pool", wu.v(wu.ap.rearrange("p (k f) -> p k f", f=FH)), w_up[li, ex].rearrange("(k p) f -> p k f", p=128))
            dma("pool", wd.v(wd.ap.rearrange("p (k f) -> p k f", f=D)), w_down[li, ex].rearrange("(k p) f -> p k f", p=128))
            dma("sp", xg.v(xg.ap.rearrange("p (s d) -> p s d", d=D)),
                XG[ex * CAP:(ex + 1) * CAP, :].rearrange("(s p) d -> p s d", p=128), r=XGKEYS)
            NS = CAP // 128
            for s in range(NS):
                pb = psbf(s % 2)
                tr_group([(pb.ap[:, kc * 128:(kc + 1) * 128], xg.ap[:, s * D + kc * 128: s * D + (kc + 1) * 128], identb.ap)
                          for kc in range(8)], r=[xg.key, identb.key], w=[pb.key])
                dst = xeT.v(xeT.ap.rearrange("p (k c) -> p k c", c=CAP)[:, :, s * 128:(s + 1) * 128])
                src = pb.v(pb.ap.rearrange("p (k c) -> p k c", c=128))
                if s % 2 == 0:
                    cp("dve", dst, src)
                else:
                    act(dst, src, AF.Copy)
            for m in range(4):
                mm_group(ps[2 + (m % 2)], [(wg.ap[:, kc * FH + m * 128: kc * FH + (m + 1) * 128], xeT.ap[:, kc * CAP:(kc + 1) * CAP])
                                           for kc in range(8)], r=[wg.key, xeT.key])
                mm_group(ps[4 + (m % 2)], [(wu.ap[:, kc * FH + m * 128: kc * FH + (m + 1) * 128], xeT.ap[:, kc * CAP:(kc + 1) * CAP])
                                           for kc in range(8)], r=[wu.key, xeT.key])
                sgt = sgs.next()
                act(sgt, ps[2 + (m % 2)], AF.Silu)
                tt("dve", HT[:, m * CAP:(m + 1) * CAP], sgt, ps[4 + (m % 2)], ALU.mult)
            for s in range(NS):
                ysb = Ysb.next()
                for half in range(2):
                    pp = ps[6 + half]
                    mm_group(pp, [(HT.ap[:, m * CAP + s * 128: m * CAP + (s + 1) * 128], wd.ap[:, m * D + half * 512: m * D + (half + 1) * 512])
                                  for m in range(4)], r=[HT.key, wd.key])
                    if half == 0:
                        act(ysb[:, 0:512], pp, AF.Copy)
                    else:
                        cp("dve", ysb[:, 512:1024], pp)
                dma("sp", YG[ex * CAP + s * 128: ex * CAP + (s + 1) * 128, :], ysb, w=[("YG", ex, s)])

        YGKEYS = ["YG"] + [("YG", ex, s_) for ex in range(NE) for s_ in range(CAP // 128)]
        m3_state = {"row": None, "mset": -1, "gtf": None}

        def m3_gen(gt, row):
            if row != m3_state["row"]:
                m3_state["mset"] += 1
                gtf_ = gtF[m3_state["mset"] % 2]
                dma("sp", gtf_, modd[li, row:row + 1, 5 * D:6 * D].partition_broadcast(128), r=[("modd", li)])
                m3_state["row"] = row
                m3_state["gtf"] = gtf_
            gtf = m3_state["gtf"]
            y1, y2 = y1s.next(), y2s.next()
            for (yy, rtd) in ((y1, rt_d1), (y2, rt_d2)):
                ia = rtd.ap[:, gt:gt + 1]
                ya = yy.ap
                P.add("pool", lambda e, ia=ia, ya=ya: e.indirect_dma_start(
                    out=ya, out_offset=None, in_=YG, in_offset=bass.IndirectOffsetOnAxis(ap=ia, axis=0)),
                    [(rtd.key, gt)] + YGKEYS, [yy.key], dma=True)
            xt = xts.next()
            dma("sp", xt, xs[gt * 128:(gt + 1) * 128, :], r=[("xs", gt)])
            yield
            w1 = T(rt_w1.ap[:, gt:gt + 1], ("rt_w1", gt))
            w2 = T(rt_w2.ap[:, gt:gt + 1], ("rt_w2", gt))
            ts("dve", y1, y1, w1, ALU.mult)
            yield
            stt("dve", y1, y2, w2, y1, ALU.mult, ALU.add)
            yield
            tt("pool", y1, y1, gtf, ALU.mult)
            yield
            tt("pool", xt, xt, y1, ALU.add)
            yield
            if not last:
                dma("sp", xs[gt * 128:(gt + 1) * 128, :], xt, w=[("xs", gt)])
            else:
                st = stat_ring.next()
                stt("dve", y2, xt, 1.0 / D, xt, ALU.mult, ALU.mult, accum=st[:, 0:1])
                yield
                act(st[:, 1:2], st[:, 0:1], AF.Ln, bias=EPS)
                yield
                act(st[:, 2:3], st[:, 1:2], AF.Exp, scale=-0.5)
                yield
                stt("dve", y2, xt, st[:, 2:3], gfin, ALU.mult, ALU.mult)
                b_ = gt // NT_B
                t_ = gt % NT_B
                r0 = b_ * SEQ + t_ * 128
                dma("sp", out[r0:r0 + 128, :], y2)

        if not OPT["SKIP_M3"]:
            interleave((m3_gen(gt, row) for (gt, row) in m_tiles), OPT["W_M3"], 3)

    P.finalize()
    P.emit()
    return nc


_CACHE = {}


def _consts():
    if "c" not in _CACHE:
        cos, sin_s = _rope_tables()
        kk = np.arange(128)[:, None]
        qq = np.arange(128)[None, :]
        mlo = np.tile((kk >= qq).astype(np.float32), (1, 4))
        mhi = np.tile((kk <= qq).astype(np.float32), (1, 4))
        tri = (np.arange(128)[:, None] < np.arange(128)[None, :]).astype(np.float32)
        ec = (np.arange(NE, dtype=np.float32) * CAP).reshape(1, NE)
        _CACHE["c"] = dict(c_ident=np.eye(128, dtype=np.float32), c_cos=cos, c_sin=sin_s,
                           c_masks=np.stack([mlo, mhi]).astype(np.float32), c_tri=tri, c_ec=ec)
        _CACHE["naidx"] = _na_index_table()
    return _CACHE["c"], _CACHE["naidx"]


def make_in_maps(inputs, n_cores, NB):
    f = lambda a: np.ascontiguousarray(np.asarray(a, dtype=np.float32))
    consts, naidx = _consts()
    rpb = f(inputs["rpb_c"])[0].reshape(-1)
    rpb_ext = np.concatenate([rpb, np.array([NEG], dtype=np.float32)])
    natab = np.ascontiguousarray(rpb_ext[naidx].reshape(5, 5, 128, NH * 128))
    w_rg = np.ascontiguousarray(np.concatenate([f(inputs["w_group"]), f(inputs["w_router"])], axis=-1))
    b_rg = np.ascontiguousarray(np.concatenate([f(inputs["b_group"]), f(inputs["b_router"])], axis=-1))
    shared = dict(
        w_ada=f(inputs["w_ada"]), b_ada=f(inputs["b_ada"]), g_attn=f(inputs["g_attn"]), w_qkv=f(inputs["w_qkv"]),
        w_o=f(inputs["w_o"]), sink_a=f(inputs["sink_a"]), gq_b=f(inputs["gq_b"]), gk_b=f(inputs["gk_b"]),
        g_ffn=f(inputs["g_ffn"]), w_rg=w_rg, b_rg=b_rg, w_gate=f(inputs["w_gate"]), w_up=f(inputs["w_up"]),
        w_down=f(inputs["w_down"]), g_final=f(inputs["g_final"]).reshape(1, D), c_natab=natab, **consts)
    x = f(inputs["x"])
    ctx = f(inputs["ctx"])
    c = f(inputs["c"])
    c_ctx = f(inputs["c_ctx"]).reshape(1, D)
    maps = []
    for i in range(n_cores):
        sl = slice(i * NB, (i + 1) * NB)
        m = dict(shared)
        m["x"] = np.ascontiguousarray(x[sl].reshape(NB * SEQ, D))
        m["ctx"] = np.ascontiguousarray(ctx[sl].reshape(NB * CTX, D))
        m["cvec"] = np.ascontiguousarray(np.concatenate([c[sl], c_ctx], axis=0))
        maps.append(m)
    return maps


def kernel(**inputs):
    n_cores = 8
    NB = 2
    if "nc" not in _CACHE:
        _CACHE["nc"] = build(NB=NB)
    nc = _CACHE["nc"]
    maps = make_in_maps(inputs, n_cores, NB)
    res = run_bass_kernel_spmd(nc, maps, core_ids=list(range(n_cores)))
    outs = [r["out"].reshape(NB, SEQ, D) for r in res.results]
    return np.concatenate(outs, axis=0).astype(np.float32)
```

```python
import numpy as np
import concourse.bass as bass
import concourse.mybir as mybir
from concourse.bass_utils import run_bass_kernel_spmd

F32 = mybir.dt.float32
BF16 = mybir.dt.bfloat16
I32 = mybir.dt.int32
ALU = mybir.AluOpType
AF = mybir.ActivationFunctionType
AX = mybir.AxisListType

D = 1024
SEQ = 2048
CTX = 256
DEPTH = 4
NH = 16
NKV = 4
HD = 64
NE = 32
FH = 512
GRID_W = 64
import os as _os
OPT = dict(W_KV=int(_os.environ.get("W_KV", 2)), W_M1=int(_os.environ.get("W_M1", 2)), W_M3=int(_os.environ.get("W_M3", 2)),
           Q_OVERLAP=int(_os.environ.get("Q_OVERLAP", 1)), S_AHEAD=int(_os.environ.get("S_AHEAD", 1)),
           SINK_BC=int(_os.environ.get("SINK_BC", 1)), SKIP_ATT=int(_os.environ.get("SKIP_ATT", 0)),
           SKIP_M1=int(_os.environ.get("SKIP_M1", 0)), SKIP_M2=int(_os.environ.get("SKIP_M2", 0)), SKIP_M3=int(_os.environ.get("SKIP_M3", 0)))
CAP = 512
NT_L = SEQ // 128
NT_C = CTX // 128
NT_B = NT_L + NT_C
EPS = 1e-6
NEG = -1e30


class _Op:
    __slots__ = ("idx", "eng", "fn", "deps", "dma", "sig", "waits", "signal", "ksnap")

    def __init__(self, idx, eng, fn, dma):
        self.idx = idx
        self.eng = eng
        self.fn = fn
        self.deps = set()
        self.dma = dma
        self.sig = None
        self.waits = []
        self.signal = False
        self.ksnap = None


class Prog:
    ENGS = ("pe", "act", "dve", "pool", "sp")
    EPOCH = 20000
    NDMA = {"sp": 24, "pool": 24, "act": 8}

    def __init__(self, nc):
        self.nc = nc
        self.ops = []
        self.last_w = {}
        self.readers = {}
        self.pending = {}
        self.last_eng = {}
        self.dma_since = []

    def add(self, eng, fn, r=(), w=(), dma=False):
        op = _Op(len(self.ops), eng, fn, dma)
        ops = self.ops

        def same_eng(j):
            o = ops[j]
            return (not dma) and (not o.dma) and o.eng == eng

        for k in r:
            lw = self.last_w.get(k)
            if lw is not None and not (eng == "pe" and same_eng(lw)):
                op.deps.add(lw)
            if isinstance(k, str) and k.startswith("ps"):
                for rd in self.readers.get(k, ()):
                    if not same_eng(rd):
                        op.deps.add(rd)
        for k in w:
            lw = self.last_w.get(k)
            if lw is not None and not (eng == "pe" and same_eng(lw)):
                op.deps.add(lw)
            rs = self.readers.get(k)
            if rs:
                for rd in rs:
                    if not same_eng(rd):
                        op.deps.add(rd)
        for k in r:
            self.readers.setdefault(k, []).append(op.idx)
        for k in w:
            self.last_w[k] = op.idx
            self.readers[k] = []
        if eng in self.pending:
            op.deps.update(self.pending.pop(eng))
        op.deps.discard(op.idx)
        self.ops.append(op)
        if dma:
            self.dma_since.append(op.idx)
        else:
            self.last_eng[eng] = op.idx
        return op

    def barrier(self):
        deps = set(self.last_eng.values()) | set(self.dma_since)
        for e in self.ENGS:
            self.pending[e] = set(deps) | self.pending.get(e, set())
        self.dma_since = []

    def finalize(self, final_engine="sp"):
        ops = self.ops
        dma_rr = {q: 0 for q in self.NDMA}
        dma_last = {}
        dma_cnt = {}
        for op in ops:
            if op.dma:
                q = op.eng
                s = dma_rr[q] % self.NDMA[q]
                dma_rr[q] += 1
                key = ("dma", q, s)
                prev = dma_last.get(key)
                if prev is not None:
                    op.deps.add(prev)
                dma_last[key] = op.idx
                dma_cnt[key] = dma_cnt.get(key, 0) + 16
                op.sig = (key, dma_cnt[key])
        has_dep = [False] * len(ops)
        for op in ops:
            for d in op.deps:
                has_dep[d] = True
        cnt = {e: 0 for e in self.ENGS}
        know = {e: {} for e in self.ENGS}
        for op in ops:
            e = op.eng
            K = know[e]
            for d in sorted(op.deps):
                sk, sv = ops[d].sig
                if K.get(sk, 0) >= sv:
                    continue
                op.waits.append((sk, sv))
                K[sk] = sv
                for k2, v2 in ops[d].ksnap.items():
                    if K.get(k2, 0) < v2:
                        K[k2] = v2
            if op.dma:
                op.signal = True
            elif has_dep[op.idx]:
                cnt[e] += 1
                ep = cnt[e] // self.EPOCH
                op.sig = (("eng", e, ep), cnt[e] - ep * self.EPOCH + (1 if ep else 0))
                op.signal = True
            op.ksnap = dict(K)
        self.final_waits = []
        Kf = know[final_engine]
        for key, v in dma_cnt.items():
            if Kf.get(key, 0) < v:
                self.final_waits.append((key, v))
        self.semkeys = set(op.sig[0] for op in ops if op.signal)
        self.final_engine = final_engine

    def emit(self):
        nc = self.nc
        sems = {}
        for i, k in enumerate(sorted(self.semkeys, key=str)):
            sems[k] = nc.alloc_semaphore("s%d" % i)
        by_eng = {e: [] for e in self.ENGS}
        for op in self.ops:
            by_eng[op.eng].append(op)
        fin_eng = self.final_engine
        fin_waits = self.final_waits

        def run(eng_name, eng):
            for op in by_eng[eng_name]:
                for sk, sv in op.waits:
                    eng.wait_ge(sems[sk], sv)
                ins = op.fn(eng)
                if op.signal:
                    ins.then_inc(sems[op.sig[0]], 16 if op.dma else 1)
            if eng_name == fin_eng:
                for sk, sv in fin_waits:
                    eng.wait_ge(sems[sk], sv)

        with nc.Block() as block:
            @block.tensor
            def _(e):
                run("pe", e)

            @block.scalar
            def _(e):
                run("act", e)

            @block.vector
            def _(e):
                run("dve", e)

            @block.gpsimd
            def _(e):
                run("pool", e)

            @block.sync
            def _(e):
                run("sp", e)


class T:
    __slots__ = ("ap", "key")

    def __init__(self, ap, key):
        self.ap = ap
        self.key = key

    def __getitem__(self, idx):
        return T(self.ap[idx], self.key)

    def v(self, ap):
        return T(ap, self.key)


class Ring:
    def __init__(self, items):
        self.items = items
        self.i = 0

    def next(self):
        it = self.items[self.i % len(self.items)]
        self.i += 1
        return it


def _rope_tables():
    t = np.arange(SEQ)
    row = (t // GRID_W).astype(np.float32)
    col = (t % GRID_W).astype(np.float32)
    quarter = HD // 4
    inv = (np.float32(10000.0) ** (-np.arange(quarter, dtype=np.float32) / np.float32(quarter))).astype(np.float32)
    ar = row[:, None] * inv
    ac = col[:, None] * inv
    ang = np.concatenate([ar, ar, ac, ac], axis=-1).astype(np.float32)
    cos = np.cos(ang).astype(np.float32)
    sin = np.sin(ang).astype(np.float32)
    sgn = np.concatenate([-np.ones(16), np.ones(16), -np.ones(16), np.ones(16)]).astype(np.float32)
    return cos, (sin * sgn[None, :]).astype(np.float32)


NA_PAT_TILES = [0, 1, 2, 14, 15]


def _na_pattern(t):
    return {0: 0, 1: 1, 14: 3, 15: 4}.get(t, 2)


def _na_keytiles(t):
    if t <= 1:
        return [0, 1, 2, 3]
    if t >= 14:
        return [12, 13, 14, 15]
    return [t - 2, t - 1, t, t + 1, t + 2]


def _na_index_table():
    rows = SEQ // GRID_W
    wh, ww = 8, 16
    nrel = 15 * 31
    masked = NH * nrel
    tab = np.full((5, 5, 128, NH, 128), masked, dtype=np.int64)
    for pi, t in enumerate(NA_PAT_TILES):
        kts = _na_keytiles(t)
        q = t * 128 + np.arange(128)
        r = q // GRID_W
        c = q % GRID_W
        rs = np.clip(r - wh // 2, 0, rows - wh)
        cs = np.clip(c - ww // 2, 0, GRID_W - ww)
        for j, kt in enumerate(kts):
            k = kt * 128 + np.arange(128)
            kr = k // GRID_W
            kc = k % GRID_W
            inwin = ((kr[:, None] >= rs[None, :]) & (kr[:, None] < rs[None, :] + wh)
                     & (kc[:, None] >= cs[None, :]) & (kc[:, None] < cs[None, :] + ww))
            rel = (kr[:, None] - r[None, :] + 7) * 31 + (kc[:, None] - c[None, :] + 15)
            for h in range(NH):
                tab[pi, j, :, h, :] = np.where(inwin, h * nrel + rel, masked)
    return tab


def build(NB=2, n_layers=DEPTH):
    nc = bass.Bass("TRN2", target_bir_lowering=False)
    R = NB + 1
    NTOK = NB * NT_B * 128

    def din(name, shape, dt=F32):
        return nc.dram_tensor(name, list(shape), dt, kind="ExternalInput").ap()

    x_in = din("x", [NB * SEQ, D])
    ctx_in = din("ctx", [NB * CTX, D])
    cvec = din("cvec", [R, D])
    w_ada = din("w_ada", [DEPTH, D, 6 * D])
    b_ada = din("b_ada", [DEPTH, 6 * D])
    g_attn = din("g_attn", [DEPTH, D])
    w_qkv = din("w_qkv", [DEPTH, D, 1536])
    w_o = din("w_o", [DEPTH, D, D])
    sink_a = din("sink_a", [2, NH])
    gq_b = din("gq_b", [1, HD])
    gk_b = din("gk_b", [1, HD])
    g_ffn = din("g_ffn", [DEPTH, D])
    w_rg = din("w_rg", [DEPTH, D, 36])
    b_rg = din("b_rg", [DEPTH, 36])
    w_gate = din("w_gate", [DEPTH, NE, D, FH])
    w_up = din("w_up", [DEPTH, NE, D, FH])
    w_down = din("w_down", [DEPTH, NE, FH, D])
    g_final = din("g_final", [1, D])
    c_ident = din("c_ident", [128, 128])
    c_cos = din("c_cos", [SEQ, HD])
    c_sin = din("c_sin", [SEQ, HD])
    c_masks = din("c_masks", [2, 128, 512])
    c_tri = din("c_tri", [128, 128])
    c_ec = din("c_ec", [1, NE])
    c_natab = din("c_natab", [5, 5, 128, NH * 128])
    out = nc.dram_tensor("out", [NB * SEQ, D], F32, kind="ExternalOutput").ap()

    xs = nc.dram_tensor("xs", [NTOK, D], F32).ap()
    modd = nc.dram_tensor("modd", [DEPTH, R, 6 * D], F32).ap()
    XG = nc.dram_tensor("XG", [NE * CAP + 1, D], BF16).ap()
    YG = nc.dram_tensor("YG", [NE * CAP + 1, D], F32).ap()

    P = Prog(nc)

    POOL_KB = 180
    pool = nc.alloc_sbuf_tensor("pool", [128, POOL_KB * 256], F32)
    alloc_state = {"off": 0}

    def a32(name, n, parts=128):
        off = alloc_state["off"]
        alloc_state["off"] = off + n
        assert alloc_state["off"] <= POOL_KB * 256, (name, alloc_state["off"])
        return T(pool[0:parts, off:off + n], name)

    def a16(name, n, parts=128):
        n32 = (n + 1) // 2
        off = alloc_state["off"]
        alloc_state["off"] = off + n32
        assert alloc_state["off"] <= POOL_KB * 256, (name, alloc_state["off"])
        return T(pool[0:parts, off:off + n32].bitcast(BF16), name)

    ps = [T(nc.alloc_psum_tensor("ps%d" % i, [128, 512], F32)[:, :], "ps%d" % i) for i in range(8)]

    def psbf(i):
        return T(ps[i].ap.bitcast(BF16), ps[i].key)

    ident = a32("ident", 128)
    identb = a16("identb", 128)
    onesb = a16("onesb", 128)
    trib = a16("trib", 128)
    cos_t = a32("cos", NT_L * HD)
    sin_t = a32("sin", NT_L * HD)
    maskLo = a16("maskLo", 512)
    maskHi = a16("maskHi", 512)
    gq_bc = a32("gq_bc", HD)
    gk_bc = a32("gk_bc", HD)
    ec_bc = a32("ec_bc", NE)
    brg_bc = a32("brg_bc", 36)
    tot = a32("tot", NE)
    NTT = NB * NT_B
    rt_w1 = a32("rt_w1", NTT)
    rt_w2 = a32("rt_w2", NTT)
    rt_d1 = T(nc.alloc_sbuf_tensor("rt_d1", [128, NTT], I32)[:, :], "rt_d1")
    rt_d2 = T(nc.alloc_sbuf_tensor("rt_d2", [128, NTT], I32)[:, :], "rt_d2")
    small = a32("small", 64)
    PERSIST = alloc_state["off"]

    def dma(q, out_, in_, r=(), w=()):
        oa = out_.ap if isinstance(out_, T) else out_
        ia = in_.ap if isinstance(in_, T) else in_
        rr = list(r) + ([in_.key] if isinstance(in_, T) else [])
        ww = list(w) + ([out_.key] if isinstance(out_, T) else [])
        P.add(q, lambda e: e.dma_start(out=oa, in_=ia), rr, ww, dma=True)

    def act(out_, in_, func, scale=1.0, bias=0.0, accum=None, r=(), w=()):
        kw = {}
        rr = [in_.key] + list(r)
        ww = [out_.key] + list(w)
        if isinstance(bias, T):
            rr.append(bias.key)
            bias = bias.ap
        if isinstance(scale, T):
            rr.append(scale.key)
            scale = scale.ap
        if accum is not None:
            kw["accum_out"] = accum.ap
            ww.append(accum.key)
        oa, ia = out_.ap, in_.ap
        P.add("act", lambda e: e.activation(out=oa, in_=ia, func=func, bias=bias, scale=scale, **kw), rr, ww)

    def tt(eng, out_, a, b, op):
        oa, aa, ba = out_.ap, a.ap, b.ap
        P.add(eng, lambda e: e.tensor_tensor(out=oa, in0=aa, in1=ba, op=op), [a.key, b.key], [out_.key])

    def ts(eng, out_, a, s1, op0, s2=None, op1=None, accum=None):
        rr = [a.key]
        ww = [out_.key]
        if isinstance(s1, T):
            rr.append(s1.key)
            s1 = s1.ap
        if isinstance(s2, T):
            rr.append(s2.key)
            s2 = s2.ap
        kw = {}
        if op1 is not None:
            kw["op1"] = op1
        if accum is not None:
            kw["accum_out"] = accum.ap
            ww.append(accum.key)
        oa, aa = out_.ap, a.ap
        P.add(eng, lambda e: e.tensor_scalar(out=oa, in0=aa, scalar1=s1, scalar2=s2, op0=op0, **kw), rr, ww)

    def stt(eng, out_, a, s, b, op0, op1, accum=None):
        rr = [a.key, b.key]
        ww = [out_.key]
        if isinstance(s, T):
            rr.append(s.key)
            s = s.ap
        kw = {}
        if accum is not None:
            kw["accum_out"] = accum.ap
            ww.append(accum.key)
        oa, aa, ba = out_.ap, a.ap, b.ap
        P.add(eng, lambda e: e.scalar_tensor_tensor(out=oa, in0=aa, scalar=s, in1=ba, op0=op0, op1=op1, **kw), rr, ww)

    def cp(eng, out_, in_):
        oa, ia = out_.ap, in_.ap
        P.add(eng, lambda e: e.tensor_copy(out=oa, in_=ia), [in_.key], [out_.key])

    def recip(out_, in_):
        oa, ia = out_.ap, in_.ap
        P.add("dve", lambda e: e.reciprocal(out=oa, in_=ia), [in_.key], [out_.key])

    def red(out_, in_, op, axis=AX.X):
        oa, ia = out_.ap, in_.ap
        P.add("dve", lambda e: e.tensor_reduce(out=oa, in_=ia, axis=axis, op=op), [in_.key], [out_.key])

    def memset(eng, out_, val):
        oa = out_.ap
        P.add(eng, lambda e: e.memset(oa, val), [], [out_.key])

    def mm_group(out_, pairs, r=()):
        oa = out_.ap
        n = len(pairs)

        def fn(e):
            ins = None
            for i, (l, rh) in enumerate(pairs):
                ins = e.matmul(oa, lhsT=l, rhs=rh, start=(i == 0), stop=(i == n - 1))
            return ins
        P.add("pe", fn, list(r), [out_.key])

    def tr_group(items, r=(), w=()):
        def fn(e):
            ins = None
            for (o, i, idn) in items:
                ins = e.transpose(out=o, in_=i, identity=idn)
            return ins
        P.add("pe", fn, list(r), list(w))

    dma("sp", ident, c_ident)
    dma("pool", identb, c_ident)
    dma("pool", trib, c_tri)
    dma("sp", cos_t.v(cos_t.ap.rearrange("p (t d) -> p t d", d=HD)), c_cos.rearrange("(t p) d -> p t d", p=128))
    dma("sp", sin_t.v(sin_t.ap.rearrange("p (t d) -> p t d", d=HD)), c_sin.rearrange("(t p) d -> p t d", p=128))
    dma("pool", maskLo, c_masks[0])
    dma("pool", maskHi, c_masks[1])
    dma("sp", gq_bc, gq_b.partition_broadcast(128))
    dma("sp", gk_bc, gk_b.partition_broadcast(128))
    dma("sp", ec_bc, c_ec.partition_broadcast(128))
    memset("dve", onesb, 1.0)

    for b in range(NB):
        base = b * NT_B * 128
        dma("sp", xs[base:base + SEQ, :], x_in[b * SEQ:(b + 1) * SEQ, :], w=[("xs", b * NT_B + t) for t in range(NT_L)])
        dma("sp", xs[base + SEQ:base + SEQ + CTX, :], ctx_in[b * CTX:(b + 1) * CTX, :],
            w=[("xs", b * NT_B + NT_L + t) for t in range(NT_C)])

    alloc_state["off"] = PERSIST
    zrow = a32("zrow", D, parts=1)
    memset("dve", zrow, 0.0)
    dma("sp", YG[NE * CAP:NE * CAP + 1, :], zrow, w=["YG"])
    cv = a32("cv", D, parts=R)
    scT = a32("scT", 8 * R)
    wada = [a32("wada%d" % i, 8 * 512) for i in range(2)]
    modsb = a32("modsb", 6 * D, parts=R)
    bada = a32("bada", 6 * D, parts=R)
    gA = a32("gA", D, parts=R)
    gF = a32("gF", D, parts=R)
    dma("sp", cv, cvec)
    act(cv, cv, AF.Silu)
    tr_group([(ps[0].ap[:, kc * R:(kc + 1) * R], cv.ap[:, kc * 128:(kc + 1) * 128], ident.ap[0:R, 0:R]) for kc in range(8)],
             r=[cv.key, ident.key], w=[ps[0].key])
    cp("dve", scT, ps[0][:, 0:8 * R])
    for li in range(n_layers):
        dma("sp", bada, b_ada[li:li + 1, :].partition_broadcast(R))
        dma("sp", gA, g_attn[li:li + 1, :].partition_broadcast(R))
        dma("sp", gF, g_ffn[li:li + 1, :].partition_broadcast(R))
        for n in range(12):
            wt = wada[n % 2]
            dma("sp" if n % 2 == 0 else "act", wt.v(wt.ap.rearrange("p (k f) -> p k f", f=512)),
                w_ada[li, :, n * 512:(n + 1) * 512].rearrange("(k p) f -> p k f", p=128))
            pp = ps[1 + (n % 2)]
            mm_group(pp[0:R, :], [(scT.ap[:, kc * R:(kc + 1) * R], wt.ap[:, kc * 512:(kc + 1) * 512]) for kc in range(8)],
                     r=[scT.key, wt.key])
            tt("dve", modsb[:, n * 512:(n + 1) * 512], pp[0:R, :], bada[:, n * 512:(n + 1) * 512], ALU.add)
        stt("dve", modsb[:, D:2 * D], modsb[:, D:2 * D], 1.0, gA, ALU.add, ALU.mult)
        stt("dve", modsb[:, 4 * D:5 * D], modsb[:, 4 * D:5 * D], 1.0, gF, ALU.add, ALU.mult)
        dma("sp", modd[li], modsb, w=[("modd", li)])

    def norm_h(xt, h, gs_bc, sh_bc, st):
        stt("dve", h, xt, 1.0 / D, xt, ALU.mult, ALU.mult, accum=st[:, 0:1])
        act(st[:, 1:2], st[:, 0:1], AF.Ln, bias=EPS)
        act(st[:, 2:3], st[:, 1:2], AF.Exp, scale=-0.5)
        stt("dve", h, xt, st[:, 2:3], gs_bc, ALU.mult, ALU.mult)
        tt("pool", h, h, sh_bc, ALU.add)

    def transpose_h(h, dst, dst_is_bf16, pbanks):
        for half in range(2):
            pp = pbanks[half]
            tr_group([(pp.ap[:, j * 128:(j + 1) * 128], h.ap[:, (half * 4 + j) * 128:(half * 4 + j + 1) * 128], ident.ap)
                      for j in range(4)], r=[h.key, ident.key], w=[pp.key])
            d = dst[:, half * 512:(half + 1) * 512]
            if half == 0:
                act(d, pp, AF.Copy)
            else:
                cp("dve", d, pp)

    stat_ring = Ring([T(small.ap[:, i * 4:(i + 1) * 4], ("stat", i)) for i in range(4)])

    for li in range(n_layers):
        mtype = li % 3
        jidx = li // 3
        last = li == n_layers - 1
        rope = mtype in (0, 1)

        P.barrier()
        alloc_state["off"] = PERSIST
        wqkv = a16("wqkv", 8 * 1536)
        wo = a16("wo", NH * D, parts=64)
        kT = a16("kT", NKV * NT_B * 128, parts=64)
        Vs = a16("Vs", NT_B * 256)
        modA = [a32("modA%d" % i, D) for i in range(3)]
        xts = Ring([a32("xt%d" % i, D) for i in range(3)])
        hs = Ring([a32("h%d" % i, D) for i in range(2)])
        hTs = Ring([a16("hT%d" % i, 8 * 128) for i in range(2)])
        q32 = a32("q32", D)
        qro = a32("qro", D) if rope else None
        qtmp = a32("qtmp", 512)
        qbf = [a16("qbf%d" % i, D) for i in range(2 if rope else 1)]
        qT = [[a16("qT%d_%d" % (sl, i), NH * 128, parts=64) for i in range(2 if rope else 1)] for sl in range(2)]
        k32s = [q32[:, i * 256:(i + 1) * 256] for i in range(2)]
        kros = [a32("kro_%d" % i, 256) if rope else None for i in range(2)]
        ktms = [qtmp[:, i * 256:(i + 1) * 256] for i in range(2)]
        kbs = [a16("kb%d" % i, 256) for i in range(2)]
        hsts = [a32("hst%d" % i, 64) for i in range(2)]
        pTs = Ring([a16("pT%d" % i, 512) for i in range(3)])
        if mtype == 2:
            sbS = Ring([a32("sbS%d" % i, 512) for i in range(2)])
            nat = Ring([a32("nat%d" % i, 512) for i in range(2)])
        den = a32("den", 512, parts=64)
        rden = a32("rden", 512, parts=64)
        oT = a16("oT", NH * 128, parts=64)
        ytmp = Ring([a32("ytmp%d" % i, 512) for i in range(1)])
        sink_s = a32("sink_s", NH, parts=64)
        hst = a32("hst", 64)
        print("attn SBUF KB", alloc_state["off"] / 256.0)

        dma("pool", wqkv.v(wqkv.ap.rearrange("p (k n) -> p k n", n=1536)),
            w_qkv[li].rearrange("(k p) n -> p k n", p=128))
        dma("pool", wo.v(wo.ap.rearrange("p (h n) -> p h n", n=D)), w_o[li].rearrange("(h p) n -> p h n", p=64))
        use_sink = mtype == 0
        if use_sink:
            dma("sp", sink_s, sink_a[jidx:jidx + 1, :].partition_broadcast(64))
            act(sink_s, sink_s, AF.Exp)

        def load_modA(row):
            for i, c in enumerate((1, 0, 2)):
                dma("sp", modA[i], modd[li, row:row + 1, c * D:(c + 1) * D].partition_broadcast(128), r=[("modd", li)])

        def head_norm(src, nheads, g_bc, dst, scratch, hst):
            W = nheads * HD
            sq = scratch[:, 0:W]
            act(sq, src, AF.Square)
            ssum = hst[:, 0:nheads]
            red(ssum, sq.v(sq.ap.rearrange("p (h d) -> p h d", d=HD)), ALU.add)
            act(hst[:, 16:16 + nheads], ssum, AF.Ln, scale=1.0 / HD, bias=EPS)
            act(hst[:, 32:32 + nheads], hst[:, 16:16 + nheads], AF.Exp, scale=-0.5)
            rs_b = hst.v(hst.ap[:, 32:32 + nheads].unsqueeze(2).to_broadcast([128, nheads, HD]))
            d3 = dst.v(dst.ap.rearrange("p (h d) -> p h d", d=HD))
            s3 = src.v(src.ap.rearrange("p (h d) -> p h d", d=HD))
            tt("dve", d3, s3, rs_b, ALU.mult)
            g_b = g_bc.v(g_bc.ap.unsqueeze(1).to_broadcast([128, nheads, HD]))
            tt("pool", d3, d3, g_b, ALU.mult)

        def apply_rope(src, nheads, t, dst, qtmp):
            W = nheads * HD
            cs = cos_t.v(cos_t.ap[:, t * HD:(t + 1) * HD].unsqueeze(1).to_broadcast([128, nheads, HD]))
            s3 = src.v(src.ap.rearrange("p (h d) -> p h d", d=HD))
            d3 = dst.v(dst.ap.rearrange("p (h d) -> p h d", d=HD))
            t3 = qtmp.v(qtmp.ap[:, 0:W].rearrange("p (h d) -> p h d", d=HD))
            tt("dve", d3, s3, cs, ALU.mult)
            s5 = src.ap.rearrange("p (h a b d) -> p h a b d", a=2, b=2, d=16)
            t5 = qtmp.ap[:, 0:W].rearrange("p (h a b d) -> p h a b d", a=2, b=2, d=16)
            sn5 = sin_t.ap[:, t * HD:(t + 1) * HD].rearrange("p (a b d) -> p a b d", a=2, b=2, d=16)
            for bsel in range(2):
                o_ = T(t5[:, :, :, bsel, :], qtmp.key)
                i_ = T(s5[:, :, :, 1 - bsel, :], src.key)
                sn = T(sn5[:, :, bsel, :].unsqueeze(1).to_broadcast([128, nheads, 2, 16]), sin_t.key)
                tt("dve" if bsel == 0 else "pool", o_, i_, sn, ALU.mult)
            tt("pool", d3, d3, t3, ALU.add)

        def interleave(gens, width, stagger):
            active = []
            it = iter(gens)
            pending_start = 0
            done = False
            while True:
                if not done and len(active) < width and pending_start <= 0:
                    g_ = next(it, None)
                    if g_ is None:
                        done = True
                    else:
                        active.append(g_)
                        pending_start = stagger
                if not active:
                    if done:
                        break
                    pending_start = 0
                    continue
                pending_start -= 1
                for g_ in list(active):
                    try:
                        next(g_)
                    except StopIteration:
                        active.remove(g_)

        cur_gs_row = [None]
        cur_gt_row = [None]

        def need_gs(row):
            if cur_gs_row[0] != row:
                for i, c in enumerate((1, 0)):
                    dma("sp", modA[i], modd[li, row:row + 1, c * D:(c + 1) * D].partition_broadcast(128), r=[("modd", li)])
                cur_gs_row[0] = row

        def need_gt(row):
            if cur_gt_row[0] != row:
                dma("sp", modA[2], modd[li, row:row + 1, 2 * D:3 * D].partition_broadcast(128), r=[("modd", li)])
                cur_gt_row[0] = row

        def kv_gen(b, t, slot):
            is_ctx = t >= NT_L
            gt = b * NT_B + t
            pbase = 4 * slot
            need_gs(NB if is_ctx else b)
            xt = xts.next()
            dma("sp", xt, xs[gt * 128:(gt + 1) * 128, :], r=[("xs", gt)])
            hh_ = hs.next()
            st = stat_ring.next()
            norm_h(xt, hh_, modA[0], modA[1], st)
            yield
            hT = hTs.next()
            transpose_h(hh_, hT, True, (ps[pbase], ps[pbase + 1]))
            yield
            pk = ps[pbase + 2]
            mm_group(pk, [(hT.ap[:, kc * 128:(kc + 1) * 128], wqkv.ap[:, kc * 1536 + 1024:kc * 1536 + 1536])
                          for kc in range(8)], r=[hT.key, wqkv.key])
            yield
            k32, kro, ktm = k32s[slot], kros[slot], ktms[slot]
            ksrc = pk[:, 0:256]
            act(Vs[:, t * 256:(t + 1) * 256], pk[:, 256:512], AF.Copy)
            if mtype == 1:
                head_norm(ksrc, NKV, gk_bc, k32, ktm, hsts[slot])
                ksrc = k32
                yield
            if rope and not is_ctx:
                if mtype != 1:
                    cp("dve", k32, ksrc)
                    ksrc = k32
                apply_rope(ksrc, NKV, t, kro, ktm)
                ksrc = kro
                yield
            kb = kbs[slot]
            act(kb, ksrc, AF.Copy)
            yield
            pb = psbf(pbase + 3)
            tr_group([(pb.ap[0:64, g * 128:(g + 1) * 128], kb.ap[:, g * HD:(g + 1) * HD], identb.ap) for g in range(NKV)],
                     r=[kb.key, identb.key], w=[pb.key])
            kTv = kT.v(kT.ap.rearrange("p (g n) -> p g n", g=NKV)[:, :, t * 128:(t + 1) * 128])
            cp("dve", kTv, pb.v(pb.ap[0:64, 0:512].rearrange("p (g n) -> p g n", g=NKV)))
            yield

        def q_prologue(b, t, qslot, info):
            is_ctx = t >= NT_L
            gt = b * NT_B + t
            need_gs(NB if is_ctx else b)
            xt = xts.next()
            info["xt"] = xt
            dma("sp", xt, xs[gt * 128:(gt + 1) * 128, :], r=[("xs", gt)])
            hh_ = hs.next()
            st = stat_ring.next()
            norm_h(xt, hh_, modA[0], modA[1], st)
            yield
            hT = hTs.next()
            transpose_h(hh_, hT, True, (ps[0], ps[1]))
            yield
            for half in range(2):
                mm_group(ps[2 + half], [(hT.ap[:, kc * 128:(kc + 1) * 128],
                                         wqkv.ap[:, kc * 1536 + half * 512:kc * 1536 + (half + 1) * 512])
                                        for kc in range(8)], r=[hT.key, wqkv.key])
            yield
            for half in range(2):
                if mtype == 1:
                    head_norm(ps[2 + half], 8, gq_bc, q32[:, half * 512:(half + 1) * 512], qtmp[:, 0:512], hst)
                else:
                    cp("dve", q32[:, half * 512:(half + 1) * 512], ps[2 + half])
                yield
            variants = [0]
            act(qbf[0], q32, AF.Copy, scale=0.125)
            if rope and not is_ctx:
                for half in range(2):
                    apply_rope(q32[:, half * 512:(half + 1) * 512], 8, t, qro[:, half * 512:(half + 1) * 512], qtmp[:, 0:512])
                    yield
                act(qbf[1], qro, AF.Copy, scale=0.125)
                variants = [0, 1]
            yield
            qTs_ = qT[qslot]
            for vi in variants:
                for hh in range(2):
                    pb = psbf(hh)
                    tr_group([(pb.ap[0:64, j * 128:(j + 1) * 128], qbf[vi].ap[:, (hh * 8 + j) * HD:(hh * 8 + j + 1) * HD],
                               identb.ap) for j in range(8)], r=[qbf[vi].key, identb.key], w=[pb.key])
                    if hh == 0:
                        cp("dve", qTs_[vi][:, hh * 1024:(hh + 1) * 1024], pb[0:64, :])
                    else:
                        act(qTs_[vi][:, hh * 1024:(hh + 1) * 1024], pb[0:64, :], AF.Copy)
                    yield

        def attend(b, t, qslot, info, nxt):
            is_ctx = t >= NT_L
            gt = b * NT_B + t
            xt = info["xt"]
            qTs_ = qT[qslot]
            need_gt(NB if is_ctx else b)
            if is_ctx:
                KL = [(NT_L + j, 0, None, None) for j in range(NT_C)]
            else:
                KL = []
                if mtype == 0:
                    for kt_ in (t - 1, t, t + 1):
                        if 0 <= kt_ < NT_L:
                            m = maskLo if kt_ == t - 1 else (maskHi if kt_ == t + 1 else None)
                            KL.append((kt_, 1, m, None))
                elif mtype == 1:
                    KL = [(kt_, 1, None, None) for kt_ in range(NT_L)]
                else:
                    for j, kt_ in enumerate(_na_keytiles(t)):
                        KL.append((kt_, 0, None, (_na_pattern(t), j)))
                KL += [(NT_L + j, 0, None, None) for j in range(NT_C)]
            nk = len(KL)
            items = [(g, ki) + KL[ki] for g in range(NKV) for ki in range(nk)]

            def emit_S(i):
                g, ki, kt_, vi, msk, natp = items[i]
                pS = ps[4 + (i % 2)]
                kslice = kT.ap[:, g * NT_B * 128 + kt_ * 128: g * NT_B * 128 + (kt_ + 1) * 128]
                mm_group(pS, [(kslice, qTs_[vi].ap[:, g * 512:(g + 1) * 512])], r=[kT.key, qTs_[vi].key])

            if OPT["S_AHEAD"]:
                emit_S(0)
            for i, (g, ki, kt_, vi, msk, natp) in enumerate(items):
                if not OPT["S_AHEAD"]:
                    emit_S(i)
                elif i + 1 < len(items):
                    emit_S(i + 1)
                pS = ps[4 + (i % 2)]
                pT = pTs.next()
                if natp is not None:
                    nb_ = nat.next()
                    dma("sp" if i % 2 == 0 else "act", nb_, c_natab[natp[0], natp[1], :, g * 512:(g + 1) * 512])
                    sS = sbS.next()
                    tt("dve", sS, pS, nb_, ALU.add)
                    act(pT, sS, AF.Exp)
                else:
                    act(pT, pS, AF.Exp)
                if msk is not None:
                    tt("pool", pT, pT, msk, ALU.mult)
                oa6, oa7 = ps[6].ap[0:64, :], ps[7].ap[0:64, :]
                vsl = Vs.ap[:, kt_ * 256 + g * HD: kt_ * 256 + (g + 1) * HD]
                pTa = pT.ap
                first, lastk = (ki == 0), (ki == nk - 1)

                def pv(e, oa6=oa6, oa7=oa7, vsl=vsl, pTa=pTa, first=first, lastk=lastk):
                    e.matmul(oa6, lhsT=vsl, rhs=pTa, start=first, stop=lastk)
                    return e.matmul(oa7, lhsT=onesb.ap[:, 0:64], rhs=pTa, start=first, stop=lastk)
                P.add("pe", pv, [Vs.key, pT.key, onesb.key], [ps[6].key, ps[7].key])
                if lastk:
                    if use_sink and not OPT["SINK_BC"]:
                        for hq in range(4):
                            ts("dve", den[:, hq * 128:(hq + 1) * 128], ps[7][0:64, hq * 128:(hq + 1) * 128],
                               sink_s[:, 4 * g + hq:4 * g + hq + 1], ALU.add)
                        recip(rden, den)
                    elif use_sink:
                        sk = sink_s.v(sink_s.ap[:, 4 * g:4 * g + 4].unsqueeze(2).to_broadcast([64, 4, 128]))
                        tt("dve", den.v(den.ap.rearrange("p (h q) -> p h q", q=128)),
                           ps[7].v(ps[7].ap[0:64, :].rearrange("p (h q) -> p h q", q=128)), sk, ALU.add)
                        recip(rden, den)
                    else:
                        recip(rden, ps[7][0:64, :])
                    tt("dve", oT[:, g * 512:(g + 1) * 512], ps[6][0:64, :], rden, ALU.mult)
                if nxt is not None and i >= 1:
                    next(nxt, None)
            if nxt is not None:
                for _ in nxt:
                    pass
            for half in range(2):
                mm_group(ps[2 + half], [(oT.ap[:, hh * 128:(hh + 1) * 128], wo.ap[:, hh * D + half * 512: hh * D + (half + 1) * 512])
                                        for hh in range(NH)], r=[oT.key, wo.key])
                yt = ytmp.next()
                tt("dve", yt, ps[2 + half], modA[2][:, half * 512:(half + 1) * 512], ALU.mult)
                tt("pool", xt[:, half * 512:(half + 1) * 512], xt[:, half * 512:(half + 1) * 512], yt, ALU.add)
            dma("sp", xs[gt * 128:(gt + 1) * 128, :], xt, w=[("xs", gt)])

        for b in (range(NB) if not OPT["SKIP_ATT"] else []):
            interleave((kv_gen(b, t, t % 2) for t in range(NT_B)), OPT["W_KV"], 3)
            q_tiles = list(range(NT_L)) + ([] if last else list(range(NT_L, NT_B)))
            infos = [dict() for _ in q_tiles]
            g0 = q_prologue(b, q_tiles[0], 0, infos[0])
            for _ in g0:
                pass
            for qi, t in enumerate(q_tiles):
                nxt = None
                if qi + 1 < len(q_tiles) and OPT["Q_OVERLAP"]:
                    nxt = q_prologue(b, q_tiles[qi + 1], (qi + 1) % 2, infos[qi + 1])
                attend(b, t, qi % 2, infos[qi], nxt)
                if qi + 1 < len(q_tiles) and not OPT["Q_OVERLAP"]:
                    for _ in q_prologue(b, q_tiles[qi + 1], (qi + 1) % 2, infos[qi + 1]):
                        pass

        P.barrier()
        alloc_state["off"] = PERSIST
        wgs = [a16("wg%d" % i, 8 * FH) for i in range(2)]
        wus = [a16("wu%d" % i, 8 * FH) for i in range(2)]
        wds = [a16("wd%d" % i, 4 * D) for i in range(2)]
        xgs = [a16("xg%d" % i, (CAP // 128) * D) for i in range(2)]
        xeT = a16("xeT", 8 * CAP)
        HT = a16("HT", 4 * CAP)
        sgs = Ring([a32("sg%d" % i, CAP) for i in range(2)])
        Ysb = Ring([a32("Ysb%d" % i, D) for i in range(2)])
        xts = Ring([a32("mxt%d" % i, D) for i in range(2)])
        h = a32("mh", D)
        hbfs = Ring([a16("hbf%d" % i, D) for i in range(2)])
        hT32 = a32("hT32", 8 * 128)
        modF = [[a32("modF%d_%d" % (s, i), D) for i in range(2)] for s in range(2)]
        gtF = [a32("gtF%d" % s, D) for s in range(2)]
        y1s = Ring([a32("y1_%d" % i, D) for i in range(2)])
        y2s = Ring([a32("y2_%d" % i, D) for i in range(2)])
        gfin = a32("gfin", D) if last else None
        wrg = a32("wrg", 8 * 36)
        print("moe SBUF KB (before rl)", alloc_state["off"] / 256.0)
        rls = [a32("rl%d" % i, 512) for i in range(2)]
        hT32s = [hT32, a32("hT32b", 8 * 128)]
        mhs = [h, a32("mh2", D)]

        def mk_rl(si):
            rl = rls[si]

            def RL(name, off, n):
                return T(rl.ap[:, off:off + n], ("rl", si, name))
            d = dict(L=RL("L", 0, 36), mg=RL("mg", 40, 1), nmg=RL("nmg", 41, 1), eg=RL("eg", 44, 4), sg=RL("sg", 48, 1),
                     ptop=RL("ptop", 49, 1), ohg=RL("ohg", 52, 4), pen=RL("pen", 56, 4), Lm=RL("Lm", 64, 32),
                     mx8=RL("mx8", 96, 8), oh1=RL("oh1", 104, 32), oh2=RL("oh2", 136, 32), dd=RL("dd", 168, 1),
                     ed=RL("ed", 169, 1), rd=RL("rd", 170, 1), A=RL("A", 172, 32),
                     Abf=T(rl.ap[:, 204:220].bitcast(BF16), ("rl", si, "Abf")), slot=RL("slot", 224, 32),
                     tmpa=RL("tmpa", 256, 32), tmpb=RL("tmpb", 328, 32), d1f=RL("d1f", 288, 1), d2f=RL("d2f", 289, 1),
                     ov=RL("ov", 290, 1), nov=RL("nov", 291, 1), slotp=RL("slotp", 296, 32))
            return d
        RLS = [mk_rl(0), mk_rl(1)]

        m_tiles = []
        for b in range(NB):
            m_tiles += [(b * NT_B + t, b) for t in range(NT_L)]
        if not last:
            for b in range(NB):
                m_tiles += [(b * NT_B + NT_L + t, NB) for t in range(NT_C)]
        XGKEYS = [("XG", gt) for (gt, _) in m_tiles]

        dma("sp", wrg.v(wrg.ap.rearrange("p (k n) -> p k n", n=36)), w_rg[li].rearrange("(k p) n -> p k n", p=128))
        dma("sp", brg_bc, b_rg[li:li + 1, :].partition_broadcast(128))
        memset("dve", tot, 0.0)
        if last:
            dma("sp", gfin, g_final.partition_broadcast(128))

        m1_state = {"row": None, "mset": -1, "mf": None}

        def m1_gen(gt, row, si):
            R_ = RLS[si]
            pbase = 4 * si
            if row != m1_state["row"]:
                m1_state["mset"] += 1
                mf_ = modF[m1_state["mset"] % 2]
                dma("sp", mf_[0], modd[li, row:row + 1, 4 * D:5 * D].partition_broadcast(128), r=[("modd", li)])
                dma("sp", mf_[1], modd[li, row:row + 1, 3 * D:4 * D].partition_broadcast(128), r=[("modd", li)])
                m1_state["row"] = row
                m1_state["mf"] = mf_
            mf = m1_state["mf"]
            xt = xts.next()
            dma("sp", xt, xs[gt * 128:(gt + 1) * 128, :], r=[("xs", gt)])
            st = stat_ring.next()
            hh_ = mhs[si]
            norm_h(xt, hh_, mf[0], mf[1], st)
            yield
            hbf = hbfs.next()
            act(hbf, hh_, AF.Copy)
            hT32_ = hT32s[si]
            transpose_h(hh_, hT32_, False, (ps[pbase], ps[pbase + 1]))
            yield
            mm_group(ps[pbase + 2][:, 0:36], [(hT32_.ap[:, kc * 128:(kc + 1) * 128], wrg.ap[:, kc * 36:(kc + 1) * 36]) for kc in range(8)],
                     r=[hT32_.key, wrg.key])
            yield
            L, mg, nmg, eg, sg_, ptop, ohg, pen, Lm, mx8 = (R_[k] for k in ("L", "mg", "nmg", "eg", "sg", "ptop", "ohg", "pen", "Lm", "mx8"))
            oh1, oh2, dd, ed, rd, Asum, Abf, slot = (R_[k] for k in ("oh1", "oh2", "dd", "ed", "rd", "A", "Abf", "slot"))
            tt("dve", L, ps[pbase + 2][:, 0:36], brg_bc, ALU.add)
            red(mg, L[:, 0:4], ALU.max)
            yield
            ts("dve", nmg, mg, -1.0, ALU.mult)
            ts("dve", ohg, L[:, 0:4], mg, ALU.is_equal)
            yield
            act(eg, L[:, 0:4], AF.Exp, bias=nmg, accum=sg_)
            ts("dve", pen, ohg, -1.0, ALU.add, 1e30, ALU.mult)
            yield
            recip(ptop, sg_)
            tt("dve", Lm.v(Lm.ap.rearrange("p (g e) -> p g e", e=8)), L.v(L.ap[:, 4:36].rearrange("p (g e) -> p g e", e=8)),
               pen.v(pen.ap.unsqueeze(2).to_broadcast([128, 4, 8])), ALU.add)
            yield
            mxo, lmi = mx8.ap, Lm.ap
            P.add("dve", lambda e, mxo=mxo, lmi=lmi: e.max(out=mxo, in_=lmi), [Lm.key], [mx8.key])
            yield
            ts("dve", oh1, Lm, mx8[:, 0:1], ALU.is_equal)
            ts("dve", oh2, Lm, mx8[:, 1:2], ALU.is_equal)
            tt("dve", dd, mx8[:, 1:2], mx8[:, 0:1], ALU.subtract)
            yield
            act(ed, dd, AF.Exp)
            tt("dve", Asum, oh1, oh2, ALU.add)
            yield
            ts("dve", ed, ed, 1.0, ALU.add)
            cp("dve", Abf, Asum)
            yield
            recip(rd, ed)
            w1 = T(rt_w1.ap[:, gt:gt + 1], ("rt_w1", gt))
            w2 = T(rt_w2.ap[:, gt:gt + 1], ("rt_w2", gt))
            mm_group(ps[pbase + 3][:, 0:32], [(trib.ap, Abf.ap)], r=[trib.key, Abf.key])
            mm_group(ps[pbase + 3][:, 32:64], [(onesb.ap, Abf.ap)], r=[onesb.key, Abf.key])
            stt("dve", slot, ps[pbase + 3][:, 0:32], 0.0, tot, ALU.add, ALU.add)
            tt("dve", tot, tot, ps[pbase + 3][:, 32:64], ALU.add)
            yield
            tt("dve", w1, ptop, rd, ALU.mult)
            tt("dve", R_["slotp"], slot, ec_bc, ALU.add)
            yield
            tt("dve", w2, ptop, w1, ALU.subtract)
            tt("dve", R_["tmpa"], oh1, slot, ALU.mult)
            tt("dve", R_["tmpb"], oh2, slot, ALU.mult)
            yield
            ovs = []
            for ci, (oh, df, rtd, tmp) in enumerate(((oh1, R_["d1f"], rt_d1, R_["tmpa"]), (oh2, R_["d2f"], rt_d2, R_["tmpb"]))):
                ov = T(rls[si].ap[:, 400 + ci:401 + ci], ("rl", si, "ov%d" % ci))
                nov = T(rls[si].ap[:, 404 + ci:405 + ci], ("rl", si, "nov%d" % ci))
                red(ov, tmp, ALU.add)
                yield
                ts("dve", ov, ov, float(CAP), ALU.is_ge)
                tt("dve", tmp, oh, R_["slotp"], ALU.mult)
                yield
                ts("dve", nov, ov, -1.0, ALU.mult, 1.0, ALU.add)
                red(df, tmp, ALU.add)
                yield
                tt("dve", df, df, nov, ALU.mult)
                yield
                stt("dve", df, ov, float(NE * CAP), df, ALU.mult, ALU.add)
                yield
                rcol = T(rtd.ap[:, gt:gt + 1], (rtd.key, gt))
                cp("dve", rcol, df)
                yield
                ia = rcol.ap
                ha = hbf.ap
                P.add("pool", lambda e, ia=ia, ha=ha: e.indirect_dma_start(
                    out=XG, out_offset=bass.IndirectOffsetOnAxis(ap=ia, axis=0), in_=ha, in_offset=None),
                    [rcol.key, hbf.key], [("XG", gt)], dma=True)

        if not OPT["SKIP_M1"]:
            interleave((m1_gen(gt, row, i % 2) for i, (gt, row) in enumerate(m_tiles)), OPT["W_M1"], 9)

        for ex in (range(NE) if not OPT["SKIP_M2"] else []):
            wg, wu, wd, xg = wgs[ex % 2], wus[ex % 2], wds[ex % 2], xgs[ex % 2]
            dma("pool", wg.v(wg.ap.rearrange("p (k f) -> p k f", f=FH)), w_gate[li, ex].rearrange("(k p) f -> p k f", p=128))
            dma("pool", wu.v(wu.ap.rearrange("p (k f) -> p k f", f=FH)), w_up[li, ex].rearrange("(k p) f -> p k f", p=128))
            dma("pool", wd.v(wd.ap.rearrange("p (k f) -> p k f", f=D)), w_down[li, ex].rearrange("(k p) f -> p k f", p=128))
            dma("sp", xg.v(xg.ap.rearrange("p (s d) -> p s d", d=D)),
                XG[ex * CAP:(ex + 1) * CAP, :].rearrange("(s p) d -> p s d", p=128), r=XGKEYS)
            NS = CAP // 128
            for s in range(NS):
                pb = psbf(s % 2)
                tr_group([(pb.ap[:, kc * 128:(kc + 1) * 128], xg.ap[:, s * D + kc * 128: s * D + (kc + 1) * 128], identb.ap)
                          for kc in range(8)], r=[xg.key, identb.key], w=[pb.key])
                dst = xeT.v(xeT.ap.rearrange("p (k c) -> p k c", c=CAP)[:, :, s * 128:(s + 1) * 128])
                src = pb.v(pb.ap.rearrange("p (k c) -> p k c", c=128))
                if s % 2 == 0:
                    cp("dve", dst, src)
                else:
                    act(dst, src, AF.Copy)
            for m in range(4):
                mm_group(ps[2 + (m % 2)], [(wg.ap[:, kc * FH + m * 128: kc * FH + (m + 1) * 128], xeT.ap[:, kc * CAP:(kc + 1) * CAP])
                                           for kc in range(8)], r=[wg.key, xeT.key])
                mm_group(ps[4 + (m % 2)], [(wu.ap[:, kc * FH + m * 128: kc * FH + (m + 1) * 128], xeT.ap[:, kc * CAP:(kc + 1) * CAP])
                                           for kc in range(8)], r=[wu.key, xeT.key])
                sgt = sgs.next()
                act(sgt, ps[2 + (m % 2)], AF.Silu)
                tt("dve", HT[:, m * CAP:(m + 1) * CAP], sgt, ps[4 + (m % 2)], ALU.mult)
            for s in range(NS):
                ysb = Ysb.next()
                for half in range(2):
                    pp = ps[6 + half]
                    mm_group(pp, [(HT.ap[:, m * CAP + s * 128: m * CAP + (s + 1) * 128], wd.ap[:, m * D + half * 512: m * D + (half + 1) * 512])
                                  for m in range(4)], r=[HT.key, wd.key])
                    if half == 0:
                        act(ysb[:, 0:512], pp, AF.Copy)
                    else:
                        cp("dve", ysb[:, 512:1024], pp)
                dma("sp", YG[ex * CAP + s * 128: ex * CAP + (s + 1) * 128, :], ysb, w=[("YG", ex, s)])

        YGKEYS = ["YG"] + [("YG", ex, s_) for ex in range(NE) for s_ in range(CAP // 128)]
        m3_state = {"row": None, "mset": -1, "gtf": None}

        def m3_gen(gt, row):
            if row != m3_state["row"]:
                m3_state["mset"] += 1
                gtf_ = gtF[m3_state["mset"] % 2]
                dma("sp", gtf_, modd[li, row:row + 1, 5 * D:6 * D].partition_broadcast(128), r=[("modd", li)])
                m3_state["row"] = row
                m3_state["gtf"] = gtf_
            gtf = m3_state["gtf"]
            y1, y2 = y1s.next(), y2s.next()
            for (yy, rtd) in ((y1, rt_d1), (y2, rt_d2)):
                ia = rtd.ap[:, gt:gt + 1]
                ya = yy.ap
                P.add("pool", lambda e, ia=ia, ya=ya: e.indirect_dma_start(
                    out=ya, out_offset=None, in_=YG, in_offset=bass.IndirectOffsetOnAxis(ap=ia, axis=0)),
                    [(rtd.key, gt)] + YGKEYS, [yy.key], dma=True)
            xt = xts.next()
            dma("sp", xt, xs[gt * 128:(gt + 1) * 128, :], r=[("xs", gt)])
            yield
            w1 = T(rt_w1.ap[:, gt:gt + 1], ("rt_w1", gt))
            w2 = T(rt_w2.ap[:, gt:gt + 1], ("rt_w2", gt))
            ts("dve", y1, y1, w1, ALU.mult)
            yield
            stt("dve", y1, y2, w2, y1, ALU.mult, ALU.add)
            yield
            tt("pool", y1, y1, gtf, ALU.mult)
            yield
            tt("pool", xt, xt, y1, ALU.add)
            yield
            if not last:
                dma("sp", xs[gt * 128:(gt + 1) * 128, :], xt, w=[("xs", gt)])
            else:
                st = stat_ring.next()
                stt("dve", y2, xt, 1.0 / D, xt, ALU.mult, ALU.mult, accum=st[:, 0:1])
                yield
                act(st[:, 1:2], st[:, 0:1], AF.Ln, bias=EPS)
                yield
                act(st[:, 2:3], st[:, 1:2], AF.Exp, scale=-0.5)
                yield
                stt("dve", y2, xt, st[:, 2:3], gfin, ALU.mult, ALU.mult)
                b_ = gt // NT_B
                t_ = gt % NT_B
                r0 = b_ * SEQ + t_ * 128
                dma("sp", out[r0:r0 + 128, :], y2)

        if not OPT["SKIP_M3"]:
            interleave((m3_gen(gt, row) for (gt, row) in m_tiles), OPT["W_M3"], 3)

    P.finalize()
    P.emit()
    return nc


_CACHE = {}


def _consts():
    if "c" not in _CACHE:
        cos, sin_s = _rope_tables()
        kk = np.arange(128)[:, None]
        qq = np.arange(128)[None, :]
        mlo = np.tile((kk >= qq).astype(np.float32), (1, 4))
        mhi = np.tile((kk <= qq).astype(np.float32), (1, 4))
        tri = (np.arange(128)[:, None] < np.arange(128)[None, :]).astype(np.float32)
        ec = (np.arange(NE, dtype=np.float32) * CAP).reshape(1, NE)
        _CACHE["c"] = dict(c_ident=np.eye(128, dtype=np.float32), c_cos=cos, c_sin=sin_s,
                           c_masks=np.stack([mlo, mhi]).astype(np.float32), c_tri=tri, c_ec=ec)
        _CACHE["naidx"] = _na_index_table()
    return _CACHE["c"], _CACHE["naidx"]


def make_in_maps(inputs, n_cores, NB):
    f = lambda a: np.ascontiguousarray(np.asarray(a, dtype=np.float32))
    consts, naidx = _consts()
    rpb = f(inputs["rpb_c"])[0].reshape(-1)
    rpb_ext = np.concatenate([rpb, np.array([NEG], dtype=np.float32)])
    natab = np.ascontiguousarray(rpb_ext[naidx].reshape(5, 5, 128, NH * 128))
    w_rg = np.ascontiguousarray(np.concatenate([f(inputs["w_group"]), f(inputs["w_router"])], axis=-1))
    b_rg = np.ascontiguousarray(np.concatenate([f(inputs["b_group"]), f(inputs["b_router"])], axis=-1))
    shared = dict(
        w_ada=f(inputs["w_ada"]), b_ada=f(inputs["b_ada"]), g_attn=f(inputs["g_attn"]), w_qkv=f(inputs["w_qkv"]),
        w_o=f(inputs["w_o"]), sink_a=f(inputs["sink_a"]), gq_b=f(inputs["gq_b"]), gk_b=f(inputs["gk_b"]),
        g_ffn=f(inputs["g_ffn"]), w_rg=w_rg, b_rg=b_rg, w_gate=f(inputs["w_gate"]), w_up=f(inputs["w_up"]),
        w_down=f(inputs["w_down"]), g_final=f(inputs["g_final"]).reshape(1, D), c_natab=natab, **consts)
    x = f(inputs["x"])
    ctx = f(inputs["ctx"])
    c = f(inputs["c"])
    c_ctx = f(inputs["c_ctx"]).reshape(1, D)
    maps = []
    for i in range(n_cores):
        sl = slice(i * NB, (i + 1) * NB)
        m = dict(shared)
        m["x"] = np.ascontiguousarray(x[sl].reshape(NB * SEQ, D))
        m["ctx"] = np.ascontiguousarray(ctx[sl].reshape(NB * CTX, D))
        m["cvec"] = np.ascontiguousarray(np.concatenate([c[sl], c_ctx], axis=0))
        maps.append(m)
    return maps


def kernel(**inputs):
    n_cores = 8
    NB = 2
    if "nc" not in _CACHE:
        _CACHE["nc"] = build(NB=NB)
    nc = _CACHE["nc"]
    maps = make_in_maps(inputs, n_cores, NB)
    res = run_bass_kernel_spmd(nc, maps, core_ids=list(range(n_cores)))
    outs = [r["out"].reshape(NB, SEQ, D) for r in res.results]
    return np.concatenate(outs, axis=0).astype(np.float32)
```

```python
import numpy as np
import concourse.bass as bass
import concourse.mybir as mybir
from concourse.bass_utils import run_bass_kernel_spmd

F32 = mybir.dt.float32
BF16 = mybir.dt.bfloat16
I32 = mybir.dt.int32
ALU = mybir.AluOpType
AF = mybir.ActivationFunctionType
AX = mybir.AxisListType

D = 1024
SEQ = 2048
CTX = 256
DEPTH = 4
NH = 16
NKV = 4
HD = 64
NE = 32
FH = 512
GRID_W = 64
import os as _os
OPT = dict(W_KV=int(_os.environ.get("W_KV", 2)), W_M1=int(_os.environ.get("W_M1", 2)), W_M3=int(_os.environ.get("W_M3", 2)),
           Q_OVERLAP=int(_os.environ.get("Q_OVERLAP", 1)), S_AHEAD=int(_os.environ.get("S_AHEAD", 1)),
           SINK_BC=int(_os.environ.get("SINK_BC", 1)), SKIP_ATT=int(_os.environ.get("SKIP_ATT", 0)),
           SKIP_M1=int(_os.environ.get("SKIP_M1", 0)), SKIP_M2=int(_os.environ.get("SKIP_M2", 0)), SKIP_M3=int(_os.environ.get("SKIP_M3", 0)))
CAP = 512
NT_L = SEQ // 128
NT_C = CTX // 128
NT_B = NT_L + NT_C
EPS = 1e-6
NEG = -1e30


class _Op:
    __slots__ = ("idx", "eng", "fn", "deps", "dma", "sig", "waits", "signal", "ksnap")

    def __init__(self, idx, eng, fn, dma):
        self.idx = idx
        self.eng = eng
        self.fn = fn
        self.deps = set()
        self.dma = dma
        self.sig = None
        self.waits = []
        self.signal = False
        self.ksnap = None


class Prog:
    ENGS = ("pe", "act", "dve", "pool", "sp")
    EPOCH = 20000
    NDMA = {"sp": 24, "pool": 24, "act": 8}

    def __init__(self, nc):
        self.nc = nc
        self.ops = []
        self.last_w = {}
        self.readers = {}
        self.pending = {}
        self.last_eng = {}
        self.dma_since = []

    def add(self, eng, fn, r=(), w=(), dma=False):
        op = _Op(len(self.ops), eng, fn, dma)
        ops = self.ops

        def same_eng(j):
            o = ops[j]
            return (not dma) and (not o.dma) and o.eng == eng

        for k in r:
            lw = self.last_w.get(k)
            if lw is not None and not (eng == "pe" and same_eng(lw)):
                op.deps.add(lw)
            if isinstance(k, str) and k.startswith("ps"):
                for rd in self.readers.get(k, ()):
                    if not same_eng(rd):
                        op.deps.add(rd)
        for k in w:
            lw = self.last_w.get(k)
            if lw is not None and not (eng == "pe" and same_eng(lw)):
                op.deps.add(lw)
            rs = self.readers.get(k)
            if rs:
                for rd in rs:
                    if not same_eng(rd):
                        op.deps.add(rd)
        for k in r:
            self.readers.setdefault(k, []).append(op.idx)
        for k in w:
            self.last_w[k] = op.idx
            self.readers[k] = []
        if eng in self.pending:
            op.deps.update(self.pending.pop(eng))
        op.deps.discard(op.idx)
        self.ops.append(op)
        if dma:
            self.dma_since.append(op.idx)
        else:
            self.last_eng[eng] = op.idx
        return op

    def barrier(self):
        deps = set(self.last_eng.values()) | set(self.dma_since)
        for e in self.ENGS:
            self.pending[e] = set(deps) | self.pending.get(e, set())
        self.dma_since = []

    def finalize(self, final_engine="sp"):
        ops = self.ops
        dma_rr = {q: 0 for q in self.NDMA}
        dma_last = {}
        dma_cnt = {}
        for op in ops:
            if op.dma:
                q = op.eng
                s = dma_rr[q] % self.NDMA[q]
                dma_rr[q] += 1
                key = ("dma", q, s)
                prev = dma_last.get(key)
                if prev is not None:
                    op.deps.add(prev)
                dma_last[key] = op.idx
                dma_cnt[key] = dma_cnt.get(key, 0) + 16
                op.sig = (key, dma_cnt[key])
        has_dep = [False] * len(ops)
        for op in ops:
            for d in op.deps:
                has_dep[d] = True
        cnt = {e: 0 for e in self.ENGS}
        know = {e: {} for e in self.ENGS}
        for op in ops:
            e = op.eng
            K = know[e]
            for d in sorted(op.deps):
                sk, sv = ops[d].sig
                if K.get(sk, 0) >= sv:
                    continue
                op.waits.append((sk, sv))
                K[sk] = sv
                for k2, v2 in ops[d].ksnap.items():
                    if K.get(k2, 0) < v2:
                        K[k2] = v2
            if op.dma:
                op.signal = True
            elif has_dep[op.idx]:
                cnt[e] += 1
                ep = cnt[e] // self.EPOCH
                op.sig = (("eng", e, ep), cnt[e] - ep * self.EPOCH + (1 if ep else 0))
                op.signal = True
            op.ksnap = dict(K)
        self.final_waits = []
        Kf = know[final_engine]
        for key, v in dma_cnt.items():
            if Kf.get(key, 0) < v:
                self.final_waits.append((key, v))
        self.semkeys = set(op.sig[0] for op in ops if op.signal)
        self.final_engine = final_engine

    def emit(self):
        nc = self.nc
        sems = {}
        for i, k in enumerate(sorted(self.semkeys, key=str)):
            sems[k] = nc.alloc_semaphore("s%d" % i)
        by_eng = {e: [] for e in self.ENGS}
        for op in self.ops:
            by_eng[op.eng].append(op)
        fin_eng = self.final_engine
        fin_waits = self.final_waits

        def run(eng_name, eng):
            for op in by_eng[eng_name]:
                for sk, sv in op.waits:
                    eng.wait_ge(sems[sk], sv)
                ins = op.fn(eng)
                if op.signal:
                    ins.then_inc(sems[op.sig[0]], 16 if op.dma else 1)
            if eng_name == fin_eng:
                for sk, sv in fin_waits:
                    eng.wait_ge(sems[sk], sv)

        with nc.Block() as block:
            @block.tensor
            def _(e):
                run("pe", e)

            @block.scalar
            def _(e):
                run("act", e)

            @block.vector
            def _(e):
                run("dve", e)

            @block.gpsimd
            def _(e):
                run("pool", e)

            @block.sync
            def _(e):
                run("sp", e)


class T:
    __slots__ = ("ap", "key")

    def __init__(self, ap, key):
        self.ap = ap
        self.key = key

    def __getitem__(self, idx):
        return T(self.ap[idx], self.key)

    def v(self, ap):
        return T(ap, self.key)


class Ring:
    def __init__(self, items):
        self.items = items
        self.i = 0

    def next(self):
        it = self.items[self.i % len(self.items)]
        self.i += 1
        return it


def _rope_tables():
    t = np.arange(SEQ)
    row = (t // GRID_W).astype(np.float32)
    col = (t % GRID_W).astype(np.float32)
    quarter = HD // 4
    inv = (np.float32(10000.0) ** (-np.arange(quarter, dtype=np.float32) / np.float32(quarter))).astype(np.float32)
    ar = row[:, None] * inv
    ac = col[:, None] * inv
    ang = np.concatenate([ar, ar, ac, ac], axis=-1).astype(np.float32)
    cos = np.cos(ang).astype(np.float32)
    sin = np.sin(ang).astype(np.float32)
    sgn = np.concatenate([-np.ones(16), np.ones(16), -np.ones(16), np.ones(16)]).astype(np.float32)
    return cos, (sin * sgn[None, :]).astype(np.float32)


NA_PAT_TILES = [0, 1, 2, 14, 15]


def _na_pattern(t):
    return {0: 0, 1: 1, 14: 3, 15: 4}.get(t, 2)


def _na_keytiles(t):
    if t <= 1:
        return [0, 1, 2, 3]
    if t >= 14:
        return [12, 13, 14, 15]
    return [t - 2, t - 1, t, t + 1, t + 2]


def _na_index_table():
    rows = SEQ // GRID_W
    wh, ww = 8, 16
    nrel = 15 * 31
    masked = NH * nrel
    tab = np.full((5, 5, 128, NH, 128), masked, dtype=np.int64)
    for pi, t in enumerate(NA_PAT_TILES):
        kts = _na_keytiles(t)
        q = t * 128 + np.arange(128)
        r = q // GRID_W
        c = q % GRID_W
        rs = np.clip(r - wh // 2, 0, rows - wh)
        cs = np.clip(c - ww // 2, 0, GRID_W - ww)
        for j, kt in enumerate(kts):
            k = kt * 128 + np.arange(128)
            kr = k // GRID_W
            kc = k % GRID_W
            inwin = ((kr[:, None] >= rs[None, :]) & (kr[:, None] < rs[None, :] + wh)
                     & (kc[:, None] >= cs[None, :]) & (kc[:, None] < cs[None, :] + ww))
            rel = (kr[:, None] - r[None, :] + 7) * 31 + (kc[:, None] - c[None, :] + 15)
            for h in range(NH):
                tab[pi, j, :, h, :] = np.where(inwin, h * nrel + rel, masked)
    return tab


def build(NB=2, n_layers=DEPTH):
    nc = bass.Bass("TRN2", target_bir_lowering=False)
    R = NB + 1
    NTOK = NB * NT_B * 128

    def din(name, shape, dt=F32):
        return nc.dram_tensor(name, list(shape), dt, kind="ExternalInput").ap()

    x_in = din("x", [NB * SEQ, D])
    ctx_in = din("ctx", [NB * CTX, D])
    cvec = din("cvec", [R, D])
    w_ada = din("w_ada", [DEPTH, D, 6 * D])
    b_ada = din("b_ada", [DEPTH, 6 * D])
    g_attn = din("g_attn", [DEPTH, D])
    w_qkv = din("w_qkv", [DEPTH, D, 1536])
    w_o = din("w_o", [DEPTH, D, D])
    sink_a = din("sink_a", [2, NH])
    gq_b = din("gq_b", [1, HD])
    gk_b = din("gk_b", [1, HD])
    g_ffn = din("g_ffn", [DEPTH, D])
    w_rg = din("w_rg", [DEPTH, D, 36])
    b_rg = din("b_rg", [DEPTH, 36])
    w_gate = din("w_gate", [DEPTH, NE, D, FH])
    w_up = din("w_up", [DEPTH, NE, D, FH])
    w_down = din("w_down", [DEPTH, NE, FH, D])
    g_final = din("g_final", [1, D])
    c_ident = din("c_ident", [128, 128])
    c_cos = din("c_cos", [SEQ, HD])
    c_sin = din("c_sin", [SEQ, HD])
    c_masks = din("c_masks", [2, 128, 512])
    c_tri = din("c_tri", [128, 128])
    c_ec = din("c_ec", [1, NE])
    c_natab = din("c_natab", [5, 5, 128, NH * 128])
    out = nc.dram_tensor("out", [NB * SEQ, D], F32, kind="ExternalOutput").ap()

    xs = nc.dram_tensor("xs", [NTOK, D], F32).ap()
    modd = nc.dram_tensor("modd", [DEPTH, R, 6 * D], F32).ap()
    XG = nc.dram_tensor("XG", [NE * CAP + 1, D], BF16).ap()
    YG = nc.dram_tensor("YG", [NE * CAP + 1, D], F32).ap()

    P = Prog(nc)

    POOL_KB = 190
    pool = nc.alloc_sbuf_tensor("pool", [128, POOL_KB * 256], F32)
    alloc_state = {"off": 0}

    def a32(name, n, parts=128):
        off = alloc_state["off"]
        alloc_state["off"] = off + n
        assert alloc_state["off"] <= POOL_KB * 256, (name, alloc_state["off"])
        return T(pool[0:parts, off:off + n], name)

    def a16(name, n, parts=128):
        n32 = (n + 1) // 2
        off = alloc_state["off"]
        alloc_state["off"] = off + n32
        assert alloc_state["off"] <= POOL_KB * 256, (name, alloc_state["off"])
        return T(pool[0:parts, off:off + n32].bitcast(BF16), name)

    ps = [T(nc.alloc_psum_tensor("ps%d" % i, [128, 512], F32)[:, :], "ps%d" % i) for i in range(8)]

    def psbf(i):
        return T(ps[i].ap.bitcast(BF16), ps[i].key)

    ident = a32("ident", 128)
    identb = a16("identb", 128)
    onesb = a16("onesb", 128)
    trib = a16("trib", 128)
    cos_t = a32("cos", NT_L * HD)
    sin_t = a32("sin", NT_L * HD)
    maskLo = a16("maskLo", 512)
    maskHi = a16("maskHi", 512)
    gq_bc = a32("gq_bc", HD)
    gk_bc = a32("gk_bc", HD)
    ec_bc = a32("ec_bc", NE)
    brg_bc = a32("brg_bc", 36)
    tot = a32("tot", NE)
    NTT = NB * NT_B
    rt_w1 = a32("rt_w1", NTT)
    rt_w2 = a32("rt_w2", NTT)
    rt_d1 = T(nc.alloc_sbuf_tensor("rt_d1", [128, NTT], I32)[:, :], "rt_d1")
    rt_d2 = T(nc.alloc_sbuf_tensor("rt_d2", [128, NTT], I32)[:, :], "rt_d2")
    small = a32("small", 64)
    PERSIST = alloc_state["off"]

    def dma(q, out_, in_, r=(), w=()):
        oa = out_.ap if isinstance(out_, T) else out_
        ia = in_.ap if isinstance(in_, T) else in_
        rr = list(r) + ([in_.key] if isinstance(in_, T) else [])
        ww = list(w) + ([out_.key] if isinstance(out_, T) else [])
        P.add(q, lambda e: e.dma_start(out=oa, in_=ia), rr, ww, dma=True)

    def act(out_, in_, func, scale=1.0, bias=0.0, accum=None, r=(), w=()):
        kw = {}
        rr = [in_.key] + list(r)
        ww = [out_.key] + list(w)
        if isinstance(bias, T):
            rr.append(bias.key)
            bias = bias.ap
        if isinstance(scale, T):
            rr.append(scale.key)
            scale = scale.ap
        if accum is not None:
            kw["accum_out"] = accum.ap
            ww.append(accum.key)
        oa, ia = out_.ap, in_.ap
        P.add("act", lambda e: e.activation(out=oa, in_=ia, func=func, bias=bias, scale=scale, **kw), rr, ww)

    def tt(eng, out_, a, b, op):
        oa, aa, ba = out_.ap, a.ap, b.ap
        P.add(eng, lambda e: e.tensor_tensor(out=oa, in0=aa, in1=ba, op=op), [a.key, b.key], [out_.key])

    def ts(eng, out_, a, s1, op0, s2=None, op1=None, accum=None):
        rr = [a.key]
        ww = [out_.key]
        if isinstance(s1, T):
            rr.append(s1.key)
            s1 = s1.ap
        if isinstance(s2, T):
            rr.append(s2.key)
            s2 = s2.ap
        kw = {}
        if op1 is not None:
            kw["op1"] = op1
        if accum is not None:
            kw["accum_out"] = accum.ap
            ww.append(accum.key)
        oa, aa = out_.ap, a.ap
        P.add(eng, lambda e: e.tensor_scalar(out=oa, in0=aa, scalar1=s1, scalar2=s2, op0=op0, **kw), rr, ww)

    def stt(eng, out_, a, s, b, op0, op1, accum=None):
        rr = [a.key, b.key]
        ww = [out_.key]
        if isinstance(s, T):
            rr.append(s.key)
            s = s.ap
        kw = {}
        if accum is not None:
            kw["accum_out"] = accum.ap
            ww.append(accum.key)
        oa, aa, ba = out_.ap, a.ap, b.ap
        P.add(eng, lambda e: e.scalar_tensor_tensor(out=oa, in0=aa, scalar=s, in1=ba, op0=op0, op1=op1, **kw), rr, ww)

    def cp(eng, out_, in_):
        oa, ia = out_.ap, in_.ap
        P.add(eng, lambda e: e.tensor_copy(out=oa, in_=ia), [in_.key], [out_.key])

    def recip(out_, in_):
        oa, ia = out_.ap, in_.ap
        P.add("dve", lambda e: e.reciprocal(out=oa, in_=ia), [in_.key], [out_.key])

    def red(out_, in_, op, axis=AX.X):
        oa, ia = out_.ap, in_.ap
        P.add("dve", lambda e: e.tensor_reduce(out=oa, in_=ia, axis=axis, op=op), [in_.key], [out_.key])

    def memset(eng, out_, val):
        oa = out_.ap
        P.add(eng, lambda e: e.memset(oa, val), [], [out_.key])

    def mm_group(out_, pairs, r=()):
        oa = out_.ap
        n = len(pairs)

        def fn(e):
            ins = None
            for i, (l, rh) in enumerate(pairs):
                ins = e.matmul(oa, lhsT=l, rhs=rh, start=(i == 0), stop=(i == n - 1))
            return ins
        P.add("pe", fn, list(r), [out_.key])

    def tr_group(items, r=(), w=()):
        def fn(e):
            ins = None
            for (o, i, idn) in items:
                ins = e.transpose(out=o, in_=i, identity=idn)
            return ins
        P.add("pe", fn, list(r), list(w))

    dma("sp", ident, c_ident)
    dma("pool", identb, c_ident)
    dma("pool", trib, c_tri)
    dma("sp", cos_t.v(cos_t.ap.rearrange("p (t d) -> p t d", d=HD)), c_cos.rearrange("(t p) d -> p t d", p=128))
    dma("sp", sin_t.v(sin_t.ap.rearrange("p (t d) -> p t d", d=HD)), c_sin.rearrange("(t p) d -> p t d", p=128))
    dma("pool", maskLo, c_masks[0])
    dma("pool", maskHi, c_masks[1])
    dma("sp", gq_bc, gq_b.partition_broadcast(128))
    dma("sp", gk_bc, gk_b.partition_broadcast(128))
    dma("sp", ec_bc, c_ec.partition_broadcast(128))
    memset("dve", onesb, 1.0)

    for b in range(NB):
        base = b * NT_B * 128
        dma("sp", xs[base:base + SEQ, :], x_in[b * SEQ:(b + 1) * SEQ, :], w=[("xs", b * NT_B + t) for t in range(NT_L)])
        dma("sp", xs[base + SEQ:base + SEQ + CTX, :], ctx_in[b * CTX:(b + 1) * CTX, :],
            w=[("xs", b * NT_B + NT_L + t) for t in range(NT_C)])

    alloc_state["off"] = PERSIST
    zrow = a32("zrow", D, parts=1)
    memset("dve", zrow, 0.0)
    dma("sp", YG[NE * CAP:NE * CAP + 1, :], zrow, w=["YG"])
    cv = a32("cv", D, parts=R)
    scT = a32("scT", 8 * R)
    wada = [a32("wada%d" % i, 8 * 512) for i in range(2)]
    modsb = a32("modsb", 6 * D, parts=R)
    bada = a32("bada", 6 * D, parts=R)
    gA = a32("gA", D, parts=R)
    gF = a32("gF", D, parts=R)
    dma("sp", cv, cvec)
    act(cv, cv, AF.Silu)
    tr_group([(ps[0].ap[:, kc * R:(kc + 1) * R], cv.ap[:, kc * 128:(kc + 1) * 128], ident.ap[0:R, 0:R]) for kc in range(8)],
             r=[cv.key, ident.key], w=[ps[0].key])
    cp("dve", scT, ps[0][:, 0:8 * R])
    for li in range(n_layers):
        dma("sp", bada, b_ada[li:li + 1, :].partition_broadcast(R))
        dma("sp", gA, g_attn[li:li + 1, :].partition_broadcast(R))
        dma("sp", gF, g_ffn[li:li + 1, :].partition_broadcast(R))
        for n in range(12):
            wt = wada[n % 2]
            dma("sp" if n % 2 == 0 else "act", wt.v(wt.ap.rearrange("p (k f) -> p k f", f=512)),
                w_ada[li, :, n * 512:(n + 1) * 512].rearrange("(k p) f -> p k f", p=128))
            pp = ps[1 + (n % 2)]
            mm_group(pp[0:R, :], [(scT.ap[:, kc * R:(kc + 1) * R], wt.ap[:, kc * 512:(kc + 1) * 512]) for kc in range(8)],
                     r=[scT.key, wt.key])
            tt("dve", modsb[:, n * 512:(n + 1) * 512], pp[0:R, :], bada[:, n * 512:(n + 1) * 512], ALU.add)
        stt("dve", modsb[:, D:2 * D], modsb[:, D:2 * D], 1.0, gA, ALU.add, ALU.mult)
        stt("dve", modsb[:, 4 * D:5 * D], modsb[:, 4 * D:5 * D], 1.0, gF, ALU.add, ALU.mult)
        dma("sp", modd[li], modsb, w=[("modd", li)])

    def norm_h(xt, h, gs_bc, sh_bc, st):
        stt("dve", h, xt, 1.0 / D, xt, ALU.mult, ALU.mult, accum=st[:, 0:1])
        act(st[:, 1:2], st[:, 0:1], AF.Ln, bias=EPS)
        act(st[:, 2:3], st[:, 1:2], AF.Exp, scale=-0.5)
        stt("dve", h, xt, st[:, 2:3], gs_bc, ALU.mult, ALU.mult)
        tt("pool", h, h, sh_bc, ALU.add)

    def transpose_h(h, dst, dst_is_bf16, pbanks):
        for half in range(2):
            pp = pbanks[half]
            tr_group([(pp.ap[:, j * 128:(j + 1) * 128], h.ap[:, (half * 4 + j) * 128:(half * 4 + j + 1) * 128], ident.ap)
                      for j in range(4)], r=[h.key, ident.key], w=[pp.key])
            d = dst[:, half * 512:(half + 1) * 512]
            if half == 0:
                act(d, pp, AF.Copy)
            else:
                cp("dve", d, pp)

    stat_ring = Ring([T(small.ap[:, i * 4:(i + 1) * 4], ("stat", i)) for i in range(4)])

    for li in range(n_layers):
        mtype = li % 3
        jidx = li // 3
        last = li == n_layers - 1
        rope = mtype in (0, 1)

        P.barrier()
        alloc_state["off"] = PERSIST
        wqkv = a16("wqkv", 8 * 1536)
        wo = a16("wo", NH * D, parts=64)
        kT = a16("kT", NKV * NT_B * 128, parts=64)
        Vs = a16("Vs", NT_B * 512)
        modA = [a32("modA%d" % i, D) for i in range(3)]
        xts = Ring([a32("xt%d" % i, D) for i in range(3)])
        hs = Ring([a32("h%d" % i, D) for i in range(2)])
        hTs = Ring([a16("hT%d" % i, 8 * 128) for i in range(2)])
        q32 = a32("q32", D)
        qro = a32("qro", D) if rope else None
        qtmp = a32("qtmp", 512)
        qbf = [a16("qbf%d" % i, D) for i in range(2 if rope else 1)]
        qT = [[a16("qT%d_%d" % (sl, i), NH * 128, parts=64) for i in range(2 if rope else 1)] for sl in range(2)]
        k32s = [q32[:, i * 256:(i + 1) * 256] for i in range(2)]
        kros = [a32("kro_%d" % i, 256) if rope else None for i in range(2)]
        ktms = [qtmp[:, i * 256:(i + 1) * 256] for i in range(2)]
        kbs = [a16("kb%d" % i, 256) for i in range(2)]
        hsts = [a32("hst%d" % i, 64) for i in range(2)]
        pTs = Ring([a16("pT%d" % i, 512) for i in range(3)])
        if mtype == 2:
            sbS = Ring([a32("sbS%d" % i, 512) for i in range(2)])
            nat = Ring([a32("nat%d" % i, 512) for i in range(2)])
        den = a32("den", 512, parts=64)
        rden = a32("rden", 512, parts=64)
        oT = a16("oT", NH * 128, parts=64)
        ytmp = Ring([a32("ytmp%d" % i, 512) for i in range(1)])
        sink_s = a32("sink_s", NH, parts=64)
        hst = a32("hst", 64)
        print("attn SBUF KB", alloc_state["off"] / 256.0)

        dma("pool", wqkv.v(wqkv.ap.rearrange("p (k n) -> p k n", n=1536)),
            w_qkv[li].rearrange("(k p) n -> p k n", p=128))
        dma("pool", wo.v(wo.ap.rearrange("p (h n) -> p h n", n=D)), w_o[li].rearrange("(h p) n -> p h n", p=64))
        use_sink = mtype == 0
        memset("dve", Vs, 1.0)
        if use_sink:
            dma("sp", sink_s, sink_a[jidx:jidx + 1, :].partition_broadcast(64))
            act(sink_s, sink_s, AF.Exp)

        def load_modA(row):
            for i, c in enumerate((1, 0, 2)):
                dma("sp", modA[i], modd[li, row:row + 1, c * D:(c + 1) * D].partition_broadcast(128), r=[("modd", li)])

        def head_norm(src, nheads, g_bc, dst, scratch, hst):
            W = nheads * HD
            sq = scratch[:, 0:W]
            act(sq, src, AF.Square)
            ssum = hst[:, 0:nheads]
            red(ssum, sq.v(sq.ap.rearrange("p (h d) -> p h d", d=HD)), ALU.add)
            act(hst[:, 16:16 + nheads], ssum, AF.Ln, scale=1.0 / HD, bias=EPS)
            act(hst[:, 32:32 + nheads], hst[:, 16:16 + nheads], AF.Exp, scale=-0.5)
            rs_b = hst.v(hst.ap[:, 32:32 + nheads].unsqueeze(2).to_broadcast([128, nheads, HD]))
            d3 = dst.v(dst.ap.rearrange("p (h d) -> p h d", d=HD))
            s3 = src.v(src.ap.rearrange("p (h d) -> p h d", d=HD))
            tt("dve", d3, s3, rs_b, ALU.mult)
            g_b = g_bc.v(g_bc.ap.unsqueeze(1).to_broadcast([128, nheads, HD]))
            tt("pool", d3, d3, g_b, ALU.mult)

        def apply_rope(src, nheads, t, dst, qtmp):
            W = nheads * HD
            cs = cos_t.v(cos_t.ap[:, t * HD:(t + 1) * HD].unsqueeze(1).to_broadcast([128, nheads, HD]))
            s3 = src.v(src.ap.rearrange("p (h d) -> p h d", d=HD))
            d3 = dst.v(dst.ap.rearrange("p (h d) -> p h d", d=HD))
            t3 = qtmp.v(qtmp.ap[:, 0:W].rearrange("p (h d) -> p h d", d=HD))
            tt("dve", d3, s3, cs, ALU.mult)
            s5 = src.ap.rearrange("p (h a b d) -> p h a b d", a=2, b=2, d=16)
            t5 = qtmp.ap[:, 0:W].rearrange("p (h a b d) -> p h a b d", a=2, b=2, d=16)
            sn5 = sin_t.ap[:, t * HD:(t + 1) * HD].rearrange("p (a b d) -> p a b d", a=2, b=2, d=16)
            for bsel in range(2):
                o_ = T(t5[:, :, :, bsel, :], qtmp.key)
                i_ = T(s5[:, :, :, 1 - bsel, :], src.key)
                sn = T(sn5[:, :, bsel, :].unsqueeze(1).to_broadcast([128, nheads, 2, 16]), sin_t.key)
                tt("dve" if bsel == 0 else "pool", o_, i_, sn, ALU.mult)
            tt("pool", d3, d3, t3, ALU.add)

        def interleave(gens, width, stagger):
            active = []
            it = iter(gens)
            pending_start = 0
            done = False
            while True:
                if not done and len(active) < width and pending_start <= 0:
                    g_ = next(it, None)
                    if g_ is None:
                        done = True
                    else:
                        active.append(g_)
                        pending_start = stagger
                if not active:
                    if done:
                        break
                    pending_start = 0
                    continue
                pending_start -= 1
                for g_ in list(active):
                    try:
                        next(g_)
                    except StopIteration:
                        active.remove(g_)

        cur_gs_row = [None]
        cur_gt_row = [None]

        def need_gs(row):
            if cur_gs_row[0] != row:
                for i, c in enumerate((1, 0)):
                    dma("sp", modA[i], modd[li, row:row + 1, c * D:(c + 1) * D].partition_broadcast(128), r=[("modd", li)])
                cur_gs_row[0] = row

        def need_gt(row):
            if cur_gt_row[0] != row:
                dma("sp", modA[2], modd[li, row:row + 1, 2 * D:3 * D].partition_broadcast(128), r=[("modd", li)])
                cur_gt_row[0] = row

        def kv_gen(b, t, slot):
            is_ctx = t >= NT_L
            gt = b * NT_B + t
            pbase = 4 * slot
            need_gs(NB if is_ctx else b)
            xt = xts.next()
            dma("sp", xt, xs[gt * 128:(gt + 1) * 128, :], r=[("xs", gt)])
            hh_ = hs.next()
            st = stat_ring.next()
            norm_h(xt, hh_, modA[0], modA[1], st)
            yield
            hT = hTs.next()
            transpose_h(hh_, hT, True, (ps[pbase], ps[pbase + 1]))
            yield
            pk = ps[pbase + 2]
            mm_group(pk, [(hT.ap[:, kc * 128:(kc + 1) * 128], wqkv.ap[:, kc * 1536 + 1024:kc * 1536 + 1536])
                          for kc in range(8)], r=[hT.key, wqkv.key])
            yield
            k32, kro, ktm = k32s[slot], kros[slot], ktms[slot]
            ksrc = pk[:, 0:256]
            act(Vs.v(Vs.ap.rearrange("p (t g c) -> p t g c", g=NKV, c=128)[:, t, :, 0:64]),
                pk.v(pk.ap[:, 256:512].rearrange("p (g d) -> p g d", d=HD)), AF.Copy)
            if mtype == 1:
                head_norm(ksrc, NKV, gk_bc, k32, ktm, hsts[slot])
                ksrc = k32
                yield
            if rope and not is_ctx:
                if mtype != 1:
                    cp("dve", k32, ksrc)
                    ksrc = k32
                apply_rope(ksrc, NKV, t, kro, ktm)
                ksrc = kro
                yield
            kb = kbs[slot]
            act(kb, ksrc, AF.Copy)
            yield
            pb = psbf(pbase + 3)
            tr_group([(pb.ap[0:64, g * 128:(g + 1) * 128], kb.ap[:, g * HD:(g + 1) * HD], identb.ap) for g in range(NKV)],
                     r=[kb.key, identb.key], w=[pb.key])
            kTv = kT.v(kT.ap.rearrange("p (g n) -> p g n", g=NKV)[:, :, t * 128:(t + 1) * 128])
            cp("dve", kTv, pb.v(pb.ap[0:64, 0:512].rearrange("p (g n) -> p g n", g=NKV)))
            yield

        def q_prologue(b, t, qslot, info):
            is_ctx = t >= NT_L
            gt = b * NT_B + t
            need_gs(NB if is_ctx else b)
            xt = xts.next()
            info["xt"] = xt
            dma("sp", xt, xs[gt * 128:(gt + 1) * 128, :], r=[("xs", gt)])
            hh_ = hs.next()
            st = stat_ring.next()
            norm_h(xt, hh_, modA[0], modA[1], st)
            yield
            hT = hTs.next()
            transpose_h(hh_, hT, True, (ps[0], ps[1]))
            yield
            for half in range(2):
                mm_group(ps[2 + half], [(hT.ap[:, kc * 128:(kc + 1) * 128],
                                         wqkv.ap[:, kc * 1536 + half * 512:kc * 1536 + (half + 1) * 512])
                                        for kc in range(8)], r=[hT.key, wqkv.key])
            yield
            for half in range(2):
                if mtype == 1:
                    head_norm(ps[2 + half], 8, gq_bc, q32[:, half * 512:(half + 1) * 512], qtmp[:, 0:512], hst)
                else:
                    cp("dve", q32[:, half * 512:(half + 1) * 512], ps[2 + half])
                yield
            variants = [0]
            act(qbf[0], q32, AF.Copy, scale=0.125)
            if rope and not is_ctx:
                for half in range(2):
                    apply_rope(q32[:, half * 512:(half + 1) * 512], 8, t, qro[:, half * 512:(half + 1) * 512], qtmp[:, 0:512])
                    yield
                act(qbf[1], qro, AF.Copy, scale=0.125)
                variants = [0, 1]
            yield
            qTs_ = qT[qslot]
            for vi in variants:
                for hh in range(2):
                    pb = psbf(hh)
                    tr_group([(pb.ap[0:64, j * 128:(j + 1) * 128], qbf[vi].ap[:, (hh * 8 + j) * HD:(hh * 8 + j + 1) * HD],
                               identb.ap) for j in range(8)], r=[qbf[vi].key, identb.key], w=[pb.key])
                    if hh == 0:
                        cp("dve", qTs_[vi][:, hh * 1024:(hh + 1) * 1024], pb[0:64, :])
                    else:
                        act(qTs_[vi][:, hh * 1024:(hh + 1) * 1024], pb[0:64, :], AF.Copy)
                    yield

        def attend(b, t, qslot, info, nxt):
            is_ctx = t >= NT_L
            gt = b * NT_B + t
            xt = info["xt"]
            qTs_ = qT[qslot]
            need_gt(NB if is_ctx else b)
            if is_ctx:
                KL = [(NT_L + j, 0, None, None) for j in range(NT_C)]
            else:
                KL = []
                if mtype == 0:
                    for kt_ in (t - 1, t, t + 1):
                        if 0 <= kt_ < NT_L:
                            m = maskLo if kt_ == t - 1 else (maskHi if kt_ == t + 1 else None)
                            KL.append((kt_, 1, m, None))
                elif mtype == 1:
                    KL = [(kt_, 1, None, None) for kt_ in range(NT_L)]
                else:
                    for j, kt_ in enumerate(_na_keytiles(t)):
                        KL.append((kt_, 0, None, (_na_pattern(t), j)))
                KL += [(NT_L + j, 0, None, None) for j in range(NT_C)]
            nk = len(KL)
            items = [(g, ki) + KL[ki] for g in range(NKV) for ki in range(nk)]

            def emit_S(i):
                g, ki, kt_, vi, msk, natp = items[i]
                pS = ps[4 + (i % 2)]
                kslice = kT.ap[:, g * NT_B * 128 + kt_ * 128: g * NT_B * 128 + (kt_ + 1) * 128]
                mm_group(pS, [(kslice, qTs_[vi].ap[:, g * 512:(g + 1) * 512])], r=[kT.key, qTs_[vi].key])

            if OPT["S_AHEAD"]:
                emit_S(0)
            for i, (g, ki, kt_, vi, msk, natp) in enumerate(items):
                if not OPT["S_AHEAD"]:
                    emit_S(i)
                elif i + 1 < len(items):
                    emit_S(i + 1)
                pS = ps[4 + (i % 2)]
                pT = pTs.next()
                if natp is not None:
                    nb_ = nat.next()
                    dma("sp" if i % 2 == 0 else "act", nb_, c_natab[natp[0], natp[1], :, g * 512:(g + 1) * 512])
                    sS = sbS.next()
                    tt("dve", sS, pS, nb_, ALU.add)
                    act(pT, sS, AF.Exp)
                else:
                    act(pT, pS, AF.Exp)
                if msk is not None:
                    tt("pool", pT, pT, msk, ALU.mult)
                acc = ps[6 + (g % 2)]
                acca = acc.ap
                vsl = Vs.ap[:, kt_ * 512 + g * 128: kt_ * 512 + (g + 1) * 128]
                pTa = pT.ap
                first, lastk = (ki == 0), (ki == nk - 1)

                def pv(e, acca=acca, vsl=vsl, pTa=pTa, first=first, lastk=lastk):
                    return e.matmul(acca, lhsT=vsl, rhs=pTa, start=first, stop=lastk)
                P.add("pe", pv, [Vs.key, pT.key], [acc.key])
                if lastk:
                    if use_sink:
                        cp("dve", den, acc[64:128, :])
                        sk = sink_s.v(sink_s.ap[:, 4 * g:4 * g + 4].unsqueeze(2).to_broadcast([64, 4, 128]))
                        d3 = den.v(den.ap.rearrange("p (h q) -> p h q", q=128))
                        tt("dve", d3, d3, sk, ALU.add)
                        recip(rden, den)
                    else:
                        recip(rden, acc[64:128, :])
                    tt("dve", oT[:, g * 512:(g + 1) * 512], acc[0:64, :], rden, ALU.mult)
                if nxt is not None and i >= 1:
                    next(nxt, None)
            if nxt is not None:
                for _ in nxt:
                    pass
            for half in range(2):
                mm_group(ps[2 + half], [(oT.ap[:, hh * 128:(hh + 1) * 128], wo.ap[:, hh * D + half * 512: hh * D + (half + 1) * 512])
                                        for hh in range(NH)], r=[oT.key, wo.key])
                yt = ytmp.next()
                tt("dve", yt, ps[2 + half], modA[2][:, half * 512:(half + 1) * 512], ALU.mult)
                tt("pool", xt[:, half * 512:(half + 1) * 512], xt[:, half * 512:(half + 1) * 512], yt, ALU.add)
            dma("sp", xs[gt * 128:(gt + 1) * 128, :], xt, w=[("xs", gt)])

        for b in (range(NB) if not OPT["SKIP_ATT"] else []):
            interleave((kv_gen(b, t, t % 2) for t in range(NT_B)), OPT["W_KV"], 3)
            q_tiles = list(range(NT_L)) + ([] if last else list(range(NT_L, NT_B)))
            infos = [dict() for _ in q_tiles]
            g0 = q_prologue(b, q_tiles[0], 0, infos[0])
            for _ in g0:
                pass
            for qi, t in enumerate(q_tiles):
                nxt = None
                if qi + 1 < len(q_tiles) and OPT["Q_OVERLAP"]:
                    nxt = q_prologue(b, q_tiles[qi + 1], (qi + 1) % 2, infos[qi + 1])
                attend(b, t, qi % 2, infos[qi], nxt)
                if qi + 1 < len(q_tiles) and not OPT["Q_OVERLAP"]:
                    for _ in q_prologue(b, q_tiles[qi + 1], (qi + 1) % 2, infos[qi + 1]):
                        pass

        P.barrier()
        alloc_state["off"] = PERSIST
        wgs = [a16("wg%d" % i, 8 * FH) for i in range(2)]
        wus = [a16("wu%d" % i, 8 * FH) for i in range(2)]
        wds = [a16("wd%d" % i, 4 * D) for i in range(2)]
        xgs = [a16("xg%d" % i, (CAP // 128) * D) for i in range(2)]
        xeT = a16("xeT", 8 * CAP)
        HT = a16("HT", 4 * CAP)
        sgs = Ring([a32("sg%d" % i, CAP) for i in range(2)])
        Ysb = Ring([a32("Ysb%d" % i, D) for i in range(2)])
        xts = Ring([a32("mxt%d" % i, D) for i in range(2)])
        h = a32("mh", D)
        hbfs = Ring([a16("hbf%d" % i, D) for i in range(2)])
        hT32 = a32("hT32", 8 * 128)
        modF = [[a32("modF%d_%d" % (s, i), D) for i in range(2)] for s in range(2)]
        gtF = [a32("gtF%d" % s, D) for s in range(2)]
        y1s = Ring([a32("y1_%d" % i, D) for i in range(2)])
        y2s = Ring([a32("y2_%d" % i, D) for i in range(2)])
        gfin = a32("gfin", D) if last else None
        wrg = a32("wrg", 8 * 36)
        print("moe SBUF KB (before rl)", alloc_state["off"] / 256.0)
        rls = [a32("rl%d" % i, 512) for i in range(2)]
        hT32s = [hT32, a32("hT32b", 8 * 128)]
        mhs = [h, a32("mh2", D)]

        def mk_rl(si):
            rl = rls[si]

            def RL(name, off, n):
                return T(rl.ap[:, off:off + n], ("rl", si, name))
            d = dict(L=RL("L", 0, 36), mg=RL("mg", 40, 1), nmg=RL("nmg", 41, 1), eg=RL("eg", 44, 4), sg=RL("sg", 48, 1),
                     ptop=RL("ptop", 49, 1), ohg=RL("ohg", 52, 4), pen=RL("pen", 56, 4), Lm=RL("Lm", 64, 32),
                     mx8=RL("mx8", 96, 8), oh1=RL("oh1", 104, 32), oh2=RL("oh2", 136, 32), dd=RL("dd", 168, 1),
                     ed=RL("ed", 169, 1), rd=RL("rd", 170, 1), A=RL("A", 172, 32),
                     Abf=T(rl.ap[:, 204:220].bitcast(BF16), ("rl", si, "Abf")), slot=RL("slot", 224, 32),
                     tmpa=RL("tmpa", 256, 32), tmpb=RL("tmpb", 328, 32), d1f=RL("d1f", 288, 1), d2f=RL("d2f", 289, 1),
                     ov=RL("ov", 290, 1), nov=RL("nov", 291, 1), slotp=RL("slotp", 296, 32))
            return d
        RLS = [mk_rl(0), mk_rl(1)]

        m_tiles = []
        for b in range(NB):
            m_tiles += [(b * NT_B + t, b) for t in range(NT_L)]
        if not last:
            for b in range(NB):
                m_tiles += [(b * NT_B + NT_L + t, NB) for t in range(NT_C)]
        XGKEYS = [("XG", gt) for (gt, _) in m_tiles]

        dma("sp", wrg.v(wrg.ap.rearrange("p (k n) -> p k n", n=36)), w_rg[li].rearrange("(k p) n -> p k n", p=128))
        dma("sp", brg_bc, b_rg[li:li + 1, :].partition_broadcast(128))
        memset("dve", tot, 0.0)
        if last:
            dma("sp", gfin, g_final.partition_broadcast(128))

        m1_state = {"row": None, "mset": -1, "mf": None}

        def m1_gen(gt, row, si):
            R_ = RLS[si]
            pbase = 4 * si
            if row != m1_state["row"]:
                m1_state["mset"] += 1
                mf_ = modF[m1_state["mset"] % 2]
                dma("sp", mf_[0], modd[li, row:row + 1, 4 * D:5 * D].partition_broadcast(128), r=[("modd", li)])
                dma("sp", mf_[1], modd[li, row:row + 1, 3 * D:4 * D].partition_broadcast(128), r=[("modd", li)])
                m1_state["row"] = row
                m1_state["mf"] = mf_
            mf = m1_state["mf"]
            xt = xts.next()
            dma("sp", xt, xs[gt * 128:(gt + 1) * 128, :], r=[("xs", gt)])
            st = stat_ring.next()
            hh_ = mhs[si]
            norm_h(xt, hh_, mf[0], mf[1], st)
            yield
            hbf = hbfs.next()
            act(hbf, hh_, AF.Copy)
            hT32_ = hT32s[si]
            transpose_h(hh_, hT32_, False, (ps[pbase], ps[pbase + 1]))
            yield
            mm_group(ps[pbase + 2][:, 0:36], [(hT32_.ap[:, kc * 128:(kc + 1) * 128], wrg.ap[:, kc * 36:(kc + 1) * 36]) for kc in range(8)],
                     r=[hT32_.key, wrg.key])
            yield
            L, mg, nmg, eg, sg_, ptop, ohg, pen, Lm, mx8 = (R_[k] for k in ("L", "mg", "nmg", "eg", "sg", "ptop", "ohg", "pen", "Lm", "mx8"))
            oh1, oh2, dd, ed, rd, Asum, Abf, slot = (R_[k] for k in ("oh1", "oh2", "dd", "ed", "rd", "A", "Abf", "slot"))
            tt("dve", L, ps[pbase + 2][:, 0:36], brg_bc, ALU.add)
            red(mg, L[:, 0:4], ALU.max)
            yield
            ts("dve", nmg, mg, -1.0, ALU.mult)
            ts("dve", ohg, L[:, 0:4], mg, ALU.is_equal)
            yield
            act(eg, L[:, 0:4], AF.Exp, bias=nmg, accum=sg_)
            ts("dve", pen, ohg, -1.0, ALU.add, 1e30, ALU.mult)
            yield
            recip(ptop, sg_)
            tt("dve", Lm.v(Lm.ap.rearrange("p (g e) -> p g e", e=8)), L.v(L.ap[:, 4:36].rearrange("p (g e) -> p g e", e=8)),
               pen.v(pen.ap.unsqueeze(2).to_broadcast([128, 4, 8])), ALU.add)
            yield
            mxo, lmi = mx8.ap, Lm.ap
            P.add("dve", lambda e, mxo=mxo, lmi=lmi: e.max(out=mxo, in_=lmi), [Lm.key], [mx8.key])
            yield
            ts("dve", oh1, Lm, mx8[:, 0:1], ALU.is_equal)
            ts("dve", oh2, Lm, mx8[:, 1:2], ALU.is_equal)
            tt("dve", dd, mx8[:, 1:2], mx8[:, 0:1], ALU.subtract)
            yield
            act(ed, dd, AF.Exp)
            tt("dve", Asum, oh1, oh2, ALU.add)
            yield
            ts("dve", ed, ed, 1.0, ALU.add)
            cp("dve", Abf, Asum)
            yield
            recip(rd, ed)
            w1 = T(rt_w1.ap[:, gt:gt + 1], ("rt_w1", gt))
            w2 = T(rt_w2.ap[:, gt:gt + 1], ("rt_w2", gt))
            mm_group(ps[pbase + 3][:, 0:32], [(trib.ap, Abf.ap)], r=[trib.key, Abf.key])
            mm_group(ps[pbase + 3][:, 32:64], [(onesb.ap, Abf.ap)], r=[onesb.key, Abf.key])
            stt("dve", slot, ps[pbase + 3][:, 0:32], 0.0, tot, ALU.add, ALU.add)
            tt("dve", tot, tot, ps[pbase + 3][:, 32:64], ALU.add)
            yield
            tt("dve", w1, ptop, rd, ALU.mult)
            tt("dve", R_["slotp"], slot, ec_bc, ALU.add)
            yield
            tt("dve", w2, ptop, w1, ALU.subtract)
            tt("dve", R_["tmpa"], oh1, slot, ALU.mult)
            tt("dve", R_["tmpb"], oh2, slot, ALU.mult)
            yield
            ovs = []
            for ci, (oh, df, rtd, tmp) in enumerate(((oh1, R_["d1f"], rt_d1, R_["tmpa"]), (oh2, R_["d2f"], rt_d2, R_["tmpb"]))):
                ov = T(rls[si].ap[:, 400 + ci:401 + ci], ("rl", si, "ov%d" % ci))
                nov = T(rls[si].ap[:, 404 + ci:405 + ci], ("rl", si, "nov%d" % ci))
                red(ov, tmp, ALU.add)
                yield
                ts("dve", ov, ov, float(CAP), ALU.is_ge)
                tt("dve", tmp, oh, R_["slotp"], ALU.mult)
                yield
                ts("dve", nov, ov, -1.0, ALU.mult, 1.0, ALU.add)
                red(df, tmp, ALU.add)
                yield
                tt("dve", df, df, nov, ALU.mult)
                yield
                stt("dve", df, ov, float(NE * CAP), df, ALU.mult, ALU.add)
                yield
                rcol = T(rtd.ap[:, gt:gt + 1], (rtd.key, gt))
                cp("dve", rcol, df)
                yield
                ia = rcol.ap
                ha = hbf.ap
                P.add("pool", lambda e, ia=ia, ha=ha: e.indirect_dma_start(
                    out=XG, out_offset=bass.IndirectOffsetOnAxis(ap=ia, axis=0), in_=ha, in_offset=None),
                    [rcol.key, hbf.key], [("XG", gt)], dma=True)

        if not OPT["SKIP_M1"]:
            interleave((m1_gen(gt, row, i % 2) for i, (gt, row) in enumerate(m_tiles)), OPT["W_M1"], 9)

        for ex in (range(NE) if not OPT["SKIP_M2"] else []):
            wg, wu, wd, xg = wgs[ex % 2], wus[ex % 2], wds[ex % 2], xgs[ex % 2]
            dma("pool", wg.v(wg.ap.rearrange("p (k f) -> p k f", f=FH)), w_gate[li, ex].rearrange("(k p) f -> p k f", p=128))
            dma("pool", wu.v(wu.ap.rearrange("p (k f) -> p k f", f=FH)), w_up[li, ex].rearrange("(k p) f -> p k f", p=128))
            dma("pool", wd.v(wd.ap.rearrange("p (k f) -> p k f", f=D)), w_down[li, ex].rearrange("(k p) f -> p k f", p=128))
            dma("sp", xg.v(xg.ap.rearrange("p (s d) -> p s d", d=D)),
                XG[ex * CAP:(ex + 1) * CAP, :].rearrange("(s p) d -> p s d", p=128), r=XGKEYS)
            NS = CAP // 128
            for s in range(NS):
                pb = psbf(s % 2)
                tr_group([(pb.ap[:, kc * 128:(kc + 1) * 128], xg.ap[:, s * D + kc * 128: s * D + (kc + 1) * 128], identb.ap)
                          for kc in range(8)], r=[xg.key, identb.key], w=[pb.key])
                dst = xeT.v(xeT.ap.rearrange("p (k c) -> p k c", c=CAP)[:, :, s * 128:(s + 1) * 128])
                src = pb.v(pb.ap.rearrange("p (k c) -> p k c", c=128))
                if s % 2 == 0:
                    cp("dve", dst, src)
                else:
                    act(dst, src, AF.Copy)
            for m in range(4):
                mm_group(ps[2 + (m % 2)], [(wg.ap[:, kc * FH + m * 128: kc * FH + (m + 1) * 128], xeT.ap[:, kc * CAP:(kc + 1) * CAP])
                                           for kc in range(8)], r=[wg.key, xeT.key])
                mm_group(ps[4 + (m % 2)], [(wu.ap[:, kc * FH + m * 128: kc * FH + (m + 1) * 128], xeT.ap[:, kc * CAP:(kc + 1) * CAP])
                                           for kc in range(8)], r=[wu.key, xeT.key])
                sgt = sgs.next()
                act(sgt, ps[2 + (m % 2)], AF.Silu)
                tt("dve", HT[:, m * CAP:(m + 1) * CAP], sgt, ps[4 + (m % 2)], ALU.mult)
            for s in range(NS):
                ysb = Ysb.next()
                for half in range(2):
                    pp = ps[6 + half]
                    mm_group(pp, [(HT.ap[:, m * CAP + s * 128: m * CAP + (s + 1) * 128], wd.ap[:, m * D + half * 512: m * D + (half + 1) * 512])
                                  for m in range(4)], r=[HT.key, wd.key])
                    if half == 0:
                        act(ysb[:, 0:512], pp, AF.Copy)
                    else:
                        cp("dve", ysb[:, 512:1024], pp)
                dma("sp", YG[ex * CAP + s * 128: ex * CAP + (s + 1) * 128, :], ysb, w=[("YG", ex, s)])

        YGKEYS = ["YG"] + [("YG", ex, s_) for ex in range(NE) for s_ in range(CAP // 128)]
        m3_state = {"row": None, "mset": -1, "gtf": None}

        def m3_gen(gt, row):
            if row != m3_state["row"]:
                m3_state["mset"] += 1
                gtf_ = gtF[m3_state["mset"] % 2]
                dma("sp", gtf_, modd[li, row:row + 1, 5 * D:6 * D].partition_broadcast(128), r=[("modd", li)])
                m3_state["row"] = row
                m3_state["gtf"] = gtf_
            gtf = m3_state["gtf"]
            y1, y2 = y1s.next(), y2s.next()
            for (yy, rtd) in ((y1, rt_d1), (y2, rt_d2)):
                ia = rtd.ap[:, gt:gt + 1]
                ya = yy.ap
                P.add("pool", lambda e, ia=ia, ya=ya: e.indirect_dma_start(
                    out=ya, out_offset=None, in_=YG, in_offset=bass.IndirectOffsetOnAxis(ap=ia, axis=0)),
                    [(rtd.key, gt)] + YGKEYS, [yy.key], dma=True)
            xt = xts.next()
            dma("sp", xt, xs[gt * 128:(gt + 1) * 128, :], r=[("xs", gt)])
            yield
            w1 = T(rt_w1.ap[:, gt:gt + 1], ("rt_w1", gt))
            w2 = T(rt_w2.ap[:, gt:gt + 1], ("rt_w2", gt))
            ts("dve", y1, y1, w1, ALU.mult)
            yield
            stt("dve", y1, y2, w2, y1, ALU.mult, ALU.add)
            yield
            tt("pool", y1, y1, gtf, ALU.mult)
            yield
            tt("pool", xt, xt, y1, ALU.add)
            yield
            if not last:
                dma("sp", xs[gt * 128:(gt + 1) * 128, :], xt, w=[("xs", gt)])
            else:
                st = stat_ring.next()
                stt("dve", y2, xt, 1.0 / D, xt, ALU.mult, ALU.mult, accum=st[:, 0:1])
                yield
                act(st[:, 1:2], st[:, 0:1], AF.Ln, bias=EPS)
                yield
                act(st[:, 2:3], st[:, 1:2], AF.Exp, scale=-0.5)
                yield
                stt("dve", y2, xt, st[:, 2:3], gfin, ALU.mult, ALU.mult)
                b_ = gt // NT_B
                t_ = gt % NT_B
                r0 = b_ * SEQ + t_ * 128
                dma("sp", out[r0:r0 + 128, :], y2)

        if not OPT["SKIP_M3"]:
            interleave((m3_gen(gt, row) for (gt, row) in m_tiles), OPT["W_M3"], 3)

    P.finalize()
    P.emit()
    return nc


_CACHE = {}


def _consts():
    if "c" not in _CACHE:
        cos, sin_s = _rope_tables()
        kk = np.arange(128)[:, None]
        qq = np.arange(128)[None, :]
        mlo = np.tile((kk >= qq).astype(np.float32), (1, 4))
        mhi = np.tile((kk <= qq).astype(np.float32), (1, 4))
        tri = (np.arange(128)[:, None] < np.arange(128)[None, :]).astype(np.float32)
        ec = (np.arange(NE, dtype=np.float32) * CAP).reshape(1, NE)
        _CACHE["c"] = dict(c_ident=np.eye(128, dtype=np.float32), c_cos=cos, c_sin=sin_s,
                           c_masks=np.stack([mlo, mhi]).astype(np.float32), c_tri=tri, c_ec=ec)
        _CACHE["naidx"] = _na_index_table()
    return _CACHE["c"], _CACHE["naidx"]


def make_in_maps(inputs, n_cores, NB):
    f = lambda a: np.ascontiguousarray(np.asarray(a, dtype=np.float32))
    consts, naidx = _consts()
    rpb = f(inputs["rpb_c"])[0].reshape(-1)
    rpb_ext = np.concatenate([rpb, np.array([NEG], dtype=np.float32)])
    natab = np.ascontiguousarray(rpb_ext[naidx].reshape(5, 5, 128, NH * 128))
    w_rg = np.ascontiguousarray(np.concatenate([f(inputs["w_group"]), f(inputs["w_router"])], axis=-1))
    b_rg = np.ascontiguousarray(np.concatenate([f(inputs["b_group"]), f(inputs["b_router"])], axis=-1))
    shared = dict(
        w_ada=f(inputs["w_ada"]), b_ada=f(inputs["b_ada"]), g_attn=f(inputs["g_attn"]), w_qkv=f(inputs["w_qkv"]),
        w_o=f(inputs["w_o"]), sink_a=f(inputs["sink_a"]), gq_b=f(inputs["gq_b"]), gk_b=f(inputs["gk_b"]),
        g_ffn=f(inputs["g_ffn"]), w_rg=w_rg, b_rg=b_rg, w_gate=f(inputs["w_gate"]), w_up=f(inputs["w_up"]),
        w_down=f(inputs["w_down"]), g_final=f(inputs["g_final"]).reshape(1, D), c_natab=natab, **consts)
    x = f(inputs["x"])
    ctx = f(inputs["ctx"])
    c = f(inputs["c"])
    c_ctx = f(inputs["c_ctx"]).reshape(1, D)
    maps = []
    for i in range(n_cores):
        sl = slice(i * NB, (i + 1) * NB)
        m = dict(shared)
        m["x"] = np.ascontiguousarray(x[sl].reshape(NB * SEQ, D))
        m["ctx"] = np.ascontiguousarray(ctx[sl].reshape(NB * CTX, D))
        m["cvec"] = np.ascontiguousarray(np.concatenate([c[sl], c_ctx], axis=0))
        maps.append(m)
    return maps


def kernel(**inputs):
    n_cores = 8
    NB = 2
    if "nc" not in _CACHE:
        _CACHE["nc"] = build(NB=NB)
    nc = _CACHE["nc"]
    maps = make_in_maps(inputs, n_cores, NB)
    res = run_bass_kernel_spmd(nc, maps, core_ids=list(range(n_cores)))
    outs = [r["out"].reshape(NB, SEQ, D) for r in res.results]
    return np.concatenate(outs, axis=0).astype(np.float32)
```

```python
import numpy as np
import concourse.bass as bass
import concourse.mybir as mybir
from concourse.bass_utils import run_bass_kernel_spmd

F32 = mybir.dt.float32
BF16 = mybir.dt.bfloat16
I32 = mybir.dt.int32
ALU = mybir.AluOpType
AF = mybir.ActivationFunctionType
AX = mybir.AxisListType

D = 1024
SEQ = 2048
CTX = 256
DEPTH = 4
NH = 16
NKV = 4
HD = 64
NE = 32
FH = 512
GRID_W = 64
import os as _os
OPT = dict(W_KV=int(_os.environ.get("W_KV", 2)), W_M1=int(_os.environ.get("W_M1", 2)), W_M3=int(_os.environ.get("W_M3", 2)),
           Q_OVERLAP=int(_os.environ.get("Q_OVERLAP", 1)), S_AHEAD=int(_os.environ.get("S_AHEAD", 1)),
           SINK_BC=int(_os.environ.get("SINK_BC", 1)), SKIP_ATT=int(_os.environ.get("SKIP_ATT", 0)),
           SKIP_M1=int(_os.environ.get("SKIP_M1", 0)), SKIP_M2=int(_os.environ.get("SKIP_M2", 0)), SKIP_M3=int(_os.environ.get("SKIP_M3", 0)))
CAP = 512
NT_L = SEQ // 128
NT_C = CTX // 128
NT_B = NT_L + NT_C
EPS = 1e-6
NEG = -1e30


class _Op:
    __slots__ = ("idx", "eng", "fn", "deps", "dma", "sig", "waits", "signal", "ksnap")

    def __init__(self, idx, eng, fn, dma):
        self.idx = idx
        self.eng = eng
        self.fn = fn
        self.deps = set()
        self.dma = dma
        self.sig = None
        self.waits = []
        self.signal = False
        self.ksnap = None


class Prog:
    ENGS = ("pe", "act", "dve", "pool", "sp")
    EPOCH = 20000
    NDMA = {"sp": 24, "pool": 24, "act": 8}

    def __init__(self, nc):
        self.nc = nc
        self.ops = []
        self.last_w = {}
        self.readers = {}
        self.pending = {}
        self.last_eng = {}
        self.dma_since = []

    def add(self, eng, fn, r=(), w=(), dma=False):
        op = _Op(len(self.ops), eng, fn, dma)
        ops = self.ops

        def same_eng(j):
            o = ops[j]
            return (not dma) and (not o.dma) and o.eng == eng

        for k in r:
            lw = self.last_w.get(k)
            if lw is not None and not (eng == "pe" and same_eng(lw)):
                op.deps.add(lw)
            if isinstance(k, str) and k.startswith("ps"):
                for rd in self.readers.get(k, ()):
                    if not same_eng(rd):
                        op.deps.add(rd)
        for k in w:
            lw = self.last_w.get(k)
            if lw is not None and not (eng == "pe" and same_eng(lw)):
                op.deps.add(lw)
            rs = self.readers.get(k)
            if rs:
                for rd in rs:
                    if not same_eng(rd):
                        op.deps.add(rd)
        for k in r:
            self.readers.setdefault(k, []).append(op.idx)
        for k in w:
            self.last_w[k] = op.idx
            self.readers[k] = []
        if eng in self.pending:
            op.deps.update(self.pending.pop(eng))
        op.deps.discard(op.idx)
        self.ops.append(op)
        if dma:
            self.dma_since.append(op.idx)
        else:
            self.last_eng[eng] = op.idx
        return op

    def barrier(self):
        deps = set(self.last_eng.values()) | set(self.dma_since)
        for e in self.ENGS:
            self.pending[e] = set(deps) | self.pending.get(e, set())
        self.dma_since = []

    def finalize(self, final_engine="sp"):
        ops = self.ops
        dma_rr = {q: 0 for q in self.NDMA}
        dma_last = {}
        dma_cnt = {}
        for op in ops:
            if op.dma:
                q = op.eng
                s = dma_rr[q] % self.NDMA[q]
                dma_rr[q] += 1
                key = ("dma", q, s)
                prev = dma_last.get(key)
                if prev is not None:
                    op.deps.add(prev)
                dma_last[key] = op.idx
                dma_cnt[key] = dma_cnt.get(key, 0) + 16
                op.sig = (key, dma_cnt[key])
        has_dep = [False] * len(ops)
        for op in ops:
            for d in op.deps:
                has_dep[d] = True
        cnt = {e: 0 for e in self.ENGS}
        know = {e: {} for e in self.ENGS}
        for op in ops:
            e = op.eng
            K = know[e]
            for d in sorted(op.deps):
                sk, sv = ops[d].sig
                if K.get(sk, 0) >= sv:
                    continue
                op.waits.append((sk, sv))
                K[sk] = sv
                for k2, v2 in ops[d].ksnap.items():
                    if K.get(k2, 0) < v2:
                        K[k2] = v2
            if op.dma:
                op.signal = True
            elif has_dep[op.idx]:
                cnt[e] += 1
                ep = cnt[e] // self.EPOCH
                op.sig = (("eng", e, ep), cnt[e] - ep * self.EPOCH + (1 if ep else 0))
                op.signal = True
            op.ksnap = dict(K)
        self.final_waits = []
        Kf = know[final_engine]
        for key, v in dma_cnt.items():
            if Kf.get(key, 0) < v:
                self.final_waits.append((key, v))
        self.semkeys = set(op.sig[0] for op in ops if op.signal)
        self.final_engine = final_engine

    def emit(self):
        nc = self.nc
        sems = {}
        for i, k in enumerate(sorted(self.semkeys, key=str)):
            sems[k] = nc.alloc_semaphore("s%d" % i)
        by_eng = {e: [] for e in self.ENGS}
        for op in self.ops:
            by_eng[op.eng].append(op)
        fin_eng = self.final_engine
        fin_waits = self.final_waits

        def run(eng_name, eng):
            for op in by_eng[eng_name]:
                for sk, sv in op.waits:
                    eng.wait_ge(sems[sk], sv)
                ins = op.fn(eng)
                if op.signal:
                    ins.then_inc(sems[op.sig[0]], 16 if op.dma else 1)
            if eng_name == fin_eng:
                for sk, sv in fin_waits:
                    eng.wait_ge(sems[sk], sv)

        with nc.Block() as block:
            @block.tensor
            def _(e):
                run("pe", e)

            @block.scalar
            def _(e):
                run("act", e)

            @block.vector
            def _(e):
                run("dve", e)

            @block.gpsimd
            def _(e):
                run("pool", e)

            @block.sync
            def _(e):
                run("sp", e)


class T:
    __slots__ = ("ap", "key")

    def __init__(self, ap, key):
        self.ap = ap
        self.key = key

    def __getitem__(self, idx):
        return T(self.ap[idx], self.key)

    def v(self, ap):
        return T(ap, self.key)


class Ring:
    def __init__(self, items):
        self.items = items
        self.i = 0

    def next(self):
        it = self.items[self.i % len(self.items)]
        self.i += 1
        return it


def _rope_tables():
    t = np.arange(SEQ)
    row = (t // GRID_W).astype(np.float32)
    col = (t % GRID_W).astype(np.float32)
    quarter = HD // 4
    inv = (np.float32(10000.0) ** (-np.arange(quarter, dtype=np.float32) / np.float32(quarter))).astype(np.float32)
    ar = row[:, None] * inv
    ac = col[:, None] * inv
    ang = np.concatenate([ar, ar, ac, ac], axis=-1).astype(np.float32)
    cos = np.cos(ang).astype(np.float32)
    sin = np.sin(ang).astype(np.float32)
    sgn = np.concatenate([-np.ones(16), np.ones(16), -np.ones(16), np.ones(16)]).astype(np.float32)
    return cos, (sin * sgn[None, :]).astype(np.float32)


NA_PAT_TILES = [0, 1, 2, 14, 15]


def _na_pattern(t):
    return {0: 0, 1: 1, 14: 3, 15: 4}.get(t, 2)


def _na_keytiles(t):
    if t <= 1:
        return [0, 1, 2, 3]
    if t >= 14:
        return [12, 13, 14, 15]
    return [t - 2, t - 1, t, t + 1, t + 2]


def _na_index_table():
    rows = SEQ // GRID_W
    wh, ww = 8, 16
    nrel = 15 * 31
    masked = NH * nrel
    tab = np.full((5, 5, 128, NH, 128), masked, dtype=np.int64)
    for pi, t in enumerate(NA_PAT_TILES):
        kts = _na_keytiles(t)
        q = t * 128 + np.arange(128)
        r = q // GRID_W
        c = q % GRID_W
        rs = np.clip(r - wh // 2, 0, rows - wh)
        cs = np.clip(c - ww // 2, 0, GRID_W - ww)
        for j, kt in enumerate(kts):
            k = kt * 128 + np.arange(128)
            kr = k // GRID_W
            kc = k % GRID_W
            inwin = ((kr[:, None] >= rs[None, :]) & (kr[:, None] < rs[None, :] + wh)
                     & (kc[:, None] >= cs[None, :]) & (kc[:, None] < cs[None, :] + ww))
            rel = (kr[:, None] - r[None, :] + 7) * 31 + (kc[:, None] - c[None, :] + 15)
            for h in range(NH):
                tab[pi, j, :, h, :] = np.where(inwin, h * nrel + rel, masked)
    return tab


def build(NB=2, n_layers=DEPTH):
    nc = bass.Bass("TRN2", target_bir_lowering=False)
    R = NB + 1
    NTOK = NB * NT_B * 128

    def din(name, shape, dt=F32):
        return nc.dram_tensor(name, list(shape), dt, kind="ExternalInput").ap()

    x_in = din("x", [NB * SEQ, D])
    ctx_in = din("ctx", [NB * CTX, D])
    cvec = din("cvec", [R, D])
    w_ada = din("w_ada", [DEPTH, D, 6 * D])
    b_ada = din("b_ada", [DEPTH, 6 * D])
    g_attn = din("g_attn", [DEPTH, D])
    w_qkv = din("w_qkv", [DEPTH, D, 1536])
    w_o = din("w_o", [DEPTH, D, D])
    sink_a = din("sink_a", [2, NH])
    gq_b = din("gq_b", [1, HD])
    gk_b = din("gk_b", [1, HD])
    g_ffn = din("g_ffn", [DEPTH, D])
    w_rg = din("w_rg", [DEPTH, D, 36])
    b_rg = din("b_rg", [DEPTH, 36])
    w_gate = din("w_gate", [DEPTH, NE, D, FH])
    w_up = din("w_up", [DEPTH, NE, D, FH])
    w_down = din("w_down", [DEPTH, NE, FH, D])
    g_final = din("g_final", [1, D])
    c_ident = din("c_ident", [128, 128])
    c_cos = din("c_cos", [SEQ, HD])
    c_sin = din("c_sin", [SEQ, HD])
    c_masks = din("c_masks", [2, 128, 512])
    c_tri = din("c_tri", [128, 128])
    c_ec = din("c_ec", [1, NE])
    c_natab = din("c_natab", [5, 5, 128, NH * 128])
    out = nc.dram_tensor("out", [NB * SEQ, D], F32, kind="ExternalOutput").ap()

    xs = nc.dram_tensor("xs", [NTOK, D], F32).ap()
    modd = nc.dram_tensor("modd", [DEPTH, R, 6 * D], F32).ap()
    XG = nc.dram_tensor("XG", [NE * CAP + 1, D], BF16).ap()
    YG = nc.dram_tensor("YG", [NE * CAP + 1, D], F32).ap()

    P = Prog(nc)

    POOL_KB = 190
    pool = nc.alloc_sbuf_tensor("pool", [128, POOL_KB * 256], F32)
    alloc_state = {"off": 0}

    def a32(name, n, parts=128):
        off = alloc_state["off"]
        alloc_state["off"] = off + n
        assert alloc_state["off"] <= POOL_KB * 256, (name, alloc_state["off"])
        return T(pool[0:parts, off:off + n], name)

    def a16(name, n, parts=128):
        n32 = (n + 1) // 2
        off = alloc_state["off"]
        alloc_state["off"] = off + n32
        assert alloc_state["off"] <= POOL_KB * 256, (name, alloc_state["off"])
        return T(pool[0:parts, off:off + n32].bitcast(BF16), name)

    ps = [T(nc.alloc_psum_tensor("ps%d" % i, [128, 512], F32)[:, :], "ps%d" % i) for i in range(8)]

    def psbf(i):
        return T(ps[i].ap.bitcast(BF16), ps[i].key)

    ident = a32("ident", 128)
    identb = a16("identb", 128)
    onesb = a16("onesb", 128)
    trib = a16("trib", 128)
    cos_t = a32("cos", NT_L * HD)
    sin_t = a32("sin", NT_L * HD)
    maskLo = a16("maskLo", 512)
    maskHi = a16("maskHi", 512)
    gq_bc = a32("gq_bc", HD)
    gk_bc = a32("gk_bc", HD)
    ec_bc = a32("ec_bc", NE)
    brg_bc = a32("brg_bc", 36)
    tot = a32("tot", NE)
    NTT = NB * NT_B
    rt_w1 = a32("rt_w1", NTT)
    rt_w2 = a32("rt_w2", NTT)
    rt_d1 = T(nc.alloc_sbuf_tensor("rt_d1", [128, NTT], I32)[:, :], "rt_d1")
    rt_d2 = T(nc.alloc_sbuf_tensor("rt_d2", [128, NTT], I32)[:, :], "rt_d2")
    small = a32("small", 64)
    PERSIST = alloc_state["off"]

    def dma(q, out_, in_, r=(), w=()):
        oa = out_.ap if isinstance(out_, T) else out_
        ia = in_.ap if isinstance(in_, T) else in_
        rr = list(r) + ([in_.key] if isinstance(in_, T) else [])
        ww = list(w) + ([out_.key] if isinstance(out_, T) else [])
        P.add(q, lambda e: e.dma_start(out=oa, in_=ia), rr, ww, dma=True)

    def act(out_, in_, func, scale=1.0, bias=0.0, accum=None, r=(), w=()):
        kw = {}
        rr = [in_.key] + list(r)
        ww = [out_.key] + list(w)
        if isinstance(bias, T):
            rr.append(bias.key)
            bias = bias.ap
        if isinstance(scale, T):
            rr.append(scale.key)
            scale = scale.ap
        if accum is not None:
            kw["accum_out"] = accum.ap
            ww.append(accum.key)
        oa, ia = out_.ap, in_.ap
        P.add("act", lambda e: e.activation(out=oa, in_=ia, func=func, bias=bias, scale=scale, **kw), rr, ww)

    def tt(eng, out_, a, b, op):
        oa, aa, ba = out_.ap, a.ap, b.ap
        P.add(eng, lambda e: e.tensor_tensor(out=oa, in0=aa, in1=ba, op=op), [a.key, b.key], [out_.key])

    def ts(eng, out_, a, s1, op0, s2=None, op1=None, accum=None):
        rr = [a.key]
        ww = [out_.key]
        if isinstance(s1, T):
            rr.append(s1.key)
            s1 = s1.ap
        if isinstance(s2, T):
            rr.append(s2.key)
            s2 = s2.ap
        kw = {}
        if op1 is not None:
            kw["op1"] = op1
        if accum is not None:
            kw["accum_out"] = accum.ap
            ww.append(accum.key)
        oa, aa = out_.ap, a.ap
        P.add(eng, lambda e: e.tensor_scalar(out=oa, in0=aa, scalar1=s1, scalar2=s2, op0=op0, **kw), rr, ww)

    def stt(eng, out_, a, s, b, op0, op1, accum=None):
        rr = [a.key, b.key]
        ww = [out_.key]
        if isinstance(s, T):
            rr.append(s.key)
            s = s.ap
        kw = {}
        if accum is not None:
            kw["accum_out"] = accum.ap
            ww.append(accum.key)
        oa, aa, ba = out_.ap, a.ap, b.ap
        P.add(eng, lambda e: e.scalar_tensor_tensor(out=oa, in0=aa, scalar=s, in1=ba, op0=op0, op1=op1, **kw), rr, ww)

    def cp(eng, out_, in_):
        oa, ia = out_.ap, in_.ap
        P.add(eng, lambda e: e.tensor_copy(out=oa, in_=ia), [in_.key], [out_.key])

    def recip(out_, in_):
        oa, ia = out_.ap, in_.ap
        P.add("dve", lambda e: e.reciprocal(out=oa, in_=ia), [in_.key], [out_.key])

    def red(out_, in_, op, axis=AX.X):
        oa, ia = out_.ap, in_.ap
        P.add("dve", lambda e: e.tensor_reduce(out=oa, in_=ia, axis=axis, op=op), [in_.key], [out_.key])

    def memset(eng, out_, val):
        oa = out_.ap
        P.add(eng, lambda e: e.memset(oa, val), [], [out_.key])

    def mm_group(out_, pairs, r=()):
        oa = out_.ap
        n = len(pairs)

        def fn(e):
            ins = None
            for i, (l, rh) in enumerate(pairs):
                ins = e.matmul(oa, lhsT=l, rhs=rh, start=(i == 0), stop=(i == n - 1))
            return ins
        P.add("pe", fn, list(r), [out_.key])

    def tr_group(items, r=(), w=()):
        def fn(e):
            ins = None
            for (o, i, idn) in items:
                ins = e.transpose(out=o, in_=i, identity=idn)
            return ins
        P.add("pe", fn, list(r), list(w))

    dma("sp", ident, c_ident)
    dma("pool", identb, c_ident)
    dma("pool", trib, c_tri)
    dma("sp", cos_t.v(cos_t.ap.rearrange("p (t d) -> p t d", d=HD)), c_cos.rearrange("(t p) d -> p t d", p=128))
    dma("sp", sin_t.v(sin_t.ap.rearrange("p (t d) -> p t d", d=HD)), c_sin.rearrange("(t p) d -> p t d", p=128))
    dma("pool", maskLo, c_masks[0])
    dma("pool", maskHi, c_masks[1])
    dma("sp", gq_bc, gq_b.partition_broadcast(128))
    dma("sp", gk_bc, gk_b.partition_broadcast(128))
    dma("sp", ec_bc, c_ec.partition_broadcast(128))
    memset("dve", onesb, 1.0)

    for b in range(NB):
        base = b * NT_B * 128
        dma("sp", xs[base:base + SEQ, :], x_in[b * SEQ:(b + 1) * SEQ, :], w=[("xs", b * NT_B + t) for t in range(NT_L)])
        dma("sp", xs[base + SEQ:base + SEQ + CTX, :], ctx_in[b * CTX:(b + 1) * CTX, :],
            w=[("xs", b * NT_B + NT_L + t) for t in range(NT_C)])

    alloc_state["off"] = PERSIST
    zrow = a32("zrow", D, parts=1)
    memset("dve", zrow, 0.0)
    dma("sp", YG[NE * CAP:NE * CAP + 1, :], zrow, w=["YG"])
    cv = a32("cv", D, parts=R)
    scT = a32("scT", 8 * R)
    wada = [a32("wada%d" % i, 8 * 512) for i in range(2)]
    modsb = a32("modsb", 6 * D, parts=R)
    bada = a32("bada", 6 * D, parts=R)
    gA = a32("gA", D, parts=R)
    gF = a32("gF", D, parts=R)
    dma("sp", cv, cvec)
    act(cv, cv, AF.Silu)
    tr_group([(ps[0].ap[:, kc * R:(kc + 1) * R], cv.ap[:, kc * 128:(kc + 1) * 128], ident.ap[0:R, 0:R]) for kc in range(8)],
             r=[cv.key, ident.key], w=[ps[0].key])
    cp("dve", scT, ps[0][:, 0:8 * R])
    for li in range(n_layers):
        dma("sp", bada, b_ada[li:li + 1, :].partition_broadcast(R))
        dma("sp", gA, g_attn[li:li + 1, :].partition_broadcast(R))
        dma("sp", gF, g_ffn[li:li + 1, :].partition_broadcast(R))
        for n in range(12):
            wt = wada[n % 2]
            dma("sp" if n % 2 == 0 else "act", wt.v(wt.ap.rearrange("p (k f) -> p k f", f=512)),
                w_ada[li, :, n * 512:(n + 1) * 512].rearrange("(k p) f -> p k f", p=128))
            pp = ps[1 + (n % 2)]
            mm_group(pp[0:R, :], [(scT.ap[:, kc * R:(kc + 1) * R], wt.ap[:, kc * 512:(kc + 1) * 512]) for kc in range(8)],
                     r=[scT.key, wt.key])
            tt("dve", modsb[:, n * 512:(n + 1) * 512], pp[0:R, :], bada[:, n * 512:(n + 1) * 512], ALU.add)
        stt("dve", modsb[:, D:2 * D], modsb[:, D:2 * D], 1.0, gA, ALU.add, ALU.mult)
        stt("dve", modsb[:, 4 * D:5 * D], modsb[:, 4 * D:5 * D], 1.0, gF, ALU.add, ALU.mult)
        dma("sp", modd[li], modsb, w=[("modd", li)])

    def norm_h(xt, h, gs_bc, sh_bc, st):
        stt("dve", h, xt, 1.0 / D, xt, ALU.mult, ALU.mult, accum=st[:, 0:1])
        act(st[:, 1:2], st[:, 0:1], AF.Ln, bias=EPS)
        act(st[:, 2:3], st[:, 1:2], AF.Exp, scale=-0.5)
        stt("dve", h, xt, st[:, 2:3], gs_bc, ALU.mult, ALU.mult)
        tt("pool", h, h, sh_bc, ALU.add)

    def transpose_h(h, dst, dst_is_bf16, pbanks):
        for half in range(2):
            pp = pbanks[half]
            tr_group([(pp.ap[:, j * 128:(j + 1) * 128], h.ap[:, (half * 4 + j) * 128:(half * 4 + j + 1) * 128], ident.ap)
                      for j in range(4)], r=[h.key, ident.key], w=[pp.key])
            d = dst[:, half * 512:(half + 1) * 512]
            if half == 0:
                act(d, pp, AF.Copy)
            else:
                cp("dve", d, pp)

    stat_ring = Ring([T(small.ap[:, i * 4:(i + 1) * 4], ("stat", i)) for i in range(4)])

    for li in range(n_layers):
        mtype = li % 3
        jidx = li // 3
        last = li == n_layers - 1
        rope = mtype in (0, 1)

        P.barrier()
        alloc_state["off"] = PERSIST
        wqkv = a16("wqkv", 8 * 1536)
        wo = a16("wo", NH * D, parts=64)
        kT = a16("kT", NKV * NT_B * 128, parts=64)
        Vs = a16("Vs", NT_B * 512)
        modA = [a32("modA%d" % i, D) for i in range(3)]
        xts = Ring([a32("xt%d" % i, D) for i in range(3)])
        hs = Ring([a32("h%d" % i, D) for i in range(2)])
        hTs = Ring([a16("hT%d" % i, 8 * 128) for i in range(2)])
        q32 = a32("q32", D)
        qro = a32("qro", D) if rope else None
        qtmp = a32("qtmp", 512)
        qbf = [a16("qbf%d" % i, D) for i in range(2 if rope else 1)]
        qT = [[a16("qT%d_%d" % (sl, i), NH * 128, parts=64) for i in range(2 if rope else 1)] for sl in range(2)]
        k32s = [q32[:, i * 256:(i + 1) * 256] for i in range(2)]
        kros = [a32("kro_%d" % i, 256) if rope else None for i in range(2)]
        ktms = [qtmp[:, i * 256:(i + 1) * 256] for i in range(2)]
        kbs = [a16("kb%d" % i, 256) for i in range(2)]
        hsts = [a32("hst%d" % i, 64) for i in range(2)]
        pTs = Ring([a16("pT%d" % i, 512) for i in range(3)])
        if mtype == 2:
            sbS = Ring([a32("sbS%d" % i, 512) for i in range(2)])
            nat = Ring([a32("nat%d" % i, 512) for i in range(2)])
        den = a32("den", 512, parts=64)
        rden = a32("rden", 512, parts=64)
        oT = a16("oT", NH * 128, parts=64)
        ytmp = Ring([a32("ytmp%d" % i, 512) for i in range(1)])
        sink_s = a32("sink_s", NH, parts=64)
        hst = a32("hst", 64)
        print("attn SBUF KB", alloc_state["off"] / 256.0)

        dma("pool", wqkv.v(wqkv.ap.rearrange("p (k n) -> p k n", n=1536)),
            w_qkv[li].rearrange("(k p) n -> p k n", p=128))
        dma("pool", wo.v(wo.ap.rearrange("p (h n) -> p h n", n=D)), w_o[li].rearrange("(h p) n -> p h n", p=64))
        use_sink = mtype == 0
        memset("dve", Vs, 1.0)
        if use_sink:
            dma("sp", sink_s, sink_a[jidx:jidx + 1, :].partition_broadcast(64))
            act(sink_s, sink_s, AF.Exp)

        def load_modA(row):
            for i, c in enumerate((1, 0, 2)):
                dma("sp", modA[i], modd[li, row:row + 1, c * D:(c + 1) * D].partition_broadcast(128), r=[("modd", li)])

        def head_norm(src, nheads, g_bc, dst, scratch, hst):
            W = nheads * HD
            sq = scratch[:, 0:W]
            act(sq, src, AF.Square)
            ssum = hst[:, 0:nheads]
            red(ssum, sq.v(sq.ap.rearrange("p (h d) -> p h d", d=HD)), ALU.add)
            act(hst[:, 16:16 + nheads], ssum, AF.Ln, scale=1.0 / HD, bias=EPS)
            act(hst[:, 32:32 + nheads], hst[:, 16:16 + nheads], AF.Exp, scale=-0.5)
            rs_b = hst.v(hst.ap[:, 32:32 + nheads].unsqueeze(2).to_broadcast([128, nheads, HD]))
            d3 = dst.v(dst.ap.rearrange("p (h d) -> p h d", d=HD))
            s3 = src.v(src.ap.rearrange("p (h d) -> p h d", d=HD))
            tt("dve", d3, s3, rs_b, ALU.mult)
            g_b = g_bc.v(g_bc.ap.unsqueeze(1).to_broadcast([128, nheads, HD]))
            tt("pool", d3, d3, g_b, ALU.mult)

        def apply_rope(src, nheads, t, dst, qtmp):
            W = nheads * HD
            cs = cos_t.v(cos_t.ap[:, t * HD:(t + 1) * HD].unsqueeze(1).to_broadcast([128, nheads, HD]))
            s3 = src.v(src.ap.rearrange("p (h d) -> p h d", d=HD))
            d3 = dst.v(dst.ap.rearrange("p (h d) -> p h d", d=HD))
            t3 = qtmp.v(qtmp.ap[:, 0:W].rearrange("p (h d) -> p h d", d=HD))
            tt("dve", d3, s3, cs, ALU.mult)
            s5 = src.ap.rearrange("p (h a b d) -> p h a b d", a=2, b=2, d=16)
            t5 = qtmp.ap[:, 0:W].rearrange("p (h a b d) -> p h a b d", a=2, b=2, d=16)
            sn5 = sin_t.ap[:, t * HD:(t + 1) * HD].rearrange("p (a b d) -> p a b d", a=2, b=2, d=16)
            for bsel in range(2):
                o_ = T(t5[:, :, :, bsel, :], qtmp.key)
                i_ = T(s5[:, :, :, 1 - bsel, :], src.key)
                sn = T(sn5[:, :, bsel, :].unsqueeze(1).to_broadcast([128, nheads, 2, 16]), sin_t.key)
                tt("dve" if bsel == 0 else "pool", o_, i_, sn, ALU.mult)
            tt("pool", d3, d3, t3, ALU.add)

        def interleave(gens, width, stagger):
            active = []
            it = iter(gens)
            pending_start = 0
            done = False
            while True:
                if not done and len(active) < width and pending_start <= 0:
                    g_ = next(it, None)
                    if g_ is None:
                        done = True
                    else:
                        active.append(g_)
                        pending_start = stagger
                if not active:
                    if done:
                        break
                    pending_start = 0
                    continue
                pending_start -= 1
                for g_ in list(active):
                    try:
                        next(g_)
                    except StopIteration:
                        active.remove(g_)

        cur_gs_row = [None]
        cur_gt_row = [None]

        def need_gs(row):
            if cur_gs_row[0] != row:
                for i, c in enumerate((1, 0)):
                    dma("sp", modA[i], modd[li, row:row + 1, c * D:(c + 1) * D].partition_broadcast(128), r=[("modd", li)])
                cur_gs_row[0] = row

        def need_gt(row):
            if cur_gt_row[0] != row:
                dma("sp", modA[2], modd[li, row:row + 1, 2 * D:3 * D].partition_broadcast(128), r=[("modd", li)])
                cur_gt_row[0] = row

        def kv_gen(b, t, slot):
            is_ctx = t >= NT_L
            gt = b * NT_B + t
            pbase = 4 * slot
            need_gs(NB if is_ctx else b)
            xt = xts.next()
            dma("sp", xt, xs[gt * 128:(gt + 1) * 128, :], r=[("xs", gt)])
            hh_ = hs.next()
            st = stat_ring.next()
            norm_h(xt, hh_, modA[0], modA[1], st)
            yield
            hT = hTs.next()
            transpose_h(hh_, hT, True, (ps[pbase], ps[pbase + 1]))
            yield
            pk = ps[pbase + 2]
            mm_group(pk, [(hT.ap[:, kc * 128:(kc + 1) * 128], wqkv.ap[:, kc * 1536 + 1024:kc * 1536 + 1536])
                          for kc in range(8)], r=[hT.key, wqkv.key])
            yield
            k32, kro, ktm = k32s[slot], kros[slot], ktms[slot]
            ksrc = pk[:, 0:256]
            act(Vs.v(Vs.ap.rearrange("p (t g c) -> p t g c", g=NKV, c=128)[:, t, :, 0:64]),
                pk.v(pk.ap[:, 256:512].rearrange("p (g d) -> p g d", d=HD)), AF.Copy)
            if mtype == 1:
                head_norm(ksrc, NKV, gk_bc, k32, ktm, hsts[slot])
                ksrc = k32
                yield
            if rope and not is_ctx:
                if mtype != 1:
                    cp("dve", k32, ksrc)
                    ksrc = k32
                apply_rope(ksrc, NKV, t, kro, ktm)
                ksrc = kro
                yield
            kb = kbs[slot]
            act(kb, ksrc, AF.Copy)
            yield
            pb = psbf(pbase + 3)
            tr_group([(pb.ap[0:64, g * 128:(g + 1) * 128], kb.ap[:, g * HD:(g + 1) * HD], identb.ap) for g in range(NKV)],
                     r=[kb.key, identb.key], w=[pb.key])
            kTv = kT.v(kT.ap.rearrange("p (g n) -> p g n", g=NKV)[:, :, t * 128:(t + 1) * 128])
            cp("dve", kTv, pb.v(pb.ap[0:64, 0:512].rearrange("p (g n) -> p g n", g=NKV)))
            yield

        def q_prologue(b, t, qslot, info):
            is_ctx = t >= NT_L
            gt = b * NT_B + t
            need_gs(NB if is_ctx else b)
            xt = xts.next()
            info["xt"] = xt
            dma("sp", xt, xs[gt * 128:(gt + 1) * 128, :], r=[("xs", gt)])
            hh_ = hs.next()
            st = stat_ring.next()
            norm_h(xt, hh_, modA[0], modA[1], st)
            yield
            hT = hTs.next()
            transpose_h(hh_, hT, True, (ps[0], ps[1]))
            yield
            for half in range(2):
                mm_group(ps[2 + half], [(hT.ap[:, kc * 128:(kc + 1) * 128],
                                         wqkv.ap[:, kc * 1536 + half * 512:kc * 1536 + (half + 1) * 512])
                                        for kc in range(8)], r=[hT.key, wqkv.key])
            yield
            for half in range(2):
                if mtype == 1:
                    head_norm(ps[2 + half], 8, gq_bc, q32[:, half * 512:(half + 1) * 512], qtmp[:, 0:512], hst)
                else:
                    cp("dve", q32[:, half * 512:(half + 1) * 512], ps[2 + half])
                yield
            variants = [0]
            act(qbf[0], q32, AF.Copy, scale=0.125)
            if rope and not is_ctx:
                for half in range(2):
                    apply_rope(q32[:, half * 512:(half + 1) * 512], 8, t, qro[:, half * 512:(half + 1) * 512], qtmp[:, 0:512])
                    yield
                act(qbf[1], qro, AF.Copy, scale=0.125)
                variants = [0, 1]
            yield
            qTs_ = qT[qslot]
            for vi in variants:
                for hh in range(2):
                    pb = psbf(hh)
                    tr_group([(pb.ap[0:64, j * 128:(j + 1) * 128], qbf[vi].ap[:, (hh * 8 + j) * HD:(hh * 8 + j + 1) * HD],
                               identb.ap) for j in range(8)], r=[qbf[vi].key, identb.key], w=[pb.key])
                    if hh == 0:
                        cp("dve", qTs_[vi][:, hh * 1024:(hh + 1) * 1024], pb[0:64, :])
                    else:
                        act(qTs_[vi][:, hh * 1024:(hh + 1) * 1024], pb[0:64, :], AF.Copy)
                    yield

        def attend(b, t, qslot, info, nxt):
            is_ctx = t >= NT_L
            gt = b * NT_B + t
            xt = info["xt"]
            qTs_ = qT[qslot]
            need_gt(NB if is_ctx else b)
            if is_ctx:
                KL = [(NT_L + j, 0, None, None) for j in range(NT_C)]
            else:
                KL = []
                if mtype == 0:
                    for kt_ in (t - 1, t, t + 1):
                        if 0 <= kt_ < NT_L:
                            m = maskLo if kt_ == t - 1 else (maskHi if kt_ == t + 1 else None)
                            KL.append((kt_, 1, m, None))
                elif mtype == 1:
                    KL = [(kt_, 1, None, None) for kt_ in range(NT_L)]
                else:
                    for j, kt_ in enumerate(_na_keytiles(t)):
                        KL.append((kt_, 0, None, (_na_pattern(t), j)))
                KL += [(NT_L + j, 0, None, None) for j in range(NT_C)]
            nk = len(KL)
            items = [(g, ki) + KL[ki] for g in range(NKV) for ki in range(nk)]

            def emit_S(i):
                g, ki, kt_, vi, msk, natp = items[i]
                pS = ps[4 + (i % 2)]
                kslice = kT.ap[:, g * NT_B * 128 + kt_ * 128: g * NT_B * 128 + (kt_ + 1) * 128]
                mm_group(pS, [(kslice, qTs_[vi].ap[:, g * 512:(g + 1) * 512])], r=[kT.key, qTs_[vi].key])

            if OPT["S_AHEAD"]:
                emit_S(0)
            for i, (g, ki, kt_, vi, msk, natp) in enumerate(items):
                if not OPT["S_AHEAD"]:
                    emit_S(i)
                elif i + 1 < len(items):
                    emit_S(i + 1)
                pS = ps[4 + (i % 2)]
                pT = pTs.next()
                if natp is not None:
                    nb_ = nat.next()
                    dma("sp" if i % 2 == 0 else "act", nb_, c_natab[natp[0], natp[1], :, g * 512:(g + 1) * 512])
                    sS = sbS.next()
                    tt("dve", sS, pS, nb_, ALU.add)
                    act(pT, sS, AF.Exp)
                else:
                    act(pT, pS, AF.Exp)
                if msk is not None:
                    tt("pool", pT, pT, msk, ALU.mult)
                acc = ps[6 + (g % 2)]
                acca = acc.ap
                vsl = Vs.ap[:, kt_ * 512 + g * 128: kt_ * 512 + (g + 1) * 128]
                pTa = pT.ap
                first, lastk = (ki == 0), (ki == nk - 1)

                def pv(e, acca=acca, vsl=vsl, pTa=pTa, first=first, lastk=lastk):
                    return e.matmul(acca, lhsT=vsl, rhs=pTa, start=first, stop=lastk)
                P.add("pe", pv, [Vs.key, pT.key], [acc.key])
                if lastk:
                    if use_sink:
                        cp("dve", den, acc[64:128, :])
                        sk = sink_s.v(sink_s.ap[:, 4 * g:4 * g + 4].unsqueeze(2).to_broadcast([64, 4, 128]))
                        d3 = den.v(den.ap.rearrange("p (h q) -> p h q", q=128))
                        tt("dve", d3, d3, sk, ALU.add)
                        recip(rden, den)
                    else:
                        recip(rden, acc[64:128, :])
                    tt("dve", oT[:, g * 512:(g + 1) * 512], acc[0:64, :], rden, ALU.mult)
                if nxt is not None and i >= 1:
                    next(nxt, None)
            if nxt is not None:
                for _ in nxt:
                    pass
            for half in range(2):
                mm_group(ps[2 + half], [(oT.ap[:, hh * 128:(hh + 1) * 128], wo.ap[:, hh * D + half * 512: hh * D + (half + 1) * 512])
                                        for hh in range(NH)], r=[oT.key, wo.key])
                yt = ytmp.next()
                tt("dve", yt, ps[2 + half], modA[2][:, half * 512:(half + 1) * 512], ALU.mult)
                tt("pool", xt[:, half * 512:(half + 1) * 512], xt[:, half * 512:(half + 1) * 512], yt, ALU.add)
            dma("sp", xs[gt * 128:(gt + 1) * 128, :], xt, w=[("xs", gt)])

        for b in (range(NB) if not OPT["SKIP_ATT"] else []):
            interleave((kv_gen(b, t, t % 2) for t in range(NT_B)), OPT["W_KV"], 3)
            q_tiles = list(range(NT_L)) + ([] if last else list(range(NT_L, NT_B)))
            infos = [dict() for _ in q_tiles]
            g0 = q_prologue(b, q_tiles[0], 0, infos[0])
            for _ in g0:
                pass
            for qi, t in enumerate(q_tiles):
                nxt = None
                if qi + 1 < len(q_tiles) and OPT["Q_OVERLAP"]:
                    nxt = q_prologue(b, q_tiles[qi + 1], (qi + 1) % 2, infos[qi + 1])
                attend(b, t, qi % 2, infos[qi], nxt)
                if qi + 1 < len(q_tiles) and not OPT["Q_OVERLAP"]:
                    for _ in q_prologue(b, q_tiles[qi + 1], (qi + 1) % 2, infos[qi + 1]):
                        pass

        P.barrier()
        alloc_state["off"] = PERSIST
        wgs = [a16("wg%d" % i, 8 * FH) for i in range(2)]
        wus = [a16("wu%d" % i, 8 * FH) for i in range(2)]
        wds = [a16("wd%d" % i, 4 * D) for i in range(2)]
        xgs = [a16("xg%d" % i, (CAP // 128) * D) for i in range(2)]
        xeT = a16("xeT", 8 * CAP)
        HT = a16("HT", 4 * CAP)
        sgs = Ring([a32("sg%d" % i, CAP) for i in range(2)])
        Ysb = Ring([a32("Ysb%d" % i, D) for i in range(2)])
        xts = Ring([a32("mxt%d" % i, D) for i in range(2)])
        h = a32("mh", D)
        hbfs = Ring([a16("hbf%d" % i, D) for i in range(2)])
        hT32 = a32("hT32", 8 * 128)
        modF = [[a32("modF%d_%d" % (s, i), D) for i in range(2)] for s in range(2)]
        gtF = [a32("gtF%d" % s, D) for s in range(2)]
        y1s = Ring([a32("y1_%d" % i, D) for i in range(2)])
        y2s = Ring([a32("y2_%d" % i, D) for i in range(2)])
        gfin = a32("gfin", D) if last else None
        wrg = a32("wrg", 8 * 36)
        print("moe SBUF KB (before rl)", alloc_state["off"] / 256.0)
        rls = [a32("rl%d" % i, 512) for i in range(2)]
        hT32s = [hT32, a32("hT32b", 8 * 128)]
        mhs = [h, a32("mh2", D)]

        def mk_rl(si):
            rl = rls[si]

            def RL(name, off, n):
                return T(rl.ap[:, off:off + n], ("rl", si, name))
            d = dict(L=RL("L", 0, 36), mg=RL("mg", 40, 1), nmg=RL("nmg", 41, 1), eg=RL("eg", 44, 4), sg=RL("sg", 48, 1),
                     ptop=RL("ptop", 49, 1), ohg=RL("ohg", 52, 4), pen=RL("pen", 56, 4), Lm=RL("Lm", 64, 32),
                     mx8=RL("mx8", 96, 8), oh1=RL("oh1", 104, 32), oh2=RL("oh2", 136, 32), dd=RL("dd", 168, 1),
                     ed=RL("ed", 169, 1), rd=RL("rd", 170, 1), A=RL("A", 172, 32),
                     Abf=T(rl.ap[:, 204:220].bitcast(BF16), ("rl", si, "Abf")), slot=RL("slot", 224, 32),
                     tmpa=RL("tmpa", 256, 32), tmpb=RL("tmpb", 328, 32), d1f=RL("d1f", 288, 1), d2f=RL("d2f", 289, 1),
                     ov=RL("ov", 290, 1), nov=RL("nov", 291, 1), slotp=RL("slotp", 296, 32))
            return d
        RLS = [mk_rl(0), mk_rl(1)]

        m_tiles = []
        for b in range(NB):
            m_tiles += [(b * NT_B + t, b) for t in range(NT_L)]
        if not last:
            for b in range(NB):
                m_tiles += [(b * NT_B + NT_L + t, NB) for t in range(NT_C)]
        XGKEYS = [("XG", gt) for (gt, _) in m_tiles]

        dma("sp", wrg.v(wrg.ap.rearrange("p (k n) -> p k n", n=36)), w_rg[li].rearrange("(k p) n -> p k n", p=128))
        dma("sp", brg_bc, b_rg[li:li + 1, :].partition_broadcast(128))
        memset("dve", tot, 0.0)
        if last:
            dma("sp", gfin, g_final.partition_broadcast(128))

        m1_state = {"row": None, "mset": -1, "mf": None}

        def m1_gen(gt, row, si):
            R_ = RLS[si]
            pbase = 4 * si
            if row != m1_state["row"]:
                m1_state["mset"] += 1
                mf_ = modF[m1_state["mset"] % 2]
                dma("sp", mf_[0], modd[li, row:row + 1, 4 * D:5 * D].partition_broadcast(128), r=[("modd", li)])
                dma("sp", mf_[1], modd[li, row:row + 1, 3 * D:4 * D].partition_broadcast(128), r=[("modd", li)])
                m1_state["row"] = row
                m1_state["mf"] = mf_
            mf = m1_state["mf"]
            xt = xts.next()
            dma("sp", xt, xs[gt * 128:(gt + 1) * 128, :], r=[("xs", gt)])
            st = stat_ring.next()
            hh_ = mhs[si]
            norm_h(xt, hh_, mf[0], mf[1], st)
            yield
            hbf = hbfs.next()
            act(hbf, hh_, AF.Copy)
            hT32_ = hT32s[si]
            transpose_h(hh_, hT32_, False, (ps[pbase], ps[pbase + 1]))
            yield
            mm_group(ps[pbase + 2][:, 0:36], [(hT32_.ap[:, kc * 128:(kc + 1) * 128], wrg.ap[:, kc * 36:(kc + 1) * 36]) for kc in range(8)],
                     r=[hT32_.key, wrg.key])
            yield
            L, mg, nmg, eg, sg_, ptop, ohg, pen, Lm, mx8 = (R_[k] for k in ("L", "mg", "nmg", "eg", "sg", "ptop", "ohg", "pen", "Lm", "mx8"))
            oh1, oh2, dd, ed, rd, Asum, Abf, slot = (R_[k] for k in ("oh1", "oh2", "dd", "ed", "rd", "A", "Abf", "slot"))
            tt("dve", L, ps[pbase + 2][:, 0:36], brg_bc, ALU.add)
            red(mg, L[:, 0:4], ALU.max)
            yield
            ts("dve", nmg, mg, -1.0, ALU.mult)
            ts("dve", ohg, L[:, 0:4], mg, ALU.is_equal)
            yield
            act(eg, L[:, 0:4], AF.Exp, bias=nmg, accum=sg_)
            ts("dve", pen, ohg, -1.0, ALU.add, 1e30, ALU.mult)
            yield
            recip(ptop, sg_)
            tt("dve", Lm.v(Lm.ap.rearrange("p (g e) -> p g e", e=8)), L.v(L.ap[:, 4:36].rearrange("p (g e) -> p g e", e=8)),
               pen.v(pen.ap.unsqueeze(2).to_broadcast([128, 4, 8])), ALU.add)
            yield
            mxo, lmi = mx8.ap, Lm.ap
            P.add("dve", lambda e, mxo=mxo, lmi=lmi: e.max(out=mxo, in_=lmi), [Lm.key], [mx8.key])
            yield
            ts("dve", oh1, Lm, mx8[:, 0:1], ALU.is_equal)
            ts("dve", oh2, Lm, mx8[:, 1:2], ALU.is_equal)
            tt("dve", dd, mx8[:, 1:2], mx8[:, 0:1], ALU.subtract)
            yield
            act(ed, dd, AF.Exp)
            tt("dve", Asum, oh1, oh2, ALU.add)
            yield
            ts("dve", ed, ed, 1.0, ALU.add)
            cp("dve", Abf, Asum)
            yield
            recip(rd, ed)
            w1 = T(rt_w1.ap[:, gt:gt + 1], ("rt_w1", gt))
            w2 = T(rt_w2.ap[:, gt:gt + 1], ("rt_w2", gt))
            mm_group(ps[pbase + 3][:, 0:32], [(trib.ap, Abf.ap)], r=[trib.key, Abf.key])
            mm_group(ps[pbase + 3][:, 32:64], [(onesb.ap, Abf.ap)], r=[onesb.key, Abf.key])
            stt("dve", slot, ps[pbase + 3][:, 0:32], 0.0, tot, ALU.add, ALU.add)
            tt("dve", tot, tot, ps[pbase + 3][:, 32:64], ALU.add)
            yield
            tt("dve", w1, ptop, rd, ALU.mult)
            tt("dve", R_["slotp"], slot, ec_bc, ALU.add)
            yield
            tt("dve", w2, ptop, w1, ALU.subtract)
            tt("dve", R_["tmpa"], oh1, slot, ALU.mult)
            tt("dve", R_["tmpb"], oh2, slot, ALU.mult)
            yield
            ovs = []
            for ci, (oh, df, rtd, tmp) in enumerate(((oh1, R_["d1f"], rt_d1, R_["tmpa"]), (oh2, R_["d2f"], rt_d2, R_["tmpb"]))):
                ov = T(rls[si].ap[:, 400 + ci:401 + ci], ("rl", si, "ov%d" % ci))
                nov = T(rls[si].ap[:, 404 + ci:405 + ci], ("rl", si, "nov%d" % ci))
                red(ov, tmp, ALU.add)
                yield
                ts("dve", ov, ov, float(CAP), ALU.is_ge)
                tt("dve", tmp, oh, R_["slotp"], ALU.mult)
                yield
                ts("dve", nov, ov, -1.0, ALU.mult, 1.0, ALU.add)
                red(df, tmp, ALU.add)
                yield
                tt("dve", df, df, nov, ALU.mult)
                yield
                stt("dve", df, ov, float(NE * CAP), df, ALU.mult, ALU.add)
                yield
                rcol = T(rtd.ap[:, gt:gt + 1], (rtd.key, gt))
                cp("dve", rcol, df)
                yield
                ia = rcol.ap
                ha = hbf.ap
                P.add("pool", lambda e, ia=ia, ha=ha: e.indirect_dma_start(
                    out=XG, out_offset=bass.IndirectOffsetOnAxis(ap=ia, axis=0), in_=ha, in_offset=None),
                    [rcol.key, hbf.key], [("XG", gt)], dma=True)

        if not OPT["SKIP_M1"]:
            interleave((m1_gen(gt, row, i % 2) for i, (gt, row) in enumerate(m_tiles)), OPT["W_M1"], 9)

        P.barrier()
        stg = Ring([T(b_.ap, "stg%d" % i) for i, b_ in enumerate(y1s.items + y2s.items + modF[0] + modF[1])])
        NS = CAP // 128

        def prefetch(ex):
            wg, wu, wd, xg = wgs[ex % 2], wus[ex % 2], wds[ex % 2], xgs[ex % 2]
            dma("pool", xg.v(xg.ap.rearrange("p (s d) -> p s d", d=D)),
                XG[ex * CAP:(ex + 1) * CAP, :].rearrange("(s p) d -> p s d", p=128), r=XGKEYS)
            chunks = []
            for (wdst, wsrc, fw) in ((wg, w_gate[li, ex], FH), (wu, w_up[li, ex], FH), (wd, w_down[li, ex], D)):
                src3 = wsrc.rearrange("(k p) f -> p k f", p=128)
                kper = 1024 // fw
                for c in range(4):
                    chunks.append((wdst[:, c * 1024:(c + 1) * 1024], src3[:, c * kper:(c + 1) * kper, :], fw))
            bufs = []

            def issue(i):
                sb = stg.next()
                bufs.append(sb)
                dma("sp", sb.v(sb.ap.rearrange("p (k f) -> p k f", f=chunks[i][2])), chunks[i][1])
            AHEAD = 3
            for i in range(AHEAD):
                issue(i)
            yield
            for i in range(len(chunks)):
                if i + AHEAD < len(chunks):
                    issue(i + AHEAD)
                if i % 2 == 0:
                    act(chunks[i][0], bufs[i], AF.Copy)
                else:
                    cp("dve", chunks[i][0], bufs[i])
                yield

        for _ in prefetch(0):
            pass
        for ex in (range(NE) if not OPT["SKIP_M2"] else []):
            wg, wu, wd, xg = wgs[ex % 2], wus[ex % 2], wds[ex % 2], xgs[ex % 2]
            nxt = prefetch(ex + 1) if ex + 1 < NE else None

            def step(nxt=nxt):
                if nxt is not None:
                    next(nxt, None)
            for s in range(NS):
                pb = psbf(s % 2)
                tr_group([(pb.ap[:, kc * 128:(kc + 1) * 128], xg.ap[:, s * D + kc * 128: s * D + (kc + 1) * 128], identb.ap)
                          for kc in range(8)], r=[xg.key, identb.key], w=[pb.key])
                dst = xeT.v(xeT.ap.rearrange("p (k c) -> p k c", c=CAP)[:, :, s * 128:(s + 1) * 128])
                src = pb.v(pb.ap.rearrange("p (k c) -> p k c", c=128))
                if s % 2 == 0:
                    cp("dve", dst, src)
                else:
                    act(dst, src, AF.Copy)
                step()
            for m in range(4):
                mm_group(ps[2 + (m % 2)], [(wg.ap[:, kc * FH + m * 128: kc * FH + (m + 1) * 128], xeT.ap[:, kc * CAP:(kc + 1) * CAP])
                                           for kc in range(8)], r=[wg.key, xeT.key])
                mm_group(ps[4 + (m % 2)], [(wu.ap[:, kc * FH + m * 128: kc * FH + (m + 1) * 128], xeT.ap[:, kc * CAP:(kc + 1) * CAP])
                                           for kc in range(8)], r=[wu.key, xeT.key])
                sgt = sgs.next()
                act(sgt, ps[2 + (m % 2)], AF.Silu)
                tt("dve", HT[:, m * CAP:(m + 1) * CAP], sgt, ps[4 + (m % 2)], ALU.mult)
                step()
            for s in range(NS):
                ysb = Ysb.next()
                for half in range(2):
                    pp = ps[6 + half]
                    mm_group(pp, [(HT.ap[:, m * CAP + s * 128: m * CAP + (s + 1) * 128], wd.ap[:, m * D + half * 512: m * D + (half + 1) * 512])
                                  for m in range(4)], r=[HT.key, wd.key])
                    if half == 0:
                        act(ysb[:, 0:512], pp, AF.Copy)
                    else:
                        cp("dve", ysb[:, 512:1024], pp)
                dma("pool", YG[ex * CAP + s * 128: ex * CAP + (s + 1) * 128, :], ysb, w=[("YG", ex, s)])
                step()
            if nxt is not None:
                for _ in nxt:
                    pass
        P.barrier()

        YGKEYS = ["YG"] + [("YG", ex, s_) for ex in range(NE) for s_ in range(CAP // 128)]
        m3_state = {"row": None, "mset": -1, "gtf": None}

        def m3_gen(gt, row):
            if row != m3_state["row"]:
                m3_state["mset"] += 1
                gtf_ = gtF[m3_state["mset"] % 2]
                dma("sp", gtf_, modd[li, row:row + 1, 5 * D:6 * D].partition_broadcast(128), r=[("modd", li)])
                m3_state["row"] = row
                m3_state["gtf"] = gtf_
            gtf = m3_state["gtf"]
            y1, y2 = y1s.next(), y2s.next()
            for (yy, rtd) in ((y1, rt_d1), (y2, rt_d2)):
                ia = rtd.ap[:, gt:gt + 1]
                ya = yy.ap
                P.add("pool", lambda e, ia=ia, ya=ya: e.indirect_dma_start(
                    out=ya, out_offset=None, in_=YG, in_offset=bass.IndirectOffsetOnAxis(ap=ia, axis=0)),
                    [(rtd.key, gt)] + YGKEYS, [yy.key], dma=True)
            xt = xts.next()
            dma("sp", xt, xs[gt * 128:(gt + 1) * 128, :], r=[("xs", gt)])
            yield
            w1 = T(rt_w1.ap[:, gt:gt + 1], ("rt_w1", gt))
            w2 = T(rt_w2.ap[:, gt:gt + 1], ("rt_w2", gt))
            ts("dve", y1, y1, w1, ALU.mult)
            yield
            stt("dve", y1, y2, w2, y1, ALU.mult, ALU.add)
            yield
            tt("pool", y1, y1, gtf, ALU.mult)
            yield
            tt("pool", xt, xt, y1, ALU.add)
            yield
            if not last:
                dma("sp", xs[gt * 128:(gt + 1) * 128, :], xt, w=[("xs", gt)])
            else:
                st = stat_ring.next()
                stt("dve", y2, xt, 1.0 / D, xt, ALU.mult, ALU.mult, accum=st[:, 0:1])
                yield
                act(st[:, 1:2], st[:, 0:1], AF.Ln, bias=EPS)
                yield
                act(st[:, 2:3], st[:, 1:2], AF.Exp, scale=-0.5)
                yield
                stt("dve", y2, xt, st[:, 2:3], gfin, ALU.mult, ALU.mult)
                b_ = gt // NT_B
                t_ = gt % NT_B
                r0 = b_ * SEQ + t_ * 128
                dma("sp", out[r0:r0 + 128, :], y2)

        if not OPT["SKIP_M3"]:
            interleave((m3_gen(gt, row) for (gt, row) in m_tiles), OPT["W_M3"], 3)

    P.finalize()
    P.emit()
    return nc


_CACHE = {}


def _consts():
    if "c" not in _CACHE:
        cos, sin_s = _rope_tables()
        kk = np.arange(128)[:, None]
        qq = np.arange(128)[None, :]
        mlo = np.tile((kk >= qq).astype(np.float32), (1, 4))
        mhi = np.tile((kk <= qq).astype(np.float32), (1, 4))
        tri = (np.arange(128)[:, None] < np.arange(128)[None, :]).astype(np.float32)
        ec = (np.arange(NE, dtype=np.float32) * CAP).reshape(1, NE)
        _CACHE["c"] = dict(c_ident=np.eye(128, dtype=np.float32), c_cos=cos, c_sin=sin_s,
                           c_masks=np.stack([mlo, mhi]).astype(np.float32), c_tri=tri, c_ec=ec)
        _CACHE["naidx"] = _na_index_table()
    return _CACHE["c"], _CACHE["naidx"]


def make_in_maps(inputs, n_cores, NB):
    f = lambda a: np.ascontiguousarray(np.asarray(a, dtype=np.float32))
    consts, naidx = _consts()
    rpb = f(inputs["rpb_c"])[0].reshape(-1)
    rpb_ext = np.concatenate([rpb, np.array([NEG], dtype=np.float32)])
    natab = np.ascontiguousarray(rpb_ext[naidx].reshape(5, 5, 128, NH * 128))
    w_rg = np.ascontiguousarray(np.concatenate([f(inputs["w_group"]), f(inputs["w_router"])], axis=-1))
    b_rg = np.ascontiguousarray(np.concatenate([f(inputs["b_group"]), f(inputs["b_router"])], axis=-1))
    shared = dict(
        w_ada=f(inputs["w_ada"]), b_ada=f(inputs["b_ada"]), g_attn=f(inputs["g_attn"]), w_qkv=f(inputs["w_qkv"]),
        w_o=f(inputs["w_o"]), sink_a=f(inputs["sink_a"]), gq_b=f(inputs["gq_b"]), gk_b=f(inputs["gk_b"]),
        g_ffn=f(inputs["g_ffn"]), w_rg=w_rg, b_rg=b_rg, w_gate=f(inputs["w_gate"]), w_up=f(inputs["w_up"]),
        w_down=f(inputs["w_down"]), g_final=f(inputs["g_final"]).reshape(1, D), c_natab=natab, **consts)
    x = f(inputs["x"])
    ctx = f(inputs["ctx"])
    c = f(inputs["c"])
    c_ctx = f(inputs["c_ctx"]).reshape(1, D)
    maps = []
    for i in range(n_cores):
        sl = slice(i * NB, (i + 1) * NB)
        m = dict(shared)
        m["x"] = np.ascontiguousarray(x[sl].reshape(NB * SEQ, D))
        m["ctx"] = np.ascontiguousarray(ctx[sl].reshape(NB * CTX, D))
        m["cvec"] = np.ascontiguousarray(np.concatenate([c[sl], c_ctx], axis=0))
        maps.append(m)
    return maps


def kernel(**inputs):
    n_cores = 8
    NB = 2
    if "nc" not in _CACHE:
        _CACHE["nc"] = build(NB=NB)
    nc = _CACHE["nc"]
    maps = make_in_maps(inputs, n_cores, NB)
    res = run_bass_kernel_spmd(nc, maps, core_ids=list(range(n_cores)))
    outs = [r["out"].reshape(NB, SEQ, D) for r in res.results]
    return np.concatenate(outs, axis=0).astype(np.float32)
```

```python
import numpy as np
import concourse.bass as bass
import concourse.mybir as mybir
from concourse.bass_utils import run_bass_kernel_spmd

F32 = mybir.dt.float32
BF16 = mybir.dt.bfloat16
I32 = mybir.dt.int32
ALU = mybir.AluOpType
AF = mybir.ActivationFunctionType
AX = mybir.AxisListType

D = 1024
SEQ = 2048
CTX = 256
DEPTH = 4
NH = 16
NKV = 4
HD = 64
NE = 32
FH = 512
GRID_W = 64
import os as _os
OPT = dict(W_KV=int(_os.environ.get("W_KV", 2)), W_M1=int(_os.environ.get("W_M1", 2)), W_M3=int(_os.environ.get("W_M3", 2)),
           Q_OVERLAP=int(_os.environ.get("Q_OVERLAP", 1)), S_AHEAD=int(_os.environ.get("S_AHEAD", 1)),
           SINK_BC=int(_os.environ.get("SINK_BC", 1)), SKIP_ATT=int(_os.environ.get("SKIP_ATT", 0)),
           SKIP_M1=int(_os.environ.get("SKIP_M1", 0)), SKIP_M2=int(_os.environ.get("SKIP_M2", 0)), SKIP_M3=int(_os.environ.get("SKIP_M3", 0)))
CAP = 512
NT_L = SEQ // 128
NT_C = CTX // 128
NT_B = NT_L + NT_C
EPS = 1e-6
NEG = -1e30


class _Op:
    __slots__ = ("idx", "eng", "fn", "deps", "dma", "sig", "waits", "signal", "ksnap")

    def __init__(self, idx, eng, fn, dma):
        self.idx = idx
        self.eng = eng
        self.fn = fn
        self.deps = set()
        self.dma = dma
        self.sig = None
        self.waits = []
        self.signal = False
        self.ksnap = None


class Prog:
    ENGS = ("pe", "act", "dve", "pool", "sp")
    EPOCH = 20000
    NDMA = {"sp": 24, "pool": 24, "act": 8}

    def __init__(self, nc):
        self.nc = nc
        self.ops = []
        self.last_w = {}
        self.readers = {}
        self.pending = {}
        self.last_eng = {}
        self.dma_since = []

    def add(self, eng, fn, r=(), w=(), dma=False):
        op = _Op(len(self.ops), eng, fn, dma)
        ops = self.ops

        def same_eng(j):
            o = ops[j]
            return (not dma) and (not o.dma) and o.eng == eng

        for k in r:
            lw = self.last_w.get(k)
            if lw is not None and not (eng == "pe" and same_eng(lw)):
                op.deps.add(lw)
            if isinstance(k, str) and k.startswith("ps"):
                for rd in self.readers.get(k, ()):
                    if not same_eng(rd):
                        op.deps.add(rd)
        for k in w:
            lw = self.last_w.get(k)
            if lw is not None and not (eng == "pe" and same_eng(lw)):
                op.deps.add(lw)
            rs = self.readers.get(k)
            if rs:
                for rd in rs:
                    if not same_eng(rd):
                        op.deps.add(rd)
        for k in r:
            self.readers.setdefault(k, []).append(op.idx)
        for k in w:
            self.last_w[k] = op.idx
            self.readers[k] = []
        if eng in self.pending:
            op.deps.update(self.pending.pop(eng))
        op.deps.discard(op.idx)
        self.ops.append(op)
        if dma:
            self.dma_since.append(op.idx)
        else:
            self.last_eng[eng] = op.idx
        return op

    def barrier(self):
        deps = set(self.last_eng.values()) | set(self.dma_since)
        for e in self.ENGS:
            self.pending[e] = set(deps) | self.pending.get(e, set())
        self.dma_since = []

    def finalize(self, final_engine="sp"):
        ops = self.ops
        dma_rr = {q: 0 for q in self.NDMA}
        dma_last = {}
        dma_cnt = {}
        for op in ops:
            if op.dma:
                q = op.eng
                s = dma_rr[q] % self.NDMA[q]
                dma_rr[q] += 1
                key = ("dma", q, s)
                prev = dma_last.get(key)
                if prev is not None:
                    op.deps.add(prev)
                dma_last[key] = op.idx
                dma_cnt[key] = dma_cnt.get(key, 0) + 16
                op.sig = (key, dma_cnt[key])
        has_dep = [False] * len(ops)
        for op in ops:
            for d in op.deps:
                has_dep[d] = True
        cnt = {e: 0 for e in self.ENGS}
        know = {e: {} for e in self.ENGS}
        for op in ops:
            e = op.eng
            K = know[e]
            for d in sorted(op.deps):
                sk, sv = ops[d].sig
                if K.get(sk, 0) >= sv:
                    continue
                op.waits.append((sk, sv))
                K[sk] = sv
                for k2, v2 in ops[d].ksnap.items():
                    if K.get(k2, 0) < v2:
                        K[k2] = v2
            if op.dma:
                op.signal = True
            elif has_dep[op.idx]:
                cnt[e] += 1
                ep = cnt[e] // self.EPOCH
                op.sig = (("eng", e, ep), cnt[e] - ep * self.EPOCH + (1 if ep else 0))
                op.signal = True
            op.ksnap = dict(K)
        self.final_waits = []
        Kf = know[final_engine]
        for key, v in dma_cnt.items():
            if Kf.get(key, 0) < v:
                self.final_waits.append((key, v))
        self.semkeys = set(op.sig[0] for op in ops if op.signal)
        self.final_engine = final_engine

    def emit(self):
        nc = self.nc
        sems = {}
        for i, k in enumerate(sorted(self.semkeys, key=str)):
            sems[k] = nc.alloc_semaphore("s%d" % i)
        by_eng = {e: [] for e in self.ENGS}
        for op in self.ops:
            by_eng[op.eng].append(op)
        fin_eng = self.final_engine
        fin_waits = self.final_waits

        def run(eng_name, eng):
            for op in by_eng[eng_name]:
                for sk, sv in op.waits:
                    eng.wait_ge(sems[sk], sv)
                ins = op.fn(eng)
                if op.signal:
                    ins.then_inc(sems[op.sig[0]], 16 if op.dma else 1)
            if eng_name == fin_eng:
                for sk, sv in fin_waits:
                    eng.wait_ge(sems[sk], sv)

        with nc.Block() as block:
            @block.tensor
            def _(e):
                run("pe", e)

            @block.scalar
            def _(e):
                run("act", e)

            @block.vector
            def _(e):
                run("dve", e)

            @block.gpsimd
            def _(e):
                run("pool", e)

            @block.sync
            def _(e):
                run("sp", e)


class T:
    __slots__ = ("ap", "key")

    def __init__(self, ap, key):
        self.ap = ap
        self.key = key

    def __getitem__(self, idx):
        return T(self.ap[idx], self.key)

    def v(self, ap):
        return T(ap, self.key)


class Ring:
    def __init__(self, items):
        self.items = items
        self.i = 0

    def next(self):
        it = self.items[self.i % len(self.items)]
        self.i += 1
        return it


def _rope_tables():
    t = np.arange(SEQ)
    row = (t // GRID_W).astype(np.float32)
    col = (t % GRID_W).astype(np.float32)
    quarter = HD // 4
    inv = (np.float32(10000.0) ** (-np.arange(quarter, dtype=np.float32) / np.float32(quarter))).astype(np.float32)
    ar = row[:, None] * inv
    ac = col[:, None] * inv
    ang = np.concatenate([ar, ar, ac, ac], axis=-1).astype(np.float32)
    cos = np.cos(ang).astype(np.float32)
    sin = np.sin(ang).astype(np.float32)
    sgn = np.concatenate([-np.ones(16), np.ones(16), -np.ones(16), np.ones(16)]).astype(np.float32)
    return cos, (sin * sgn[None, :]).astype(np.float32)


NA_PAT_TILES = [0, 1, 2, 14, 15]


def _na_pattern(t):
    return {0: 0, 1: 1, 14: 3, 15: 4}.get(t, 2)


def _na_keytiles(t):
    if t <= 1:
        return [0, 1, 2, 3]
    if t >= 14:
        return [12, 13, 14, 15]
    return [t - 2, t - 1, t, t + 1, t + 2]


def _na_index_table():
    rows = SEQ // GRID_W
    wh, ww = 8, 16
    nrel = 15 * 31
    masked = NH * nrel
    tab = np.full((5, 5, 128, NH, 128), masked, dtype=np.int64)
    for pi, t in enumerate(NA_PAT_TILES):
        kts = _na_keytiles(t)
        q = t * 128 + np.arange(128)
        r = q // GRID_W
        c = q % GRID_W
        rs = np.clip(r - wh // 2, 0, rows - wh)
        cs = np.clip(c - ww // 2, 0, GRID_W - ww)
        for j, kt in enumerate(kts):
            k = kt * 128 + np.arange(128)
            kr = k // GRID_W
            kc = k % GRID_W
            inwin = ((kr[:, None] >= rs[None, :]) & (kr[:, None] < rs[None, :] + wh)
                     & (kc[:, None] >= cs[None, :]) & (kc[:, None] < cs[None, :] + ww))
            rel = (kr[:, None] - r[None, :] + 7) * 31 + (kc[:, None] - c[None, :] + 15)
            for h in range(NH):
                tab[pi, j, :, h, :] = np.where(inwin, h * nrel + rel, masked)
    return tab


def build(NB=2, n_layers=DEPTH):
    nc = bass.Bass("TRN2", target_bir_lowering=False)
    R = NB + 1
    NTOK = NB * NT_B * 128

    def din(name, shape, dt=F32):
        return nc.dram_tensor(name, list(shape), dt, kind="ExternalInput").ap()

    x_in = din("x", [NB * SEQ, D])
    ctx_in = din("ctx", [NB * CTX, D])
    cvec = din("cvec", [R, D])
    w_ada = din("w_ada", [DEPTH, D, 6 * D])
    b_ada = din("b_ada", [DEPTH, 6 * D])
    g_attn = din("g_attn", [DEPTH, D])
    w_qkv = din("w_qkv", [DEPTH, D, 1536])
    w_o = din("w_o", [DEPTH, D, D])
    sink_a = din("sink_a", [2, NH])
    gq_b = din("gq_b", [1, HD])
    gk_b = din("gk_b", [1, HD])
    g_ffn = din("g_ffn", [DEPTH, D])
    w_rg = din("w_rg", [DEPTH, D, 36])
    b_rg = din("b_rg", [DEPTH, 36])
    w_gate = din("w_gate", [DEPTH, NE, D, FH])
    w_up = din("w_up", [DEPTH, NE, D, FH])
    w_down = din("w_down", [DEPTH, NE, FH, D])
    g_final = din("g_final", [1, D])
    c_ident = din("c_ident", [128, 128])
    c_cos = din("c_cos", [SEQ, HD])
    c_sin = din("c_sin", [SEQ, HD])
    c_masks = din("c_masks", [2, 128, 512])
    c_tri = din("c_tri", [128, 128])
    c_ec = din("c_ec", [1, NE])
    c_natab = din("c_natab", [5, 5, 128, NH * 128])
    out = nc.dram_tensor("out", [NB * SEQ, D], F32, kind="ExternalOutput").ap()

    xs = nc.dram_tensor("xs", [NTOK, D], F32).ap()
    modd = nc.dram_tensor("modd", [DEPTH, R, 6 * D], F32).ap()
    XG = nc.dram_tensor("XG", [NE * CAP + 1, D], BF16).ap()
    YG = nc.dram_tensor("YG", [NE * CAP + 1, D], F32).ap()

    P = Prog(nc)

    POOL_KB = 190
    pool = nc.alloc_sbuf_tensor("pool", [128, POOL_KB * 256], F32)
    alloc_state = {"off": 0}

    def a32(name, n, parts=128):
        off = alloc_state["off"]
        alloc_state["off"] = off + n
        assert alloc_state["off"] <= POOL_KB * 256, (name, alloc_state["off"])
        return T(pool[0:parts, off:off + n], name)

    def a16(name, n, parts=128):
        n32 = (n + 1) // 2
        off = alloc_state["off"]
        alloc_state["off"] = off + n32
        assert alloc_state["off"] <= POOL_KB * 256, (name, alloc_state["off"])
        return T(pool[0:parts, off:off + n32].bitcast(BF16), name)

    ps = [T(nc.alloc_psum_tensor("ps%d" % i, [128, 512], F32)[:, :], "ps%d" % i) for i in range(8)]

    def psbf(i):
        return T(ps[i].ap.bitcast(BF16), ps[i].key)

    ident = a32("ident", 128)
    identb = a16("identb", 128)
    onesb = a16("onesb", 128)
    trib = a16("trib", 128)
    cos_t = a32("cos", NT_L * HD)
    sin_t = a32("sin", NT_L * HD)
    maskLo = a16("maskLo", 512)
    maskHi = a16("maskHi", 512)
    gq_bc = a32("gq_bc", HD)
    gk_bc = a32("gk_bc", HD)
    ec_bc = a32("ec_bc", NE)
    brg_bc = a32("brg_bc", 36)
    tot = a32("tot", NE)
    NTT = NB * NT_B
    rt_w1 = a32("rt_w1", NTT)
    rt_w2 = a32("rt_w2", NTT)
    rt_d1 = T(nc.alloc_sbuf_tensor("rt_d1", [128, NTT], I32)[:, :], "rt_d1")
    rt_d2 = T(nc.alloc_sbuf_tensor("rt_d2", [128, NTT], I32)[:, :], "rt_d2")
    small = a32("small", 64)
    PERSIST = alloc_state["off"]

    def dma(q, out_, in_, r=(), w=()):
        oa = out_.ap if isinstance(out_, T) else out_
        ia = in_.ap if isinstance(in_, T) else in_
        rr = list(r) + ([in_.key] if isinstance(in_, T) else [])
        ww = list(w) + ([out_.key] if isinstance(out_, T) else [])
        P.add(q, lambda e: e.dma_start(out=oa, in_=ia), rr, ww, dma=True)

    def act(out_, in_, func, scale=1.0, bias=0.0, accum=None, r=(), w=()):
        kw = {}
        rr = [in_.key] + list(r)
        ww = [out_.key] + list(w)
        if isinstance(bias, T):
            rr.append(bias.key)
            bias = bias.ap
        if isinstance(scale, T):
            rr.append(scale.key)
            scale = scale.ap
        if accum is not None:
            kw["accum_out"] = accum.ap
            ww.append(accum.key)
        oa, ia = out_.ap, in_.ap
        P.add("act", lambda e: e.activation(out=oa, in_=ia, func=func, bias=bias, scale=scale, **kw), rr, ww)

    def tt(eng, out_, a, b, op):
        oa, aa, ba = out_.ap, a.ap, b.ap
        P.add(eng, lambda e: e.tensor_tensor(out=oa, in0=aa, in1=ba, op=op), [a.key, b.key], [out_.key])

    def ts(eng, out_, a, s1, op0, s2=None, op1=None, accum=None):
        rr = [a.key]
        ww = [out_.key]
        if isinstance(s1, T):
            rr.append(s1.key)
            s1 = s1.ap
        if isinstance(s2, T):
            rr.append(s2.key)
            s2 = s2.ap
        kw = {}
        if op1 is not None:
            kw["op1"] = op1
        if accum is not None:
            kw["accum_out"] = accum.ap
            ww.append(accum.key)
        oa, aa = out_.ap, a.ap
        P.add(eng, lambda e: e.tensor_scalar(out=oa, in0=aa, scalar1=s1, scalar2=s2, op0=op0, **kw), rr, ww)

    def stt(eng, out_, a, s, b, op0, op1, accum=None):
        rr = [a.key, b.key]
        ww = [out_.key]
        if isinstance(s, T):
            rr.append(s.key)
            s = s.ap
        kw = {}
        if accum is not None:
            kw["accum_out"] = accum.ap
            ww.append(accum.key)
        oa, aa, ba = out_.ap, a.ap, b.ap
        P.add(eng, lambda e: e.scalar_tensor_tensor(out=oa, in0=aa, scalar=s, in1=ba, op0=op0, op1=op1, **kw), rr, ww)

    def cp(eng, out_, in_):
        oa, ia = out_.ap, in_.ap
        P.add(eng, lambda e: e.tensor_copy(out=oa, in_=ia), [in_.key], [out_.key])

    def recip(out_, in_):
        oa, ia = out_.ap, in_.ap
        P.add("dve", lambda e: e.reciprocal(out=oa, in_=ia), [in_.key], [out_.key])

    def red(out_, in_, op, axis=AX.X):
        oa, ia = out_.ap, in_.ap
        P.add("dve", lambda e: e.tensor_reduce(out=oa, in_=ia, axis=axis, op=op), [in_.key], [out_.key])

    def memset(eng, out_, val):
        oa = out_.ap
        P.add(eng, lambda e: e.memset(oa, val), [], [out_.key])

    def mm_group(out_, pairs, r=()):
        oa = out_.ap
        n = len(pairs)

        def fn(e):
            ins = None
            for i, (l, rh) in enumerate(pairs):
                ins = e.matmul(oa, lhsT=l, rhs=rh, start=(i == 0), stop=(i == n - 1))
            return ins
        P.add("pe", fn, list(r), [out_.key])

    def tr_group(items, r=(), w=()):
        def fn(e):
            ins = None
            for (o, i, idn) in items:
                ins = e.transpose(out=o, in_=i, identity=idn)
            return ins
        P.add("pe", fn, list(r), list(w))

    dma("sp", ident, c_ident)
    dma("pool", identb, c_ident)
    dma("pool", trib, c_tri)
    dma("sp", cos_t.v(cos_t.ap.rearrange("p (t d) -> p t d", d=HD)), c_cos.rearrange("(t p) d -> p t d", p=128))
    dma("sp", sin_t.v(sin_t.ap.rearrange("p (t d) -> p t d", d=HD)), c_sin.rearrange("(t p) d -> p t d", p=128))
    dma("pool", maskLo, c_masks[0])
    dma("pool", maskHi, c_masks[1])
    dma("sp", gq_bc, gq_b.partition_broadcast(128))
    dma("sp", gk_bc, gk_b.partition_broadcast(128))
    dma("sp", ec_bc, c_ec.partition_broadcast(128))
    memset("dve", onesb, 1.0)

    for b in range(NB):
        base = b * NT_B * 128
        dma("sp", xs[base:base + SEQ, :], x_in[b * SEQ:(b + 1) * SEQ, :], w=[("xs", b * NT_B + t) for t in range(NT_L)])
        dma("sp", xs[base + SEQ:base + SEQ + CTX, :], ctx_in[b * CTX:(b + 1) * CTX, :],
            w=[("xs", b * NT_B + NT_L + t) for t in range(NT_C)])

    alloc_state["off"] = PERSIST
    zrow = a32("zrow", D, parts=1)
    memset("dve", zrow, 0.0)
    dma("sp", YG[NE * CAP:NE * CAP + 1, :], zrow, w=["YG"])
    cv = a32("cv", D, parts=R)
    scT = a32("scT", 8 * R)
    wada = [a32("wada%d" % i, 8 * 512) for i in range(2)]
    modsb = a32("modsb", 6 * D, parts=R)
    bada = a32("bada", 6 * D, parts=R)
    gA = a32("gA", D, parts=R)
    gF = a32("gF", D, parts=R)
    dma("sp", cv, cvec)
    act(cv, cv, AF.Silu)
    tr_group([(ps[0].ap[:, kc * R:(kc + 1) * R], cv.ap[:, kc * 128:(kc + 1) * 128], ident.ap[0:R, 0:R]) for kc in range(8)],
             r=[cv.key, ident.key], w=[ps[0].key])
    cp("dve", scT, ps[0][:, 0:8 * R])
    for li in range(n_layers):
        dma("sp", bada, b_ada[li:li + 1, :].partition_broadcast(R))
        dma("sp", gA, g_attn[li:li + 1, :].partition_broadcast(R))
        dma("sp", gF, g_ffn[li:li + 1, :].partition_broadcast(R))
        for n in range(12):
            wt = wada[n % 2]
            dma("sp" if n % 2 == 0 else "act", wt.v(wt.ap.rearrange("p (k f) -> p k f", f=512)),
                w_ada[li, :, n * 512:(n + 1) * 512].rearrange("(k p) f -> p k f", p=128))
            pp = ps[1 + (n % 2)]
            mm_group(pp[0:R, :], [(scT.ap[:, kc * R:(kc + 1) * R], wt.ap[:, kc * 512:(kc + 1) * 512]) for kc in range(8)],
                     r=[scT.key, wt.key])
            tt("dve", modsb[:, n * 512:(n + 1) * 512], pp[0:R, :], bada[:, n * 512:(n + 1) * 512], ALU.add)
        stt("dve", modsb[:, D:2 * D], modsb[:, D:2 * D], 1.0, gA, ALU.add, ALU.mult)
        stt("dve", modsb[:, 4 * D:5 * D], modsb[:, 4 * D:5 * D], 1.0, gF, ALU.add, ALU.mult)
        dma("sp", modd[li], modsb, w=[("modd", li)])

    def norm_h(xt, h, gs_bc, sh_bc, st):
        stt("dve", h, xt, 1.0 / D, xt, ALU.mult, ALU.mult, accum=st[:, 0:1])
        act(st[:, 1:2], st[:, 0:1], AF.Ln, bias=EPS)
        act(st[:, 2:3], st[:, 1:2], AF.Exp, scale=-0.5)
        stt("dve", h, xt, st[:, 2:3], gs_bc, ALU.mult, ALU.mult)
        tt("pool", h, h, sh_bc, ALU.add)

    def transpose_h(h, dst, dst_is_bf16, pbanks):
        for half in range(2):
            pp = pbanks[half]
            tr_group([(pp.ap[:, j * 128:(j + 1) * 128], h.ap[:, (half * 4 + j) * 128:(half * 4 + j + 1) * 128], ident.ap)
                      for j in range(4)], r=[h.key, ident.key], w=[pp.key])
            d = dst[:, half * 512:(half + 1) * 512]
            if half == 0:
                act(d, pp, AF.Copy)
            else:
                cp("dve", d, pp)

    stat_ring = Ring([T(small.ap[:, i * 4:(i + 1) * 4], ("stat", i)) for i in range(4)])

    for li in range(n_layers):
        mtype = li % 3
        jidx = li // 3
        last = li == n_layers - 1
        rope = mtype in (0, 1)

        P.barrier()
        alloc_state["off"] = PERSIST
        wqkv = a16("wqkv", 8 * 1536)
        wo = a16("wo", NH * D, parts=64)
        kT = a16("kT", NKV * NT_B * 128, parts=64)
        Vs = a16("Vs", NT_B * 512)
        modA = [a32("modA%d" % i, D) for i in range(3)]
        xts = Ring([a32("xt%d" % i, D) for i in range(3)])
        hs = Ring([a32("h%d" % i, D) for i in range(2)])
        hTs = Ring([a16("hT%d" % i, 8 * 128) for i in range(2)])
        q32 = a32("q32", D)
        qro = a32("qro", D) if rope else None
        qtmp = a32("qtmp", 512)
        qbf = [a16("qbf%d" % i, D) for i in range(2 if rope else 1)]
        qT = [[a16("qT%d_%d" % (sl, i), NH * 128, parts=64) for i in range(2 if rope else 1)] for sl in range(2)]
        k32s = [q32[:, i * 256:(i + 1) * 256] for i in range(2)]
        kros = [a32("kro_%d" % i, 256) if rope else None for i in range(2)]
        ktms = [qtmp[:, i * 256:(i + 1) * 256] for i in range(2)]
        kbs = [a16("kb%d" % i, 256) for i in range(2)]
        hsts = [a32("hst%d" % i, 64) for i in range(2)]
        pTs = Ring([a16("pT%d" % i, 512) for i in range(3)])
        if mtype == 2:
            sbS = Ring([a32("sbS%d" % i, 512) for i in range(2)])
            nat = Ring([a32("nat%d" % i, 512) for i in range(2)])
        den = a32("den", 512, parts=64)
        rden = a32("rden", 512, parts=64)
        oT = a16("oT", NH * 128, parts=64)
        ytmp = Ring([a32("ytmp%d" % i, 512) for i in range(1)])
        sink_s = a32("sink_s", NH, parts=64)
        hst = a32("hst", 64)
        print("attn SBUF KB", alloc_state["off"] / 256.0)

        dma("pool", wqkv.v(wqkv.ap.rearrange("p (k n) -> p k n", n=1536)),
            w_qkv[li].rearrange("(k p) n -> p k n", p=128))
        dma("pool", wo.v(wo.ap.rearrange("p (h n) -> p h n", n=D)), w_o[li].rearrange("(h p) n -> p h n", p=64))
        use_sink = mtype == 0
        memset("dve", Vs, 1.0)
        if use_sink:
            dma("sp", sink_s, sink_a[jidx:jidx + 1, :].partition_broadcast(64))
            act(sink_s, sink_s, AF.Exp)

        def load_modA(row):
            for i, c in enumerate((1, 0, 2)):
                dma("sp", modA[i], modd[li, row:row + 1, c * D:(c + 1) * D].partition_broadcast(128), r=[("modd", li)])

        def head_norm(src, nheads, g_bc, dst, scratch, hst):
            W = nheads * HD
            sq = scratch[:, 0:W]
            act(sq, src, AF.Square)
            ssum = hst[:, 0:nheads]
            red(ssum, sq.v(sq.ap.rearrange("p (h d) -> p h d", d=HD)), ALU.add)
            act(hst[:, 16:16 + nheads], ssum, AF.Ln, scale=1.0 / HD, bias=EPS)
            act(hst[:, 32:32 + nheads], hst[:, 16:16 + nheads], AF.Exp, scale=-0.5)
            rs_b = hst.v(hst.ap[:, 32:32 + nheads].unsqueeze(2).to_broadcast([128, nheads, HD]))
            d3 = dst.v(dst.ap.rearrange("p (h d) -> p h d", d=HD))
            s3 = src.v(src.ap.rearrange("p (h d) -> p h d", d=HD))
            tt("dve", d3, s3, rs_b, ALU.mult)
            g_b = g_bc.v(g_bc.ap.unsqueeze(1).to_broadcast([128, nheads, HD]))
            tt("pool", d3, d3, g_b, ALU.mult)

        def apply_rope(src, nheads, t, dst, qtmp):
            W = nheads * HD
            cs = cos_t.v(cos_t.ap[:, t * HD:(t + 1) * HD].unsqueeze(1).to_broadcast([128, nheads, HD]))
            s3 = src.v(src.ap.rearrange("p (h d) -> p h d", d=HD))
            d3 = dst.v(dst.ap.rearrange("p (h d) -> p h d", d=HD))
            t3 = qtmp.v(qtmp.ap[:, 0:W].rearrange("p (h d) -> p h d", d=HD))
            tt("dve", d3, s3, cs, ALU.mult)
            s5 = src.ap.rearrange("p (h a b d) -> p h a b d", a=2, b=2, d=16)
            t5 = qtmp.ap[:, 0:W].rearrange("p (h a b d) -> p h a b d", a=2, b=2, d=16)
            sn5 = sin_t.ap[:, t * HD:(t + 1) * HD].rearrange("p (a b d) -> p a b d", a=2, b=2, d=16)
            for bsel in range(2):
                o_ = T(t5[:, :, :, bsel, :], qtmp.key)
                i_ = T(s5[:, :, :, 1 - bsel, :], src.key)
                sn = T(sn5[:, :, bsel, :].unsqueeze(1).to_broadcast([128, nheads, 2, 16]), sin_t.key)
                tt("dve" if bsel == 0 else "pool", o_, i_, sn, ALU.mult)
            tt("pool", d3, d3, t3, ALU.add)

        def interleave(gens, width, stagger):
            active = []
            it = iter(gens)
            pending_start = 0
            done = False
            while True:
                if not done and len(active) < width and pending_start <= 0:
                    g_ = next(it, None)
                    if g_ is None:
                        done = True
                    else:
                        active.append(g_)
                        pending_start = stagger
                if not active:
                    if done:
                        break
                    pending_start = 0
                    continue
                pending_start -= 1
                for g_ in list(active):
                    try:
                        next(g_)
                    except StopIteration:
                        active.remove(g_)

        cur_gs_row = [None]
        cur_gt_row = [None]

        def need_gs(row):
            if cur_gs_row[0] != row:
                for i, c in enumerate((1, 0)):
                    dma("sp", modA[i], modd[li, row:row + 1, c * D:(c + 1) * D].partition_broadcast(128), r=[("modd", li)])
                cur_gs_row[0] = row

        def need_gt(row):
            if cur_gt_row[0] != row:
                dma("sp", modA[2], modd[li, row:row + 1, 2 * D:3 * D].partition_broadcast(128), r=[("modd", li)])
                cur_gt_row[0] = row

        def kv_gen(b, t, slot):
            is_ctx = t >= NT_L
            gt = b * NT_B + t
            pbase = 4 * slot
            need_gs(NB if is_ctx else b)
            xt = xts.next()
            dma("sp", xt, xs[gt * 128:(gt + 1) * 128, :], r=[("xs", gt)])
            hh_ = hs.next()
            st = stat_ring.next()
            norm_h(xt, hh_, modA[0], modA[1], st)
            yield
            hT = hTs.next()
            transpose_h(hh_, hT, True, (ps[pbase], ps[pbase + 1]))
            yield
            pk = ps[pbase + 2]
            mm_group(pk, [(hT.ap[:, kc * 128:(kc + 1) * 128], wqkv.ap[:, kc * 1536 + 1024:kc * 1536 + 1536])
                          for kc in range(8)], r=[hT.key, wqkv.key])
            yield
            k32, kro, ktm = k32s[slot], kros[slot], ktms[slot]
            ksrc = pk[:, 0:256]
            act(Vs.v(Vs.ap.rearrange("p (t g c) -> p t g c", g=NKV, c=128)[:, t, :, 0:64]),
                pk.v(pk.ap[:, 256:512].rearrange("p (g d) -> p g d", d=HD)), AF.Copy)
            if mtype == 1:
                head_norm(ksrc, NKV, gk_bc, k32, ktm, hsts[slot])
                ksrc = k32
                yield
            if rope and not is_ctx:
                if mtype != 1:
                    cp("dve", k32, ksrc)
                    ksrc = k32
                apply_rope(ksrc, NKV, t, kro, ktm)
                ksrc = kro
                yield
            kb = kbs[slot]
            act(kb, ksrc, AF.Copy)
            yield
            pb = psbf(pbase + 3)
            tr_group([(pb.ap[0:64, g * 128:(g + 1) * 128], kb.ap[:, g * HD:(g + 1) * HD], identb.ap) for g in range(NKV)],
                     r=[kb.key, identb.key], w=[pb.key])
            kTv = kT.v(kT.ap.rearrange("p (g n) -> p g n", g=NKV)[:, :, t * 128:(t + 1) * 128])
            cp("dve", kTv, pb.v(pb.ap[0:64, 0:512].rearrange("p (g n) -> p g n", g=NKV)))
            yield

        def q_prologue(b, t, qslot, info):
            is_ctx = t >= NT_L
            gt = b * NT_B + t
            need_gs(NB if is_ctx else b)
            xt = xts.next()
            info["xt"] = xt
            dma("sp", xt, xs[gt * 128:(gt + 1) * 128, :], r=[("xs", gt)])
            hh_ = hs.next()
            st = stat_ring.next()
            norm_h(xt, hh_, modA[0], modA[1], st)
            yield
            hT = hTs.next()
            transpose_h(hh_, hT, True, (ps[0], ps[1]))
            yield
            for half in range(2):
                mm_group(ps[2 + half], [(hT.ap[:, kc * 128:(kc + 1) * 128],
                                         wqkv.ap[:, kc * 1536 + half * 512:kc * 1536 + (half + 1) * 512])
                                        for kc in range(8)], r=[hT.key, wqkv.key])
            yield
            for half in range(2):
                if mtype == 1:
                    head_norm(ps[2 + half], 8, gq_bc, q32[:, half * 512:(half + 1) * 512], qtmp[:, 0:512], hst)
                else:
                    cp("dve", q32[:, half * 512:(half + 1) * 512], ps[2 + half])
                yield
            variants = [0]
            act(qbf[0], q32, AF.Copy, scale=0.125)
            if rope and not is_ctx:
                for half in range(2):
                    apply_rope(q32[:, half * 512:(half + 1) * 512], 8, t, qro[:, half * 512:(half + 1) * 512], qtmp[:, 0:512])
                    yield
                act(qbf[1], qro, AF.Copy, scale=0.125)
                variants = [0, 1]
            yield
            qTs_ = qT[qslot]
            for vi in variants:
                for hh in range(2):
                    pb = psbf(hh)
                    tr_group([(pb.ap[0:64, j * 128:(j + 1) * 128], qbf[vi].ap[:, (hh * 8 + j) * HD:(hh * 8 + j + 1) * HD],
                               identb.ap) for j in range(8)], r=[qbf[vi].key, identb.key], w=[pb.key])
                    if hh == 0:
                        cp("dve", qTs_[vi][:, hh * 1024:(hh + 1) * 1024], pb[0:64, :])
                    else:
                        act(qTs_[vi][:, hh * 1024:(hh + 1) * 1024], pb[0:64, :], AF.Copy)
                    yield

        def attend(b, t, qslot, info, nxt):
            is_ctx = t >= NT_L
            gt = b * NT_B + t
            xt = info["xt"]
            qTs_ = qT[qslot]
            need_gt(NB if is_ctx else b)
            if is_ctx:
                KL = [(NT_L + j, 0, None, None) for j in range(NT_C)]
            else:
                KL = []
                if mtype == 0:
                    for kt_ in (t - 1, t, t + 1):
                        if 0 <= kt_ < NT_L:
                            m = maskLo if kt_ == t - 1 else (maskHi if kt_ == t + 1 else None)
                            KL.append((kt_, 1, m, None))
                elif mtype == 1:
                    KL = [(kt_, 1, None, None) for kt_ in range(NT_L)]
                else:
                    for j, kt_ in enumerate(_na_keytiles(t)):
                        KL.append((kt_, 0, None, (_na_pattern(t), j)))
                KL += [(NT_L + j, 0, None, None) for j in range(NT_C)]
            nk = len(KL)
            items = [(g, ki) + KL[ki] for g in range(NKV) for ki in range(nk)]

            def emit_S(i):
                g, ki, kt_, vi, msk, natp = items[i]
                pS = ps[4 + (i % 2)]
                kslice = kT.ap[:, g * NT_B * 128 + kt_ * 128: g * NT_B * 128 + (kt_ + 1) * 128]
                mm_group(pS, [(kslice, qTs_[vi].ap[:, g * 512:(g + 1) * 512])], r=[kT.key, qTs_[vi].key])

            if OPT["S_AHEAD"]:
                emit_S(0)
            for i, (g, ki, kt_, vi, msk, natp) in enumerate(items):
                if not OPT["S_AHEAD"]:
                    emit_S(i)
                elif i + 1 < len(items):
                    emit_S(i + 1)
                pS = ps[4 + (i % 2)]
                pT = pTs.next()
                if natp is not None:
                    nb_ = nat.next()
                    dma("sp" if i % 2 == 0 else "act", nb_, c_natab[natp[0], natp[1], :, g * 512:(g + 1) * 512])
                    sS = sbS.next()
                    tt("dve", sS, pS, nb_, ALU.add)
                    act(pT, sS, AF.Exp)
                else:
                    act(pT, pS, AF.Exp)
                if msk is not None:
                    tt("pool", pT, pT, msk, ALU.mult)
                acc = ps[6 + (g % 2)]
                acca = acc.ap
                vsl = Vs.ap[:, kt_ * 512 + g * 128: kt_ * 512 + (g + 1) * 128]
                pTa = pT.ap
                first, lastk = (ki == 0), (ki == nk - 1)

                def pv(e, acca=acca, vsl=vsl, pTa=pTa, first=first, lastk=lastk):
                    return e.matmul(acca, lhsT=vsl, rhs=pTa, start=first, stop=lastk)
                P.add("pe", pv, [Vs.key, pT.key], [acc.key])
                if lastk:
                    if use_sink:
                        cp("dve", den, acc[64:128, :])
                        sk = sink_s.v(sink_s.ap[:, 4 * g:4 * g + 4].unsqueeze(2).to_broadcast([64, 4, 128]))
                        d3 = den.v(den.ap.rearrange("p (h q) -> p h q", q=128))
                        tt("dve", d3, d3, sk, ALU.add)
                        recip(rden, den)
                    else:
                        recip(rden, acc[64:128, :])
                    tt("dve", oT[:, g * 512:(g + 1) * 512], acc[0:64, :], rden, ALU.mult)
                if nxt is not None and i >= 1:
                    next(nxt, None)
            if nxt is not None:
                for _ in nxt:
                    pass
            for half in range(2):
                mm_group(ps[2 + half], [(oT.ap[:, hh * 128:(hh + 1) * 128], wo.ap[:, hh * D + half * 512: hh * D + (half + 1) * 512])
                                        for hh in range(NH)], r=[oT.key, wo.key])
                yt = ytmp.next()
                tt("dve", yt, ps[2 + half], modA[2][:, half * 512:(half + 1) * 512], ALU.mult)
                tt("pool", xt[:, half * 512:(half + 1) * 512], xt[:, half * 512:(half + 1) * 512], yt, ALU.add)
            dma("sp", xs[gt * 128:(gt + 1) * 128, :], xt, w=[("xs", gt)])

        for b in (range(NB) if not OPT["SKIP_ATT"] else []):
            interleave((kv_gen(b, t, t % 2) for t in range(NT_B)), OPT["W_KV"], 3)
            q_tiles = list(range(NT_L)) + ([] if last else list(range(NT_L, NT_B)))
            infos = [dict() for _ in q_tiles]
            g0 = q_prologue(b, q_tiles[0], 0, infos[0])
            for _ in g0:
                pass
            for qi, t in enumerate(q_tiles):
                nxt = None
                if qi + 1 < len(q_tiles) and OPT["Q_OVERLAP"]:
                    nxt = q_prologue(b, q_tiles[qi + 1], (qi + 1) % 2, infos[qi + 1])
                attend(b, t, qi % 2, infos[qi], nxt)
                if qi + 1 < len(q_tiles) and not OPT["Q_OVERLAP"]:
                    for _ in q_prologue(b, q_tiles[qi + 1], (qi + 1) % 2, infos[qi + 1]):
                        pass

        P.barrier()
        alloc_state["off"] = PERSIST
        wgs = [a16("wg%d" % i, 8 * FH) for i in range(2)]
        wus = [a16("wu%d" % i, 8 * FH) for i in range(2)]
        wds = [a16("wd%d" % i, 4 * D) for i in range(2)]
        xgs = [a16("xg%d" % i, (CAP // 128) * D) for i in range(2)]
        xeT = a16("xeT", 8 * CAP)
        HT = a16("HT", 4 * CAP)
        sgs = Ring([a32("sg%d" % i, CAP) for i in range(2)])
        Ysb = Ring([a32("Ysb%d" % i, D) for i in range(2)])
        xts = Ring([a32("mxt%d" % i, D) for i in range(2)])
        h = a32("mh", D)
        hbfs = Ring([a16("hbf%d" % i, D) for i in range(2)])
        hT32 = a32("hT32", 8 * 128)
        modF = [[a32("modF%d_%d" % (s, i), D) for i in range(2)] for s in range(2)]
        gtF = [a32("gtF%d" % s, D) for s in range(2)]
        y1s = Ring([a32("y1_%d" % i, D) for i in range(2)])
        y2s = Ring([a32("y2_%d" % i, D) for i in range(2)])
        gfin = a32("gfin", D) if last else None
        wrg = a32("wrg", 8 * 36)
        print("moe SBUF KB (before rl)", alloc_state["off"] / 256.0)
        rls = [a32("rl%d" % i, 512) for i in range(2)]
        hT32s = [hT32, a32("hT32b", 8 * 128)]
        mhs = [h, a32("mh2", D)]

        def mk_rl(si):
            rl = rls[si]

            def RL(name, off, n):
                return T(rl.ap[:, off:off + n], ("rl", si, name))
            d = dict(L=RL("L", 0, 36), mg=RL("mg", 40, 1), nmg=RL("nmg", 41, 1), eg=RL("eg", 44, 4), sg=RL("sg", 48, 1),
                     ptop=RL("ptop", 49, 1), ohg=RL("ohg", 52, 4), pen=RL("pen", 56, 4), Lm=RL("Lm", 64, 32),
                     mx8=RL("mx8", 96, 8), oh1=RL("oh1", 104, 32), oh2=RL("oh2", 136, 32), dd=RL("dd", 168, 1),
                     ed=RL("ed", 169, 1), rd=RL("rd", 170, 1), A=RL("A", 172, 32),
                     Abf=T(rl.ap[:, 204:220].bitcast(BF16), ("rl", si, "Abf")), slot=RL("slot", 224, 32),
                     tmpa=RL("tmpa", 256, 32), tmpb=RL("tmpb", 328, 32), d1f=RL("d1f", 288, 1), d2f=RL("d2f", 289, 1),
                     ov=RL("ov", 290, 1), nov=RL("nov", 291, 1), slotp=RL("slotp", 296, 32))
            return d
        RLS = [mk_rl(0), mk_rl(1)]

        m_tiles = []
        for b in range(NB):
            m_tiles += [(b * NT_B + t, b) for t in range(NT_L)]
        if not last:
            for b in range(NB):
                m_tiles += [(b * NT_B + NT_L + t, NB) for t in range(NT_C)]
        XGKEYS = [("XG", gt) for (gt, _) in m_tiles]

        dma("sp", wrg.v(wrg.ap.rearrange("p (k n) -> p k n", n=36)), w_rg[li].rearrange("(k p) n -> p k n", p=128))
        dma("sp", brg_bc, b_rg[li:li + 1, :].partition_broadcast(128))
        memset("dve", tot, 0.0)
        if last:
            dma("sp", gfin, g_final.partition_broadcast(128))

        m1_state = {"row": None, "mset": -1, "mf": None}

        def m1_gen(gt, row, si):
            R_ = RLS[si]
            pbase = 4 * si
            if row != m1_state["row"]:
                m1_state["mset"] += 1
                mf_ = modF[m1_state["mset"] % 2]
                dma("sp", mf_[0], modd[li, row:row + 1, 4 * D:5 * D].partition_broadcast(128), r=[("modd", li)])
                dma("sp", mf_[1], modd[li, row:row + 1, 3 * D:4 * D].partition_broadcast(128), r=[("modd", li)])
                m1_state["row"] = row
                m1_state["mf"] = mf_
            mf = m1_state["mf"]
            xt = xts.next()
            dma("sp", xt, xs[gt * 128:(gt + 1) * 128, :], r=[("xs", gt)])
            st = stat_ring.next()
            hh_ = mhs[si]
            norm_h(xt, hh_, mf[0], mf[1], st)
            yield
            hbf = hbfs.next()
            act(hbf, hh_, AF.Copy)
            hT32_ = hT32s[si]
            transpose_h(hh_, hT32_, False, (ps[pbase], ps[pbase + 1]))
            yield
            mm_group(ps[pbase + 2][:, 0:36], [(hT32_.ap[:, kc * 128:(kc + 1) * 128], wrg.ap[:, kc * 36:(kc + 1) * 36]) for kc in range(8)],
                     r=[hT32_.key, wrg.key])
            yield
            L, mg, nmg, eg, sg_, ptop, ohg, pen, Lm, mx8 = (R_[k] for k in ("L", "mg", "nmg", "eg", "sg", "ptop", "ohg", "pen", "Lm", "mx8"))
            oh1, oh2, dd, ed, rd, Asum, Abf, slot = (R_[k] for k in ("oh1", "oh2", "dd", "ed", "rd", "A", "Abf", "slot"))
            tt("dve", L, ps[pbase + 2][:, 0:36], brg_bc, ALU.add)
            red(mg, L[:, 0:4], ALU.max)
            yield
            ts("dve", nmg, mg, -1.0, ALU.mult)
            ts("dve", ohg, L[:, 0:4], mg, ALU.is_equal)
            yield
            act(eg, L[:, 0:4], AF.Exp, bias=nmg, accum=sg_)
            ts("dve", pen, ohg, -1.0, ALU.add, 1e30, ALU.mult)
            yield
            recip(ptop, sg_)
            tt("dve", Lm.v(Lm.ap.rearrange("p (g e) -> p g e", e=8)), L.v(L.ap[:, 4:36].rearrange("p (g e) -> p g e", e=8)),
               pen.v(pen.ap.unsqueeze(2).to_broadcast([128, 4, 8])), ALU.add)
            yield
            mxo, lmi = mx8.ap, Lm.ap
            P.add("dve", lambda e, mxo=mxo, lmi=lmi: e.max(out=mxo, in_=lmi), [Lm.key], [mx8.key])
            yield
            ts("dve", oh1, Lm, mx8[:, 0:1], ALU.is_equal)
            ts("dve", oh2, Lm, mx8[:, 1:2], ALU.is_equal)
            tt("dve", dd, mx8[:, 1:2], mx8[:, 0:1], ALU.subtract)
            yield
            act(ed, dd, AF.Exp)
            tt("dve", Asum, oh1, oh2, ALU.add)
            yield
            ts("dve", ed, ed, 1.0, ALU.add)
            cp("dve", Abf, Asum)
            yield
            recip(rd, ed)
            w1 = T(rt_w1.ap[:, gt:gt + 1], ("rt_w1", gt))
            w2 = T(rt_w2.ap[:, gt:gt + 1], ("rt_w2", gt))
            mm_group(ps[pbase + 3][:, 0:32], [(trib.ap, Abf.ap)], r=[trib.key, Abf.key])
            mm_group(ps[pbase + 3][:, 32:64], [(onesb.ap, Abf.ap)], r=[onesb.key, Abf.key])
            stt("dve", slot, ps[pbase + 3][:, 0:32], 0.0, tot, ALU.add, ALU.add)
            tt("dve", tot, tot, ps[pbase + 3][:, 32:64], ALU.add)
            yield
            tt("dve", w1, ptop, rd, ALU.mult)
            tt("dve", R_["slotp"], slot, ec_bc, ALU.add)
            yield
            tt("dve", w2, ptop, w1, ALU.subtract)
            tt("dve", R_["tmpa"], oh1, slot, ALU.mult)
            tt("dve", R_["tmpb"], oh2, slot, ALU.mult)
            yield
            ovs = []
            for ci, (oh, df, rtd, tmp) in enumerate(((oh1, R_["d1f"], rt_d1, R_["tmpa"]), (oh2, R_["d2f"], rt_d2, R_["tmpb"]))):
                ov = T(rls[si].ap[:, 400 + ci:401 + ci], ("rl", si, "ov%d" % ci))
                nov = T(rls[si].ap[:, 404 + ci:405 + ci], ("rl", si, "nov%d" % ci))
                red(ov, tmp, ALU.add)
                yield
                ts("dve", ov, ov, float(CAP), ALU.is_ge)
                tt("dve", tmp, oh, R_["slotp"], ALU.mult)
                yield
                ts("dve", nov, ov, -1.0, ALU.mult, 1.0, ALU.add)
                red(df, tmp, ALU.add)
                yield
                tt("dve", df, df, nov, ALU.mult)
                yield
                stt("dve", df, ov, float(NE * CAP), df, ALU.mult, ALU.add)
                yield
                rcol = T(rtd.ap[:, gt:gt + 1], (rtd.key, gt))
                cp("dve", rcol, df)
                yield
                ia = rcol.ap
                ha = hbf.ap
                P.add("pool", lambda e, ia=ia, ha=ha: e.indirect_dma_start(
                    out=XG, out_offset=bass.IndirectOffsetOnAxis(ap=ia, axis=0), in_=ha, in_offset=None),
                    [rcol.key, hbf.key], [("XG", gt)], dma=True)

        if not OPT["SKIP_M1"]:
            interleave((m1_gen(gt, row, i % 2) for i, (gt, row) in enumerate(m_tiles)), OPT["W_M1"], 9)

        P.barrier()
        stg = Ring([T(b_.ap, "stg%d" % i) for i, b_ in enumerate(y1s.items + y2s.items + modF[0] + modF[1])])
        NS = CAP // 128

        def prefetch(ex):
            wg, wu, wd, xg = wgs[ex % 2], wus[ex % 2], wds[ex % 2], xgs[ex % 2]
            dma("pool", xg.v(xg.ap.rearrange("p (s d) -> p s d", d=D)),
                XG[ex * CAP:(ex + 1) * CAP, :].rearrange("(s p) d -> p s d", p=128), r=XGKEYS)
            chunks = []
            for (wdst, wsrc, fw) in ((wg, w_gate[li, ex], FH), (wu, w_up[li, ex], FH), (wd, w_down[li, ex], D)):
                src3 = wsrc.rearrange("(k p) f -> p k f", p=128)
                kper = 1024 // fw
                for c in range(4):
                    chunks.append((wdst[:, c * 1024:(c + 1) * 1024], src3[:, c * kper:(c + 1) * kper, :], fw))
            bufs = []

            def issue(i):
                sb = stg.next()
                bufs.append(sb)
                dma("sp", sb.v(sb.ap.rearrange("p (k f) -> p k f", f=chunks[i][2])), chunks[i][1])
            AHEAD = 3
            for i in range(AHEAD):
                issue(i)
            yield
            for i in range(len(chunks)):
                if i + AHEAD < len(chunks):
                    issue(i + AHEAD)
                if i % 2 == 0:
                    act(chunks[i][0], bufs[i], AF.Copy)
                else:
                    cp("dve", chunks[i][0], bufs[i])
                yield

        for _ in prefetch(0):
            pass
        for ex in (range(NE) if not OPT["SKIP_M2"] else []):
            wg, wu, wd, xg = wgs[ex % 2], wus[ex % 2], wds[ex % 2], xgs[ex % 2]
            nxt = prefetch(ex + 1) if ex + 1 < NE else None

            def step(nxt=nxt):
                if nxt is not None:
                    next(nxt, None)
            for s in range(NS):
                pb = psbf(s % 2)
                tr_group([(pb.ap[:, kc * 128:(kc + 1) * 128], xg.ap[:, s * D + kc * 128: s * D + (kc + 1) * 128], identb.ap)
                          for kc in range(8)], r=[xg.key, identb.key], w=[pb.key])
                dst = xeT.v(xeT.ap.rearrange("p (k c) -> p k c", c=CAP)[:, :, s * 128:(s + 1) * 128])
                src = pb.v(pb.ap.rearrange("p (k c) -> p k c", c=128))
                if s % 2 == 0:
                    cp("dve", dst, src)
                else:
                    act(dst, src, AF.Copy)
                step()
            for m in range(4):
                mm_group(ps[2 + (m % 2)], [(wg.ap[:, kc * FH + m * 128: kc * FH + (m + 1) * 128], xeT.ap[:, kc * CAP:(kc + 1) * CAP])
                                           for kc in range(8)], r=[wg.key, xeT.key])
                mm_group(ps[4 + (m % 2)], [(wu.ap[:, kc * FH + m * 128: kc * FH + (m + 1) * 128], xeT.ap[:, kc * CAP:(kc + 1) * CAP])
                                           for kc in range(8)], r=[wu.key, xeT.key])
                sgt = sgs.next()
                act(sgt, ps[2 + (m % 2)], AF.Silu)
                tt("dve", HT[:, m * CAP:(m + 1) * CAP], sgt, ps[4 + (m % 2)], ALU.mult)
                step()
            for s in range(NS):
                ysb = Ysb.next()
                for half in range(2):
                    pp = ps[6 + half]
                    mm_group(pp, [(HT.ap[:, m * CAP + s * 128: m * CAP + (s + 1) * 128], wd.ap[:, m * D + half * 512: m * D + (half + 1) * 512])
                                  for m in range(4)], r=[HT.key, wd.key])
                    if half == 0:
                        act(ysb[:, 0:512], pp, AF.Copy)
                    else:
                        cp("dve", ysb[:, 512:1024], pp)
                dma("pool", YG[ex * CAP + s * 128: ex * CAP + (s + 1) * 128, :], ysb, w=[("YG", ex, s)])
                step()
            if nxt is not None:
                for _ in nxt:
                    pass
        P.barrier()

        YGKEYS = ["YG"] + [("YG", ex, s_) for ex in range(NE) for s_ in range(CAP // 128)]
        m3_state = {"row": None, "mset": -1, "gtf": None}

        def m3_gen(gt, row):
            if row != m3_state["row"]:
                m3_state["mset"] += 1
                gtf_ = gtF[m3_state["mset"] % 2]
                dma("sp", gtf_, modd[li, row:row + 1, 5 * D:6 * D].partition_broadcast(128), r=[("modd", li)])
                m3_state["row"] = row
                m3_state["gtf"] = gtf_
            gtf = m3_state["gtf"]
            y1, y2 = y1s.next(), y2s.next()
            for (yy, rtd) in ((y1, rt_d1), (y2, rt_d2)):
                ia = rtd.ap[:, gt:gt + 1]
                ya = yy.ap
                P.add("pool", lambda e, ia=ia, ya=ya: e.indirect_dma_start(
                    out=ya, out_offset=None, in_=YG, in_offset=bass.IndirectOffsetOnAxis(ap=ia, axis=0)),
                    [(rtd.key, gt)] + YGKEYS, [yy.key], dma=True)
            xt = xts.next()
            dma("sp", xt, xs[gt * 128:(gt + 1) * 128, :], r=[("xs", gt)])
            yield
            w1 = T(rt_w1.ap[:, gt:gt + 1], ("rt_w1", gt))
            w2 = T(rt_w2.ap[:, gt:gt + 1], ("rt_w2", gt))
            ts("dve", y1, y1, w1, ALU.mult)
            yield
            stt("dve", y1, y2, w2, y1, ALU.mult, ALU.add)
            yield
            tt("dve", y1, y1, gtf, ALU.mult)
            yield
            tt("dve", xt, xt, y1, ALU.add)
            yield
            if not last:
                dma("sp", xs[gt * 128:(gt + 1) * 128, :], xt, w=[("xs", gt)])
            else:
                st = stat_ring.next()
                stt("dve", y2, xt, 1.0 / D, xt, ALU.mult, ALU.mult, accum=st[:, 0:1])
                yield
                act(st[:, 1:2], st[:, 0:1], AF.Ln, bias=EPS)
                yield
                act(st[:, 2:3], st[:, 1:2], AF.Exp, scale=-0.5)
                yield
                stt("dve", y2, xt, st[:, 2:3], gfin, ALU.mult, ALU.mult)
                b_ = gt // NT_B
                t_ = gt % NT_B
                r0 = b_ * SEQ + t_ * 128
                dma("sp", out[r0:r0 + 128, :], y2)

        if not OPT["SKIP_M3"]:
            interleave((m3_gen(gt, row) for (gt, row) in m_tiles), OPT["W_M3"], 3)

    P.finalize()
    P.emit()
    return nc


_CACHE = {}


def _consts():
    if "c" not in _CACHE:
        cos, sin_s = _rope_tables()
        kk = np.arange(128)[:, None]
        qq = np.arange(128)[None, :]
        mlo = np.tile((kk >= qq).astype(np.float32), (1, 4))
        mhi = np.tile((kk <= qq).astype(np.float32), (1, 4))
        tri = (np.arange(128)[:, None] < np.arange(128)[None, :]).astype(np.float32)
        ec = (np.arange(NE, dtype=np.float32) * CAP).reshape(1, NE)
        _CACHE["c"] = dict(c_ident=np.eye(128, dtype=np.float32), c_cos=cos, c_sin=sin_s,
                           c_masks=np.stack([mlo, mhi]).astype(np.float32), c_tri=tri, c_ec=ec)
        _CACHE["naidx"] = _na_index_table()
    return _CACHE["c"], _CACHE["naidx"]


def make_in_maps(inputs, n_cores, NB):
    f = lambda a: np.ascontiguousarray(np.asarray(a, dtype=np.float32))
    consts, naidx = _consts()
    rpb = f(inputs["rpb_c"])[0].reshape(-1)
    rpb_ext = np.concatenate([rpb, np.array([NEG], dtype=np.float32)])
    natab = np.ascontiguousarray(rpb_ext[naidx].reshape(5, 5, 128, NH * 128))
    w_rg = np.ascontiguousarray(np.concatenate([f(inputs["w_group"]), f(inputs["w_router"])], axis=-1))
    b_rg = np.ascontiguousarray(np.concatenate([f(inputs["b_group"]), f(inputs["b_router"])], axis=-1))
    shared = dict(
        w_ada=f(inputs["w_ada"]), b_ada=f(inputs["b_ada"]), g_attn=f(inputs["g_attn"]), w_qkv=f(inputs["w_qkv"]),
        w_o=f(inputs["w_o"]), sink_a=f(inputs["sink_a"]), gq_b=f(inputs["gq_b"]), gk_b=f(inputs["gk_b"]),
        g_ffn=f(inputs["g_ffn"]), w_rg=w_rg, b_rg=b_rg, w_gate=f(inputs["w_gate"]), w_up=f(inputs["w_up"]),
        w_down=f(inputs["w_down"]), g_final=f(inputs["g_final"]).reshape(1, D), c_natab=natab, **consts)
    x = f(inputs["x"])
    ctx = f(inputs["ctx"])
    c = f(inputs["c"])
    c_ctx = f(inputs["c_ctx"]).reshape(1, D)
    maps = []
    for i in range(n_cores):
        sl = slice(i * NB, (i + 1) * NB)
        m = dict(shared)
        m["x"] = np.ascontiguousarray(x[sl].reshape(NB * SEQ, D))
        m["ctx"] = np.ascontiguousarray(ctx[sl].reshape(NB * CTX, D))
        m["cvec"] = np.ascontiguousarray(np.concatenate([c[sl], c_ctx], axis=0))
        maps.append(m)
    return maps


def kernel(**inputs):
    n_cores = 8
    NB = 2
    if "nc" not in _CACHE:
        _CACHE["nc"] = build(NB=NB)
    nc = _CACHE["nc"]
    maps = make_in_maps(inputs, n_cores, NB)
    res = run_bass_kernel_spmd(nc, maps, core_ids=list(range(n_cores)))
    outs = [r["out"].reshape(NB, SEQ, D) for r in res.results]
    return np.concatenate(outs, axis=0).astype(np.float32)
```

```python
import numpy as np
import concourse.bass as bass
import concourse.mybir as mybir
from concourse.bass_utils import run_bass_kernel_spmd

F32 = mybir.dt.float32
BF16 = mybir.dt.bfloat16
I32 = mybir.dt.int32
ALU = mybir.AluOpType
AF = mybir.ActivationFunctionType
AX = mybir.AxisListType

D = 1024
SEQ = 2048
CTX = 256
DEPTH = 4
NH = 16
NKV = 4
HD = 64
NE = 32
FH = 512
GRID_W = 64
import os as _os
OPT = dict(W_KV=int(_os.environ.get("W_KV", 2)), W_M1=int(_os.environ.get("W_M1", 2)), W_M3=int(_os.environ.get("W_M3", 2)),
           Q_OVERLAP=int(_os.environ.get("Q_OVERLAP", 1)), S_AHEAD=int(_os.environ.get("S_AHEAD", 1)),
           SINK_BC=int(_os.environ.get("SINK_BC", 1)), SKIP_ATT=int(_os.environ.get("SKIP_ATT", 0)),
           SKIP_M1=int(_os.environ.get("SKIP_M1", 0)), SKIP_M2=int(_os.environ.get("SKIP_M2", 0)), SKIP_M3=int(_os.environ.get("SKIP_M3", 0)))
CAP = 512
NT_L = SEQ // 128
NT_C = CTX // 128
NT_B = NT_L + NT_C
EPS = 1e-6
NEG = -1e30


class _Op:
    __slots__ = ("idx", "eng", "fn", "deps", "dma", "sig", "waits", "signal", "ksnap")

    def __init__(self, idx, eng, fn, dma):
        self.idx = idx
        self.eng = eng
        self.fn = fn
        self.deps = set()
        self.dma = dma
        self.sig = None
        self.waits = []
        self.signal = False
        self.ksnap = None


class Prog:
    ENGS = ("pe", "act", "dve", "pool", "sp")
    EPOCH = 20000
    NDMA = {"sp": 24, "pool": 24, "act": 8}

    def __init__(self, nc):
        self.nc = nc
        self.ops = []
        self.last_w = {}
        self.readers = {}
        self.pending = {}
        self.last_eng = {}
        self.dma_since = []

    def add(self, eng, fn, r=(), w=(), dma=False):
        op = _Op(len(self.ops), eng, fn, dma)
        ops = self.ops

        def same_eng(j):
            o = ops[j]
            return (not dma) and (not o.dma) and o.eng == eng

        for k in r:
            lw = self.last_w.get(k)
            if lw is not None and not (eng == "pe" and same_eng(lw)):
                op.deps.add(lw)
            if isinstance(k, str) and k.startswith("ps"):
                for rd in self.readers.get(k, ()):
                    if not same_eng(rd):
                        op.deps.add(rd)
        for k in w:
            lw = self.last_w.get(k)
            if lw is not None and not (eng == "pe" and same_eng(lw)):
                op.deps.add(lw)
            rs = self.readers.get(k)
            if rs:
                for rd in rs:
                    if not same_eng(rd):
                        op.deps.add(rd)
        for k in r:
            self.readers.setdefault(k, []).append(op.idx)
        for k in w:
            self.last_w[k] = op.idx
            self.readers[k] = []
        if eng in self.pending:
            op.deps.update(self.pending.pop(eng))
        op.deps.discard(op.idx)
        self.ops.append(op)
        if dma:
            self.dma_since.append(op.idx)
        else:
            self.last_eng[eng] = op.idx
        return op

    def barrier(self):
        deps = set(self.last_eng.values()) | set(self.dma_since)
        for e in self.ENGS:
            self.pending[e] = set(deps) | self.pending.get(e, set())
        self.dma_since = []

    def finalize(self, final_engine="sp"):
        ops = self.ops
        dma_rr = {q: 0 for q in self.NDMA}
        dma_last = {}
        dma_cnt = {}
        for op in ops:
            if op.dma:
                q = op.eng
                s = dma_rr[q] % self.NDMA[q]
                dma_rr[q] += 1
                key = ("dma", q, s)
                prev = dma_last.get(key)
                if prev is not None:
                    op.deps.add(prev)
                dma_last[key] = op.idx
                dma_cnt[key] = dma_cnt.get(key, 0) + 16
                op.sig = (key, dma_cnt[key])
        has_dep = [False] * len(ops)
        for op in ops:
            for d in op.deps:
                has_dep[d] = True
        cnt = {e: 0 for e in self.ENGS}
        know = {e: {} for e in self.ENGS}
        for op in ops:
            e = op.eng
            K = know[e]
            for d in sorted(op.deps):
                sk, sv = ops[d].sig
                if K.get(sk, 0) >= sv:
                    continue
                op.waits.append((sk, sv))
                K[sk] = sv
                for k2, v2 in ops[d].ksnap.items():
                    if K.get(k2, 0) < v2:
                        K[k2] = v2
            if op.dma:
                op.signal = True
            elif has_dep[op.idx]:
                cnt[e] += 1
                ep = cnt[e] // self.EPOCH
                op.sig = (("eng", e, ep), cnt[e] - ep * self.EPOCH + (1 if ep else 0))
                op.signal = True
            op.ksnap = dict(K)
        self.final_waits = []
        Kf = know[final_engine]
        for key, v in dma_cnt.items():
            if Kf.get(key, 0) < v:
                self.final_waits.append((key, v))
        self.semkeys = set(op.sig[0] for op in ops if op.signal)
        self.final_engine = final_engine

    def emit(self):
        nc = self.nc
        sems = {}
        for i, k in enumerate(sorted(self.semkeys, key=str)):
            sems[k] = nc.alloc_semaphore("s%d" % i)
        by_eng = {e: [] for e in self.ENGS}
        for op in self.ops:
            by_eng[op.eng].append(op)
        fin_eng = self.final_engine
        fin_waits = self.final_waits

        def run(eng_name, eng):
            for op in by_eng[eng_name]:
                for sk, sv in op.waits:
                    eng.wait_ge(sems[sk], sv)
                ins = op.fn(eng)
                if op.signal:
                    ins.then_inc(sems[op.sig[0]], 16 if op.dma else 1)
            if eng_name == fin_eng:
                for sk, sv in fin_waits:
                    eng.wait_ge(sems[sk], sv)

        with nc.Block() as block:
            @block.tensor
            def _(e):
                run("pe", e)

            @block.scalar
            def _(e):
                run("act", e)

            @block.vector
            def _(e):
                run("dve", e)

            @block.gpsimd
            def _(e):
                run("pool", e)

            @block.sync
            def _(e):
                run("sp", e)


class T:
    __slots__ = ("ap", "key")

    def __init__(self, ap, key):
        self.ap = ap
        self.key = key

    def __getitem__(self, idx):
        return T(self.ap[idx], self.key)

    def v(self, ap):
        return T(ap, self.key)


class Ring:
    def __init__(self, items):
        self.items = items
        self.i = 0

    def next(self):
        it = self.items[self.i % len(self.items)]
        self.i += 1
        return it


def _rope_tables():
    t = np.arange(SEQ)
    row = (t // GRID_W).astype(np.float32)
    col = (t % GRID_W).astype(np.float32)
    quarter = HD // 4
    inv = (np.float32(10000.0) ** (-np.arange(quarter, dtype=np.float32) / np.float32(quarter))).astype(np.float32)
    ar = row[:, None] * inv
    ac = col[:, None] * inv
    ang = np.concatenate([ar, ar, ac, ac], axis=-1).astype(np.float32)
    cos = np.cos(ang).astype(np.float32)
    sin = np.sin(ang).astype(np.float32)
    sgn = np.concatenate([-np.ones(16), np.ones(16), -np.ones(16), np.ones(16)]).astype(np.float32)
    return cos, (sin * sgn[None, :]).astype(np.float32)


NA_PAT_TILES = [0, 1, 2, 14, 15]


def _na_pattern(t):
    return {0: 0, 1: 1, 14: 3, 15: 4}.get(t, 2)


def _na_keytiles(t):
    if t <= 1:
        return [0, 1, 2, 3]
    if t >= 14:
        return [12, 13, 14, 15]
    return [t - 2, t - 1, t, t + 1, t + 2]


def _na_index_table():
    rows = SEQ // GRID_W
    wh, ww = 8, 16
    nrel = 15 * 31
    masked = NH * nrel
    tab = np.full((5, 5, 128, NH, 128), masked, dtype=np.int64)
    for pi, t in enumerate(NA_PAT_TILES):
        kts = _na_keytiles(t)
        q = t * 128 + np.arange(128)
        r = q // GRID_W
        c = q % GRID_W
        rs = np.clip(r - wh // 2, 0, rows - wh)
        cs = np.clip(c - ww // 2, 0, GRID_W - ww)
        for j, kt in enumerate(kts):
            k = kt * 128 + np.arange(128)
            kr = k // GRID_W
            kc = k % GRID_W
            inwin = ((kr[:, None] >= rs[None, :]) & (kr[:, None] < rs[None, :] + wh)
                     & (kc[:, None] >= cs[None, :]) & (kc[:, None] < cs[None, :] + ww))
            rel = (kr[:, None] - r[None, :] + 7) * 31 + (kc[:, None] - c[None, :] + 15)
            for h in range(NH):
                tab[pi, j, :, h, :] = np.where(inwin, h * nrel + rel, masked)
    return tab


def build(NB=2, n_layers=DEPTH):
    nc = bass.Bass("TRN2", target_bir_lowering=False)
    R = NB + 1
    NTOK = NB * NT_B * 128

    def din(name, shape, dt=F32):
        return nc.dram_tensor(name, list(shape), dt, kind="ExternalInput").ap()

    x_in = din("x", [NB * SEQ, D])
    ctx_in = din("ctx", [NB * CTX, D])
    cvec = din("cvec", [R, D])
    w_ada = din("w_ada", [DEPTH, D, 6 * D])
    b_ada = din("b_ada", [DEPTH, 6 * D])
    g_attn = din("g_attn", [DEPTH, D])
    w_qkv = din("w_qkv", [DEPTH, D, 1536])
    w_o = din("w_o", [DEPTH, D, D])
    sink_a = din("sink_a", [2, NH])
    gq_b = din("gq_b", [1, HD])
    gk_b = din("gk_b", [1, HD])
    g_ffn = din("g_ffn", [DEPTH, D])
    w_rg = din("w_rg", [DEPTH, D, 36])
    b_rg = din("b_rg", [DEPTH, 36])
    w_gate = din("w_gate", [DEPTH, NE, D, FH])
    w_up = din("w_up", [DEPTH, NE, D, FH])
    w_down = din("w_down", [DEPTH, NE, FH, D])
    g_final = din("g_final", [1, D])
    c_ident = din("c_ident", [128, 128])
    c_cos = din("c_cos", [SEQ, HD])
    c_sin = din("c_sin", [SEQ, HD])
    c_masks = din("c_masks", [2, 128, 512])
    c_tri = din("c_tri", [128, 128])
    c_ec = din("c_ec", [1, NE])
    c_natab = din("c_natab", [5, 5, 128, NH * 128])
    out = nc.dram_tensor("out", [NB * SEQ, D], F32, kind="ExternalOutput").ap()

    xs = nc.dram_tensor("xs", [NTOK, D], F32).ap()
    modd = nc.dram_tensor("modd", [DEPTH, R, 6 * D], F32).ap()
    XG = nc.dram_tensor("XG", [NE * CAP + 1, D], BF16).ap()
    YG = nc.dram_tensor("YG", [NE * CAP + 1, D], F32).ap()

    P = Prog(nc)

    POOL_KB = 190
    pool = nc.alloc_sbuf_tensor("pool", [128, POOL_KB * 256], F32)
    alloc_state = {"off": 0}

    def a32(name, n, parts=128):
        off = alloc_state["off"]
        alloc_state["off"] = off + n
        assert alloc_state["off"] <= POOL_KB * 256, (name, alloc_state["off"])
        return T(pool[0:parts, off:off + n], name)

    def a16(name, n, parts=128):
        n32 = (n + 1) // 2
        off = alloc_state["off"]
        alloc_state["off"] = off + n32
        assert alloc_state["off"] <= POOL_KB * 256, (name, alloc_state["off"])
        return T(pool[0:parts, off:off + n32].bitcast(BF16), name)

    ps = [T(nc.alloc_psum_tensor("ps%d" % i, [128, 512], F32)[:, :], "ps%d" % i) for i in range(8)]

    def psbf(i):
        return T(ps[i].ap.bitcast(BF16), ps[i].key)

    ident = a32("ident", 128)
    identb = a16("identb", 128)
    onesb = a16("onesb", 128)
    trib = a16("trib", 128)
    cos_t = a32("cos", NT_L * HD)
    sin_t = a32("sin", NT_L * HD)
    maskLo = a16("maskLo", 512)
    maskHi = a16("maskHi", 512)
    gq_bc = a32("gq_bc", HD)
    gk_bc = a32("gk_bc", HD)
    ec_bc = a32("ec_bc", NE)
    brg_bc = a32("brg_bc", 36)
    tot = a32("tot", NE)
    NTT = NB * NT_B
    rt_w1 = a32("rt_w1", NTT)
    rt_w2 = a32("rt_w2", NTT)
    rt_d1 = T(nc.alloc_sbuf_tensor("rt_d1", [128, NTT], I32)[:, :], "rt_d1")
    rt_d2 = T(nc.alloc_sbuf_tensor("rt_d2", [128, NTT], I32)[:, :], "rt_d2")
    small = a32("small", 64)
    PERSIST = alloc_state["off"]

    def dma(q, out_, in_, r=(), w=()):
        oa = out_.ap if isinstance(out_, T) else out_
        ia = in_.ap if isinstance(in_, T) else in_
        rr = list(r) + ([in_.key] if isinstance(in_, T) else [])
        ww = list(w) + ([out_.key] if isinstance(out_, T) else [])
        P.add(q, lambda e: e.dma_start(out=oa, in_=ia), rr, ww, dma=True)

    def act(out_, in_, func, scale=1.0, bias=0.0, accum=None, r=(), w=()):
        kw = {}
        rr = [in_.key] + list(r)
        ww = [out_.key] + list(w)
        if isinstance(bias, T):
            rr.append(bias.key)
            bias = bias.ap
        if isinstance(scale, T):
            rr.append(scale.key)
            scale = scale.ap
        if accum is not None:
            kw["accum_out"] = accum.ap
            ww.append(accum.key)
        oa, ia = out_.ap, in_.ap
        P.add("act", lambda e: e.activation(out=oa, in_=ia, func=func, bias=bias, scale=scale, **kw), rr, ww)

    def tt(eng, out_, a, b, op):
        oa, aa, ba = out_.ap, a.ap, b.ap
        P.add(eng, lambda e: e.tensor_tensor(out=oa, in0=aa, in1=ba, op=op), [a.key, b.key], [out_.key])

    def ts(eng, out_, a, s1, op0, s2=None, op1=None, accum=None):
        rr = [a.key]
        ww = [out_.key]
        if isinstance(s1, T):
            rr.append(s1.key)
            s1 = s1.ap
        if isinstance(s2, T):
            rr.append(s2.key)
            s2 = s2.ap
        kw = {}
        if op1 is not None:
            kw["op1"] = op1
        if accum is not None:
            kw["accum_out"] = accum.ap
            ww.append(accum.key)
        oa, aa = out_.ap, a.ap
        P.add(eng, lambda e: e.tensor_scalar(out=oa, in0=aa, scalar1=s1, scalar2=s2, op0=op0, **kw), rr, ww)

    def stt(eng, out_, a, s, b, op0, op1, accum=None):
        rr = [a.key, b.key]
        ww = [out_.key]
        if isinstance(s, T):
            rr.append(s.key)
            s = s.ap
        kw = {}
        if accum is not None:
            kw["accum_out"] = accum.ap
            ww.append(accum.key)
        oa, aa, ba = out_.ap, a.ap, b.ap
        P.add(eng, lambda e: e.scalar_tensor_tensor(out=oa, in0=aa, scalar=s, in1=ba, op0=op0, op1=op1, **kw), rr, ww)

    def cp(eng, out_, in_):
        oa, ia = out_.ap, in_.ap
        P.add(eng, lambda e: e.tensor_copy(out=oa, in_=ia), [in_.key], [out_.key])

    def recip(out_, in_):
        oa, ia = out_.ap, in_.ap
        P.add("dve", lambda e: e.reciprocal(out=oa, in_=ia), [in_.key], [out_.key])

    def red(out_, in_, op, axis=AX.X):
        oa, ia = out_.ap, in_.ap
        P.add("dve", lambda e: e.tensor_reduce(out=oa, in_=ia, axis=axis, op=op), [in_.key], [out_.key])

    def memset(eng, out_, val):
        oa = out_.ap
        P.add(eng, lambda e: e.memset(oa, val), [], [out_.key])

    def mm_group(out_, pairs, r=()):
        oa = out_.ap
        n = len(pairs)

        def fn(e):
            ins = None
            for i, (l, rh) in enumerate(pairs):
                ins = e.matmul(oa, lhsT=l, rhs=rh, start=(i == 0), stop=(i == n - 1))
            return ins
        P.add("pe", fn, list(r), [out_.key])

    def tr_group(items, r=(), w=()):
        def fn(e):
            ins = None
            for (o, i, idn) in items:
                ins = e.transpose(out=o, in_=i, identity=idn)
            return ins
        P.add("pe", fn, list(r), list(w))

    dma("sp", ident, c_ident)
    dma("pool", identb, c_ident)
    dma("pool", trib, c_tri)
    dma("sp", cos_t.v(cos_t.ap.rearrange("p (t d) -> p t d", d=HD)), c_cos.rearrange("(t p) d -> p t d", p=128))
    dma("sp", sin_t.v(sin_t.ap.rearrange("p (t d) -> p t d", d=HD)), c_sin.rearrange("(t p) d -> p t d", p=128))
    dma("pool", maskLo, c_masks[0])
    dma("pool", maskHi, c_masks[1])
    dma("sp", gq_bc, gq_b.partition_broadcast(128))
    dma("sp", gk_bc, gk_b.partition_broadcast(128))
    dma("sp", ec_bc, c_ec.partition_broadcast(128))
    memset("dve", onesb, 1.0)

    for b in range(NB):
        base = b * NT_B * 128
        dma("sp", xs[base:base + SEQ, :], x_in[b * SEQ:(b + 1) * SEQ, :], w=[("xs", b * NT_B + t) for t in range(NT_L)])
        dma("sp", xs[base + SEQ:base + SEQ + CTX, :], ctx_in[b * CTX:(b + 1) * CTX, :],
            w=[("xs", b * NT_B + NT_L + t) for t in range(NT_C)])

    alloc_state["off"] = PERSIST
    zrow = a32("zrow", D, parts=1)
    memset("dve", zrow, 0.0)
    dma("sp", YG[NE * CAP:NE * CAP + 1, :], zrow, w=["YG"])
    cv = a32("cv", D, parts=R)
    scT = a32("scT", 8 * R)
    wada = [a32("wada%d" % i, 8 * 512) for i in range(2)]
    modsb = a32("modsb", 6 * D, parts=R)
    bada = a32("bada", 6 * D, parts=R)
    gA = a32("gA", D, parts=R)
    gF = a32("gF", D, parts=R)
    dma("sp", cv, cvec)
    act(cv, cv, AF.Silu)
    tr_group([(ps[0].ap[:, kc * R:(kc + 1) * R], cv.ap[:, kc * 128:(kc + 1) * 128], ident.ap[0:R, 0:R]) for kc in range(8)],
             r=[cv.key, ident.key], w=[ps[0].key])
    cp("dve", scT, ps[0][:, 0:8 * R])
    for li in range(n_layers):
        dma("sp", bada, b_ada[li:li + 1, :].partition_broadcast(R))
        dma("sp", gA, g_attn[li:li + 1, :].partition_broadcast(R))
        dma("sp", gF, g_ffn[li:li + 1, :].partition_broadcast(R))
        for n in range(12):
            wt = wada[n % 2]
            dma("sp" if n % 2 == 0 else "act", wt.v(wt.ap.rearrange("p (k f) -> p k f", f=512)),
                w_ada[li, :, n * 512:(n + 1) * 512].rearrange("(k p) f -> p k f", p=128))
            pp = ps[1 + (n % 2)]
            mm_group(pp[0:R, :], [(scT.ap[:, kc * R:(kc + 1) * R], wt.ap[:, kc * 512:(kc + 1) * 512]) for kc in range(8)],
                     r=[scT.key, wt.key])
            tt("dve", modsb[:, n * 512:(n + 1) * 512], pp[0:R, :], bada[:, n * 512:(n + 1) * 512], ALU.add)
        stt("dve", modsb[:, D:2 * D], modsb[:, D:2 * D], 1.0, gA, ALU.add, ALU.mult)
        stt("dve", modsb[:, 4 * D:5 * D], modsb[:, 4 * D:5 * D], 1.0, gF, ALU.add, ALU.mult)
        dma("sp", modd[li], modsb, w=[("modd", li)])

    def norm_h(xt, h, gs_bc, sh_bc, st):
        stt("dve", h, xt, 1.0 / D, xt, ALU.mult, ALU.mult, accum=st[:, 0:1])
        act(st[:, 1:2], st[:, 0:1], AF.Ln, bias=EPS)
        act(st[:, 2:3], st[:, 1:2], AF.Exp, scale=-0.5)
        stt("dve", h, xt, st[:, 2:3], gs_bc, ALU.mult, ALU.mult)
        tt("pool", h, h, sh_bc, ALU.add)

    def transpose_h(h, dst, dst_is_bf16, pbanks):
        for half in range(2):
            pp = pbanks[half]
            tr_group([(pp.ap[:, j * 128:(j + 1) * 128], h.ap[:, (half * 4 + j) * 128:(half * 4 + j + 1) * 128], ident.ap)
                      for j in range(4)], r=[h.key, ident.key], w=[pp.key])
            d = dst[:, half * 512:(half + 1) * 512]
            if half == 0:
                act(d, pp, AF.Copy)
            else:
                cp("dve", d, pp)

    stat_ring = Ring([T(small.ap[:, i * 4:(i + 1) * 4], ("stat", i)) for i in range(4)])

    for li in range(n_layers):
        mtype = li % 3
        jidx = li // 3
        last = li == n_layers - 1
        rope = mtype in (0, 1)

        P.barrier()
        alloc_state["off"] = PERSIST
        wqkv = a16("wqkv", 8 * 1536)
        wo = a16("wo", NH * D, parts=64)
        kT = a16("kT", NKV * NT_B * 128, parts=64)
        Vs = a16("Vs", NT_B * 512)
        modA = [a32("modA%d" % i, D) for i in range(3)]
        xts = Ring([a32("xt%d" % i, D) for i in range(3)])
        hs = Ring([a32("h%d" % i, D) for i in range(2)])
        hTs = Ring([a16("hT%d" % i, 8 * 128) for i in range(2)])
        q32 = a32("q32", D)
        qro = a32("qro", D) if rope else None
        qtmp = a32("qtmp", 512)
        qbf = [a16("qbf%d" % i, D) for i in range(2 if rope else 1)]
        qT = [[a16("qT%d_%d" % (sl, i), NH * 128, parts=64) for i in range(2 if rope else 1)] for sl in range(2)]
        k32s = [q32[:, i * 256:(i + 1) * 256] for i in range(2)]
        kros = [a32("kro_%d" % i, 256) if rope else None for i in range(2)]
        ktms = [qtmp[:, i * 256:(i + 1) * 256] for i in range(2)]
        kbs = [a16("kb%d" % i, 256) for i in range(2)]
        hsts = [a32("hst%d" % i, 64) for i in range(2)]
        pTs = Ring([a16("pT%d" % i, 512) for i in range(3)])
        if mtype == 2:
            sbS = Ring([a32("sbS%d" % i, 512) for i in range(2)])
            nat = Ring([a32("nat%d" % i, 512) for i in range(2)])
        den = a32("den", 512, parts=64)
        rden = a32("rden", 512, parts=64)
        oT = a16("oT", NH * 128, parts=64)
        ytmp = Ring([a32("ytmp%d" % i, 512) for i in range(1)])
        sink_s = a32("sink_s", NH, parts=64)
        hst = a32("hst", 64)
        print("attn SBUF KB", alloc_state["off"] / 256.0)

        wq3 = wqkv.ap.rearrange("p (k n) -> p k n", n=1536)
        wsrc3 = w_qkv[li].rearrange("(k p) n -> p k n", p=128)
        dma("pool", T(wq3[:, :, 1024:1536], "wqkv_kv"), wsrc3[:, :, 1024:1536])
        dma("pool", T(wq3[:, :, 0:1024], "wqkv_q"), wsrc3[:, :, 0:1024])
        dma("pool", wo.v(wo.ap.rearrange("p (h n) -> p h n", n=D)), w_o[li].rearrange("(h p) n -> p h n", p=64))
        use_sink = mtype == 0
        memset("dve", Vs, 1.0)
        if use_sink:
            dma("sp", sink_s, sink_a[jidx:jidx + 1, :].partition_broadcast(64))
            act(sink_s, sink_s, AF.Exp)

        def load_modA(row):
            for i, c in enumerate((1, 0, 2)):
                dma("sp", modA[i], modd[li, row:row + 1, c * D:(c + 1) * D].partition_broadcast(128), r=[("modd", li)])

        def head_norm(src, nheads, g_bc, dst, scratch, hst):
            W = nheads * HD
            sq = scratch[:, 0:W]
            act(sq, src, AF.Square)
            ssum = hst[:, 0:nheads]
            red(ssum, sq.v(sq.ap.rearrange("p (h d) -> p h d", d=HD)), ALU.add)
            act(hst[:, 16:16 + nheads], ssum, AF.Ln, scale=1.0 / HD, bias=EPS)
            act(hst[:, 32:32 + nheads], hst[:, 16:16 + nheads], AF.Exp, scale=-0.5)
            rs_b = hst.v(hst.ap[:, 32:32 + nheads].unsqueeze(2).to_broadcast([128, nheads, HD]))
            d3 = dst.v(dst.ap.rearrange("p (h d) -> p h d", d=HD))
            s3 = src.v(src.ap.rearrange("p (h d) -> p h d", d=HD))
            tt("dve", d3, s3, rs_b, ALU.mult)
            g_b = g_bc.v(g_bc.ap.unsqueeze(1).to_broadcast([128, nheads, HD]))
            tt("pool", d3, d3, g_b, ALU.mult)

        def apply_rope(src, nheads, t, dst, qtmp):
            W = nheads * HD
            cs = cos_t.v(cos_t.ap[:, t * HD:(t + 1) * HD].unsqueeze(1).to_broadcast([128, nheads, HD]))
            s3 = src.v(src.ap.rearrange("p (h d) -> p h d", d=HD))
            d3 = dst.v(dst.ap.rearrange("p (h d) -> p h d", d=HD))
            t3 = qtmp.v(qtmp.ap[:, 0:W].rearrange("p (h d) -> p h d", d=HD))
            tt("dve", d3, s3, cs, ALU.mult)
            s5 = src.ap.rearrange("p (h a b d) -> p h a b d", a=2, b=2, d=16)
            t5 = qtmp.ap[:, 0:W].rearrange("p (h a b d) -> p h a b d", a=2, b=2, d=16)
            sn5 = sin_t.ap[:, t * HD:(t + 1) * HD].rearrange("p (a b d) -> p a b d", a=2, b=2, d=16)
            for bsel in range(2):
                o_ = T(t5[:, :, :, bsel, :], qtmp.key)
                i_ = T(s5[:, :, :, 1 - bsel, :], src.key)
                sn = T(sn5[:, :, bsel, :].unsqueeze(1).to_broadcast([128, nheads, 2, 16]), sin_t.key)
                tt("dve" if bsel == 0 else "pool", o_, i_, sn, ALU.mult)
            tt("pool", d3, d3, t3, ALU.add)

        def interleave(gens, width, stagger):
            active = []
            it = iter(gens)
            pending_start = 0
            done = False
            while True:
                if not done and len(active) < width and pending_start <= 0:
                    g_ = next(it, None)
                    if g_ is None:
                        done = True
                    else:
                        active.append(g_)
                        pending_start = stagger
                if not active:
                    if done:
                        break
                    pending_start = 0
                    continue
                pending_start -= 1
                for g_ in list(active):
                    try:
                        next(g_)
                    except StopIteration:
                        active.remove(g_)

        cur_gs_row = [None]
        cur_gt_row = [None]

        def need_gs(row):
            if cur_gs_row[0] != row:
                for i, c in enumerate((1, 0)):
                    dma("sp", modA[i], modd[li, row:row + 1, c * D:(c + 1) * D].partition_broadcast(128), r=[("modd", li)])
                cur_gs_row[0] = row

        def need_gt(row):
            if cur_gt_row[0] != row:
                dma("sp", modA[2], modd[li, row:row + 1, 2 * D:3 * D].partition_broadcast(128), r=[("modd", li)])
                cur_gt_row[0] = row

        def kv_gen(b, t, slot):
            is_ctx = t >= NT_L
            gt = b * NT_B + t
            pbase = 4 * slot
            need_gs(NB if is_ctx else b)
            xt = xts.next()
            dma("sp", xt, xs[gt * 128:(gt + 1) * 128, :], r=[("xs", gt)])
            hh_ = hs.next()
            st = stat_ring.next()
            norm_h(xt, hh_, modA[0], modA[1], st)
            yield
            hT = hTs.next()
            transpose_h(hh_, hT, True, (ps[pbase], ps[pbase + 1]))
            yield
            pk = ps[pbase + 2]
            mm_group(pk, [(hT.ap[:, kc * 128:(kc + 1) * 128], wqkv.ap[:, kc * 1536 + 1024:kc * 1536 + 1536])
                          for kc in range(8)], r=[hT.key, "wqkv_kv"])
            yield
            k32, kro, ktm = k32s[slot], kros[slot], ktms[slot]
            ksrc = pk[:, 0:256]
            act(Vs.v(Vs.ap.rearrange("p (t g c) -> p t g c", g=NKV, c=128)[:, t, :, 0:64]),
                pk.v(pk.ap[:, 256:512].rearrange("p (g d) -> p g d", d=HD)), AF.Copy)
            if mtype == 1:
                head_norm(ksrc, NKV, gk_bc, k32, ktm, hsts[slot])
                ksrc = k32
                yield
            if rope and not is_ctx:
                if mtype != 1:
                    cp("dve", k32, ksrc)
                    ksrc = k32
                apply_rope(ksrc, NKV, t, kro, ktm)
                ksrc = kro
                yield
            kb = kbs[slot]
            act(kb, ksrc, AF.Copy)
            yield
            pb = psbf(pbase + 3)
            tr_group([(pb.ap[0:64, g * 128:(g + 1) * 128], kb.ap[:, g * HD:(g + 1) * HD], identb.ap) for g in range(NKV)],
                     r=[kb.key, identb.key], w=[pb.key])
            kTv = kT.v(kT.ap.rearrange("p (g n) -> p g n", g=NKV)[:, :, t * 128:(t + 1) * 128])
            cp("dve", kTv, pb.v(pb.ap[0:64, 0:512].rearrange("p (g n) -> p g n", g=NKV)))
            yield

        def q_prologue(b, t, qslot, info):
            is_ctx = t >= NT_L
            gt = b * NT_B + t
            need_gs(NB if is_ctx else b)
            xt = xts.next()
            info["xt"] = xt
            dma("sp", xt, xs[gt * 128:(gt + 1) * 128, :], r=[("xs", gt)])
            hh_ = hs.next()
            st = stat_ring.next()
            norm_h(xt, hh_, modA[0], modA[1], st)
            yield
            hT = hTs.next()
            transpose_h(hh_, hT, True, (ps[0], ps[1]))
            yield
            for half in range(2):
                mm_group(ps[2 + half], [(hT.ap[:, kc * 128:(kc + 1) * 128],
                                         wqkv.ap[:, kc * 1536 + half * 512:kc * 1536 + (half + 1) * 512])
                                        for kc in range(8)], r=[hT.key, "wqkv_q"])
            yield
            for half in range(2):
                if mtype == 1:
                    head_norm(ps[2 + half], 8, gq_bc, q32[:, half * 512:(half + 1) * 512], qtmp[:, 0:512], hst)
                else:
                    cp("dve", q32[:, half * 512:(half + 1) * 512], ps[2 + half])
                yield
            variants = [0]
            act(qbf[0], q32, AF.Copy, scale=0.125)
            if rope and not is_ctx:
                for half in range(2):
                    apply_rope(q32[:, half * 512:(half + 1) * 512], 8, t, qro[:, half * 512:(half + 1) * 512], qtmp[:, 0:512])
                    yield
                act(qbf[1], qro, AF.Copy, scale=0.125)
                variants = [0, 1]
            yield
            qTs_ = qT[qslot]
            for vi in variants:
                for hh in range(2):
                    pb = psbf(hh)
                    tr_group([(pb.ap[0:64, j * 128:(j + 1) * 128], qbf[vi].ap[:, (hh * 8 + j) * HD:(hh * 8 + j + 1) * HD],
                               identb.ap) for j in range(8)], r=[qbf[vi].key, identb.key], w=[pb.key])
                    if hh == 0:
                        cp("dve", qTs_[vi][:, hh * 1024:(hh + 1) * 1024], pb[0:64, :])
                    else:
                        act(qTs_[vi][:, hh * 1024:(hh + 1) * 1024], pb[0:64, :], AF.Copy)
                    yield

        def attend(b, t, qslot, info, nxt):
            is_ctx = t >= NT_L
            gt = b * NT_B + t
            xt = info["xt"]
            qTs_ = qT[qslot]
            need_gt(NB if is_ctx else b)
            if is_ctx:
                KL = [(NT_L + j, 0, None, None) for j in range(NT_C)]
            else:
                KL = []
                if mtype == 0:
                    for kt_ in (t - 1, t, t + 1):
                        if 0 <= kt_ < NT_L:
                            m = maskLo if kt_ == t - 1 else (maskHi if kt_ == t + 1 else None)
                            KL.append((kt_, 1, m, None))
                elif mtype == 1:
                    KL = [(kt_, 1, None, None) for kt_ in range(NT_L)]
                else:
                    for j, kt_ in enumerate(_na_keytiles(t)):
                        KL.append((kt_, 0, None, (_na_pattern(t), j)))
                KL += [(NT_L + j, 0, None, None) for j in range(NT_C)]
            nk = len(KL)
            items = [(g, ki) + KL[ki] for g in range(NKV) for ki in range(nk)]

            def emit_S(i):
                g, ki, kt_, vi, msk, natp = items[i]
                pS = ps[4 + (i % 2)]
                kslice = kT.ap[:, g * NT_B * 128 + kt_ * 128: g * NT_B * 128 + (kt_ + 1) * 128]
                mm_group(pS, [(kslice, qTs_[vi].ap[:, g * 512:(g + 1) * 512])], r=[kT.key, qTs_[vi].key])

            if OPT["S_AHEAD"]:
                emit_S(0)
            for i, (g, ki, kt_, vi, msk, natp) in enumerate(items):
                if not OPT["S_AHEAD"]:
                    emit_S(i)
                elif i + 1 < len(items):
                    emit_S(i + 1)
                pS = ps[4 + (i % 2)]
                pT = pTs.next()
                if natp is not None:
                    nb_ = nat.next()
                    dma("sp" if i % 2 == 0 else "act", nb_, c_natab[natp[0], natp[1], :, g * 512:(g + 1) * 512])
                    sS = sbS.next()
                    tt("dve", sS, pS, nb_, ALU.add)
                    act(pT, sS, AF.Exp)
                else:
                    act(pT, pS, AF.Exp)
                if msk is not None:
                    tt("pool", pT, pT, msk, ALU.mult)
                acc = ps[6 + (g % 2)]
                acca = acc.ap
                vsl = Vs.ap[:, kt_ * 512 + g * 128: kt_ * 512 + (g + 1) * 128]
                pTa = pT.ap
                first, lastk = (ki == 0), (ki == nk - 1)

                def pv(e, acca=acca, vsl=vsl, pTa=pTa, first=first, lastk=lastk):
                    return e.matmul(acca, lhsT=vsl, rhs=pTa, start=first, stop=lastk)
                P.add("pe", pv, [Vs.key, pT.key], [acc.key])
                if lastk:
                    if use_sink:
                        cp("dve", den, acc[64:128, :])
                        sk = sink_s.v(sink_s.ap[:, 4 * g:4 * g + 4].unsqueeze(2).to_broadcast([64, 4, 128]))
                        d3 = den.v(den.ap.rearrange("p (h q) -> p h q", q=128))
                        tt("dve", d3, d3, sk, ALU.add)
                        recip(rden, den)
                    else:
                        recip(rden, acc[64:128, :])
                    tt("dve", oT[:, g * 512:(g + 1) * 512], acc[0:64, :], rden, ALU.mult)
                if nxt is not None and i >= 1:
                    next(nxt, None)
            if nxt is not None:
                for _ in nxt:
                    pass
            for half in range(2):
                mm_group(ps[2 + half], [(oT.ap[:, hh * 128:(hh + 1) * 128], wo.ap[:, hh * D + half * 512: hh * D + (half + 1) * 512])
                                        for hh in range(NH)], r=[oT.key, wo.key])
                yt = ytmp.next()
                tt("dve", yt, ps[2 + half], modA[2][:, half * 512:(half + 1) * 512], ALU.mult)
                tt("pool", xt[:, half * 512:(half + 1) * 512], xt[:, half * 512:(half + 1) * 512], yt, ALU.add)
            dma("sp", xs[gt * 128:(gt + 1) * 128, :], xt, w=[("xs", gt)])

        for b in (range(NB) if not OPT["SKIP_ATT"] else []):
            interleave((kv_gen(b, t, t % 2) for t in range(NT_B)), OPT["W_KV"], 3)
            q_tiles = list(range(NT_L)) + ([] if last else list(range(NT_L, NT_B)))
            infos = [dict() for _ in q_tiles]
            g0 = q_prologue(b, q_tiles[0], 0, infos[0])
            for _ in g0:
                pass
            for qi, t in enumerate(q_tiles):
                nxt = None
                if qi + 1 < len(q_tiles) and OPT["Q_OVERLAP"]:
                    nxt = q_prologue(b, q_tiles[qi + 1], (qi + 1) % 2, infos[qi + 1])
                attend(b, t, qi % 2, infos[qi], nxt)
                if qi + 1 < len(q_tiles) and not OPT["Q_OVERLAP"]:
                    for _ in q_prologue(b, q_tiles[qi + 1], (qi + 1) % 2, infos[qi + 1]):
                        pass

        P.barrier()
        alloc_state["off"] = PERSIST
        wgs = [a16("wg%d" % i, 8 * FH) for i in range(2)]
        wus = [a16("wu%d" % i, 8 * FH) for i in range(2)]
        wds = [a16("wd%d" % i, 4 * D) for i in range(2)]
        xgs = [a16("xg%d" % i, (CAP // 128) * D) for i in range(2)]
        xeT = a16("xeT", 8 * CAP)
        HT = a16("HT", 4 * CAP)
        sgs = Ring([a32("sg%d" % i, CAP) for i in range(2)])
        Ysb = Ring([a32("Ysb%d" % i, D) for i in range(2)])
        xts = Ring([a32("mxt%d" % i, D) for i in range(2)])
        h = a32("mh", D)
        hbfs = Ring([a16("hbf%d" % i, D) for i in range(2)])
        hT32 = a32("hT32", 8 * 128)
        modF = [[a32("modF%d_%d" % (s, i), D) for i in range(2)] for s in range(2)]
        gtF = [a32("gtF%d" % s, D) for s in range(2)]
        y1s = Ring([a32("y1_%d" % i, D) for i in range(2)])
        y2s = Ring([a32("y2_%d" % i, D) for i in range(2)])
        gfin = a32("gfin", D) if last else None
        wrg = a32("wrg", 8 * 36)
        print("moe SBUF KB (before rl)", alloc_state["off"] / 256.0)
        rls = [a32("rl%d" % i, 512) for i in range(2)]
        hT32s = [hT32, a32("hT32b", 8 * 128)]
        mhs = [h, a32("mh2", D)]

        def mk_rl(si):
            rl = rls[si]

            def RL(name, off, n):
                return T(rl.ap[:, off:off + n], ("rl", si, name))
            d = dict(L=RL("L", 0, 36), mg=RL("mg", 40, 1), nmg=RL("nmg", 41, 1), eg=RL("eg", 44, 4), sg=RL("sg", 48, 1),
                     ptop=RL("ptop", 49, 1), ohg=RL("ohg", 52, 4), pen=RL("pen", 56, 4), Lm=RL("Lm", 64, 32),
                     mx8=RL("mx8", 96, 8), oh1=RL("oh1", 104, 32), oh2=RL("oh2", 136, 32), dd=RL("dd", 168, 1),
                     ed=RL("ed", 169, 1), rd=RL("rd", 170, 1), A=RL("A", 172, 32),
                     Abf=T(rl.ap[:, 204:220].bitcast(BF16), ("rl", si, "Abf")), slot=RL("slot", 224, 32),
                     tmpa=RL("tmpa", 256, 32), tmpb=RL("tmpb", 328, 32), d1f=RL("d1f", 288, 1), d2f=RL("d2f", 289, 1),
                     ov=RL("ov", 290, 1), nov=RL("nov", 291, 1), slotp=RL("slotp", 296, 32))
            return d
        RLS = [mk_rl(0), mk_rl(1)]

        m_tiles = []
        for b in range(NB):
            m_tiles += [(b * NT_B + t, b) for t in range(NT_L)]
        if not last:
            for b in range(NB):
                m_tiles += [(b * NT_B + NT_L + t, NB) for t in range(NT_C)]
        XGKEYS = [("XG", gt) for (gt, _) in m_tiles]

        dma("sp", wrg.v(wrg.ap.rearrange("p (k n) -> p k n", n=36)), w_rg[li].rearrange("(k p) n -> p k n", p=128))
        dma("sp", brg_bc, b_rg[li:li + 1, :].partition_broadcast(128))
        memset("dve", tot, 0.0)
        if last:
            dma("sp", gfin, g_final.partition_broadcast(128))

        m1_state = {"row": None, "mset": -1, "mf": None}

        def m1_gen(gt, row, si):
            R_ = RLS[si]
            pbase = 4 * si
            if row != m1_state["row"]:
                m1_state["mset"] += 1
                mf_ = modF[m1_state["mset"] % 2]
                dma("sp", mf_[0], modd[li, row:row + 1, 4 * D:5 * D].partition_broadcast(128), r=[("modd", li)])
                dma("sp", mf_[1], modd[li, row:row + 1, 3 * D:4 * D].partition_broadcast(128), r=[("modd", li)])
                m1_state["row"] = row
                m1_state["mf"] = mf_
            mf = m1_state["mf"]
            xt = xts.next()
            dma("sp", xt, xs[gt * 128:(gt + 1) * 128, :], r=[("xs", gt)])
            st = stat_ring.next()
            hh_ = mhs[si]
            norm_h(xt, hh_, mf[0], mf[1], st)
            yield
            hbf = hbfs.next()
            act(hbf, hh_, AF.Copy)
            hT32_ = hT32s[si]
            transpose_h(hh_, hT32_, False, (ps[pbase], ps[pbase + 1]))
            yield
            mm_group(ps[pbase + 2][:, 0:36], [(hT32_.ap[:, kc * 128:(kc + 1) * 128], wrg.ap[:, kc * 36:(kc + 1) * 36]) for kc in range(8)],
                     r=[hT32_.key, wrg.key])
            yield
            L, mg, nmg, eg, sg_, ptop, ohg, pen, Lm, mx8 = (R_[k] for k in ("L", "mg", "nmg", "eg", "sg", "ptop", "ohg", "pen", "Lm", "mx8"))
            oh1, oh2, dd, ed, rd, Asum, Abf, slot = (R_[k] for k in ("oh1", "oh2", "dd", "ed", "rd", "A", "Abf", "slot"))
            tt("dve", L, ps[pbase + 2][:, 0:36], brg_bc, ALU.add)
            red(mg, L[:, 0:4], ALU.max)
            yield
            ts("dve", nmg, mg, -1.0, ALU.mult)
            ts("dve", ohg, L[:, 0:4], mg, ALU.is_equal)
            yield
            act(eg, L[:, 0:4], AF.Exp, bias=nmg, accum=sg_)
            ts("dve", pen, ohg, -1.0, ALU.add, 1e30, ALU.mult)
            yield
            recip(ptop, sg_)
            tt("dve", Lm.v(Lm.ap.rearrange("p (g e) -> p g e", e=8)), L.v(L.ap[:, 4:36].rearrange("p (g e) -> p g e", e=8)),
               pen.v(pen.ap.unsqueeze(2).to_broadcast([128, 4, 8])), ALU.add)
            yield
            mxo, lmi = mx8.ap, Lm.ap
            P.add("dve", lambda e, mxo=mxo, lmi=lmi: e.max(out=mxo, in_=lmi), [Lm.key], [mx8.key])
            yield
            ts("dve", oh1, Lm, mx8[:, 0:1], ALU.is_equal)
            ts("dve", oh2, Lm, mx8[:, 1:2], ALU.is_equal)
            tt("dve", dd, mx8[:, 1:2], mx8[:, 0:1], ALU.subtract)
            yield
            act(ed, dd, AF.Exp)
            tt("dve", Asum, oh1, oh2, ALU.add)
            yield
            ts("dve", ed, ed, 1.0, ALU.add)
            cp("dve", Abf, Asum)
            yield
            recip(rd, ed)
            w1 = T(rt_w1.ap[:, gt:gt + 1], ("rt_w1", gt))
            w2 = T(rt_w2.ap[:, gt:gt + 1], ("rt_w2", gt))
            mm_group(ps[pbase + 3][:, 0:32], [(trib.ap, Abf.ap)], r=[trib.key, Abf.key])
            mm_group(ps[pbase + 3][:, 32:64], [(onesb.ap, Abf.ap)], r=[onesb.key, Abf.key])
            stt("dve", slot, ps[pbase + 3][:, 0:32], 0.0, tot, ALU.add, ALU.add)
            tt("dve", tot, tot, ps[pbase + 3][:, 32:64], ALU.add)
            yield
            tt("dve", w1, ptop, rd, ALU.mult)
            tt("dve", R_["slotp"], slot, ec_bc, ALU.add)
            yield
            tt("dve", w2, ptop, w1, ALU.subtract)
            tt("dve", R_["tmpa"], oh1, slot, ALU.mult)
            tt("dve", R_["tmpb"], oh2, slot, ALU.mult)
            yield
            ovs = []
            for ci, (oh, df, rtd, tmp) in enumerate(((oh1, R_["d1f"], rt_d1, R_["tmpa"]), (oh2, R_["d2f"], rt_d2, R_["tmpb"]))):
                ov = T(rls[si].ap[:, 400 + ci:401 + ci], ("rl", si, "ov%d" % ci))
                nov = T(rls[si].ap[:, 404 + ci:405 + ci], ("rl", si, "nov%d" % ci))
                red(ov, tmp, ALU.add)
                yield
                ts("dve", ov, ov, float(CAP), ALU.is_ge)
                tt("dve", tmp, oh, R_["slotp"], ALU.mult)
                yield
                ts("dve", nov, ov, -1.0, ALU.mult, 1.0, ALU.add)
                red(df, tmp, ALU.add)
                yield
                tt("dve", df, df, nov, ALU.mult)
                yield
                stt("dve", df, ov, float(NE * CAP), df, ALU.mult, ALU.add)
                yield
                rcol = T(rtd.ap[:, gt:gt + 1], (rtd.key, gt))
                cp("dve", rcol, df)
                yield
                ia = rcol.ap
                ha = hbf.ap
                P.add("pool", lambda e, ia=ia, ha=ha: e.indirect_dma_start(
                    out=XG, out_offset=bass.IndirectOffsetOnAxis(ap=ia, axis=0), in_=ha, in_offset=None),
                    [rcol.key, hbf.key], [("XG", gt)], dma=True)

        if not OPT["SKIP_M1"]:
            interleave((m1_gen(gt, row, i % 2) for i, (gt, row) in enumerate(m_tiles)), OPT["W_M1"], 9)

        P.barrier()
        stg = Ring([T(b_.ap, "stg%d" % i) for i, b_ in enumerate(y1s.items + y2s.items + modF[0] + modF[1])])
        NS = CAP // 128

        def prefetch(ex):
            wg, wu, wd, xg = wgs[ex % 2], wus[ex % 2], wds[ex % 2], xgs[ex % 2]
            dma("pool", xg.v(xg.ap.rearrange("p (s d) -> p s d", d=D)),
                XG[ex * CAP:(ex + 1) * CAP, :].rearrange("(s p) d -> p s d", p=128), r=XGKEYS)
            chunks = []
            for (wdst, wsrc, fw) in ((wg, w_gate[li, ex], FH), (wu, w_up[li, ex], FH), (wd, w_down[li, ex], D)):
                src3 = wsrc.rearrange("(k p) f -> p k f", p=128)
                kper = 1024 // fw
                for c in range(4):
                    chunks.append((wdst[:, c * 1024:(c + 1) * 1024], src3[:, c * kper:(c + 1) * kper, :], fw))
            bufs = []

            def issue(i):
                sb = stg.next()
                bufs.append(sb)
                dma("sp", sb.v(sb.ap.rearrange("p (k f) -> p k f", f=chunks[i][2])), chunks[i][1])
            AHEAD = 3
            for i in range(AHEAD):
                issue(i)
            yield
            for i in range(len(chunks)):
                if i + AHEAD < len(chunks):
                    issue(i + AHEAD)
                if i % 2 == 0:
                    act(chunks[i][0], bufs[i], AF.Copy)
                else:
                    cp("dve", chunks[i][0], bufs[i])
                yield

        for _ in prefetch(0):
            pass
        for ex in (range(NE) if not OPT["SKIP_M2"] else []):
            wg, wu, wd, xg = wgs[ex % 2], wus[ex % 2], wds[ex % 2], xgs[ex % 2]
            nxt = prefetch(ex + 1) if ex + 1 < NE else None

            def step(nxt=nxt):
                if nxt is not None:
                    next(nxt, None)
            for s in range(NS):
                pb = psbf(s % 2)
                tr_group([(pb.ap[:, kc * 128:(kc + 1) * 128], xg.ap[:, s * D + kc * 128: s * D + (kc + 1) * 128], identb.ap)
                          for kc in range(8)], r=[xg.key, identb.key], w=[pb.key])
                dst = xeT.v(xeT.ap.rearrange("p (k c) -> p k c", c=CAP)[:, :, s * 128:(s + 1) * 128])
                src = pb.v(pb.ap.rearrange("p (k c) -> p k c", c=128))
                if s % 2 == 0:
                    cp("dve", dst, src)
                else:
                    act(dst, src, AF.Copy)
                step()
            for m in range(4):
                mm_group(ps[2 + (m % 2)], [(wg.ap[:, kc * FH + m * 128: kc * FH + (m + 1) * 128], xeT.ap[:, kc * CAP:(kc + 1) * CAP])
                                           for kc in range(8)], r=[wg.key, xeT.key])
                mm_group(ps[4 + (m % 2)], [(wu.ap[:, kc * FH + m * 128: kc * FH + (m + 1) * 128], xeT.ap[:, kc * CAP:(kc + 1) * CAP])
                                           for kc in range(8)], r=[wu.key, xeT.key])
                sgt = sgs.next()
                act(sgt, ps[2 + (m % 2)], AF.Silu)
                tt("dve", HT[:, m * CAP:(m + 1) * CAP], sgt, ps[4 + (m % 2)], ALU.mult)
                step()
            for s in range(NS):
                ysb = Ysb.next()
                for half in range(2):
                    pp = ps[6 + half]
                    mm_group(pp, [(HT.ap[:, m * CAP + s * 128: m * CAP + (s + 1) * 128], wd.ap[:, m * D + half * 512: m * D + (half + 1) * 512])
                                  for m in range(4)], r=[HT.key, wd.key])
                    if half == 0:
                        act(ysb[:, 0:512], pp, AF.Copy)
                    else:
                        cp("dve", ysb[:, 512:1024], pp)
                dma("pool", YG[ex * CAP + s * 128: ex * CAP + (s + 1) * 128, :], ysb, w=[("YG", ex, s)])
                step()
            if nxt is not None:
                for _ in nxt:
                    pass
        P.barrier()

        YGKEYS = ["YG"] + [("YG", ex, s_) for ex in range(NE) for s_ in range(CAP // 128)]
        m3_state = {"row": None, "mset": -1, "gtf": None}

        def m3_gen(gt, row):
            if row != m3_state["row"]:
                m3_state["mset"] += 1
                gtf_ = gtF[m3_state["mset"] % 2]
                dma("sp", gtf_, modd[li, row:row + 1, 5 * D:6 * D].partition_broadcast(128), r=[("modd", li)])
                m3_state["row"] = row
                m3_state["gtf"] = gtf_
            gtf = m3_state["gtf"]
            y1, y2 = y1s.next(), y2s.next()
            for (yy, rtd) in ((y1, rt_d1), (y2, rt_d2)):
                ia = rtd.ap[:, gt:gt + 1]
                ya = yy.ap
                P.add("pool", lambda e, ia=ia, ya=ya: e.indirect_dma_start(
                    out=ya, out_offset=None, in_=YG, in_offset=bass.IndirectOffsetOnAxis(ap=ia, axis=0)),
                    [(rtd.key, gt)] + YGKEYS, [yy.key], dma=True)
            xt = xts.next()
            dma("sp", xt, xs[gt * 128:(gt + 1) * 128, :], r=[("xs", gt)])
            yield
            w1 = T(rt_w1.ap[:, gt:gt + 1], ("rt_w1", gt))
            w2 = T(rt_w2.ap[:, gt:gt + 1], ("rt_w2", gt))
            ts("dve", y1, y1, w1, ALU.mult)
            yield
            stt("dve", y1, y2, w2, y1, ALU.mult, ALU.add)
            yield
            tt("dve", y1, y1, gtf, ALU.mult)
            yield
            tt("dve", xt, xt, y1, ALU.add)
            yield
            if not last:
                dma("sp", xs[gt * 128:(gt + 1) * 128, :], xt, w=[("xs", gt)])
            else:
                st = stat_ring.next()
                stt("dve", y2, xt, 1.0 / D, xt, ALU.mult, ALU.mult, accum=st[:, 0:1])
                yield
                act(st[:, 1:2], st[:, 0:1], AF.Ln, bias=EPS)
                yield
                act(st[:, 2:3], st[:, 1:2], AF.Exp, scale=-0.5)
                yield
                stt("dve", y2, xt, st[:, 2:3], gfin, ALU.mult, ALU.mult)
                b_ = gt // NT_B
                t_ = gt % NT_B
                r0 = b_ * SEQ + t_ * 128
                dma("sp", out[r0:r0 + 128, :], y2)

        if not OPT["SKIP_M3"]:
            interleave((m3_gen(gt, row) for (gt, row) in m_tiles), OPT["W_M3"], 3)

    P.finalize()
    P.emit()
    return nc


_CACHE = {}


def _consts():
    if "c" not in _CACHE:
        cos, sin_s = _rope_tables()
        kk = np.arange(128)[:, None]
        qq = np.arange(128)[None, :]
        mlo = np.tile((kk >= qq).astype(np.float32), (1, 4))
        mhi = np.tile((kk <= qq).astype(np.float32), (1, 4))
        tri = (np.arange(128)[:, None] < np.arange(128)[None, :]).astype(np.float32)
        ec = (np.arange(NE, dtype=np.float32) * CAP).reshape(1, NE)
        _CACHE["c"] = dict(c_ident=np.eye(128, dtype=np.float32), c_cos=cos, c_sin=sin_s,
                           c_masks=np.stack([mlo, mhi]).astype(np.float32), c_tri=tri, c_ec=ec)
        _CACHE["naidx"] = _na_index_table()
    return _CACHE["c"], _CACHE["naidx"]


def make_in_maps(inputs, n_cores, NB):
    f = lambda a: np.ascontiguousarray(np.asarray(a, dtype=np.float32))
    consts, naidx = _consts()
    rpb = f(inputs["rpb_c"])[0].reshape(-1)
    rpb_ext = np.concatenate([rpb, np.array([NEG], dtype=np.float32)])
    natab = np.ascontiguousarray(rpb_ext[naidx].reshape(5, 5, 128, NH * 128))
    w_rg = np.ascontiguousarray(np.concatenate([f(inputs["w_group"]), f(inputs["w_router"])], axis=-1))
    b_rg = np.ascontiguousarray(np.concatenate([f(inputs["b_group"]), f(inputs["b_router"])], axis=-1))
    shared = dict(
        w_ada=f(inputs["w_ada"]), b_ada=f(inputs["b_ada"]), g_attn=f(inputs["g_attn"]), w_qkv=f(inputs["w_qkv"]),
        w_o=f(inputs["w_o"]), sink_a=f(inputs["sink_a"]), gq_b=f(inputs["gq_b"]), gk_b=f(inputs["gk_b"]),
        g_ffn=f(inputs["g_ffn"]), w_rg=w_rg, b_rg=b_rg, w_gate=f(inputs["w_gate"]), w_up=f(inputs["w_up"]),
        w_down=f(inputs["w_down"]), g_final=f(inputs["g_final"]).reshape(1, D), c_natab=natab, **consts)
    x = f(inputs["x"])
    ctx = f(inputs["ctx"])
    c = f(inputs["c"])
    c_ctx = f(inputs["c_ctx"]).reshape(1, D)
    maps = []
    for i in range(n_cores):
        sl = slice(i * NB, (i + 1) * NB)
        m = dict(shared)
        m["x"] = np.ascontiguousarray(x[sl].reshape(NB * SEQ, D))
        m["ctx"] = np.ascontiguousarray(ctx[sl].reshape(NB * CTX, D))
        m["cvec"] = np.ascontiguousarray(np.concatenate([c[sl], c_ctx], axis=0))
        maps.append(m)
    return maps


def kernel(**inputs):
    n_cores = 8
    NB = 2
    if "nc" not in _CACHE:
        _CACHE["nc"] = build(NB=NB)
    nc = _CACHE["nc"]
    maps = make_in_maps(inputs, n_cores, NB)
    res = run_bass_kernel_spmd(nc, maps, core_ids=list(range(n_cores)))
    outs = [r["out"].reshape(NB, SEQ, D) for r in res.results]
    return np.concatenate(outs, axis=0).astype(np.float32)
```
